# Optimizing a Trainium2 kernel written in Bass

```python
import math
import jax, jax.numpy as jnp
from jax import lax
import numpy as np

D_MODEL = 1024
BATCH = 16
SEQ = 4096
DEPTH = 2

HEAD_DIM = 64
N_Q_HEADS = 8
N_KV_HEADS = 2
GQA_GROUP = N_Q_HEADS // N_KV_HEADS
ATTN_WIDTH = N_Q_HEADS * HEAD_DIM
KV_WIDTH = N_KV_HEADS * HEAD_DIM
N_BRANCH = 3
SSM_WIDTH = D_MODEL // 4
SSM_GROUP = 16
SSM_N_GROUPS = SSM_WIDTH // SSM_GROUP
SSM_STATE = 64
CONV_WIDTH = D_MODEL // 4
CONV_K = 31
MIX_WIDTH = ATTN_WIDTH + SSM_WIDTH + CONV_WIDTH
CMP_LEN = 32
CMP_STRIDE = 16
CMP_HIDDEN = 256
SLC_BLOCK = 64
SLC_TOPN = 16
WINDOW = 512
Q_BLOCK = 32
ROPE_THETA = 10000.0
D_FF = ((8 * D_MODEL // 3 + 255) // 256) * 256
EPS = 1e-6
NEG_INF = -1e30
FORCE = 1e9

_SIZES = (ATTN_WIDTH, KV_WIDTH, KV_WIDTH, KV_WIDTH, KV_WIDTH, KV_WIDTH, KV_WIDTH,
          N_BRANCH * N_Q_HEADS, SSM_WIDTH, 2 * CONV_WIDTH)
IN_WIDTH = sum(_SIZES)
SPLIT_POINTS = tuple(sum(_SIZES[:i + 1]) for i in range(len(_SIZES) - 1))

kernel_name = "hymba_nsa_s5_conformer_block"


def rmsnorm(x, g):
    xf = x.astype(jnp.float32)
    y = xf * lax.rsqrt(jnp.mean(xf * xf, axis=-1, keepdims=True) + EPS)
    return (y * g.astype(jnp.float32)).astype(x.dtype)


def layernorm(x, g, b):
    xf = x.astype(jnp.float32)
    mu = jnp.mean(xf, axis=-1, keepdims=True)
    var = jnp.mean(jnp.square(xf - mu), axis=-1, keepdims=True)
    y = (xf - mu) * lax.rsqrt(var + EPS) * g.astype(jnp.float32) + b.astype(jnp.float32)
    return y.astype(x.dtype)


def rope(x, pos):
    half = x.shape[-1] // 2
    freqs = ROPE_THETA ** (-jnp.arange(half, dtype=jnp.float32) / half)
    ang = pos.astype(jnp.float32)[:, None] * freqs[None, :]
    cos = jnp.cos(ang)[:, None, :]
    sin = jnp.sin(ang)[:, None, :]
    xf = x.astype(jnp.float32)
    x1, x2 = xf[..., :half], xf[..., half:]
    return jnp.concatenate([x1 * cos - x2 * sin, x2 * cos + x1 * sin], axis=-1).astype(x.dtype)


def masked_softmax(s, mask):
    s = jnp.where(mask, s, NEG_INF)
    m = jnp.max(s, axis=-1, keepdims=True)
    p = jnp.where(mask, jnp.exp(s - m), 0.0)
    return p / jnp.maximum(jnp.sum(p, axis=-1, keepdims=True), 1e-30)


def compress_blocks(x, pe, w1, w2):
    b, s, h, d = x.shape
    halves = x.reshape(b, s // CMP_STRIDE, CMP_STRIDE, h, d)
    blocks = jnp.concatenate([halves[:, :-1], halves[:, 1:]], axis=2)
    blocks = blocks + pe[None, None, :, None, :]
    nc = s // CMP_STRIDE - 1
    flat = blocks.transpose(0, 1, 3, 2, 4).reshape(b, nc, h, CMP_LEN * d)
    return jax.nn.gelu(flat @ w1) @ w2


def nsa_attention(q, kc, vc, ks, vs, kw, vw, gates):
    b, s, _, d = q.shape
    nc = kc.shape[1]
    n_sel = s // SLC_BLOCK
    top_n = min(SLC_TOPN, n_sel)
    qg = (q * (d ** -0.5)).reshape(b, s, N_KV_HEADS, GQA_GROUP, d)
    cmp_end = jnp.arange(nc) * CMP_STRIDE + CMP_LEN - 1
    ci = np.arange(nc)[:, None]
    sj = np.arange(n_sel)[None, :]
    overlap = jnp.asarray(((ci * CMP_STRIDE < (sj + 1) * SLC_BLOCK) &
                           (ci * CMP_STRIDE + CMP_LEN > sj * SLC_BLOCK)).astype(np.float32))
    ks_blocks = ks.reshape(b, n_sel, SLC_BLOCK, N_KV_HEADS, d).transpose(0, 3, 1, 2, 4)
    vs_blocks = vs.reshape(b, n_sel, SLC_BLOCK, N_KV_HEADS, d).transpose(0, 3, 1, 2, 4)
    kw_pad = jnp.pad(kw, ((0, 0), (WINDOW, 0), (0, 0), (0, 0)))
    vw_pad = jnp.pad(vw, ((0, 0), (WINDOW, 0), (0, 0), (0, 0)))
    gather = jax.vmap(jax.vmap(lambda blk, idx: blk[idx]))
    blk_ids = jnp.arange(n_sel)
    offs = jnp.arange(SLC_BLOCK)

    def block_fn(s0):
        t = s0 + jnp.arange(Q_BLOCK)
        qb = lax.dynamic_slice_in_dim(qg, s0, Q_BLOCK, axis=1)
        sc = jnp.einsum('bqgrd,bngd->bgrqn', qb, kc).astype(jnp.float32)
        pc = masked_softmax(sc, cmp_end[None, :] <= t[:, None])
        oc = jnp.einsum('bgrqn,bngd->bqgrd', pc, vc)
        imp = jnp.einsum('bgrqn,nj->bgqj', pc, overlap)
        cur = t // SLC_BLOCK
        forced = (blk_ids[None, :] == 0) | (blk_ids[None, :] == cur[:, None]) | (blk_ids[None, :] == cur[:, None] - 1)
        causal = blk_ids[None, :] * SLC_BLOCK <= t[:, None]
        imp = jnp.where(forced, FORCE, jnp.where(causal, imp, -FORCE))
        _, idx = lax.top_k(imp, top_n)
        kg = gather(ks_blocks, idx)
        vg = gather(vs_blocks, idx)
        ss = jnp.einsum('bqgrd,bgqnld->bgrqnl', qb, kg).astype(jnp.float32)
        ss = ss.reshape(b, N_KV_HEADS, GQA_GROUP, Q_BLOCK, top_n * SLC_BLOCK)
        kpos = idx[..., None] * SLC_BLOCK + offs
        ms = (kpos <= t[None, None, :, None, None]).reshape(b, N_KV_HEADS, 1, Q_BLOCK, top_n * SLC_BLOCK)
        ps = masked_softmax(ss, ms)
        os_ = jnp.einsum('bgrqm,bgqmd->bqgrd', ps,
                         vg.reshape(b, N_KV_HEADS, Q_BLOCK, top_n * SLC_BLOCK, d))
        kwb = lax.dynamic_slice_in_dim(kw_pad, s0, WINDOW + Q_BLOCK, axis=1)
        vwb = lax.dynamic_slice_in_dim(vw_pad, s0, WINDOW + Q_BLOCK, axis=1)
        sw = jnp.einsum('bqgrd,bkgd->bgrqk', qb, kwb).astype(jnp.float32)
        kp = s0 - WINDOW + jnp.arange(WINDOW + Q_BLOCK)
        mw = (kp[None, :] <= t[:, None]) & (kp[None, :] > t[:, None] - WINDOW) & (kp[None, :] >= 0)
        pw = masked_softmax(sw, mw)
        ow = jnp.einsum('bgrqk,bkgd->bqgrd', pw, vwb)
        gb = lax.dynamic_slice_in_dim(gates, s0, Q_BLOCK, axis=1).astype(jnp.float32)
        return gb[:, :, 0, :, :, None] * oc + gb[:, :, 1, :, :, None] * os_ + gb[:, :, 2, :, :, None] * ow

    starts = jnp.arange(s // Q_BLOCK) * Q_BLOCK
    outs = lax.map(block_fn, starts)
    return outs.transpose(1, 0, 2, 3, 4, 5).reshape(b, s, N_Q_HEADS * d).astype(q.dtype)


def s5_ssm(u, a_re, a_im, log_dt, b_re, b_im, c_re, c_im, d_skip, w_glu):
    bsz, s, _ = u.shape
    f32 = jnp.float32
    uf = u.astype(f32).reshape(bsz, s, SSM_N_GROUPS, SSM_GROUP)
    a_re, a_im = a_re.astype(f32), a_im.astype(f32)
    b_re, b_im = b_re.astype(f32), b_im.astype(f32)
    dt = jnp.exp(log_dt.astype(f32))[:, None]
    mag = jnp.exp(a_re * dt)
    lam_re = mag * jnp.cos(a_im * dt)
    lam_im = mag * jnp.sin(a_im * dt)
    den = a_re * a_re + a_im * a_im
    nr, ni = lam_re - 1.0, lam_im
    f_re = (nr * a_re + ni * a_im) / den
    f_im = (ni * a_re - nr * a_im) / den
    bb_re = f_re[..., None] * b_re - f_im[..., None] * b_im
    bb_im = f_re[..., None] * b_im + f_im[..., None] * b_re
    bu_re = jnp.einsum('gpc,bsgc->bsgp', bb_re, uf)
    bu_im = jnp.einsum('gpc,bsgc->bsgp', bb_im, uf)
    shape = (1, s, SSM_N_GROUPS, SSM_STATE)
    la_re = jnp.broadcast_to(lam_re[None, None], shape)
    la_im = jnp.broadcast_to(lam_im[None, None], shape)

    def combine(e1, e2):
        a1r, a1i, b1r, b1i = e1
        a2r, a2i, b2r, b2i = e2
        return (a2r * a1r - a2i * a1i, a2r * a1i + a2i * a1r,
                a2r * b1r - a2i * b1i + b2r, a2r * b1i + a2i * b1r + b2i)

    _, _, xr, xi = lax.associative_scan(combine, (la_re, la_im, bu_re, bu_im), axis=1)
    y = jnp.einsum('gcp,bsgp->bsgc', c_re.astype(f32), xr) - jnp.einsum('gcp,bsgp->bsgc', c_im.astype(f32), xi)
    y = (y + d_skip.astype(f32).reshape(SSM_N_GROUPS, SSM_GROUP) * uf).reshape(bsz, s, SSM_WIDTH)
    y = jax.nn.gelu(y)
    a, g = jnp.split(y @ w_glu.astype(f32), 2, axis=-1)
    return (a * jax.nn.sigmoid(g)).astype(u.dtype)


def conformer_conv(h, w_dw, b_dw, ln_g, ln_b, w_pw, b_pw):
    a, g = jnp.split(h, 2, axis=-1)
    u = a * jax.nn.sigmoid(g)
    y = lax.conv_general_dilated(u, w_dw[:, None, :], window_strides=(1,),
                                 padding=[(CONV_K - 1, 0)],
                                 dimension_numbers=('NWC', 'WIO', 'NWC'),
                                 feature_group_count=CONV_WIDTH) + b_dw
    y = jax.nn.silu(layernorm(y, ln_g, ln_b))
    return y @ w_pw + b_pw


def hybrid_layer(x, norm_mix, w_in, cmp_pe_k, cmp_k_w1, cmp_k_w2, cmp_pe_v, cmp_v_w1, cmp_v_w2,
                 ssm_a_re, ssm_a_im, ssm_log_dt, ssm_b_re, ssm_b_im, ssm_c_re, ssm_c_im, ssm_d, ssm_w_glu,
                 conv_w_dw, conv_b_dw, conv_ln_g, conv_ln_b, conv_w_pw, conv_b_pw,
                 norm_out_attn, norm_out_ssm, norm_out_conv, w_out, norm_ffn, w_gate, w_up, w_down):
    b, s, _ = x.shape
    h = rmsnorm(x, norm_mix)
    proj = h @ w_in
    q, kc, vc, ks, vs, kw, vw, g_cols, u_ssm, u_conv = jnp.split(proj, SPLIT_POINTS, axis=-1)
    pos = jnp.arange(s)
    heads = lambda t, n: t.reshape(b, s, n, HEAD_DIM)
    q = rope(heads(q, N_Q_HEADS), pos)
    ks = rope(heads(ks, N_KV_HEADS), pos)
    kw = rope(heads(kw, N_KV_HEADS), pos)
    vs = heads(vs, N_KV_HEADS)
    vw = heads(vw, N_KV_HEADS)
    kc = compress_blocks(heads(kc, N_KV_HEADS), cmp_pe_k, cmp_k_w1, cmp_k_w2)
    vc = compress_blocks(heads(vc, N_KV_HEADS), cmp_pe_v, cmp_v_w1, cmp_v_w2)
    nc = kc.shape[1]
    kc = rope(kc, jnp.arange(nc) * CMP_STRIDE + CMP_LEN - 1)
    gates = jax.nn.sigmoid(g_cols).reshape(b, s, N_BRANCH, N_KV_HEADS, GQA_GROUP)
    y_attn = nsa_attention(q, kc, vc, ks, vs, kw, vw, gates)
    y_ssm = s5_ssm(u_ssm, ssm_a_re, ssm_a_im, ssm_log_dt, ssm_b_re, ssm_b_im, ssm_c_re, ssm_c_im, ssm_d, ssm_w_glu)
    y_conv = conformer_conv(u_conv, conv_w_dw, conv_b_dw, conv_ln_g, conv_ln_b, conv_w_pw, conv_b_pw)
    mixed = jnp.concatenate([rmsnorm(y_attn, norm_out_attn), rmsnorm(y_ssm, norm_out_ssm),
                             rmsnorm(y_conv, norm_out_conv)], axis=-1)
    x = x + mixed @ w_out
    h = rmsnorm(x, norm_ffn)
    x = x + (jax.nn.silu(h @ w_gate) * (h @ w_up)) @ w_down
    return x


def _normal(k, shape, scale):
    return scale * jax.random.normal(k, shape, jnp.float32)


def setup_inputs(seed: int = 0) -> dict:
    key = jax.random.key(seed)
    ks = jax.random.split(key, 40)
    L = DEPTH
    gain = lambda k, n: 1.0 + _normal(k, (L, n), 0.02)
    return {
        "x": _normal(ks[0], (BATCH, SEQ, D_MODEL), 1.0),
        "norm_mix": gain(ks[1], D_MODEL),
        "w_in": _normal(ks[2], (L, D_MODEL, IN_WIDTH), D_MODEL ** -0.5),
        "cmp_pe_k": _normal(ks[3], (L, CMP_LEN, HEAD_DIM), 0.02),
        "cmp_k_w1": _normal(ks[4], (L, CMP_LEN * HEAD_DIM, CMP_HIDDEN), (CMP_LEN * HEAD_DIM) ** -0.5),
        "cmp_k_w2": _normal(ks[5], (L, CMP_HIDDEN, HEAD_DIM), CMP_HIDDEN ** -0.5),
        "cmp_pe_v": _normal(ks[6], (L, CMP_LEN, HEAD_DIM), 0.02),
        "cmp_v_w1": _normal(ks[7], (L, CMP_LEN * HEAD_DIM, CMP_HIDDEN), (CMP_LEN * HEAD_DIM) ** -0.5),
        "cmp_v_w2": _normal(ks[8], (L, CMP_HIDDEN, HEAD_DIM), CMP_HIDDEN ** -0.5),
        "ssm_a_re": -0.5 + _normal(ks[9], (L, SSM_N_GROUPS, SSM_STATE), 0.01),
        "ssm_a_im": jnp.pi * jnp.arange(SSM_STATE, dtype=jnp.float32)[None, None, :]
                    + _normal(ks[10], (L, SSM_N_GROUPS, SSM_STATE), 0.01),
        "ssm_log_dt": jax.random.uniform(ks[11], (L, SSM_N_GROUPS), jnp.float32,
                                         minval=math.log(0.001), maxval=math.log(0.1)),
        "ssm_b_re": _normal(ks[12], (L, SSM_N_GROUPS, SSM_STATE, SSM_GROUP), (2 * SSM_GROUP) ** -0.5),
        "ssm_b_im": _normal(ks[13], (L, SSM_N_GROUPS, SSM_STATE, SSM_GROUP), (2 * SSM_GROUP) ** -0.5),
        "ssm_c_re": _normal(ks[14], (L, SSM_N_GROUPS, SSM_GROUP, SSM_STATE), SSM_STATE ** -0.5),
        "ssm_c_im": _normal(ks[15], (L, SSM_N_GROUPS, SSM_GROUP, SSM_STATE), SSM_STATE ** -0.5),
        "ssm_d": _normal(ks[16], (L, SSM_WIDTH), 1.0),
        "ssm_w_glu": _normal(ks[17], (L, SSM_WIDTH, 2 * SSM_WIDTH), SSM_WIDTH ** -0.5),
        "conv_w_dw": _normal(ks[18], (L, CONV_K, CONV_WIDTH), CONV_K ** -0.5),
        "conv_b_dw": _normal(ks[19], (L, CONV_WIDTH), 0.02),
        "conv_ln_g": gain(ks[20], CONV_WIDTH),
        "conv_ln_b": _normal(ks[21], (L, CONV_WIDTH), 0.02),
        "conv_w_pw": _normal(ks[22], (L, CONV_WIDTH, CONV_WIDTH), CONV_WIDTH ** -0.5),
        "conv_b_pw": _normal(ks[23], (L, CONV_WIDTH), 0.02),
        "norm_out_attn": gain(ks[24], ATTN_WIDTH),
        "norm_out_ssm": gain(ks[25], SSM_WIDTH),
        "norm_out_conv": gain(ks[26], CONV_WIDTH),
        "w_out": _normal(ks[27], (L, MIX_WIDTH, D_MODEL), MIX_WIDTH ** -0.5),
        "norm_ffn": gain(ks[28], D_MODEL),
        "w_gate": _normal(ks[29], (L, D_MODEL, D_FF), D_MODEL ** -0.5),
        "w_up": _normal(ks[30], (L, D_MODEL, D_FF), D_MODEL ** -0.5),
        "w_down": _normal(ks[31], (L, D_FF, D_MODEL), D_FF ** -0.5),
        "norm_final": 1.0 + _normal(ks[32], (D_MODEL,), 0.02),
    }


def reference(x, norm_mix, w_in, cmp_pe_k, cmp_k_w1, cmp_k_w2, cmp_pe_v, cmp_v_w1, cmp_v_w2,
              ssm_a_re, ssm_a_im, ssm_log_dt, ssm_b_re, ssm_b_im, ssm_c_re, ssm_c_im, ssm_d, ssm_w_glu,
              conv_w_dw, conv_b_dw, conv_ln_g, conv_ln_b, conv_w_pw, conv_b_pw,
              norm_out_attn, norm_out_ssm, norm_out_conv, w_out, norm_ffn, w_gate, w_up, w_down,
              norm_final):
    for l in range(DEPTH):
        x = hybrid_layer(x, norm_mix[l], w_in[l], cmp_pe_k[l], cmp_k_w1[l], cmp_k_w2[l],
                         cmp_pe_v[l], cmp_v_w1[l], cmp_v_w2[l],
                         ssm_a_re[l], ssm_a_im[l], ssm_log_dt[l], ssm_b_re[l], ssm_b_im[l],
                         ssm_c_re[l], ssm_c_im[l], ssm_d[l], ssm_w_glu[l],
                         conv_w_dw[l], conv_b_dw[l], conv_ln_g[l], conv_ln_b[l], conv_w_pw[l], conv_b_pw[l],
                         norm_out_attn[l], norm_out_ssm[l], norm_out_conv[l], w_out[l],
                         norm_ffn[l], w_gate[l], w_up[l], w_down[l])
    return rmsnorm(x, norm_final)
```

```python
import contextlib
import numpy as np
import ml_dtypes
import concourse.bass as bass
import concourse.mybir as mybir
from concourse.bass_utils import run_bass_kernel_spmd

F32 = mybir.dt.float32
BF16 = mybir.dt.bfloat16
AF = mybir.ActivationFunctionType
ALU = mybir.AluOpType
AX = mybir.AxisListType

SEM_L = 20000
DMA_L = 16 * 1200
NB = 2
SEQ = 4096
D = 1024
NT = SEQ // 128
NG = SEQ // 512
DFF = 2816
NFF = DFF // 128
EPS = 1e-6
NEG = -30000.0
L = 2


class T:
    __slots__ = ("name", "w", "r", "sem", "semv")

    def __init__(self, name=""):
        self.name = name
        self.w = None
        self.r = {}
        self.sem = None
        self.semv = 0


class Sched:
    def __init__(self, nc):
        self.nc = nc
        self.h = {"pe": nc.tensor, "act": nc.scalar, "dve": nc.vector,
                  "pool": nc.gpsimd, "sp": nc.sync}
        self.cnt = {k: 0 for k in self.h}
        self.sems = {k: [] for k in self.h}
        self.waited = {}
        self.same = {"dve", "act", "pool"}
        self.nsem = 0
        self.ninst = 0
        self.all_T = []

    def T(self, name=""):
        t = T(name)
        self.all_T.append(t)
        return t

    def new_sem(self, name):
        self.nsem += 1
        return self.nc.alloc_semaphore(name + f"_{self.nsem}")

    def dma_sem(self, d):
        free = getattr(self, "free_sems", None)
        if free:
            d.sem, d.semv = free.pop()
        else:
            d.sem, d.semv = self.new_sem("d"), 0

    def _eng_sem(self, e):
        idx = self.cnt[e] // SEM_L
        while len(self.sems[e]) <= idx:
            self.sems[e].append(self.new_sem(f"s_{e}"))
        return self.sems[e][idx]

    def _wait(self, e, tok):
        if tok is None:
            return
        sem, val, src = tok
        if src == e and e not in self.same:
            return
        key = (e, id(sem))
        if self.waited.get(key, 0) >= val:
            return
        self.waited[key] = val
        self.h[e].wait_ge(sem, val)

    def _deps(self, e, reads, writes):
        for t in reads:
            self._wait(e, t.w)
        for t in writes:
            self._wait(e, t.w)
            for tok in t.r.values():
                self._wait(e, tok)

    def op(self, e, fn, reads=(), writes=()):
        self._deps(e, reads, writes)
        sem = self._eng_sem(e)
        val = self.cnt[e] % SEM_L + 1
        fn(self.h[e]).then_inc(sem, 1)
        self.cnt[e] += 1
        self.ninst += 1
        tok = (sem, val, e)
        for t in reads:
            t.r[id(sem)] = tok
        for t in writes:
            t.w = tok
            t.r = {}
        return tok

    def dma(self, q, out, in_, reads=(), writes=(), **kw):
        self._deps(q, reads, writes)
        d = writes[0]
        if d.sem is None or d.semv >= DMA_L:
            self.dma_sem(d)
        d.semv += 16
        self.h[q].dma_start(out=out, in_=in_, **kw).then_inc(d.sem, 16)
        self.ninst += 1
        tok = (d.sem, d.semv, "dma")
        for t in reads:
            t.r[id(d.sem)] = tok
        d.w = tok
        d.r = {}
        return tok

    def barrier(self):
        e0 = "dve"
        for t in self.all_T:
            self._wait(e0, t.w)
            for tok in t.r.values():
                self._wait(e0, tok)
            t.r = {}
        for e in self.h:
            if self.cnt[e] > 0 and e != e0:
                idx = (self.cnt[e] - 1) // SEM_L
                self._wait(e0, (self.sems[e][idx], (self.cnt[e] - 1) % SEM_L + 1, e))
        if not hasattr(self, "Tbar"):
            self.Tbar = T("bar")
        tok = self.op(e0, lambda h: h.memset(self.bar_tile[:], 0.0), writes=[self.Tbar])
        for e in self.h:
            if e != e0:
                self._wait(e, tok)
        if not hasattr(self, "free_sems"):
            self.free_sems = []
        for t in self.all_T:
            t.w = None
            t.r = {}
            if t.sem is not None:
                if t.semv < DMA_L - 4096:
                    self.free_sems.append((t.sem, t.semv))
                t.sem = None
                t.semv = 0


class K:
    def __init__(self, debug=False, phases=None, nlayers=L):
        self.debug = debug
        self.phases = phases
        self.nlayers = nlayers
        nc = bass.Bass("TRN2", target_bir_lowering=False)
        self.nc = nc
        self.S = Sched(nc)
        self.inp = {}
        self.scr = {}
        self.S.bar_tile = nc.alloc_sbuf_tensor("bar_tile", [128, 8], F32)
        self.st_i = 0
        self.ld_i = 0

    def din(self, name, shape, dt=F32):
        self.inp[name] = self.nc.dram_tensor(name, list(shape), dt, kind="ExternalInput").ap()
        return self.inp[name]

    def dscr(self, name, shape, dt=F32, out=False):
        kind = "ExternalOutput" if (self.debug or out) else "Internal"
        self.scr[name] = self.nc.dram_tensor(name, list(shape), dt, kind=kind).ap()
        return self.scr[name]

    def sb(self, es, name, shape, dt=F32):
        self.uid = getattr(self, "uid", 0) + 1
        return es.enter_context(self.nc.sbuf_tensor(f"sb{self.uid}_{name}", list(shape), dt))

    def ps(self, es, name, shape, dt=F32):
        self.uid = getattr(self, "uid", 0) + 1
        return es.enter_context(self.nc.psum_tensor(f"ps{self.uid}_{name}", list(shape), dt))

    def store(self, out, in_, reads, q=None):
        import os
        q = q or os.environ.get("STQ", "pool")
        if not hasattr(self, "_st"):
            self._st = [self.S.T(f"st{i}") for i in range(8)]
        i = self.st_i % 8
        self.st_i += 1
        self.S.dma(q, out, in_, reads=reads, writes=[self._st[i]])

    def load(self, out, in_, Tt, q="sp"):
        self.S.dma(q, out, in_, writes=[Tt])


def _rope_tables():
    half = 32
    freqs = (np.float32(10000.0) ** (-np.arange(half, dtype=np.float32) / np.float32(half))).astype(np.float32)
    pos = np.arange(SEQ, dtype=np.float32)
    ang = (pos[:, None] * freqs[None, :]).astype(np.float32)
    cos = np.cos(ang).astype(np.float32).T
    sin = np.sin(ang).astype(np.float32).T
    cosT = np.tile(cos, (4, 1))
    sinT = np.tile(sin, (4, 1))
    posc = (np.arange(256, dtype=np.float32) * 16 + 31).astype(np.float32)
    angc = (posc[:, None] * freqs[None, :]).astype(np.float32)
    cosC = np.tile(np.cos(angc).astype(np.float32).T, (2, 1))
    sinC = np.tile(np.sin(angc).astype(np.float32).T, (2, 1))
    return cosT, sinT, cosC, sinC


def _consts():
    c = {}
    cosT, sinT, cosC, sinC = _rope_tables()
    c["cosT"], c["sinT"], c["cosC"], c["sinC"] = cosT, sinT, cosC, sinC
    c["identf"] = np.eye(128, dtype=np.float32)
    c["identb"] = np.eye(128, dtype=np.float32).astype(ml_dtypes.bfloat16)
    key = np.arange(SEQ)
    c["Erows"] = (key[None, :] // 64 == np.arange(64)[:, None]).astype(np.float32).astype(ml_dtypes.bfloat16)
    i = np.arange(256)
    c["cmask"] = np.where((i[:, None] * 16 + 31) <= key[None, :], 0.0, NEG).astype(np.float32).astype(ml_dtypes.bfloat16)
    v = np.arange(128)[:, None]
    u = np.arange(512)[None, :]
    wm = np.zeros((8, 128, 512), np.float32)
    for ri, r in enumerate(range(-4, 4)):
        kk = 128 * r + v
        wm[ri] = np.where((kk <= u) & (kk > u - 512), 0.0, NEG)
    c["wmask"] = wm.astype(ml_dtypes.bfloat16)
    cm = np.zeros((4, 128, 512), np.float32)
    for r in range(4):
        cm[r] = np.where(128 * r + v <= u, 0.0, NEG)
    c["dmask"] = cm.astype(ml_dtypes.bfloat16)
    t = np.arange(SEQ)[:, None]
    j = np.arange(64)[None, :]
    cur = t // 64
    forced = (j == 0) | (j == cur) | (j == cur - 1)
    causal = j * 64 <= t
    add = np.where(forced, 1e9, np.where(causal, 0.0, -1e9)).astype(np.float32)
    c["addtab"] = np.ascontiguousarray(add.reshape(NT, 128, 64).transpose(1, 0, 2))
    ii = np.arange(256)[:, None]
    ov = ((ii * 16 < (j + 1) * 64) & (ii * 16 + 32 > j * 64) & (ii < 255)).astype(np.float32)
    c["overlap"] = ov.astype(ml_dtypes.bfloat16)
    return c


def declare_io(self):
    k = self
    k.din("x", [NB, SEQ, D])
    k.din("w_in_p", [L, D, 2072])
    k.din("norm_mix_t", [L, 128, 8])
    k.din("norm_final", [D])
    for n, sh, dt in [("cosT", [128, SEQ], F32), ("sinT", [128, SEQ], F32), ("cosC", [64, 256], F32),
                      ("sinC", [64, 256], F32), ("identf", [128, 128], F32), ("identb", [128, 128], BF16),
                      ("Erows", [64, SEQ], BF16), ("cmask", [256, SEQ], BF16), ("wmask", [8, 128, 512], BF16),
                      ("dmask", [4, 128, 512], BF16), ("addtab", [128, NT, 64], F32), ("overlap", [256, 64], BF16)]:
        k.din(n, sh, dt)
    k.dscr("qT", [NB, 512, SEQ], BF16)
    k.dscr("ksT", [NB, 128, SEQ], BF16)
    k.dscr("kwT", [NB, 128, SEQ], BF16)
    k.dscr("kcrT", [NB, 128, SEQ], BF16)
    k.dscr("vcrT", [NB, 128, SEQ], BF16)
    k.dscr("ucT", [NB, 256, SEQ], BF16)
    k.dscr("usT", [NB, 256, SEQ], BF16)
    k.dscr("vsw", [NB, SEQ, 256], BF16)
    k.dscr("gat", [NB, SEQ, 32], F32)
    k.dscr("ycat", [NB, SEQ, D], F32)
    k.dscr("xres", [NB, SEQ, D], F32)
    k.dscr("h2T", [NB, D, SEQ], BF16)
    k.dscr("kcT", [NB, 2, 64, 256], BF16)
    k.dscr("vc", [NB, 2, 256, 64], BF16)
    for n in ["cmp_k_w1", "cmp_v_w1"]:
        k.din(n, [L, 2048, 256])
    for n in ["cmp_k_w2", "cmp_v_w2"]:
        k.din(n, [L, 256, 64])
    k.din("pe_t", [L, 2, 128, 16])
    k.din("wdw_t", [L, 256, 31])
    for n in ["ssm_are_t", "ssm_aim_t", "ssm_ldt_t"]:
        k.din(n, [L, 128, 8])
    for n in ["Zb_re", "Zb_im", "Cp_re", "Cp_im"]:
        k.din(n, [L, 128, 8, 128])
    k.din("ssm_d_t", [L, 128, 2])
    k.din("ssm_w_glu", [L, 256, 512])
    k.din("iota512", [512])
    k.din("gcat_t", [L, 128, 8])
    k.din("norm_ffn_t", [L, 128, 8])
    k.din("w_out", [L, D, D])
    k.din("w_gate", [L, D, DFF])
    k.din("w_up", [L, D, DFF])
    k.din("w_down", [L, DFF, D])
    for n in ["conv_b_dw", "conv_b_pw", "conv_ln_g", "conv_ln_b"]:
        k.din(n, [L, 256])
    k.din("conv_w_pw", [L, 256, 256])
    k.dscr("out", [NB, SEQ, D], F32, out=True)


def load_consts(self, es):
    k, S = self, self.S
    k.identf = k.sb(es, "identf", [128, 128], F32)
    k.identb = k.sb(es, "identb", [128, 128], BF16)
    k.Tid = S.T("ident")
    S.dma("sp", k.identf[:], k.inp["identf"], writes=[k.Tid])
    k.Tidb = S.T("identb")
    S.dma("sp", k.identb[:], k.inp["identb"], writes=[k.Tidb])
    k.ones_b = k.sb(es, "ones_b", [128, 128], BF16)
    k.Tones = S.T("ones")
    S.op("dve", lambda e: e.memset(k.ones_b[:], 1.0), writes=[k.Tones])


def phase1(self, l):
    k, S, nc = self, self.S, self.nc
    with contextlib.ExitStack() as es:
        NCOL = 2840
        W = k.sb(es, "p1_W", [128, 8, NCOL], BF16)
        TW = [S.T(f"p1_W{i}") for i in range(8)]
        stage = [k.sb(es, f"p1_stage{i}", [128, 2072], F32) for i in range(2)]
        Tstage = [S.T(f"p1_stage{i}") for i in range(2)]
        gain = k.sb(es, "p1_gain", [128, 8], F32)
        gq = k.sb(es, "p1_gq", [128, 8], F32)
        ngq = k.sb(es, "p1_ngq", [128, 8], F32)
        ngain = k.sb(es, "p1_ngain", [128, 8], F32)
        Tgain = S.T("p1_gain")
        S.dma("sp", gain[:], k.inp["norm_mix_t"][l], writes=[Tgain])
        S.op("dve", lambda e: e.tensor_scalar(out=ngain[:], in0=gain[:], scalar1=-1.0, scalar2=None, op0=ALU.mult), reads=[Tgain], writes=[Tgain])
        S.op("dve", lambda e: e.tensor_scalar(out=gq[:], in0=gain[:], scalar1=0.125, scalar2=None, op0=ALU.mult), reads=[Tgain], writes=[Tgain])
        S.op("dve", lambda e: e.tensor_scalar(out=ngq[:], in0=gain[:], scalar1=-0.125, scalar2=None, op0=ALU.mult), reads=[Tgain], writes=[Tgain])
        wsrc = k.inp["w_in_p"][l].rearrange("(kc p) n -> p kc n", p=128)
        engs = ["dve", "pool"]
        ei = 0

        def ts(out, in_, sc):
            nonlocal ei
            e_ = engs[kc % 2]
            S.op(e_, lambda e: e.tensor_scalar(out=out, in0=in_, scalar1=sc, scalar2=None, op0=ALU.mult),
                 reads=[Tstage[kc % 2], Tgain], writes=[TW[kc]])

        for kc in range(8):
            st = stage[kc % 2]
            S.dma("sp", st[:], wsrc[:, kc, :], writes=[Tstage[kc % 2]])
            g1, ng1, q1, nq1 = gain[:, kc:kc + 1], ngain[:, kc:kc + 1], gq[:, kc:kc + 1], ngq[:, kc:kc + 1]
            ts(W[:, kc, 0:512], st[:, 0:512], q1)
            sv = st[:, 0:512].rearrange("p (h t f) -> p h t f", h=8, t=2)
            dv = W[:, kc, 512:1024].rearrange("p (h t f) -> p h t f", h=8, t=2)
            ts(dv[:, :, 0, :], sv[:, :, 1, :], nq1)
            ts(dv[:, :, 1, :], sv[:, :, 0, :], q1)
            for (s0, d0) in [(512, 1024), (640, 1280)]:
                ts(W[:, kc, d0:d0 + 128], st[:, s0:s0 + 128], g1)
                sv = st[:, s0:s0 + 128].rearrange("p (h t f) -> p h t f", h=2, t=2)
                dv = W[:, kc, d0 + 128:d0 + 256].rearrange("p (h t f) -> p h t f", h=2, t=2)
                ts(dv[:, :, 0, :], sv[:, :, 1, :], ng1)
                ts(dv[:, :, 1, :], sv[:, :, 0, :], g1)
            ts(W[:, kc, 1536:2840], st[:, 768:2072], g1)

        import os
        STOP = os.environ.get("P1STOP", "")
        if STOP == "w":
            S.barrier()
            return
        xg = [k.sb(es, f"p1_xg{i}", [128, 4, D], F32) for i in range(2)]
        Txg = [S.T(f"p1_xg{i}") for i in range(2)]
        junk = k.sb(es, "p1_junk", [128, D], BF16)
        Tjunk = S.T("p1_junk")
        ss = k.sb(es, "p1_ss", [128, 4], F32)
        rstd = k.sb(es, "p1_rstd", [128, 4], F32)
        Tss = S.T("p1_ss")
        hb = k.sb(es, "p1_hb", [128, 4, D], BF16)
        Thb = [S.T(f"p1_hb{i}") for i in range(4)]
        hT = [k.sb(es, f"p1_hT{i}", [128, 8, 512], BF16) for i in range(2)]
        ThT = [[S.T(f"p1_hT{i}_{c}") for c in range(8)] for i in range(2)]
        cs = [k.sb(es, f"p1_cos{i}", [128, 512], F32) for i in range(2)]
        sn = [k.sb(es, f"p1_sin{i}", [128, 512], F32) for i in range(2)]
        Tcs = [S.T(f"p1_cs{i}") for i in range(2)]
        pT = [k.ps(es, f"p1_pT{i}", [128, 512], BF16) for i in range(2)]
        TpT = [S.T(f"p1_pT{i}") for i in range(2)]
        pm = [k.ps(es, f"p1_pm{i}", [128, 512], F32) for i in range(4)]
        Tpm = [S.T(f"p1_pm{i}") for i in range(4)]
        pk = [k.ps(es, f"p1_pk{i}", [128, 512], F32) for i in range(2)]
        Tpk = [S.T(f"p1_pk{i}") for i in range(2)]
        NE = 6
        t1 = [k.sb(es, f"p1_t1_{i}", [128, 512], F32) for i in range(NE)]
        t2 = [k.sb(es, f"p1_t2_{i}", [128, 512], F32) for i in range(NE)]
        ob = [k.sb(es, f"p1_ob{i}", [128, 512], BF16) for i in range(NE)]
        Tt1 = [S.T(f"p1_t1_{i}") for i in range(NE)]
        Tt2 = [S.T(f"p1_t2_{i}") for i in range(NE)]
        Tob = [S.T(f"p1_ob{i}") for i in range(NE)]
        Ttg_pre = [S.T(f"p1_tg{i}") for i in range(2)]
        tk = [k.sb(es, f"p1_tk{i}", [128, 256], BF16) for i in range(2)]
        tg = [k.sb(es, f"p1_tg{i}", [128, 32], F32) for i in range(2)]
        for i in range(2):
            S.op("dve", lambda e: e.memset(tg[i][:], 0.0), writes=[Ttg_pre[i]])
        Ttk = [S.T(f"p1_tk{i}") for i in range(2)]
        Ttg = Ttg_pre
        xin = k.inp["x"] if l == 0 else k.scr["xres"]
        gi = 0
        ecnt = 0
        pmi = 0
        for b in range(NB):
            for g in range(NG):
                u = gi % 2
                gi += 1
                tsl = slice(g * 512, (g + 1) * 512)
                S.dma("sp", xg[u][:], xin[b, tsl, :].rearrange("(j p) d -> p j d", p=128), writes=[Txg[u]])
                S.dma("sp", cs[u][:], k.inp["cosT"][:, tsl], writes=[Tcs[u]])
                S.dma("sp", sn[u][:], k.inp["sinT"][:, tsl], writes=[Tcs[u]])
                for j in range(4):
                    S.op("act", lambda e: e.activation(out=junk[:], in_=xg[u][:, j, :], func=AF.Square, accum_out=ss[:, j:j + 1]),
                         reads=[Txg[u]], writes=[Tjunk, Tss])
                S.op("dve", lambda e: e.tensor_scalar(out=rstd[:], in0=ss[:], scalar1=1.0 / D, scalar2=EPS, op0=ALU.mult, op1=ALU.add), reads=[Tss], writes=[Tss])
                S.op("act", lambda e: e.activation(out=rstd[:], in_=rstd[:], func=AF.Sqrt), reads=[Tss], writes=[Tss])
                S.op("dve", lambda e: e.reciprocal(out=rstd[:], in_=rstd[:]), reads=[Tss], writes=[Tss])
                for j in range(4):
                    S.op("pool" if j % 2 else "dve", lambda e: e.tensor_scalar(out=hb[:, j, :], in0=xg[u][:, j, :], scalar1=rstd[:, j:j + 1], scalar2=None, op0=ALU.mult),
                         reads=[Txg[u], Tss], writes=[Thb[j]])
                if STOP == "n":
                    continue
                for kc in range(8):
                    pu = kc % 2
                    for j in range(4):
                        S.op("pe", lambda e: e.transpose(out=pT[pu][:, j * 128:(j + 1) * 128], in_=hb[:, j, kc * 128:(kc + 1) * 128], identity=k.identb[:]),
                             reads=[Thb[j], k.Tidb], writes=[TpT[pu]])
                    if kc % 2:
                        S.op("act", lambda e: e.activation(out=hT[u][:, kc, :], in_=pT[pu][:], func=AF.Copy), reads=[TpT[pu]], writes=[ThT[u][kc]])
                    else:
                        S.op("dve", lambda e: e.tensor_copy(out=hT[u][:, kc, :], in_=pT[pu][:]), reads=[TpT[pu]], writes=[ThT[u][kc]])

                if STOP == "t":
                    continue

                def mm(c):
                    nonlocal pmi
                    i = pmi % 4
                    pmi += 1
                    for kc in range(8):
                        S.op("pe", lambda e: e.matmul(pm[i][:], lhsT=W[:, kc, c * 128:(c + 1) * 128], rhs=hT[u][:, kc, :], start=(kc == 0), stop=(kc == 7)),
                             reads=[TW[kc], ThT[u][kc]], writes=[Tpm[i]])
                    return i

                def roped(cx, cr, dst):
                    nonlocal ecnt
                    ix = mm(cx)
                    ir = mm(cr)
                    n = ecnt % NE
                    ecnt += 1
                    S.op("dve", lambda e: e.tensor_tensor(out=t1[n][:], in0=pm[ix][:], in1=cs[u][:], op=ALU.mult), reads=[Tpm[ix], Tcs[u]], writes=[Tt1[n]])
                    S.op("dve", lambda e: e.tensor_tensor(out=t2[n][:], in0=pm[ir][:], in1=sn[u][:], op=ALU.mult), reads=[Tpm[ir], Tcs[u]], writes=[Tt2[n]])
                    S.op("pool", lambda e: e.tensor_tensor(out=ob[n][:], in0=t1[n][:], in1=t2[n][:], op=ALU.add), reads=[Tt1[n], Tt2[n]], writes=[Tob[n]])
                    k.store(dst, ob[n][:], [Tob[n]])

                def plain(c, dst):
                    nonlocal ecnt
                    i = mm(c)
                    n = ecnt % NE
                    ecnt += 1
                    S.op("act", lambda e: e.activation(out=ob[n][:], in_=pm[i][:], func=AF.Copy), reads=[Tpm[i]], writes=[Tob[n]])
                    k.store(dst, ob[n][:], [Tob[n]])

                for c in range(4):
                    roped(c, c + 4, k.scr["qT"][b, c * 128:(c + 1) * 128, tsl])
                roped(8, 9, k.scr["ksT"][b, :, tsl])
                roped(10, 11, k.scr["kwT"][b, :, tsl])
                plain(12, k.scr["kcrT"][b, :, tsl])
                plain(13, k.scr["vcrT"][b, :, tsl])
                for c in range(2):
                    ia = mm(14 + c)
                    ig = mm(16 + c)
                    n = ecnt % NE
                    ecnt += 1
                    S.op("act", lambda e: e.activation(out=t1[n][:], in_=pm[ig][:], func=AF.Sigmoid), reads=[Tpm[ig]], writes=[Tt1[n]])
                    S.op("dve", lambda e: e.tensor_tensor(out=ob[n][:], in0=pm[ia][:], in1=t1[n][:], op=ALU.mult), reads=[Tpm[ia], Tt1[n]], writes=[Tob[n]])
                    k.store(k.scr["ucT"][b, c * 128:(c + 1) * 128, tsl], ob[n][:], [Tob[n]])
                for c in range(2):
                    plain(18 + c, k.scr["usT"][b, c * 128:(c + 1) * 128, tsl])
                if STOP == "f":
                    continue
                for j in range(4):
                    pu = j % 2
                    for kc in range(8):
                        S.op("pe", lambda e: e.matmul(pk[pu][:, 0:280], lhsT=hT[u][:, kc, j * 128:(j + 1) * 128], rhs=W[:, kc, 2560:2840], start=(kc == 0), stop=(kc == 7)),
                             reads=[TW[kc], ThT[u][kc]], writes=[Tpk[pu]])
                    S.op("dve", lambda e: e.tensor_copy(out=tk[pu][:], in_=pk[pu][:, 0:256]), reads=[Tpk[pu]], writes=[Ttk[pu]])
                    r0 = g * 512 + j * 128
                    if STOP != "g":
                        S.op("act", lambda e: e.activation(out=tg[pu][:, 0:24], in_=pk[pu][:, 256:280], func=AF.Sigmoid), reads=[Tpk[pu]], writes=[Ttg[pu]])
                        if STOP != "h":
                            k.store(k.scr["gat"][b, r0:r0 + 128, :], tg[pu][:], [Ttg[pu]])
                    k.store(k.scr["vsw"][b, r0:r0 + 128, :], tk[pu][:], [Ttk[pu]])
        S.barrier()


K.declare_io = declare_io
K.load_consts = load_consts
K.phase1 = phase1


def build(debug=False, phases=None, nlayers=L):
    k = K(debug=debug, phases=phases, nlayers=nlayers)
    k.declare_io()
    S = k.S
    with contextlib.ExitStack() as es:
        k.load_consts(es)
        for l in range(nlayers):
            for ph in PHASES:
                if phases is not None and (l, ph) not in phases and ph not in phases:
                    continue
                getattr(k, ph)(l)
        if phases is None or "final" in phases:
            k.final()
        S.barrier()
    return k


PHASES = ["phase1"]


def host_inputs(inputs):
    f = lambda a: np.ascontiguousarray(np.asarray(a, dtype=np.float32))
    w_in = f(inputs["w_in"])
    cols = np.concatenate([np.arange(0, 512), np.arange(768, 896), np.arange(1024, 1152), np.arange(512, 640),
                           np.arange(640, 768), np.arange(1560, 2072), np.arange(1304, 1560), np.arange(896, 1024),
                           np.arange(1152, 1280), np.arange(1280, 1304)])
    shared = {}
    shared["w_in_p"] = np.ascontiguousarray(w_in[:, :, cols])
    shared["norm_mix_t"] = np.ascontiguousarray(f(inputs["norm_mix"]).reshape(L, 8, 128).transpose(0, 2, 1))
    shared["norm_final"] = f(inputs["norm_final"])
    tl = lambda a: np.ascontiguousarray(a.reshape(L, 8, 128).transpose(0, 2, 1))
    shared["ssm_are_t"] = tl(f(inputs["ssm_a_re"]).reshape(L, 16 * 64))
    shared["ssm_aim_t"] = tl(f(inputs["ssm_a_im"]).reshape(L, 16 * 64))
    shared["ssm_ldt_t"] = tl(np.repeat(f(inputs["ssm_log_dt"]), 64, axis=1))
    def padB(bm):
        o = np.zeros((L, 128, 8, 128), np.float32)
        for g_ in range(16):
            m_, h_ = g_ // 2, g_ % 2
            col = (g_ % 8) * 16
            o[:, h_ * 64:(h_ + 1) * 64, m_, col:col + 16] = bm[:, g_]
        return o
    shared["Zb_re"] = padB(f(inputs["ssm_b_re"]))
    shared["Zb_im"] = padB(f(inputs["ssm_b_im"]))
    shared["Cp_re"] = padB(f(inputs["ssm_c_re"]).transpose(0, 1, 3, 2))
    shared["Cp_im"] = padB(f(inputs["ssm_c_im"]).transpose(0, 1, 3, 2))
    shared["ssm_d_t"] = np.ascontiguousarray(f(inputs["ssm_d"]).reshape(L, 2, 128).transpose(0, 2, 1))
    shared["ssm_w_glu"] = f(inputs["ssm_w_glu"])
    shared["iota512"] = np.arange(512, dtype=np.float32)
    gcat = np.concatenate([f(inputs["norm_out_attn"]), f(inputs["norm_out_ssm"]), f(inputs["norm_out_conv"])], axis=1)
    shared["gcat_t"] = np.ascontiguousarray(gcat.reshape(L, 8, 128).transpose(0, 2, 1))
    shared["norm_ffn_t"] = np.ascontiguousarray(f(inputs["norm_ffn"]).reshape(L, 8, 128).transpose(0, 2, 1))
    for n in ["w_out", "w_gate", "w_up", "w_down"]:
        shared[n] = f(inputs[n])
    shared["wdw_t"] = np.ascontiguousarray(f(inputs["conv_w_dw"]).transpose(0, 2, 1))
    for n in ["conv_b_dw", "conv_b_pw", "conv_ln_g", "conv_ln_b", "conv_w_pw"]:
        shared[n] = f(inputs[n])
    for n in ["cmp_k_w1", "cmp_v_w1", "cmp_k_w2", "cmp_v_w2"]:
        shared[n] = f(inputs[n])
    pe = np.stack([f(inputs["cmp_pe_k"]), f(inputs["cmp_pe_v"])], 1)
    shared["pe_t"] = np.ascontiguousarray(pe.reshape(L, 2, 2, 16, 64).transpose(0, 1, 2, 4, 3).reshape(L, 2, 128, 16))
    shared.update(_consts())
    return shared


def kernel(**inputs):
    k = build()
    shared = host_inputs(inputs)
    x = np.ascontiguousarray(np.asarray(inputs["x"], dtype=np.float32))
    names = set(k.inp.keys())
    in_maps = []
    for c in range(8):
        m = {n: shared[n] for n in names if n != "x"}
        m["x"] = np.ascontiguousarray(x[c * NB:(c + 1) * NB])
        in_maps.append(m)
    res = run_bass_kernel_spmd(k.nc, in_maps, core_ids=list(range(8)))
    return np.concatenate([np.asarray(r["out"]) for r in res.results], axis=0).astype(np.float32)


def final(self):
    k, S = self, self.S
    src = k.scr["xres"] if self.nlayers > 0 else k.inp["x"]
    with contextlib.ExitStack() as es:
        gf = k.sb(es, "fin_g", [128, D], F32)
        Tgf = S.T("fin_g")
        S.dma("sp", gf[:], k.inp["norm_final"].partition_broadcast(128), writes=[Tgf])
        xg = [k.sb(es, f"fin_x{i}", [128, 4, D], F32) for i in range(2)]
        Txg = [S.T(f"fin_x{i}") for i in range(2)]
        og = [k.sb(es, f"fin_o{i}", [128, 4, D], F32) for i in range(2)]
        Tog = [S.T(f"fin_o{i}") for i in range(2)]
        junk = k.sb(es, "fin_junk", [128, D], BF16)
        Tjunk = S.T("fin_junk")
        ss = [k.sb(es, f"fin_ss{i}", [128, 4], F32) for i in range(2)]
        Tss = [S.T(f"fin_ss{i}") for i in range(2)]
        gi = 0
        for b in range(NB):
            for g in range(NG):
                u = gi % 2
                gi += 1
                tsl = slice(g * 512, (g + 1) * 512)
                S.dma("sp", xg[u][:], src[b, tsl, :].rearrange("(j p) d -> p j d", p=128), writes=[Txg[u]])
                for j in range(4):
                    S.op("act", lambda e: e.activation(out=junk[:], in_=xg[u][:, j, :], func=AF.Square, accum_out=ss[u][:, j:j + 1]),
                         reads=[Txg[u]], writes=[Tjunk, Tss[u]])
                S.op("dve", lambda e: e.tensor_scalar(out=ss[u][:], in0=ss[u][:], scalar1=1.0 / D, scalar2=EPS, op0=ALU.mult, op1=ALU.add), reads=[Tss[u]], writes=[Tss[u]])
                S.op("act", lambda e: e.activation(out=ss[u][:], in_=ss[u][:], func=AF.Sqrt), reads=[Tss[u]], writes=[Tss[u]])
                S.op("dve", lambda e: e.reciprocal(out=ss[u][:], in_=ss[u][:]), reads=[Tss[u]], writes=[Tss[u]])
                for j in range(4):
                    S.op("dve", lambda e: e.scalar_tensor_tensor(out=og[u][:, j, :], in0=xg[u][:, j, :], scalar=ss[u][:, j:j + 1], in1=gf[:], op0=ALU.mult, op1=ALU.mult),
                         reads=[Txg[u], Tss[u], Tgf], writes=[Tog[u]])
                k.store(k.scr["out"][b, tsl, :].rearrange("(j p) d -> p j d", p=128), og[u][:], [Tog[u]])
        S.barrier()


K.final = final


def phase2(self, l):
    k, S = self, self.S
    with contextlib.ExitStack() as es:
        stage = k.sb(es, "p2_stage", [128, 16, 256], F32)
        Tstage = S.T("p2_stage")
        w1b = [k.sb(es, f"p2_w1b{i}", [128, 16, 256], BF16) for i in range(2)]
        Tw1 = [S.T(f"p2_w1b{i}") for i in range(2)]
        pes = k.sb(es, "p2_pes", [128, 2, 16], F32)
        peb = k.sb(es, "p2_peb", [128, 2, 16], BF16)
        Tpe = S.T("p2_pe")
        w2s = k.sb(es, "p2_w2s", [128, 2, 2, 64], F32)
        w2b = k.sb(es, "p2_w2b", [128, 2, 2, 64], BF16)
        w2r = k.sb(es, "p2_w2r", [128, 2, 64], BF16)
        Tw2 = S.T("p2_w2")
        bias = k.sb(es, "p2_bias", [128, 2, 2], F32)
        Tbias = S.T("p2_bias")
        csC = k.sb(es, "p2_cosC", [64, 256], F32)
        snC = k.sb(es, "p2_sinC", [64, 256], F32)
        TcsC = S.T("p2_csC")
        S.dma("sp", csC[:], k.inp["cosC"], writes=[TcsC])
        S.dma("sp", snC[:], k.inp["sinC"], writes=[TcsC])
        pb = k.ps(es, "p2_pb", [128, 512], F32)
        Tpb = S.T("p2_pb")
        S.dma("sp", pes[:], k.inp["pe_t"][l].rearrange("a p l -> p a l"), writes=[Tpe])
        S.op("dve", lambda e: e.tensor_copy(out=peb[:], in_=pes[:]), reads=[Tpe], writes=[Tpe])
        for a, nm in enumerate(["cmp_k_w1", "cmp_v_w1"]):
            src = k.inp[nm][l].rearrange("(hl d) h -> d hl h", d=64)
            S.dma("sp", stage[0:64, :, :], src[:, 0:16, :], writes=[Tstage])
            S.dma("sp", stage[64:128, :, :], src[:, 16:32, :], reads=[], writes=[Tstage])
            S.op("dve" if a == 0 else "pool", lambda e: e.tensor_copy(out=w1b[a][:], in_=stage[:]), reads=[Tstage], writes=[Tw1[a]])
            for hc in range(2):
                for ll in range(16):
                    S.op("pe", lambda e: e.matmul(pb[:, 0:1], lhsT=w1b[a][:, ll, hc * 128:(hc + 1) * 128], rhs=peb[:, a, ll:ll + 1], start=(ll == 0), stop=(ll == 15)),
                         reads=[Tw1[a], Tpe], writes=[Tpb])
                S.op("dve", lambda e: e.tensor_copy(out=bias[:, a, hc:hc + 1], in_=pb[:, 0:1]), reads=[Tpb], writes=[Tbias])
        for a, nm in enumerate(["cmp_k_w2", "cmp_v_w2"]):
            S.dma("sp", w2s[:, a, :, :], k.inp[nm][l].rearrange("(hc p) d -> p hc d", p=128), writes=[Tw2])
        S.op("dve", lambda e: e.tensor_copy(out=w2b[:], in_=w2s[:]), reads=[Tw2], writes=[Tw2])
        for hc in range(2):
            S.op("dve", lambda e: e.tensor_scalar(out=w2r[:, hc, 0:32], in0=w2s[:, 0, hc, 32:64], scalar1=-1.0, scalar2=None, op0=ALU.mult), reads=[Tw2], writes=[Tw2])
            S.op("dve", lambda e: e.tensor_copy(out=w2r[:, hc, 32:64], in_=w2s[:, 0, hc, 0:32]), reads=[Tw2], writes=[Tw2])

        X2 = [k.sb(es, f"p2_X2_{i}", [128, SEQ], BF16) for i in range(2)]
        TX2 = [S.T(f"p2_X2_{i}") for i in range(2)]
        for i in range(2):
            S.op("pool", lambda e: e.memset(X2[i][64:128, SEQ - 16:SEQ], 0.0), writes=[TX2[i]])
        hid = [k.sb(es, f"p2_hid{i}", [128, 2, 256], BF16) for i in range(2)]
        Thid = [S.T(f"p2_hid{i}") for i in range(2)]
        for i in range(2):
            S.op("pool", lambda e: e.memset(hid[i][:], 0.0), writes=[Thid[i]])
        ph = [k.ps(es, f"p2_ph{i}", [128, 512], F32) for i in range(2)]
        Tph = [S.T(f"p2_ph{i}") for i in range(2)]
        pk = k.ps(es, "p2_pk", [128, 512], F32)
        Tpk = S.T("p2_pk")
        pv = k.ps(es, "p2_pv", [128, 512], F32)
        Tpv = S.T("p2_pv")
        t1 = k.sb(es, "p2_t1", [64, 256], F32)
        t2 = k.sb(es, "p2_t2", [64, 256], F32)
        kco = k.sb(es, "p2_kco", [64, 256], BF16)
        Tko = S.T("p2_kco")
        S.op("dve", lambda e: e.memset(kco[:], 0.0), writes=[Tko])
        vco = [k.sb(es, f"p2_vco{i}", [128, 2, 64], BF16) for i in range(2)]
        Tvo = [S.T(f"p2_vco{i}") for i in range(2)]
        it = 0
        phi = 0
        for b in range(NB):
            for kv in range(2):
                for a in range(2):
                    u = it % 2
                    it += 1
                    src = k.scr["kcrT" if a == 0 else "vcrT"][b, kv * 64:(kv + 1) * 64, :]
                    S.dma("sp", X2[u][0:64, :], src, writes=[TX2[u]])
                    S.dma("sp", X2[u][64:128, 0:SEQ - 16], src[:, 16:SEQ], reads=[], writes=[TX2[u]])
                    xv = X2[u][:, :].rearrange("p (i s) -> p i s", s=16)
                    for hc in range(2):
                        pi = phi % 2
                        phi += 1
                        for ll in range(16):
                            S.op("pe", lambda e: e.matmul(ph[pi][:, 0:255], lhsT=w1b[a][:, ll, hc * 128:(hc + 1) * 128], rhs=xv[:, 0:255, ll], start=(ll == 0), stop=(ll == 15)),
                                 reads=[Tw1[a], TX2[u]], writes=[Tph[pi]])
                        S.op("act", lambda e: e.activation(out=hid[u][:, hc, 0:255], in_=ph[pi][:, 0:255], func=AF.Gelu_apprx_tanh, bias=bias[:, a, hc:hc + 1]),
                             reads=[Tph[pi], Tbias], writes=[Thid[u]])
                    if a == 0:
                        for r_, wsel in enumerate([w2b[:, 0, :, :], w2r[:, :, :]]):
                            for hc in range(2):
                                S.op("pe", lambda e: e.matmul(pk[0:64, r_ * 256:r_ * 256 + 256], lhsT=wsel[:, hc, :], rhs=hid[u][:, hc, :], start=(hc == 0), stop=(hc == 1)),
                                     reads=[Tw2, Thid[u]], writes=[Tpk])
                        S.op("dve", lambda e: e.tensor_tensor(out=t1[:], in0=pk[0:64, 0:256], in1=csC[:], op=ALU.mult), reads=[Tpk, TcsC], writes=[Tko])
                        S.op("dve", lambda e: e.tensor_tensor(out=t2[:], in0=pk[0:64, 256:512], in1=snC[:], op=ALU.mult), reads=[Tpk, TcsC], writes=[Tko])
                        S.op("dve", lambda e: e.tensor_tensor(out=kco[:, 0:255], in0=t1[:, 0:255], in1=t2[:, 0:255], op=ALU.add), reads=[Tko], writes=[Tko])
                        k.store(k.scr["kcT"][b, kv], kco[:], [Tko])
                    else:
                        for c in range(2):
                            for hc in range(2):
                                S.op("pe", lambda e: e.matmul(pv[:, c * 64:(c + 1) * 64], lhsT=hid[u][:, hc, c * 128:(c + 1) * 128], rhs=w2b[:, 1, hc, :], start=(hc == 0), stop=(hc == 1)),
                                     reads=[Tw2, Thid[u]], writes=[Tpv])
                        S.op("dve", lambda e: e.tensor_copy(out=vco[u][:].rearrange("p c d -> p (c d)"), in_=pv[:, 0:128]), reads=[Tpv], writes=[Tvo[u]])
                        k.store(k.scr["vc"][b, kv].rearrange("(c p) d -> p c d", p=128), vco[u][:], [Tvo[u]])
        S.barrier()


K.phase2 = phase2
PHASES.append("phase2")


def phase3(self, l):
    k, S = self, self.S
    with contextlib.ExitStack() as es:
        sb = lambda n, sh, dt=F32: k.sb(es, "p3_" + n, sh, dt)
        QS = [sb(f"QS{h}", [128, SEQ], BF16) for h in range(4)]
        TQd = [S.T(f"p3_Qd{h}") for h in range(4)]
        TQm = [[S.T(f"p3_Qm{h}_{g}") for g in range(NG)] for h in range(4)]
        KS = sb("KS", [128, SEQ], BF16)
        TKS, TE = S.T("p3_KS"), S.T("p3_E")
        KW = sb("KW", [64, SEQ], BF16)
        TKW = S.T("p3_KW")
        KC = sb("KC", [64, 256], BF16)
        TKC = S.T("p3_KC")
        VS = sb("VS", [128, NT, 80], BF16)
        VW = sb("VW", [128, NT, 80], BF16)
        VC = sb("VC", [128, 2, 144], BF16)
        TVS = [S.T(f"p3_VS{i}") for i in range(4)]
        TVW = [S.T(f"p3_VW{i}") for i in range(4)]
        TVC = S.T("p3_VC")
        G_ = sb("G", [128, NT, 32], F32)
        TG = S.T("p3_G")
        yacc = sb("yacc", [128, NT, 4, 64], F32)
        Ty = [[S.T(f"p3_y{g}_{h}") for h in range(4)] for g in range(NG)]
        cmaskS = sb("cmask", [128, 2, SEQ], BF16)
        wmaskS = sb("wmask", [128, 8, 512], BF16)
        dmaskS = sb("dmask", [128, 4, 512], BF16)
        addS = sb("add", [128, NT, 64], F32)
        Tc = S.T("p3_consts")
        S.dma("sp", cmaskS[:], k.inp["cmask"].rearrange("(c p) t -> p c t", p=128), writes=[Tc])
        Tc2 = S.T("p3_consts2")
        S.dma("sp", wmaskS[:], k.inp["wmask"].rearrange("r p u -> p r u"), writes=[Tc2])
        Tc3 = S.T("p3_consts3")
        S.dma("sp", dmaskS[:], k.inp["dmask"].rearrange("r p u -> p r u"), writes=[Tc3])
        Tc4 = S.T("p3_consts4")
        S.dma("sp", addS[:], k.inp["addtab"], writes=[Tc4])
        S.dma("sp", KS[64:128, :], k.inp["Erows"], writes=[TE])
        S.op("pool", lambda e: e.memset(VS[:, :, 64:65], 1.0), writes=TVS)
        S.op("pool", lambda e: e.memset(VW[:, :, 64:65], 1.0), writes=TVW)
        S.op("pool", lambda e: e.memset(VC[:, :, 64:65], 1.0), writes=[TVC])
        Tov = S.T("p3_ov")
        ovs = sb("ovs", [128, 2, 64], BF16)
        S.dma("sp", ovs[:], k.inp["overlap"].rearrange("(c p) j -> p c j", p=128), writes=[Tov])
        S.op("pool", lambda e: e.tensor_copy(out=VC[:, :, 65:129], in_=ovs[:]), reads=[Tov], writes=[TVC])
        import os
        if os.environ.get("P3STOP", "") == "c":
            S.barrier()
            return
        bank = [k.ps(es, f"p3_bank{i}", [128, 512], F32) for i in range(8)]
        Tb = [S.T(f"p3_bank{i}") for i in range(8)]
        NP = 6
        P = [sb(f"P{i}", [128, 512], BF16) for i in range(NP)]
        TP = [S.T(f"p3_P{i}") for i in range(NP)]
        EC = [sb(f"EC{i}", [128, 512], BF16) for i in range(8)]
        TEC = [S.T(f"p3_EC{i}") for i in range(8)]
        Mbp = [sb(f"Mbp{i}", [128, 128], BF16) for i in range(4)]
        TMb = [S.T(f"p3_Mbp{i}") for i in range(4)]
        for i in range(4):
            S.op("pool", lambda e: e.memset(Mbp[i][:], 0.0), writes=[TMb[i]])
        NS = 4
        rs = [sb(f"rs{i}", [128, 4], F32) for i in range(NS)]
        coef = [sb(f"coef{i}", [128, 4], F32) for i in range(NS)]
        impt = [sb(f"impt{i}", [128, 64], F32) for i in range(NS)]
        tmp = [sb(f"tmp{i}", [128, 64], F32) for i in range(NS)]
        m1 = [sb(f"m1_{i}", [128, 8], F32) for i in range(NS)]
        m2 = [sb(f"m2_{i}", [128, 8], F32) for i in range(NS)]
        Tsm = [S.T(f"p3_sm{i}") for i in range(NS)]
        ot = [sb(f"ot{i}", [65, 512], F32) for i in range(2)]
        Tot = [S.T(f"p3_ot{i}") for i in range(2)]
        cnt = {"p": 0, "s": 0, "sm": 0, "mb": 0, "ot": 0, "pa": 0, "ow": 0, "os": 0}

        def nxt(key, n):
            v = cnt[key] % n
            cnt[key] += 1
            return v

        for b in range(NB):
            for kv in range(2):
                for h in range(4):
                    r0 = (kv * 4 + h) * 64
                    S.dma("sp", QS[h][0:64, :], k.scr["qT"][b, r0:r0 + 64, :], writes=[TQd[h]])
                S.dma("sp", KS[0:64, :], k.scr["ksT"][b, kv * 64:(kv + 1) * 64, :], writes=[TKS])
                S.dma("sp", KW[:, :], k.scr["kwT"][b, kv * 64:(kv + 1) * 64, :], writes=[TKW])
                S.dma("sp", KC[:, :], k.scr["kcT"][b, kv], writes=[TKC])
                vsrc = k.scr["vsw"][b].rearrange("(n p) c -> p n c", p=128)
                for q4 in range(4):
                    nsl = slice(q4 * 8, q4 * 8 + 8)
                    S.dma("sp", VS[:, nsl, 0:64], vsrc[:, nsl, kv * 64:(kv + 1) * 64], writes=[TVS[q4]])
                    S.dma("sp", VW[:, nsl, 0:64], vsrc[:, nsl, 128 + kv * 64:128 + (kv + 1) * 64], writes=[TVW[q4]])
                S.dma("sp", VC[:, :, 0:64], k.scr["vc"][b, kv].rearrange("(c p) d -> p c d", p=128), writes=[TVC])
                if kv == 0:
                    S.dma("sp", G_[:], k.scr["gat"][b].rearrange("(n p) c -> p n c", p=128), writes=[TG])
                import os
                P3STOP = os.environ.get("P3STOP", "")
                for g in range(NG):
                    if P3STOP == "load":
                        break
                    gsl = slice(g * 512, (g + 1) * 512)
                    ncs = 2 if g >= 4 else 1
                    ecs = {}
                    for h in range(4):
                        for c in range(ncs):
                            bi = nxt("s", 2)
                            S.op("pe", lambda e: e.matmul(bank[bi][:], lhsT=KC[0:64, c * 128:(c + 1) * 128], rhs=QS[h][0:64, gsl], start=True, stop=False),
                                 reads=[TKC, TQd[h]], writes=[Tb[bi]])
                            S.op("pe", lambda e: e.matmul(bank[bi][:], lhsT=k.identb[:], rhs=cmaskS[:, c, gsl], start=False, stop=True),
                                 reads=[k.Tidb, Tc], writes=[Tb[bi]])
                            ei = h * 2 + c
                            S.op("act", lambda e: e.activation(out=EC[ei][:], in_=bank[bi][:], func=AF.Exp), reads=[Tb[bi]], writes=[TEC[ei]])
                            ecs[(h, c)] = ei
                    P3A = os.environ.get("P3A", "")
                    for jq in range(4):
                        if P3A == "s":
                            break
                        n = 4 * g + jq
                        pa = nxt("pa", 2)
                        bks = (2 + 2 * pa, 3 + 2 * pa)
                        for h in range(4):
                            bk = bks[h // 2]
                            o0 = (h % 2) * 256
                            for c in range(ncs):
                                ei = ecs[(h, c)]
                                S.op("pe", lambda e: e.matmul(bank[bk][:, o0:o0 + 129], lhsT=EC[ei][:, jq * 128:(jq + 1) * 128], rhs=VC[:, c, 0:129], start=(c == 0), stop=(c == ncs - 1)),
                                     reads=[TEC[ei], TVC], writes=[Tb[bk]])
                        if P3A == "pv":
                            continue
                        si = nxt("sm", NS)
                        T_s = Tsm[si]
                        for half in range(2):
                            bk = bks[half]
                            S.op("dve", lambda e: e.tensor_scalar(out=rs[si][:, 2 * half:2 * half + 2], in0=bank[bk][:].rearrange("p (a f) -> p a f", a=2)[:, :, 64], scalar1=1e-30, scalar2=None, op0=ALU.max),
                                 reads=[Tb[bk]], writes=[T_s])
                        S.op("dve", lambda e: e.reciprocal(out=rs[si][:], in_=rs[si][:]), reads=[T_s], writes=[T_s])
                        for half in range(0):
                            pass
                        for h in range(4):
                            bk = bks[h // 2]
                            o0 = (h % 2) * 256
                            in1 = addS[:, n, :] if h == 0 else impt[si][:]
                            S.op("dve", lambda e: e.scalar_tensor_tensor(out=impt[si][:], in0=bank[bk][:, o0 + 65:o0 + 129], scalar=rs[si][:, h:h + 1], in1=in1, op0=ALU.mult, op1=ALU.add),
                                 reads=[Tb[bk], T_s, Tc4], writes=[T_s])
                        S.op("dve", lambda e: e.tensor_tensor(out=coef[si][:], in0=rs[si][:], in1=G_[:, n, kv * 4:kv * 4 + 4], op=ALU.mult), reads=[T_s, TG], writes=[T_s])
                        if P3A == "d1":
                            continue
                        for h in range(4):
                            bk = bks[h // 2]
                            o0 = (h % 2) * 256
                            S.op("act", lambda e: e.activation(out=yacc[:, n, h, :], in_=bank[bk][:, o0:o0 + 64], func=AF.Identity, scale=coef[si][:, h:h + 1]),
                                 reads=[Tb[bk], T_s], writes=[Ty[g][h]])
                        if P3A == "y":
                            continue
                        S.op("dve", lambda e: e.max(out=m1[si][:], in_=impt[si][:]), reads=[T_s], writes=[T_s])
                        S.op("dve", lambda e: e.match_replace(out=tmp[si][:], in_to_replace=m1[si][:], in_values=impt[si][:], imm_value=-3e9), reads=[T_s], writes=[T_s])
                        S.op("dve", lambda e: e.max(out=m2[si][:], in_=tmp[si][:]), reads=[T_s], writes=[T_s])
                        if P3A == "tk":
                            continue
                        mi = nxt("mb", 4)
                        S.op("dve", lambda e: e.tensor_scalar(out=Mbp[mi][:, 64:128], in0=impt[si][:], scalar1=m2[si][:, 7:8], scalar2=NEG, op0=ALU.is_lt, op1=ALU.mult),
                             reads=[T_s], writes=[TMb[mi]])
                        S.op("pe", lambda e: e.matmul(bank[6][:, jq * 128:(jq + 1) * 128], lhsT=Mbp[mi][:], rhs=k.identb[:], start=True, stop=True),
                             reads=[TMb[mi], k.Tidb], writes=[Tb[6]])
                    S.op("dve", lambda e: e.tensor_copy(out=QS[0][64:128, gsl], in_=bank[6][64:128, :]), reads=[Tb[6]], writes=[TQm[0][g]])
                    for h in range(1, 4):
                        S.op("pool", lambda e: e.tensor_copy(out=QS[h][64:128, gsl], in_=QS[0][64:128, gsl]), reads=[TQm[0][g]], writes=[TQm[h][g]])
                    for h in range(4):
                        if P3STOP == "A":
                            break
                        for br in ((2,) if P3STOP == "W" else (2, 1)):
                            if br == 2:
                                kts = [kt for kt in range(4 * g - 4, 4 * g + 4) if kt >= 0]
                                ob = 3 + nxt("ow", 2)
                            else:
                                kts = list(range(0, 4 * g + 4))
                                ob = 5 + nxt("os", 2)
                            for idx, kt in enumerate(kts):
                                ksl = slice(kt * 128, (kt + 1) * 128)
                                bi = nxt("s", 3) if False else (nxt("p", 3))
                                if br == 2:
                                    S.op("pe", lambda e: e.matmul(bank[bi][:], lhsT=KW[0:64, ksl], rhs=QS[h][0:64, gsl], start=True, stop=False),
                                         reads=[TKW, TQd[h]], writes=[Tb[bi]])
                                    S.op("pe", lambda e: e.matmul(bank[bi][:], lhsT=k.identb[:], rhs=wmaskS[:, kt - 4 * g + 4, :], start=False, stop=True),
                                         reads=[k.Tidb, Tc2], writes=[Tb[bi]])
                                else:
                                    diag = kt >= 4 * g
                                    S.op("pe", lambda e: e.matmul(bank[bi][:], lhsT=KS[:, ksl], rhs=QS[h][:, gsl], start=True, stop=not diag),
                                         reads=[TKS, TE, TQd[h], TQm[h][g]], writes=[Tb[bi]])
                                    if diag:
                                        S.op("pe", lambda e: e.matmul(bank[bi][:], lhsT=k.identb[:], rhs=dmaskS[:, kt - 4 * g, :], start=False, stop=True),
                                             reads=[k.Tidb, Tc3], writes=[Tb[bi]])
                                pi = nxt("s", NP)
                                S.op("act", lambda e: e.activation(out=P[pi][:], in_=bank[bi][:], func=AF.Exp), reads=[Tb[bi]], writes=[TP[pi]])
                                Vt, TV = (VW, TVW) if br == 2 else (VS, TVS)
                                S.op("pe", lambda e: e.matmul(bank[ob][0:65, :], lhsT=Vt[:, kt, 0:65], rhs=P[pi][:], start=(idx == 0), stop=(idx == len(kts) - 1)),
                                     reads=[TV[kt // 8], TP[pi]], writes=[Tb[ob]])
                            oi = nxt("ot", 2)
                            S.op("dve", lambda e: e.tensor_copy(out=ot[oi][:], in_=bank[ob][0:65, :]), reads=[Tb[ob]], writes=[Tot[oi]])
                            for jq in range(4):
                                S.op("pe", lambda e: e.transpose(out=bank[7][:, jq * 128:jq * 128 + 65], in_=ot[oi][:, jq * 128:(jq + 1) * 128], identity=k.identf[0:65, 0:65]),
                                     reads=[Tot[oi], k.Tid], writes=[Tb[7]])
                            si = nxt("sm", NS)
                            T_s = Tsm[si]
                            b7 = bank[7][:].rearrange("p (a f) -> p a f", a=4)
                            S.op("dve", lambda e: e.reciprocal(out=rs[si][:], in_=b7[:, :, 64]), reads=[Tb[7]], writes=[T_s])
                            gc = br * 8 + kv * 4 + h
                            S.op("dve", lambda e: e.tensor_tensor(out=coef[si][:], in0=rs[si][:], in1=G_[:, 4 * g:4 * g + 4, gc], op=ALU.mult), reads=[T_s, TG], writes=[T_s])
                            for jq in range(4):
                                n = 4 * g + jq
                                S.op("dve", lambda e: e.scalar_tensor_tensor(out=yacc[:, n, h, :], in0=bank[7][:, jq * 128:jq * 128 + 64], scalar=coef[si][:, jq:jq + 1], in1=yacc[:, n, h, :], op0=ALU.mult, op1=ALU.add),
                                     reads=[Tb[7], T_s, Ty[g][h]], writes=[Ty[g][h]])
                if P3STOP == "load" and os.environ.get("P3NOST", ""):
                    continue
                ydst = k.scr["ycat"][b].rearrange("(n p) c -> p n c", p=128)
                for g in range(NG):
                    k.store(ydst[:, 4 * g:4 * g + 4, kv * 256:(kv + 1) * 256], yacc[:, 4 * g:4 * g + 4, :, :].rearrange("p n h d -> p n (h d)"), Ty[g])
        S.barrier()


K.phase3 = phase3
PHASES.append("phase3")


def phase5(self, l):
    k, S = self, self.S
    with contextlib.ExitStack() as es:
        sb = lambda n, sh, dt=F32: k.sb(es, "p5_" + n, sh, dt)
        wdw = sb("wdw", [128, 2, 31], F32)
        Twd = S.T("p5_wdw")
        S.dma("sp", wdw[:], k.inp["wdw_t"][l].rearrange("(ct p) kk -> p ct kk", p=128), writes=[Twd])
        Dg = sb("Dg", [128, 2, 31, 128], BF16)
        TDg = S.T("p5_Dg")
        for ct in range(2):
            for kk in range(31):
                S.op("pool" if (kk % 2) else "dve", lambda e: e.tensor_scalar(out=Dg[:, ct, kk, :], in0=k.identf[:], scalar1=wdw[:, ct, kk:kk + 1], scalar2=None, op0=ALU.mult),
                     reads=[Twd, k.Tid], writes=[TDg])
        rows = sb("rows", [1, 2, 256], F32)
        rowsb = sb("rowsb", [1, 2, 256], BF16)
        Trows = S.T("p5_rows")
        S.dma("sp", rows[:, 0, :], k.inp["conv_b_dw"][l:l + 1, :], writes=[Trows])
        S.dma("sp", rows[:, 1, :], k.inp["conv_b_pw"][l:l + 1, :], writes=[Trows])
        S.op("dve", lambda e: e.tensor_copy(out=rowsb[:], in_=rows[:]), reads=[Trows], writes=[Trows])
        lng = sb("lng", [128, 256], F32)
        lnb = sb("lnb", [128, 256], F32)
        Tln = S.T("p5_ln")
        S.dma("sp", lng[:], k.inp["conv_ln_g"][l].partition_broadcast(128), writes=[Tln])
        Tln2 = S.T("p5_ln2")
        S.dma("sp", lnb[:], k.inp["conv_ln_b"][l].partition_broadcast(128), writes=[Tln2])
        wps = sb("wps", [128, 2, 256], F32)
        wpb = sb("wpb", [128, 2, 256], BF16)
        Twp = S.T("p5_wp")
        S.dma("sp", wps[:], k.inp["conv_w_pw"][l].rearrange("(ct p) n -> p ct n", p=128), writes=[Twp])
        S.op("dve", lambda e: e.tensor_copy(out=wpb[:], in_=wps[:]), reads=[Twp], writes=[Twp])
        Ub = sb("Ub", [128, 2, 32 + SEQ], BF16)
        TUb = [S.T(f"p5_Ub{c}") for c in range(2)]
        S.op("pool", lambda e: e.memset(Ub[:, :, 0:32], 0.0), writes=TUb)
        pc = [k.ps(es, f"p5_pc{i}", [128, 512], F32) for i in range(2)]
        Tpc = [S.T(f"p5_pc{i}") for i in range(2)]
        pz = [k.ps(es, f"p5_pz{i}", [128, 256], BF16) for i in range(2)]
        Tpz = [S.T(f"p5_pz{i}") for i in range(2)]
        po = [k.ps(es, f"p5_po{i}", [128, 512], F32) for i in range(2)]
        Tpo = [S.T(f"p5_po{i}") for i in range(2)]
        NR = 3
        st = [sb(f"st{i}", [128, 6], F32) for i in range(NR)]
        mv = [sb(f"mv{i}", [128, 2], F32) for i in range(NR)]
        rstd = [sb(f"rstd{i}", [128, 1], F32) for i in range(NR)]
        xn = [sb(f"xn{i}", [128, 256], F32) for i in range(NR)]
        zb = [sb(f"zb{i}", [128, 256], BF16) for i in range(NR)]
        zT = [sb(f"zT{i}", [128, 2, 128], BF16) for i in range(NR)]
        Tr = [S.T(f"p5_r{i}") for i in range(NR)]
        TzT = [S.T(f"p5_zT{i}") for i in range(NR)]
        yo = [sb(f"yo{i}", [128, 4, 256], F32) for i in range(2)]
        Tyo = [S.T(f"p5_yo{i}") for i in range(2)]
        it = 0
        for b in range(NB):
            for ct in range(2):
                S.dma("sp", Ub[:, ct, 32:32 + SEQ], k.scr["ucT"][b, ct * 128:(ct + 1) * 128, :], writes=[TUb[ct]])
            for n in range(NT):
                u = it % 2
                r_ = it % NR
                it += 1
                t0 = n * 128 + 2
                for ct in range(2):
                    csl = slice(ct * 128, (ct + 1) * 128)
                    S.op("pe", lambda e: e.matmul(pc[u][:, csl], lhsT=k.ones_b[0:1, 0:128], rhs=rowsb[0:1, 0, csl], start=True, stop=False),
                         reads=[k.Tones, Trows], writes=[Tpc[u]])
                    for kk in range(31):
                        S.op("pe", lambda e: e.matmul(pc[u][:, csl], lhsT=Ub[:, ct, t0 + kk:t0 + kk + 128], rhs=Dg[:, ct, kk, :], start=False, stop=(kk == 30)),
                             reads=[TUb[ct], TDg], writes=[Tpc[u]])
                T_r = Tr[r_]
                S.op("dve", lambda e: e.bn_stats(out=st[r_][:], in_=pc[u][:, 0:256]), reads=[Tpc[u]], writes=[T_r])
                S.op("dve", lambda e: e.bn_aggr(out=mv[r_][:], in_=st[r_][:]), reads=[T_r], writes=[T_r])
                S.op("dve", lambda e: e.tensor_scalar(out=rstd[r_][:], in0=mv[r_][:, 1:2], scalar1=EPS, scalar2=None, op0=ALU.add), reads=[T_r], writes=[T_r])
                S.op("act", lambda e: e.activation(out=rstd[r_][:], in_=rstd[r_][:], func=AF.Sqrt), reads=[T_r], writes=[T_r])
                S.op("dve", lambda e: e.reciprocal(out=rstd[r_][:], in_=rstd[r_][:]), reads=[T_r], writes=[T_r])
                S.op("dve", lambda e: e.tensor_scalar(out=xn[r_][:], in0=pc[u][:, 0:256], scalar1=mv[r_][:, 0:1], scalar2=rstd[r_][:, 0:1], op0=ALU.subtract, op1=ALU.mult),
                     reads=[Tpc[u], T_r], writes=[T_r])
                S.op("pool", lambda e: e.tensor_tensor(out=xn[r_][:], in0=xn[r_][:], in1=lng[:], op=ALU.mult), reads=[T_r, Tln], writes=[T_r])
                S.op("pool", lambda e: e.tensor_tensor(out=xn[r_][:], in0=xn[r_][:], in1=lnb[:], op=ALU.add), reads=[T_r, Tln2], writes=[T_r])
                S.op("act", lambda e: e.activation(out=zb[r_][:], in_=xn[r_][:], func=AF.Silu), reads=[T_r], writes=[T_r])
                for ct in range(2):
                    S.op("pe", lambda e: e.transpose(out=pz[u][:, ct * 128:(ct + 1) * 128], in_=zb[r_][:, ct * 128:(ct + 1) * 128], identity=k.identb[:]),
                         reads=[T_r, k.Tidb], writes=[Tpz[u]])
                S.op("act", lambda e: e.activation(out=zT[r_][:].rearrange("p c t -> p (c t)"), in_=pz[u][:], func=AF.Copy), reads=[Tpz[u]], writes=[TzT[r_]])
                S.op("pe", lambda e: e.matmul(po[u][:, 0:256], lhsT=k.ones_b[0:1, 0:128], rhs=rowsb[0:1, 1, :], start=True, stop=False),
                     reads=[k.Tones, Trows], writes=[Tpo[u]])
                for ct in range(2):
                    S.op("pe", lambda e: e.matmul(po[u][:, 0:256], lhsT=zT[r_][:, ct, :], rhs=wpb[:, ct, :], start=False, stop=(ct == 1)),
                         reads=[TzT[r_], Twp], writes=[Tpo[u]])
                yi = (n // 4) % 2
                S.op("dve", lambda e: e.tensor_copy(out=yo[yi][:, n % 4, :], in_=po[u][:, 0:256]), reads=[Tpo[u]], writes=[Tyo[yi]])
                if n % 4 == 3:
                    ydst = k.scr["ycat"][b].rearrange("(n p) c -> p n c", p=128)
                    k.store(ydst[:, n - 3:n + 1, 768:1024], yo[yi][:], [Tyo[yi]])
        S.barrier()


K.phase5 = phase5
PHASES.append("phase5")


def phase6a(self, l):
    k, S = self, self.S
    with contextlib.ExitStack() as es:
        sb = lambda n, sh, dt=F32: k.sb(es, "p6a_" + n, sh, dt)
        Wo = sb("Wo", [128, 8, D], BF16)
        TWo = [S.T(f"p6a_Wo{i}") for i in range(8)]
        stage = [sb(f"stage{i}", [128, D], F32) for i in range(2)]
        Tst = [S.T(f"p6a_stage{i}") for i in range(2)]
        gcat = sb("gcat", [128, 8], F32)
        Tg = S.T("p6a_gcat")
        S.dma("sp", gcat[:], k.inp["gcat_t"][l], writes=[Tg])
        wsrc = k.inp["w_out"][l].rearrange("(kc p) n -> p kc n", p=128)
        for kc in range(8):
            S.dma("sp", stage[kc % 2][:], wsrc[:, kc, :], writes=[Tst[kc % 2]])
            S.op("pool" if kc % 2 else "dve", lambda e: e.tensor_scalar(out=Wo[:, kc, :], in0=stage[kc % 2][:], scalar1=gcat[:, kc:kc + 1], scalar2=None, op0=ALU.mult),
                 reads=[Tst[kc % 2], Tg], writes=[TWo[kc]])
        yg = [sb(f"yg{i}", [128, 4, D], F32) for i in range(2)]
        Tyg = [S.T(f"p6a_yg{i}") for i in range(2)]
        xg = [sb(f"xg{i}", [128, 4, D], F32) for i in range(2)]
        Txg = [[S.T(f"p6a_xg{i}_{j}") for j in range(4)] for i in range(2)]
        junk = sb("junk", [128, 512], BF16)
        Tjunk = S.T("p6a_junk")
        ss = sb("ss", [128, 4, 4], F32)
        Tss = S.T("p6a_ss")
        mb = sb("mb", [128, 4, D], BF16)
        Tmb = [S.T(f"p6a_mb{j}") for j in range(4)]
        mT = sb("mT", [128, 8, 512], BF16)
        TmT = [S.T(f"p6a_mT{c}") for c in range(8)]
        hb = sb("hb", [128, 4, D], BF16)
        Thb = [S.T(f"p6a_hb{j}") for j in range(4)]
        hT = [sb(f"hT{i}", [128, 8, 512], BF16) for i in range(2)]
        ThT = [S.T(f"p6a_hT{i}") for i in range(2)]
        pT = [k.ps(es, f"p6a_pT{i}", [128, 512], BF16) for i in range(2)]
        TpT = [S.T(f"p6a_pT{i}") for i in range(2)]
        pm = [k.ps(es, f"p6a_pm{i}", [128, 512], F32) for i in range(4)]
        Tpm = [S.T(f"p6a_pm{i}") for i in range(4)]
        xin = k.inp["x"] if l == 0 else k.scr["xres"]
        segs = [(0, 512), (512, 768), (768, 1024)]
        gi = 0
        pmi = 0
        for b in range(NB):
            for g in range(NG):
                u = gi % 2
                gi += 1
                tsl = slice(g * 512, (g + 1) * 512)
                S.dma("sp", yg[u][:], k.scr["ycat"][b, tsl, :].rearrange("(j p) d -> p j d", p=128), writes=[Tyg[u]])
                for j in range(4):
                    S.dma("sp", xg[u][:, j, :], xin[b, g * 512 + j * 128:g * 512 + (j + 1) * 128, :], writes=[Txg[u][j]])
                for j in range(4):
                    for si, (a0, a1) in enumerate(segs):
                        S.op("act", lambda e: e.activation(out=junk[:, 0:a1 - a0], in_=yg[u][:, j, a0:a1], func=AF.Square, accum_out=ss[:, j, si:si + 1]),
                             reads=[Tyg[u]], writes=[Tjunk, Tss])
                for si, (a0, a1) in enumerate(segs):
                    S.op("dve", lambda e: e.tensor_scalar(out=ss[:, :, si], in0=ss[:, :, si], scalar1=1.0 / (a1 - a0), scalar2=EPS, op0=ALU.mult, op1=ALU.add), reads=[Tss], writes=[Tss])
                S.op("act", lambda e: e.activation(out=ss[:, :, 0:3], in_=ss[:, :, 0:3], func=AF.Sqrt), reads=[Tss], writes=[Tss])
                S.op("dve", lambda e: e.reciprocal(out=ss[:, :, 0:3], in_=ss[:, :, 0:3]), reads=[Tss], writes=[Tss])
                for j in range(4):
                    for si, (a0, a1) in enumerate(segs):
                        S.op("pool" if (si == 0) else "dve", lambda e: e.tensor_scalar(out=mb[:, j, a0:a1], in0=yg[u][:, j, a0:a1], scalar1=ss[:, j, si:si + 1], scalar2=None, op0=ALU.mult),
                             reads=[Tyg[u], Tss], writes=[Tmb[j]])
                for kc in range(8):
                    pu = kc % 2
                    for j in range(4):
                        S.op("pe", lambda e: e.transpose(out=pT[pu][:, j * 128:(j + 1) * 128], in_=mb[:, j, kc * 128:(kc + 1) * 128], identity=k.identb[:]),
                             reads=[Tmb[j], k.Tidb], writes=[TpT[pu]])
                    S.op("act" if kc % 2 else "dve", (lambda e: e.activation(out=mT[:, kc, :], in_=pT[pu][:], func=AF.Copy)) if kc % 2 else (lambda e: e.tensor_copy(out=mT[:, kc, :], in_=pT[pu][:])),
                         reads=[TpT[pu]], writes=[TmT[kc]])
                for j in range(4):
                    for half in range(2):
                        i = pmi % 4
                        pmi += 1
                        hsl = slice(half * 512, (half + 1) * 512)
                        for kc in range(8):
                            S.op("pe", lambda e: e.matmul(pm[i][:], lhsT=mT[:, kc, j * 128:(j + 1) * 128], rhs=Wo[:, kc, hsl], start=(kc == 0), stop=(kc == 7)),
                                 reads=[TmT[kc], TWo[kc]], writes=[Tpm[i]])
                        S.op("dve", lambda e: e.tensor_tensor(out=xg[u][:, j, hsl], in0=pm[i][:], in1=xg[u][:, j, hsl], op=ALU.add), reads=[Tpm[i], Txg[u][j]], writes=[Txg[u][j]])
                    k.store(k.scr["xres"][b, g * 512 + j * 128:g * 512 + (j + 1) * 128, :], xg[u][:, j, :], [Txg[u][j]])
                    S.op("act", lambda e: e.activation(out=junk[:, 0:512], in_=xg[u][:, j, 0:512], func=AF.Square, accum_out=ss[:, j, 3:4]),
                         reads=[Txg[u][j]], writes=[Tjunk, Tss])
                    S.op("act", lambda e: e.activation(out=junk[:, 0:512], in_=xg[u][:, j, 512:1024], func=AF.Square, accum_out=ss[:, j, 0:1]),
                         reads=[Txg[u][j]], writes=[Tjunk, Tss])
                S.op("dve", lambda e: e.tensor_tensor(out=ss[:, :, 3], in0=ss[:, :, 3], in1=ss[:, :, 0], op=ALU.add), reads=[Tss], writes=[Tss])
                S.op("dve", lambda e: e.tensor_scalar(out=ss[:, :, 3], in0=ss[:, :, 3], scalar1=1.0 / D, scalar2=EPS, op0=ALU.mult, op1=ALU.add), reads=[Tss], writes=[Tss])
                S.op("act", lambda e: e.activation(out=ss[:, :, 3], in_=ss[:, :, 3], func=AF.Sqrt), reads=[Tss], writes=[Tss])
                S.op("dve", lambda e: e.reciprocal(out=ss[:, :, 3], in_=ss[:, :, 3]), reads=[Tss], writes=[Tss])
                for j in range(4):
                    S.op("pool" if j % 2 else "dve", lambda e: e.tensor_scalar(out=hb[:, j, :], in0=xg[u][:, j, :], scalar1=ss[:, j, 3:4], scalar2=None, op0=ALU.mult),
                         reads=[Txg[u][j], Tss], writes=[Thb[j]])
                for kc in range(8):
                    pu = kc % 2
                    for j in range(4):
                        S.op("pe", lambda e: e.transpose(out=pT[pu][:, j * 128:(j + 1) * 128], in_=hb[:, j, kc * 128:(kc + 1) * 128], identity=k.identb[:]),
                             reads=[Thb[j], k.Tidb], writes=[TpT[pu]])
                    S.op("act" if kc % 2 else "dve", (lambda e: e.activation(out=hT[u][:, kc, :], in_=pT[pu][:], func=AF.Copy)) if kc % 2 else (lambda e: e.tensor_copy(out=hT[u][:, kc, :], in_=pT[pu][:])),
                         reads=[TpT[pu]], writes=[ThT[u]])
                k.store(k.scr["h2T"][b, :, tsl].rearrange("(kc p) t -> p kc t", p=128), hT[u][:], [ThT[u]])
        S.barrier()


def phase6b(self, l):
    k, S = self, self.S
    with contextlib.ExitStack() as es:
        sb = lambda n, sh, dt=F32: k.sb(es, "p6b_" + n, sh, dt)
        Wg = sb("Wg", [128, 8, DFF], BF16)
        Wu = sb("Wu", [128, 8, DFF], BF16)
        Wd = sb("Wd", [128, NFF, D], BF16)
        TWg = [S.T(f"p6b_Wg{i}") for i in range(8)]
        TWu = [S.T(f"p6b_Wu{i}") for i in range(8)]
        TWd = [S.T(f"p6b_Wd{i}") for i in range(NFF)]
        with contextlib.ExitStack() as es2:
            stage = [k.sb(es2, f"p6b_stage{i}", [128, DFF], F32) for i in range(2)]
            Tst = [S.T(f"p6b_stage{i}") for i in range(2)]
            gn = k.sb(es2, "p6b_gn", [128, 8], F32)
            Tgn = S.T("p6b_gn")
            S.dma("sp", gn[:], k.inp["norm_ffn_t"][l], writes=[Tgn])
            si = 0
            for nm, Wt, TWt in [("w_gate", Wg, TWg), ("w_up", Wu, TWu)]:
                wsrc = k.inp[nm][l].rearrange("(kc p) n -> p kc n", p=128)
                for kc in range(8):
                    u = si % 2
                    si += 1
                    S.dma("sp", stage[u][:], wsrc[:, kc, :], writes=[Tst[u]])
                    S.op("pool" if u else "dve", lambda e: e.tensor_scalar(out=Wt[:, kc, :], in0=stage[u][:], scalar1=gn[:, kc:kc + 1], scalar2=None, op0=ALU.mult),
                         reads=[Tst[u], Tgn], writes=[TWt[kc]])
            wsrc = k.inp["w_down"][l].rearrange("(fc p) n -> p fc n", p=128)
            for fc in range(0, NFF, 2):
                u = si % 2
                si += 1
                S.dma("sp", stage[u][:, 0:2 * D].rearrange("p (a n) -> p a n", a=2), wsrc[:, fc:fc + 2, :], writes=[Tst[u]])
                S.op("pool" if u else "act", (lambda e: e.tensor_copy(out=Wd[:, fc:fc + 2, :].rearrange("p a n -> p (a n)"), in_=stage[u][:, 0:2 * D])) if u else
                     (lambda e: e.activation(out=Wd[:, fc:fc + 2, :].rearrange("p a n -> p (a n)"), in_=stage[u][:, 0:2 * D], func=AF.Copy)),
                     reads=[Tst[u]], writes=[TWd[fc], TWd[fc + 1]])
            S.barrier()
        hT = [sb(f"hT{i}", [128, 8, 512], BF16) for i in range(2)]
        ThT = [S.T(f"p6b_hT{i}") for i in range(2)]
        NX = 3
        xt = [sb(f"xt{i}", [128, D], F32) for i in range(NX)]
        Txt = [S.T(f"p6b_xt{i}") for i in range(NX)]
        actT = sb("actT", [128, NFF, 512], BF16)
        Tact = [S.T(f"p6b_act{i}") for i in range(NFF)]
        sg = [sb(f"sg{i}", [128, 512], BF16) for i in range(2)]
        Tsg = [S.T(f"p6b_sg{i}") for i in range(2)]
        pg = [k.ps(es, f"p6b_pg{i}", [128, 512], F32) for i in range(2)]
        pu_ = [k.ps(es, f"p6b_pu{i}", [128, 512], F32) for i in range(2)]
        pd = [k.ps(es, f"p6b_pd{i}", [128, 512], F32) for i in range(2)]
        Tpg = [S.T(f"p6b_pg{i}") for i in range(2)]
        Tpu = [S.T(f"p6b_pu{i}") for i in range(2)]
        Tpd = [S.T(f"p6b_pd{i}") for i in range(2)]
        gi = 0
        xi = 0
        pdi = 0
        for b in range(NB):
            for g in range(NG):
                u = gi % 2
                gi += 1
                tsl = slice(g * 512, (g + 1) * 512)
                S.dma("sp", hT[u][:], k.scr["h2T"][b, :, tsl].rearrange("(kc p) t -> p kc t", p=128), writes=[ThT[u]])
                for fc in range(NFF):
                    v = fc % 2
                    fsl = slice(fc * 128, (fc + 1) * 128)
                    for kc in range(8):
                        S.op("pe", lambda e: e.matmul(pg[v][:], lhsT=Wg[:, kc, fsl], rhs=hT[u][:, kc, :], start=(kc == 0), stop=(kc == 7)),
                             reads=[TWg[kc], ThT[u]], writes=[Tpg[v]])
                    for kc in range(8):
                        S.op("pe", lambda e: e.matmul(pu_[v][:], lhsT=Wu[:, kc, fsl], rhs=hT[u][:, kc, :], start=(kc == 0), stop=(kc == 7)),
                             reads=[TWu[kc], ThT[u]], writes=[Tpu[v]])
                    S.op("act", lambda e: e.activation(out=sg[v][:], in_=pg[v][:], func=AF.Silu), reads=[Tpg[v]], writes=[Tsg[v]])
                    S.op("dve", lambda e: e.tensor_tensor(out=actT[:, fc, :], in0=pu_[v][:], in1=sg[v][:], op=ALU.mult), reads=[Tpu[v], Tsg[v]], writes=[Tact[fc]])
                for j in range(4):
                    xx = xi % NX
                    xi += 1
                    r0 = g * 512 + j * 128
                    S.dma("sp", xt[xx][:], k.scr["xres"][b, r0:r0 + 128, :], writes=[Txt[xx]])
                    for half in range(2):
                        pi = pdi % 2
                        pdi += 1
                        hsl = slice(half * 512, (half + 1) * 512)
                        for fc in range(NFF):
                            S.op("pe", lambda e: e.matmul(pd[pi][:], lhsT=actT[:, fc, j * 128:(j + 1) * 128], rhs=Wd[:, fc, hsl], start=(fc == 0), stop=(fc == NFF - 1)),
                                 reads=[Tact[fc], TWd[fc]], writes=[Tpd[pi]])
                        S.op("dve", lambda e: e.tensor_tensor(out=xt[xx][:, hsl], in0=pd[pi][:], in1=xt[xx][:, hsl], op=ALU.add), reads=[Tpd[pi], Txt[xx]], writes=[Txt[xx]])
                    k.store(k.scr["xres"][b, r0:r0 + 128, :], xt[xx][:], [Txt[xx]])
        S.barrier()


K.phase6a = phase6a
K.phase6b = phase6b


def phase4(self, l):
    k, S = self, self.S
    PI = float(np.pi)
    C1 = float(np.float32(2 * np.pi))
    C2 = float(2 * np.pi - float(np.float32(2 * np.pi)))
    with contextlib.ExitStack() as es:
        sb = lambda n, sh, dt=F32: k.sb(es, "p4_" + n, sh, dt)
        Tp = S.T("p4_par")
        are, aim, ldt = sb("are", [128, 8]), sb("aim", [128, 8]), sb("ldt", [128, 8])
        S.dma("sp", are[:], k.inp["ssm_are_t"][l], writes=[Tp])
        Tp2 = S.T("p4_par2")
        S.dma("sp", aim[:], k.inp["ssm_aim_t"][l], writes=[Tp2])
        Tp3 = S.T("p4_par3")
        S.dma("sp", ldt[:], k.inp["ssm_ldt_t"][l], writes=[Tp3])
        dtt, z, th, mag, q = sb("dtt", [128, 8]), sb("z", [128, 8]), sb("th", [128, 8]), sb("mag", [128, 8]), sb("q", [128, 8])

        def dv(fn, reads=(), writes=(Tp,)):
            S.op("dve", fn, reads=list(reads) + [Tp], writes=list(writes))

        S.op("act", lambda e: e.activation(out=dtt[:], in_=ldt[:], func=AF.Exp), reads=[Tp3], writes=[Tp])
        dv(lambda e: e.tensor_tensor(out=z[:], in0=are[:], in1=dtt[:], op=ALU.mult))
        dv(lambda e: e.tensor_tensor(out=th[:], in0=aim[:], in1=dtt[:], op=ALU.mult), reads=[Tp2])
        dv(lambda e: e.tensor_scalar(out=q[:], in0=z[:], scalar1=1.0 / 6.0, scalar2=1.0, op0=ALU.mult, op1=ALU.add))
        for kk in (5.0, 4.0, 3.0, 2.0, 1.0):
            dv(lambda e: e.tensor_tensor(out=q[:], in0=q[:], in1=z[:], op=ALU.mult))
            dv(lambda e: e.tensor_scalar(out=q[:], in0=q[:], scalar1=1.0 / kk, scalar2=1.0, op0=ALU.mult, op1=ALU.add))
        dv(lambda e: e.tensor_copy(out=mag[:], in_=q[:]))

        iot = sb("iota", [128, 512])
        Tio = S.T("p4_iota")
        S.dma("sp", iot[:], k.inp["iota512"].partition_broadcast(128), writes=[Tio])
        Ct = sb("Ct", [128, 8, 512])
        St = sb("St", [128, 8, 512])
        TCt = [S.T(f"p4_Ct{m}") for m in range(8)]
        ang = [sb(f"ang{i}", [128, 512]) for i in range(2)]
        kf = [sb(f"kf{i}", [128, 512]) for i in range(2)]
        ki = [sb(f"ki{i}", [128, 512], mybir.dt.int32) for i in range(2)]
        Tang = [S.T(f"p4_ang{i}") for i in range(2)]
        ai = 0
        for m in range(8):
            for which in range(2):
                a_ = ai % 2
                ai += 1
                eng = "dve" if which == 0 else "pool"
                Ta = Tang[a_]
                A, KF, KI = ang[a_], kf[a_], ki[a_]
                off = 0.0 if which == 0 else PI / 2
                S.op("dve", lambda e: e.tensor_scalar(out=A[:], in0=iot[:], scalar1=th[:, m:m + 1], scalar2=off, op0=ALU.mult, op1=ALU.add), reads=[Tio, Tp], writes=[Ta])
                S.op("dve", lambda e: e.tensor_scalar(out=KI[:], in0=A[:], scalar1=1.0 / (2 * PI), scalar2=None, op0=ALU.mult), reads=[Ta], writes=[Ta])
                S.op("dve", lambda e: e.tensor_copy(out=KF[:], in_=KI[:]), reads=[Ta], writes=[Ta])
                S.op("dve", lambda e: e.scalar_tensor_tensor(out=A[:], in0=KF[:], scalar=-C1, in1=A[:], op0=ALU.mult, op1=ALU.add), reads=[Ta], writes=[Ta])
                S.op("dve", lambda e: e.scalar_tensor_tensor(out=A[:], in0=KF[:], scalar=-C2, in1=A[:], op0=ALU.mult, op1=ALU.add), reads=[Ta], writes=[Ta])
                S.op("dve", lambda e: e.tensor_scalar(out=KF[:], in0=A[:], scalar1=PI, scalar2=-2 * PI, op0=ALU.is_gt, op1=ALU.mult), reads=[Ta], writes=[Ta])
                S.op("dve", lambda e: e.tensor_tensor(out=A[:], in0=A[:], in1=KF[:], op=ALU.add), reads=[Ta], writes=[Ta])
                S.op("dve", lambda e: e.tensor_scalar(out=KF[:], in0=A[:], scalar1=-PI, scalar2=2 * PI, op0=ALU.is_lt, op1=ALU.mult), reads=[Ta], writes=[Ta])
                S.op("dve", lambda e: e.tensor_tensor(out=A[:], in0=A[:], in1=KF[:], op=ALU.add), reads=[Ta], writes=[Ta])
                S.op("dve", lambda e: e.tensor_scalar(out=A[:], in0=A[:], scalar1=PI, scalar2=-PI, op0=ALU.min, op1=ALU.max), reads=[Ta], writes=[Ta])
                dst = St if which == 0 else Ct
                S.op("act", lambda e: e.activation(out=dst[:, m, :], in_=A[:], func=AF.Sin), reads=[Ta], writes=[TCt[m]])
        c1, s1, ns1 = sb("c1", [128, 8]), sb("s1", [128, 8]), sb("ns1", [128, 8])
        TC1 = S.T("p4_c1")
        S.op("dve", lambda e: e.tensor_copy(out=c1[:], in_=Ct[:, :, 1]), reads=TCt, writes=[TC1])
        S.op("dve", lambda e: e.tensor_copy(out=s1[:], in_=St[:, :, 1]), reads=TCt, writes=[TC1])
        S.op("dve", lambda e: e.tensor_scalar(out=ns1[:], in0=s1[:], scalar1=-1.0, scalar2=None, op0=ALU.mult), reads=[TC1], writes=[TC1])
        lre, lim, den, nr, fre, fim, nfim, t8 = [sb(n, [128, 8]) for n in ("lre", "lim", "den", "nr", "fre", "fim", "nfim", "t8")]

        def d2(fn):
            S.op("dve", fn, reads=[Tp, Tp2, TC1], writes=[TC1])

        d2(lambda e: e.tensor_tensor(out=lre[:], in0=mag[:], in1=c1[:], op=ALU.mult))
        d2(lambda e: e.tensor_tensor(out=lim[:], in0=mag[:], in1=s1[:], op=ALU.mult))
        d2(lambda e: e.tensor_tensor(out=den[:], in0=are[:], in1=are[:], op=ALU.mult))
        d2(lambda e: e.tensor_tensor(out=t8[:], in0=aim[:], in1=aim[:], op=ALU.mult))
        d2(lambda e: e.tensor_tensor(out=den[:], in0=den[:], in1=t8[:], op=ALU.add))
        d2(lambda e: e.reciprocal(out=den[:], in_=den[:]))
        d2(lambda e: e.tensor_scalar(out=nr[:], in0=lre[:], scalar1=-1.0, scalar2=None, op0=ALU.add))
        d2(lambda e: e.tensor_tensor(out=fre[:], in0=nr[:], in1=are[:], op=ALU.mult))
        d2(lambda e: e.tensor_tensor(out=t8[:], in0=lim[:], in1=aim[:], op=ALU.mult))
        d2(lambda e: e.tensor_tensor(out=fre[:], in0=fre[:], in1=t8[:], op=ALU.add))
        d2(lambda e: e.tensor_tensor(out=fre[:], in0=fre[:], in1=den[:], op=ALU.mult))
        d2(lambda e: e.tensor_tensor(out=fim[:], in0=lim[:], in1=are[:], op=ALU.mult))
        d2(lambda e: e.tensor_tensor(out=t8[:], in0=nr[:], in1=aim[:], op=ALU.mult))
        d2(lambda e: e.tensor_tensor(out=fim[:], in0=fim[:], in1=t8[:], op=ALU.subtract))
        d2(lambda e: e.tensor_tensor(out=fim[:], in0=fim[:], in1=den[:], op=ALU.mult))
        d2(lambda e: e.tensor_scalar(out=nfim[:], in0=fim[:], scalar1=-1.0, scalar2=None, op0=ALU.mult))
        Mre = sb("Mre", [128, 8, 128], BF16)
        Mim = sb("Mim", [128, 8, 128], BF16)
        Cre = sb("Cre", [128, 8, 128], BF16)
        Cim = sb("Cim", [128, 8, 128], BF16)
        TM = S.T("p4_M")
        TCp = S.T("p4_Cp")
        pset = k.ps(es, "p4_pset", [128, 512], BF16)
        Tpset = S.T("p4_pset")
        with contextlib.ExitStack() as es2:
            zbr = k.sb(es2, "p4_zbr", [128, 8, 128], F32)
            zbi = k.sb(es2, "p4_zbi", [128, 8, 128], F32)
            Tz1, Tz2 = S.T("p4_zbr"), S.T("p4_zbi")
            tt = k.sb(es2, "p4_tt", [128, 128], F32)
            bbr = k.sb(es2, "p4_bbr", [128, 128], BF16)
            bbi = k.sb(es2, "p4_bbi", [128, 128], BF16)
            Tbb = S.T("p4_bb")
            S.dma("sp", zbr[:], k.inp["Zb_re"][l], writes=[Tz1])
            S.dma("sp", zbi[:], k.inp["Zb_im"][l], writes=[Tz2])
            for m in range(8):
                S.op("dve", lambda e: e.tensor_scalar(out=tt[:], in0=zbr[:, m, :], scalar1=fre[:, m:m + 1], scalar2=None, op0=ALU.mult), reads=[Tz1, TC1], writes=[Tbb])
                S.op("dve", lambda e: e.scalar_tensor_tensor(out=bbr[:], in0=zbi[:, m, :], scalar=nfim[:, m:m + 1], in1=tt[:], op0=ALU.mult, op1=ALU.add), reads=[Tz2, TC1, Tbb], writes=[Tbb])
                S.op("dve", lambda e: e.tensor_scalar(out=tt[:], in0=zbi[:, m, :], scalar1=fre[:, m:m + 1], scalar2=None, op0=ALU.mult), reads=[Tz2, TC1, Tbb], writes=[Tbb])
                S.op("dve", lambda e: e.scalar_tensor_tensor(out=bbi[:], in0=zbr[:, m, :], scalar=fim[:, m:m + 1], in1=tt[:], op0=ALU.mult, op1=ALU.add), reads=[Tz1, TC1, Tbb], writes=[Tbb])
                S.op("pe", lambda e: e.transpose(out=pset[:, 0:128], in_=bbr[:], identity=k.identb[:]), reads=[Tbb, k.Tidb], writes=[Tpset])
                S.op("pe", lambda e: e.transpose(out=pset[:, 128:256], in_=bbi[:], identity=k.identb[:]), reads=[Tbb, k.Tidb], writes=[Tpset])
                S.op("act", lambda e: e.activation(out=Mre[:, m, :], in_=pset[:, 0:128], func=AF.Copy), reads=[Tpset], writes=[TM])
                S.op("act", lambda e: e.activation(out=Mim[:, m, :], in_=pset[:, 128:256], func=AF.Copy), reads=[Tpset], writes=[TM])
            S.dma("sp", zbr[:], k.inp["Cp_re"][l], writes=[Tz1])
            S.dma("sp", zbi[:], k.inp["Cp_im"][l], writes=[Tz2])
            S.op("dve", lambda e: e.tensor_copy(out=Cre[:], in_=zbr[:]), reads=[Tz1], writes=[TCp])
            S.op("dve", lambda e: e.tensor_scalar(out=Cim[:], in0=zbi[:], scalar1=-1.0, scalar2=None, op0=ALU.mult), reads=[Tz2], writes=[TCp])
            S.barrier()
        dvec = sb("dvec", [128, 2])
        Td = S.T("p4_d")
        S.dma("sp", dvec[:], k.inp["ssm_d_t"][l], writes=[Td])
        wgs = sb("wgs", [128, 2, 512])
        wgb = sb("wgb", [128, 2, 512], BF16)
        Twg = S.T("p4_wg")
        S.dma("sp", wgs[:], k.inp["ssm_w_glu"][l].rearrange("(mt p) n -> p mt n", p=128), writes=[Twg])
        S.op("dve", lambda e: e.tensor_copy(out=wgb[:], in_=wgs[:]), reads=[Twg], writes=[Twg])
        U = sb("U", [128, 2, SEQ], BF16)
        TU = [S.T(f"p4_U{i}") for i in range(2)]
        NW = 2
        pw = [[k.ps(es, f"p4_pw{i}_{c}", [128, 512], F32) for c in range(2)] for i in range(NW)]
        Tpw = [[S.T(f"p4_pw{i}_{c}") for c in range(2)] for i in range(NW)]
        py = [k.ps(es, f"p4_py{i}", [128, 512], F32) for i in range(2)]
        Tpy = [S.T(f"p4_py{i}") for i in range(2)]
        pgl = k.ps(es, "p4_pgl", [128, 512], F32)
        Tpgl = S.T("p4_pgl")
        ta = [sb(f"ta{i}", [128, 512]) for i in range(NW)]
        tb_ = [sb(f"tb{i}", [128, 512]) for i in range(NW)]
        vre = [sb(f"vre{i}", [128, 512]) for i in range(NW)]
        vim = [sb(f"vim{i}", [128, 512]) for i in range(NW)]
        sre = [sb(f"sre{i}", [128, 512]) for i in range(NW)]
        sim_ = [sb(f"sim{i}", [128, 512]) for i in range(NW)]
        xre = [sb(f"xre{i}", [128, 512], BF16) for i in range(NW)]
        xim = [sb(f"xim{i}", [128, 512], BF16) for i in range(NW)]
        Twk = [S.T(f"p4_wk{i}") for i in range(NW)]
        Tv = [S.T(f"p4_v{i}") for i in range(NW)]
        Ts = [S.T(f"p4_s{i}") for i in range(NW)]
        Tx = [S.T(f"p4_x{i}") for i in range(NW)]
        init = sb("init", [128, 8, 2])
        Tinit = [S.T(f"p4_init{m}") for m in range(8)]
        xl = sb("xl", [128, 8, 2])
        i4 = sb("i4", [128, 8, 2])
        yf = [sb(f"yf{i}", [128, 512]) for i in range(2)]
        gT = [sb(f"gT{i}", [128, 512], BF16) for i in range(2)]
        TgT = [S.T(f"p4_gT{i}") for i in range(2)]
        Tyf = [S.T(f"p4_yf{i}") for i in range(2)]
        sgl = [sb(f"sgl{i}", [128, 256]) for i in range(2)]
        Tsgl = [S.T(f"p4_sgl{i}") for i in range(2)]
        yo = [sb(f"yo{i}", [128, 4, 256]) for i in range(2)]
        Tyo = [S.T(f"p4_yo{i}") for i in range(2)]
        wi = 0
        gi = 0
        for b in range(NB):
            for mt in range(2):
                S.dma("sp", U[:, mt, :], k.scr["usT"][b, mt * 128:(mt + 1) * 128, :], writes=[TU[mt]])
            for m in range(8):
                S.op("pool", lambda e: e.memset(init[:, m, :], 0.0), writes=[Tinit[m]])
            for g in range(NG):
                tsl = slice(g * 512, (g + 1) * 512)
                for mt in range(2):
                    yb = (g * 2 + mt) % 2
                    for m in range(4 * mt, 4 * mt + 4):
                        w = wi % NW
                        wi += 1
                        Cm, Sm = Ct[:, m, :], St[:, m, :]
                        S.op("pe", lambda e: e.matmul(pw[w][0][:], lhsT=Mre[:, m, :], rhs=U[:, mt, tsl], start=True, stop=True), reads=[TM, TU[mt]], writes=[Tpw[w][0]])
                        S.op("pe", lambda e: e.matmul(pw[w][1][:], lhsT=Mim[:, m, :], rhs=U[:, mt, tsl], start=True, stop=True), reads=[TM, TU[mt]], writes=[Tpw[w][1]])
                        S.op("dve", lambda e: e.tensor_tensor(out=ta[w][:], in0=pw[w][0][:], in1=Cm, op=ALU.mult), reads=[Tpw[w][0], TCt[m]], writes=[Twk[w]])
                        S.op("dve", lambda e: e.tensor_tensor(out=tb_[w][:], in0=pw[w][1][:], in1=Sm, op=ALU.mult), reads=[Tpw[w][1], TCt[m]], writes=[Twk[w]])
                        S.op("pool", lambda e: e.tensor_tensor(out=vre[w][:], in0=ta[w][:], in1=tb_[w][:], op=ALU.add), reads=[Twk[w]], writes=[Tv[w]])
                        S.op("dve", lambda e: e.tensor_tensor(out=ta[w][:], in0=pw[w][1][:], in1=Cm, op=ALU.mult), reads=[Tpw[w][1], TCt[m], Tv[w]], writes=[Twk[w]])
                        S.op("dve", lambda e: e.tensor_tensor(out=tb_[w][:], in0=pw[w][0][:], in1=Sm, op=ALU.mult), reads=[Tpw[w][0], TCt[m], Tv[w]], writes=[Twk[w]])
                        S.op("pool", lambda e: e.tensor_tensor(out=vim[w][:], in0=ta[w][:], in1=tb_[w][:], op=ALU.subtract), reads=[Twk[w]], writes=[Tv[w]])
                        S.op("dve", lambda e: e.tensor_tensor_scan(out=sre[w][:], data0=mag[:, m:m + 1].broadcast_to([128, 512]), data1=vre[w][:], initial=init[:, m, 0:1], op0=ALU.mult, op1=ALU.add),
                             reads=[Tv[w], Tp, Tinit[m]], writes=[Ts[w]])
                        S.op("dve", lambda e: e.tensor_tensor_scan(out=sim_[w][:], data0=mag[:, m:m + 1].broadcast_to([128, 512]), data1=vim[w][:], initial=init[:, m, 1:2], op0=ALU.mult, op1=ALU.add),
                             reads=[Tv[w], Tp, Tinit[m]], writes=[Ts[w]])
                        S.op("pool", lambda e: e.tensor_tensor(out=i4[:, m, 0:1], in0=sre[w][:, 511:512], in1=Ct[:, m, 511:512], op=ALU.mult), reads=[Ts[w], TCt[m]], writes=[Tinit[m]])
                        S.op("pool", lambda e: e.tensor_tensor(out=i4[:, m, 1:2], in0=sim_[w][:, 511:512], in1=St[:, m, 511:512], op=ALU.mult), reads=[Ts[w], TCt[m]], writes=[Tinit[m]])
                        S.op("pool", lambda e: e.tensor_tensor(out=xl[:, m, 0:1], in0=i4[:, m, 0:1], in1=i4[:, m, 1:2], op=ALU.subtract), reads=[Tinit[m]], writes=[Tinit[m]])
                        S.op("pool", lambda e: e.tensor_tensor(out=i4[:, m, 0:1], in0=sim_[w][:, 511:512], in1=Ct[:, m, 511:512], op=ALU.mult), reads=[Ts[w], TCt[m]], writes=[Tinit[m]])
                        S.op("pool", lambda e: e.tensor_tensor(out=i4[:, m, 1:2], in0=sre[w][:, 511:512], in1=St[:, m, 511:512], op=ALU.mult), reads=[Ts[w], TCt[m]], writes=[Tinit[m]])
                        S.op("pool", lambda e: e.tensor_tensor(out=xl[:, m, 1:2], in0=i4[:, m, 0:1], in1=i4[:, m, 1:2], op=ALU.add), reads=[Tinit[m]], writes=[Tinit[m]])
                        S.op("pool", lambda e: e.tensor_tensor(out=i4[:, m, 0:1], in0=xl[:, m, 0:1], in1=c1[:, m:m + 1], op=ALU.mult), reads=[Tinit[m], TC1], writes=[Tinit[m]])
                        S.op("pool", lambda e: e.tensor_tensor(out=i4[:, m, 1:2], in0=xl[:, m, 1:2], in1=s1[:, m:m + 1], op=ALU.mult), reads=[Tinit[m], TC1], writes=[Tinit[m]])
                        S.op("pool", lambda e: e.tensor_tensor(out=init[:, m, 0:1], in0=i4[:, m, 0:1], in1=i4[:, m, 1:2], op=ALU.subtract), reads=[Tinit[m]], writes=[Tinit[m]])
                        S.op("pool", lambda e: e.tensor_tensor(out=i4[:, m, 0:1], in0=xl[:, m, 1:2], in1=c1[:, m:m + 1], op=ALU.mult), reads=[Tinit[m], TC1], writes=[Tinit[m]])
                        S.op("pool", lambda e: e.tensor_tensor(out=i4[:, m, 1:2], in0=xl[:, m, 0:1], in1=s1[:, m:m + 1], op=ALU.mult), reads=[Tinit[m], TC1], writes=[Tinit[m]])
                        S.op("pool", lambda e: e.tensor_tensor(out=init[:, m, 1:2], in0=i4[:, m, 0:1], in1=i4[:, m, 1:2], op=ALU.add), reads=[Tinit[m]], writes=[Tinit[m]])
                        S.op("pool", lambda e: e.tensor_tensor(out=ta[w][:], in0=sre[w][:], in1=Cm, op=ALU.mult), reads=[Ts[w], TCt[m]], writes=[Twk[w]])
                        S.op("pool", lambda e: e.tensor_tensor(out=tb_[w][:], in0=sim_[w][:], in1=Sm, op=ALU.mult), reads=[Ts[w], TCt[m]], writes=[Twk[w]])
                        S.op("pool", lambda e: e.tensor_tensor(out=xre[w][:], in0=ta[w][:], in1=tb_[w][:], op=ALU.subtract), reads=[Twk[w]], writes=[Tx[w]])
                        S.op("dve", lambda e: e.tensor_tensor(out=vre[w][:], in0=sim_[w][:], in1=Cm, op=ALU.mult), reads=[Ts[w], TCt[m]], writes=[Tv[w]])
                        S.op("dve", lambda e: e.tensor_tensor(out=vim[w][:], in0=sre[w][:], in1=Sm, op=ALU.mult), reads=[Ts[w], TCt[m]], writes=[Tv[w]])
                        S.op("pool", lambda e: e.tensor_tensor(out=xim[w][:], in0=vre[w][:], in1=vim[w][:], op=ALU.add), reads=[Tv[w]], writes=[Tx[w]])
                        first = (m == 4 * mt)
                        last = (m == 4 * mt + 3)
                        S.op("pe", lambda e: e.matmul(py[yb][:], lhsT=Cre[:, m, :], rhs=xre[w][:], start=first, stop=False), reads=[TCp, Tx[w]], writes=[Tpy[yb]])
                        S.op("pe", lambda e: e.matmul(py[yb][:], lhsT=Cim[:, m, :], rhs=xim[w][:], start=False, stop=last), reads=[TCp, Tx[w]], writes=[Tpy[yb]])
                    S.op("dve", lambda e: e.scalar_tensor_tensor(out=yf[mt][:], in0=U[:, mt, tsl], scalar=dvec[:, mt:mt + 1], in1=py[yb][:], op0=ALU.mult, op1=ALU.add),
                         reads=[TU[mt], Td, Tpy[yb]], writes=[Tyf[mt]])
                    S.op("act", lambda e: e.activation(out=gT[mt][:], in_=yf[mt][:], func=AF.Gelu_apprx_tanh), reads=[Tyf[mt]], writes=[TgT[mt]])
                yi = gi % 2
                gi += 1
                for j in range(4):
                    for mt in range(2):
                        S.op("pe", lambda e: e.matmul(pgl[:], lhsT=gT[mt][:, j * 128:(j + 1) * 128], rhs=wgb[:, mt, :], start=(mt == 0), stop=(mt == 1)),
                             reads=[TgT[mt], Twg], writes=[Tpgl])
                    sj = j % 2
                    S.op("act", lambda e: e.activation(out=sgl[sj][:], in_=pgl[:, 256:512], func=AF.Sigmoid), reads=[Tpgl], writes=[Tsgl[sj]])
                    S.op("dve", lambda e: e.tensor_tensor(out=yo[yi][:, j, :], in0=pgl[:, 0:256], in1=sgl[sj][:], op=ALU.mult), reads=[Tpgl, Tsgl[sj]], writes=[Tyo[yi]])
                ydst = k.scr["ycat"][b].rearrange("(n p) c -> p n c", p=128)
                k.store(ydst[:, 4 * g:4 * g + 4, 512:768], yo[yi][:], [Tyo[yi]])
        S.barrier()


K.phase4 = phase4
PHASES.extend(["phase4", "phase6a", "phase6b"])
```

```python
import contextlib
import numpy as np
import ml_dtypes
import concourse.bass as bass
import concourse.mybir as mybir
from concourse.bass_utils import run_bass_kernel_spmd

F32 = mybir.dt.float32
BF16 = mybir.dt.bfloat16
AF = mybir.ActivationFunctionType
ALU = mybir.AluOpType
AX = mybir.AxisListType

SEM_L = 20000
DMA_L = 16 * 1200
NB = 2
SEQ = 4096
D = 1024
NT = SEQ // 128
NG = SEQ // 512
DFF = 2816
NFF = DFF // 128
EPS = 1e-6
NEG = -30000.0
L = 2


class T:
    __slots__ = ("name", "w", "r", "sem", "semv")

    def __init__(self, name=""):
        self.name = name
        self.w = None
        self.r = {}
        self.sem = None
        self.semv = 0


class Sched:
    def __init__(self, nc):
        self.nc = nc
        self.h = {"pe": nc.tensor, "act": nc.scalar, "dve": nc.vector,
                  "pool": nc.gpsimd, "sp": nc.sync}
        self.cnt = {k: 0 for k in self.h}
        self.sems = {k: [] for k in self.h}
        self.waited = {}
        self.same = {"dve", "act", "pool"}
        self.nsem = 0
        self.ninst = 0
        self.all_T = []

    def T(self, name=""):
        t = T(name)
        self.all_T.append(t)
        return t

    def new_sem(self, name):
        self.nsem += 1
        return self.nc.alloc_semaphore(name + f"_{self.nsem}")

    def dma_sem(self, d):
        free = getattr(self, "free_sems", None)
        if free:
            d.sem, d.semv = free.pop()
        else:
            d.sem, d.semv = self.new_sem("d"), 0

    def _eng_sem(self, e):
        idx = self.cnt[e] // SEM_L
        while len(self.sems[e]) <= idx:
            self.sems[e].append(self.new_sem(f"s_{e}"))
        return self.sems[e][idx]

    def _wait(self, e, tok):
        if tok is None:
            return
        sem, val, src = tok
        if src == e and e not in self.same:
            return
        key = (e, id(sem))
        if self.waited.get(key, 0) >= val:
            return
        self.waited[key] = val
        self.h[e].wait_ge(sem, val)

    def _deps(self, e, reads, writes):
        for t in reads:
            self._wait(e, t.w)
        for t in writes:
            self._wait(e, t.w)
            for tok in t.r.values():
                self._wait(e, tok)

    def op(self, e, fn, reads=(), writes=()):
        self._deps(e, reads, writes)
        sem = self._eng_sem(e)
        val = self.cnt[e] % SEM_L + 1
        fn(self.h[e]).then_inc(sem, 1)
        self.cnt[e] += 1
        self.ninst += 1
        tok = (sem, val, e)
        for t in reads:
            t.r[id(sem)] = tok
        for t in writes:
            t.w = tok
            t.r = {}
        return tok

    def dma(self, q, out, in_, reads=(), writes=(), **kw):
        self._deps(q, reads, writes)
        d = writes[0]
        if d.sem is None or d.semv >= DMA_L:
            self.dma_sem(d)
        d.semv += 16
        self.h[q].dma_start(out=out, in_=in_, **kw).then_inc(d.sem, 16)
        self.ninst += 1
        tok = (d.sem, d.semv, "dma")
        for t in reads:
            t.r[id(d.sem)] = tok
        d.w = tok
        d.r = {}
        return tok

    def barrier(self):
        e0 = "dve"
        for t in self.all_T:
            self._wait(e0, t.w)
            for tok in t.r.values():
                self._wait(e0, tok)
            t.r = {}
        for e in self.h:
            if self.cnt[e] > 0 and e != e0:
                idx = (self.cnt[e] - 1) // SEM_L
                self._wait(e0, (self.sems[e][idx], (self.cnt[e] - 1) % SEM_L + 1, e))
        if not hasattr(self, "Tbar"):
            self.Tbar = T("bar")
        tok = self.op(e0, lambda h: h.memset(self.bar_tile[:], 0.0), writes=[self.Tbar])
        for e in self.h:
            if e != e0:
                self._wait(e, tok)
        if not hasattr(self, "free_sems"):
            self.free_sems = []
        for t in self.all_T:
            t.w = None
            t.r = {}
            if t.sem is not None:
                if t.semv < DMA_L - 4096:
                    self.free_sems.append((t.sem, t.semv))
                t.sem = None
                t.semv = 0


class K:
    def __init__(self, debug=False, phases=None, nlayers=L):
        self.debug = debug
        self.phases = phases
        self.nlayers = nlayers
        nc = bass.Bass("TRN2", target_bir_lowering=False)
        self.nc = nc
        self.S = Sched(nc)
        self.inp = {}
        self.scr = {}
        self.S.bar_tile = nc.alloc_sbuf_tensor("bar_tile", [128, 8], F32)
        self.st_i = 0
        self.ld_i = 0

    def din(self, name, shape, dt=F32):
        self.inp[name] = self.nc.dram_tensor(name, list(shape), dt, kind="ExternalInput").ap()
        return self.inp[name]

    def dscr(self, name, shape, dt=F32, out=False):
        kind = "ExternalOutput" if (self.debug or out) else "Internal"
        self.scr[name] = self.nc.dram_tensor(name, list(shape), dt, kind=kind).ap()
        return self.scr[name]

    def sb(self, es, name, shape, dt=F32):
        self.uid = getattr(self, "uid", 0) + 1
        return es.enter_context(self.nc.sbuf_tensor(f"sb{self.uid}_{name}", list(shape), dt))

    def ps(self, es, name, shape, dt=F32):
        self.uid = getattr(self, "uid", 0) + 1
        return es.enter_context(self.nc.psum_tensor(f"ps{self.uid}_{name}", list(shape), dt))

    def store(self, out, in_, reads, q=None):
        import os
        q = q or os.environ.get("STQ", "pool")
        if not hasattr(self, "_st"):
            self._st = [self.S.T(f"st{i}") for i in range(8)]
        i = self.st_i % 8
        self.st_i += 1
        self.S.dma(q, out, in_, reads=reads, writes=[self._st[i]])

    def load(self, out, in_, Tt, q="sp"):
        self.S.dma(q, out, in_, writes=[Tt])


def _rope_tables():
    half = 32
    freqs = (np.float32(10000.0) ** (-np.arange(half, dtype=np.float32) / np.float32(half))).astype(np.float32)
    pos = np.arange(SEQ, dtype=np.float32)
    ang = (pos[:, None] * freqs[None, :]).astype(np.float32)
    cos = np.cos(ang).astype(np.float32).T
    sin = np.sin(ang).astype(np.float32).T
    cosT = np.tile(cos, (4, 1))
    sinT = np.tile(sin, (4, 1))
    posc = (np.arange(256, dtype=np.float32) * 16 + 31).astype(np.float32)
    angc = (posc[:, None] * freqs[None, :]).astype(np.float32)
    cosC = np.tile(np.cos(angc).astype(np.float32).T, (2, 1))
    sinC = np.tile(np.sin(angc).astype(np.float32).T, (2, 1))
    return cosT, sinT, cosC, sinC


def _consts():
    c = {}
    cosT, sinT, cosC, sinC = _rope_tables()
    c["cosT"], c["sinT"], c["cosC"], c["sinC"] = cosT, sinT, cosC, sinC
    c["identf"] = np.eye(128, dtype=np.float32)
    c["identb"] = np.eye(128, dtype=np.float32).astype(ml_dtypes.bfloat16)
    key = np.arange(SEQ)
    c["Erows"] = (key[None, :] // 64 == np.arange(64)[:, None]).astype(np.float32).astype(ml_dtypes.bfloat16)
    i = np.arange(256)
    c["cmask"] = np.where((i[:, None] * 16 + 31) <= key[None, :], 0.0, NEG).astype(np.float32).astype(ml_dtypes.bfloat16)
    v = np.arange(128)[:, None]
    u = np.arange(512)[None, :]
    wm = np.zeros((8, 128, 512), np.float32)
    for ri, r in enumerate(range(-4, 4)):
        kk = 128 * r + v
        wm[ri] = np.where((kk <= u) & (kk > u - 512), 0.0, NEG)
    c["wmask"] = wm.astype(ml_dtypes.bfloat16)
    cm = np.zeros((4, 128, 512), np.float32)
    for r in range(4):
        cm[r] = np.where(128 * r + v <= u, 0.0, NEG)
    c["dmask"] = cm.astype(ml_dtypes.bfloat16)
    t = np.arange(SEQ)[:, None]
    j = np.arange(64)[None, :]
    cur = t // 64
    forced = (j == 0) | (j == cur) | (j == cur - 1)
    causal = j * 64 <= t
    add = np.where(forced, 1e9, np.where(causal, 0.0, -1e9)).astype(np.float32)
    c["addtab"] = np.ascontiguousarray(add.reshape(NT, 128, 64).transpose(1, 0, 2))
    ii = np.arange(256)[:, None]
    ov = ((ii * 16 < (j + 1) * 64) & (ii * 16 + 32 > j * 64) & (ii < 255)).astype(np.float32)
    c["overlap"] = ov.astype(ml_dtypes.bfloat16)
    return c


def declare_io(self):
    k = self
    k.din("x", [NB, SEQ, D])
    k.din("w_in_p", [L, D, 2072])
    k.din("norm_mix_t", [L, 128, 8])
    k.din("norm_final", [D])
    for n, sh, dt in [("cosT", [128, SEQ], F32), ("sinT", [128, SEQ], F32), ("cosC", [64, 256], F32),
                      ("sinC", [64, 256], F32), ("identf", [128, 128], F32), ("identb", [128, 128], BF16),
                      ("Erows", [64, SEQ], BF16), ("cmask", [256, SEQ], BF16), ("wmask", [8, 128, 512], BF16),
                      ("dmask", [4, 128, 512], BF16), ("addtab", [128, NT, 64], F32), ("overlap", [256, 64], BF16)]:
        k.din(n, sh, dt)
    k.dscr("qT", [NB, 512, SEQ], BF16)
    k.dscr("ksT", [NB, 128, SEQ], BF16)
    k.dscr("kwT", [NB, 128, SEQ], BF16)
    k.dscr("kcrT", [NB, 128, SEQ], BF16)
    k.dscr("vcrT", [NB, 128, SEQ], BF16)
    k.dscr("ucT", [NB, 256, SEQ], BF16)
    k.dscr("usT", [NB, 256, SEQ], BF16)
    k.dscr("vsw", [NB, SEQ, 256], BF16)
    k.dscr("gat", [NB, SEQ, 32], F32)
    k.dscr("ycat", [NB, SEQ, D], F32)
    k.dscr("xres", [NB, SEQ, D], F32)
    k.dscr("h2T", [NB, D, SEQ], BF16)
    k.dscr("kcT", [NB, 2, 64, 256], BF16)
    k.dscr("vc", [NB, 2, 256, 64], BF16)
    for n in ["cmp_k_w1", "cmp_v_w1"]:
        k.din(n, [L, 2048, 256])
    for n in ["cmp_k_w2", "cmp_v_w2"]:
        k.din(n, [L, 256, 64])
    k.din("pe_t", [L, 2, 128, 16])
    k.din("wdw_t", [L, 256, 31])
    for n in ["ssm_are_t", "ssm_aim_t", "ssm_ldt_t"]:
        k.din(n, [L, 128, 8])
    for n in ["Zb_re", "Zb_im", "Cp_re", "Cp_im"]:
        k.din(n, [L, 128, 8, 128])
    k.din("ssm_d_t", [L, 128, 2])
    k.din("ssm_w_glu", [L, 256, 512])
    k.din("iota512", [512])
    k.din("gcat_t", [L, 128, 8])
    k.din("norm_ffn_t", [L, 128, 8])
    k.din("w_out", [L, D, D])
    k.din("w_gate", [L, D, DFF])
    k.din("w_up", [L, D, DFF])
    k.din("w_down", [L, DFF, D])
    for n in ["conv_b_dw", "conv_b_pw", "conv_ln_g", "conv_ln_b"]:
        k.din(n, [L, 256])
    k.din("conv_w_pw", [L, 256, 256])
    k.dscr("out", [NB, SEQ, D], F32, out=True)


def load_consts(self, es):
    k, S = self, self.S
    k.identf = k.sb(es, "identf", [128, 128], F32)
    k.identb = k.sb(es, "identb", [128, 128], BF16)
    k.Tid = S.T("ident")
    S.dma("sp", k.identf[:], k.inp["identf"], writes=[k.Tid])
    k.Tidb = S.T("identb")
    S.dma("sp", k.identb[:], k.inp["identb"], writes=[k.Tidb])
    k.ones_b = k.sb(es, "ones_b", [128, 128], BF16)
    k.Tones = S.T("ones")
    S.op("dve", lambda e: e.memset(k.ones_b[:], 1.0), writes=[k.Tones])


def phase1(self, l):
    k, S, nc = self, self.S, self.nc
    with contextlib.ExitStack() as es:
        NCOL = 2840
        W = k.sb(es, "p1_W", [128, 8, NCOL], BF16)
        TW = [S.T(f"p1_W{i}") for i in range(8)]
        stage = [k.sb(es, f"p1_stage{i}", [128, 2072], F32) for i in range(2)]
        Tstage = [S.T(f"p1_stage{i}") for i in range(2)]
        gain = k.sb(es, "p1_gain", [128, 8], F32)
        gq = k.sb(es, "p1_gq", [128, 8], F32)
        ngq = k.sb(es, "p1_ngq", [128, 8], F32)
        ngain = k.sb(es, "p1_ngain", [128, 8], F32)
        Tgain = S.T("p1_gain")
        S.dma("sp", gain[:], k.inp["norm_mix_t"][l], writes=[Tgain])
        S.op("dve", lambda e: e.tensor_scalar(out=ngain[:], in0=gain[:], scalar1=-1.0, scalar2=None, op0=ALU.mult), reads=[Tgain], writes=[Tgain])
        S.op("dve", lambda e: e.tensor_scalar(out=gq[:], in0=gain[:], scalar1=0.125, scalar2=None, op0=ALU.mult), reads=[Tgain], writes=[Tgain])
        S.op("dve", lambda e: e.tensor_scalar(out=ngq[:], in0=gain[:], scalar1=-0.125, scalar2=None, op0=ALU.mult), reads=[Tgain], writes=[Tgain])
        wsrc = k.inp["w_in_p"][l].rearrange("(kc p) n -> p kc n", p=128)
        engs = ["dve", "pool"]
        ei = 0

        def ts(out, in_, sc):
            nonlocal ei
            e_ = engs[kc % 2]
            S.op(e_, lambda e: e.tensor_scalar(out=out, in0=in_, scalar1=sc, scalar2=None, op0=ALU.mult),
                 reads=[Tstage[kc % 2], Tgain], writes=[TW[kc]])

        for kc in range(8):
            st = stage[kc % 2]
            S.dma("sp", st[:], wsrc[:, kc, :], writes=[Tstage[kc % 2]])
            g1, ng1, q1, nq1 = gain[:, kc:kc + 1], ngain[:, kc:kc + 1], gq[:, kc:kc + 1], ngq[:, kc:kc + 1]
            ts(W[:, kc, 0:512], st[:, 0:512], q1)
            sv = st[:, 0:512].rearrange("p (h t f) -> p h t f", h=8, t=2)
            dv = W[:, kc, 512:1024].rearrange("p (h t f) -> p h t f", h=8, t=2)
            ts(dv[:, :, 0, :], sv[:, :, 1, :], nq1)
            ts(dv[:, :, 1, :], sv[:, :, 0, :], q1)
            for (s0, d0) in [(512, 1024), (640, 1280)]:
                ts(W[:, kc, d0:d0 + 128], st[:, s0:s0 + 128], g1)
                sv = st[:, s0:s0 + 128].rearrange("p (h t f) -> p h t f", h=2, t=2)
                dv = W[:, kc, d0 + 128:d0 + 256].rearrange("p (h t f) -> p h t f", h=2, t=2)
                ts(dv[:, :, 0, :], sv[:, :, 1, :], ng1)
                ts(dv[:, :, 1, :], sv[:, :, 0, :], g1)
            ts(W[:, kc, 1536:2840], st[:, 768:2072], g1)

        import os
        STOP = os.environ.get("P1STOP", "")
        if STOP == "w":
            S.barrier()
            return
        xg = [k.sb(es, f"p1_xg{i}", [128, 4, D], F32) for i in range(2)]
        Txg = [S.T(f"p1_xg{i}") for i in range(2)]
        junk = k.sb(es, "p1_junk", [128, D], BF16)
        Tjunk = S.T("p1_junk")
        ss = k.sb(es, "p1_ss", [128, 4], F32)
        rstd = k.sb(es, "p1_rstd", [128, 4], F32)
        Tss = S.T("p1_ss")
        hb = k.sb(es, "p1_hb", [128, 4, D], BF16)
        Thb = [S.T(f"p1_hb{i}") for i in range(4)]
        hT = [k.sb(es, f"p1_hT{i}", [128, 8, 512], BF16) for i in range(2)]
        ThT = [[S.T(f"p1_hT{i}_{c}") for c in range(8)] for i in range(2)]
        cs = [k.sb(es, f"p1_cos{i}", [128, 512], F32) for i in range(2)]
        sn = [k.sb(es, f"p1_sin{i}", [128, 512], F32) for i in range(2)]
        Tcs = [S.T(f"p1_cs{i}") for i in range(2)]
        pT = [k.ps(es, f"p1_pT{i}", [128, 512], BF16) for i in range(2)]
        TpT = [S.T(f"p1_pT{i}") for i in range(2)]
        pm = [k.ps(es, f"p1_pm{i}", [128, 512], F32) for i in range(4)]
        Tpm = [S.T(f"p1_pm{i}") for i in range(4)]
        pk = [k.ps(es, f"p1_pk{i}", [128, 512], F32) for i in range(2)]
        Tpk = [S.T(f"p1_pk{i}") for i in range(2)]
        NE = 6
        t1 = [k.sb(es, f"p1_t1_{i}", [128, 512], F32) for i in range(NE)]
        t2 = [k.sb(es, f"p1_t2_{i}", [128, 512], F32) for i in range(NE)]
        ob = [k.sb(es, f"p1_ob{i}", [128, 512], BF16) for i in range(NE)]
        Tt1 = [S.T(f"p1_t1_{i}") for i in range(NE)]
        Tt2 = [S.T(f"p1_t2_{i}") for i in range(NE)]
        Tob = [S.T(f"p1_ob{i}") for i in range(NE)]
        Ttg_pre = [S.T(f"p1_tg{i}") for i in range(2)]
        tk = [k.sb(es, f"p1_tk{i}", [128, 256], BF16) for i in range(2)]
        tg = [k.sb(es, f"p1_tg{i}", [128, 32], F32) for i in range(2)]
        for i in range(2):
            S.op("dve", lambda e: e.memset(tg[i][:], 0.0), writes=[Ttg_pre[i]])
        Ttk = [S.T(f"p1_tk{i}") for i in range(2)]
        Ttg = Ttg_pre
        xin = k.inp["x"] if l == 0 else k.scr["xres"]
        gi = 0
        ecnt = 0
        pmi = 0
        for b in range(NB):
            for g in range(NG):
                u = gi % 2
                gi += 1
                tsl = slice(g * 512, (g + 1) * 512)
                S.dma("sp", xg[u][:], xin[b, tsl, :].rearrange("(j p) d -> p j d", p=128), writes=[Txg[u]])
                S.dma("sp", cs[u][:], k.inp["cosT"][:, tsl], writes=[Tcs[u]])
                S.dma("sp", sn[u][:], k.inp["sinT"][:, tsl], writes=[Tcs[u]])
                for j in range(4):
                    S.op("act", lambda e: e.activation(out=junk[:], in_=xg[u][:, j, :], func=AF.Square, accum_out=ss[:, j:j + 1]),
                         reads=[Txg[u]], writes=[Tjunk, Tss])
                S.op("dve", lambda e: e.tensor_scalar(out=rstd[:], in0=ss[:], scalar1=1.0 / D, scalar2=EPS, op0=ALU.mult, op1=ALU.add), reads=[Tss], writes=[Tss])
                S.op("act", lambda e: e.activation(out=rstd[:], in_=rstd[:], func=AF.Sqrt), reads=[Tss], writes=[Tss])
                S.op("dve", lambda e: e.reciprocal(out=rstd[:], in_=rstd[:]), reads=[Tss], writes=[Tss])
                for j in range(4):
                    S.op("pool" if j % 2 else "dve", lambda e: e.tensor_scalar(out=hb[:, j, :], in0=xg[u][:, j, :], scalar1=rstd[:, j:j + 1], scalar2=None, op0=ALU.mult),
                         reads=[Txg[u], Tss], writes=[Thb[j]])
                if STOP == "n":
                    continue
                for kc in range(8):
                    pu = kc % 2
                    for j in range(4):
                        S.op("pe", lambda e: e.transpose(out=pT[pu][:, j * 128:(j + 1) * 128], in_=hb[:, j, kc * 128:(kc + 1) * 128], identity=k.identb[:]),
                             reads=[Thb[j], k.Tidb], writes=[TpT[pu]])
                    if kc % 2:
                        S.op("act", lambda e: e.activation(out=hT[u][:, kc, :], in_=pT[pu][:], func=AF.Copy), reads=[TpT[pu]], writes=[ThT[u][kc]])
                    else:
                        S.op("dve", lambda e: e.tensor_copy(out=hT[u][:, kc, :], in_=pT[pu][:]), reads=[TpT[pu]], writes=[ThT[u][kc]])

                if STOP == "t":
                    continue

                def mm(c):
                    nonlocal pmi
                    i = pmi % 4
                    pmi += 1
                    for kc in range(8):
                        S.op("pe", lambda e: e.matmul(pm[i][:], lhsT=W[:, kc, c * 128:(c + 1) * 128], rhs=hT[u][:, kc, :], start=(kc == 0), stop=(kc == 7)),
                             reads=[TW[kc], ThT[u][kc]], writes=[Tpm[i]])
                    return i

                def roped(cx, cr, dst):
                    nonlocal ecnt
                    ix = mm(cx)
                    ir = mm(cr)
                    n = ecnt % NE
                    ecnt += 1
                    S.op("dve", lambda e: e.tensor_tensor(out=t1[n][:], in0=pm[ix][:], in1=cs[u][:], op=ALU.mult), reads=[Tpm[ix], Tcs[u]], writes=[Tt1[n]])
                    S.op("dve", lambda e: e.tensor_tensor(out=t2[n][:], in0=pm[ir][:], in1=sn[u][:], op=ALU.mult), reads=[Tpm[ir], Tcs[u]], writes=[Tt2[n]])
                    S.op("pool", lambda e: e.tensor_tensor(out=ob[n][:], in0=t1[n][:], in1=t2[n][:], op=ALU.add), reads=[Tt1[n], Tt2[n]], writes=[Tob[n]])
                    k.store(dst, ob[n][:], [Tob[n]])

                def plain(c, dst):
                    nonlocal ecnt
                    i = mm(c)
                    n = ecnt % NE
                    ecnt += 1
                    S.op("act", lambda e: e.activation(out=ob[n][:], in_=pm[i][:], func=AF.Copy), reads=[Tpm[i]], writes=[Tob[n]])
                    k.store(dst, ob[n][:], [Tob[n]])

                for c in range(4):
                    roped(c, c + 4, k.scr["qT"][b, c * 128:(c + 1) * 128, tsl])
                roped(8, 9, k.scr["ksT"][b, :, tsl])
                roped(10, 11, k.scr["kwT"][b, :, tsl])
                plain(12, k.scr["kcrT"][b, :, tsl])
                plain(13, k.scr["vcrT"][b, :, tsl])
                for c in range(2):
                    ia = mm(14 + c)
                    ig = mm(16 + c)
                    n = ecnt % NE
                    ecnt += 1
                    S.op("act", lambda e: e.activation(out=t1[n][:], in_=pm[ig][:], func=AF.Sigmoid), reads=[Tpm[ig]], writes=[Tt1[n]])
                    S.op("dve", lambda e: e.tensor_tensor(out=ob[n][:], in0=pm[ia][:], in1=t1[n][:], op=ALU.mult), reads=[Tpm[ia], Tt1[n]], writes=[Tob[n]])
                    k.store(k.scr["ucT"][b, c * 128:(c + 1) * 128, tsl], ob[n][:], [Tob[n]])
                for c in range(2):
                    plain(18 + c, k.scr["usT"][b, c * 128:(c + 1) * 128, tsl])
                if STOP == "f":
                    continue
                for j in range(4):
                    pu = j % 2
                    for kc in range(8):
                        S.op("pe", lambda e: e.matmul(pk[pu][:, 0:280], lhsT=hT[u][:, kc, j * 128:(j + 1) * 128], rhs=W[:, kc, 2560:2840], start=(kc == 0), stop=(kc == 7)),
                             reads=[TW[kc], ThT[u][kc]], writes=[Tpk[pu]])
                    S.op("dve", lambda e: e.tensor_copy(out=tk[pu][:], in_=pk[pu][:, 0:256]), reads=[Tpk[pu]], writes=[Ttk[pu]])
                    r0 = g * 512 + j * 128
                    if STOP != "g":
                        S.op("act", lambda e: e.activation(out=tg[pu][:, 0:24], in_=pk[pu][:, 256:280], func=AF.Sigmoid), reads=[Tpk[pu]], writes=[Ttg[pu]])
                        if STOP != "h":
                            k.store(k.scr["gat"][b, r0:r0 + 128, :], tg[pu][:], [Ttg[pu]])
                    k.store(k.scr["vsw"][b, r0:r0 + 128, :], tk[pu][:], [Ttk[pu]])
        S.barrier()


K.declare_io = declare_io
K.load_consts = load_consts
K.phase1 = phase1


def build(debug=False, phases=None, nlayers=L):
    k = K(debug=debug, phases=phases, nlayers=nlayers)
    k.declare_io()
    S = k.S
    with contextlib.ExitStack() as es:
        k.load_consts(es)
        for l in range(nlayers):
            for ph in PHASES:
                if phases is not None and (l, ph) not in phases and ph not in phases:
                    continue
                getattr(k, ph)(l)
        if phases is None or "final" in phases:
            k.final()
        S.barrier()
    return k


PHASES = ["phase1"]


def host_inputs(inputs):
    f = lambda a: np.ascontiguousarray(np.asarray(a, dtype=np.float32))
    w_in = f(inputs["w_in"])
    cols = np.concatenate([np.arange(0, 512), np.arange(768, 896), np.arange(1024, 1152), np.arange(512, 640),
                           np.arange(640, 768), np.arange(1560, 2072), np.arange(1304, 1560), np.arange(896, 1024),
                           np.arange(1152, 1280), np.arange(1280, 1304)])
    shared = {}
    shared["w_in_p"] = np.ascontiguousarray(w_in[:, :, cols])
    shared["norm_mix_t"] = np.ascontiguousarray(f(inputs["norm_mix"]).reshape(L, 8, 128).transpose(0, 2, 1))
    shared["norm_final"] = f(inputs["norm_final"])
    tl = lambda a: np.ascontiguousarray(a.reshape(L, 8, 128).transpose(0, 2, 1))
    shared["ssm_are_t"] = tl(f(inputs["ssm_a_re"]).reshape(L, 16 * 64))
    shared["ssm_aim_t"] = tl(f(inputs["ssm_a_im"]).reshape(L, 16 * 64))
    shared["ssm_ldt_t"] = tl(np.repeat(f(inputs["ssm_log_dt"]), 64, axis=1))
    def padB(bm):
        o = np.zeros((L, 128, 8, 128), np.float32)
        for g_ in range(16):
            m_, h_ = g_ // 2, g_ % 2
            col = (g_ % 8) * 16
            o[:, h_ * 64:(h_ + 1) * 64, m_, col:col + 16] = bm[:, g_]
        return o
    shared["Zb_re"] = padB(f(inputs["ssm_b_re"]))
    shared["Zb_im"] = padB(f(inputs["ssm_b_im"]))
    shared["Cp_re"] = padB(f(inputs["ssm_c_re"]).transpose(0, 1, 3, 2))
    shared["Cp_im"] = padB(f(inputs["ssm_c_im"]).transpose(0, 1, 3, 2))
    shared["ssm_d_t"] = np.ascontiguousarray(f(inputs["ssm_d"]).reshape(L, 2, 128).transpose(0, 2, 1))
    shared["ssm_w_glu"] = f(inputs["ssm_w_glu"])
    shared["iota512"] = np.arange(512, dtype=np.float32)
    gcat = np.concatenate([f(inputs["norm_out_attn"]), f(inputs["norm_out_ssm"]), f(inputs["norm_out_conv"])], axis=1)
    shared["gcat_t"] = np.ascontiguousarray(gcat.reshape(L, 8, 128).transpose(0, 2, 1))
    shared["norm_ffn_t"] = np.ascontiguousarray(f(inputs["norm_ffn"]).reshape(L, 8, 128).transpose(0, 2, 1))
    for n in ["w_out", "w_gate", "w_up", "w_down"]:
        shared[n] = f(inputs[n])
    shared["wdw_t"] = np.ascontiguousarray(f(inputs["conv_w_dw"]).transpose(0, 2, 1))
    for n in ["conv_b_dw", "conv_b_pw", "conv_ln_g", "conv_ln_b", "conv_w_pw"]:
        shared[n] = f(inputs[n])
    for n in ["cmp_k_w1", "cmp_v_w1", "cmp_k_w2", "cmp_v_w2"]:
        shared[n] = f(inputs[n])
    pe = np.stack([f(inputs["cmp_pe_k"]), f(inputs["cmp_pe_v"])], 1)
    shared["pe_t"] = np.ascontiguousarray(pe.reshape(L, 2, 2, 16, 64).transpose(0, 1, 2, 4, 3).reshape(L, 2, 128, 16))
    shared.update(_consts())
    return shared


def kernel(**inputs):
    k = build()
    shared = host_inputs(inputs)
    x = np.ascontiguousarray(np.asarray(inputs["x"], dtype=np.float32))
    names = set(k.inp.keys())
    in_maps = []
    for c in range(8):
        m = {n: shared[n] for n in names if n != "x"}
        m["x"] = np.ascontiguousarray(x[c * NB:(c + 1) * NB])
        in_maps.append(m)
    res = run_bass_kernel_spmd(k.nc, in_maps, core_ids=list(range(8)))
    return np.concatenate([np.asarray(r["out"]) for r in res.results], axis=0).astype(np.float32)


def final(self):
    k, S = self, self.S
    src = k.scr["xres"] if self.nlayers > 0 else k.inp["x"]
    with contextlib.ExitStack() as es:
        gf = k.sb(es, "fin_g", [128, D], F32)
        Tgf = S.T("fin_g")
        S.dma("sp", gf[:], k.inp["norm_final"].partition_broadcast(128), writes=[Tgf])
        xg = [k.sb(es, f"fin_x{i}", [128, 4, D], F32) for i in range(2)]
        Txg = [S.T(f"fin_x{i}") for i in range(2)]
        og = [k.sb(es, f"fin_o{i}", [128, 4, D], F32) for i in range(2)]
        Tog = [S.T(f"fin_o{i}") for i in range(2)]
        junk = k.sb(es, "fin_junk", [128, D], BF16)
        Tjunk = S.T("fin_junk")
        ss = [k.sb(es, f"fin_ss{i}", [128, 4], F32) for i in range(2)]
        Tss = [S.T(f"fin_ss{i}") for i in range(2)]
        gi = 0
        for b in range(NB):
            for g in range(NG):
                u = gi % 2
                gi += 1
                tsl = slice(g * 512, (g + 1) * 512)
                S.dma("sp", xg[u][:], src[b, tsl, :].rearrange("(j p) d -> p j d", p=128), writes=[Txg[u]])
                for j in range(4):
                    S.op("act", lambda e: e.activation(out=junk[:], in_=xg[u][:, j, :], func=AF.Square, accum_out=ss[u][:, j:j + 1]),
                         reads=[Txg[u]], writes=[Tjunk, Tss[u]])
                S.op("dve", lambda e: e.tensor_scalar(out=ss[u][:], in0=ss[u][:], scalar1=1.0 / D, scalar2=EPS, op0=ALU.mult, op1=ALU.add), reads=[Tss[u]], writes=[Tss[u]])
                S.op("act", lambda e: e.activation(out=ss[u][:], in_=ss[u][:], func=AF.Sqrt), reads=[Tss[u]], writes=[Tss[u]])
                S.op("dve", lambda e: e.reciprocal(out=ss[u][:], in_=ss[u][:]), reads=[Tss[u]], writes=[Tss[u]])
                for j in range(4):
                    S.op("dve", lambda e: e.scalar_tensor_tensor(out=og[u][:, j, :], in0=xg[u][:, j, :], scalar=ss[u][:, j:j + 1], in1=gf[:], op0=ALU.mult, op1=ALU.mult),
                         reads=[Txg[u], Tss[u], Tgf], writes=[Tog[u]])
                k.store(k.scr["out"][b, tsl, :].rearrange("(j p) d -> p j d", p=128), og[u][:], [Tog[u]])
        S.barrier()


K.final = final


def phase2(self, l):
    k, S = self, self.S
    with contextlib.ExitStack() as es:
        stage = k.sb(es, "p2_stage", [128, 16, 256], F32)
        Tstage = S.T("p2_stage")
        w1b = [k.sb(es, f"p2_w1b{i}", [128, 16, 256], BF16) for i in range(2)]
        Tw1 = [S.T(f"p2_w1b{i}") for i in range(2)]
        pes = k.sb(es, "p2_pes", [128, 2, 16], F32)
        peb = k.sb(es, "p2_peb", [128, 2, 16], BF16)
        Tpe = S.T("p2_pe")
        w2s = k.sb(es, "p2_w2s", [128, 2, 2, 64], F32)
        w2b = k.sb(es, "p2_w2b", [128, 2, 2, 64], BF16)
        w2r = k.sb(es, "p2_w2r", [128, 2, 64], BF16)
        Tw2 = S.T("p2_w2")
        bias = k.sb(es, "p2_bias", [128, 2, 2], F32)
        Tbias = S.T("p2_bias")
        csC = k.sb(es, "p2_cosC", [64, 256], F32)
        snC = k.sb(es, "p2_sinC", [64, 256], F32)
        TcsC = S.T("p2_csC")
        S.dma("sp", csC[:], k.inp["cosC"], writes=[TcsC])
        S.dma("sp", snC[:], k.inp["sinC"], writes=[TcsC])
        pb = k.ps(es, "p2_pb", [128, 512], F32)
        Tpb = S.T("p2_pb")
        S.dma("sp", pes[:], k.inp["pe_t"][l].rearrange("a p l -> p a l"), writes=[Tpe])
        S.op("dve", lambda e: e.tensor_copy(out=peb[:], in_=pes[:]), reads=[Tpe], writes=[Tpe])
        for a, nm in enumerate(["cmp_k_w1", "cmp_v_w1"]):
            src = k.inp[nm][l].rearrange("(hl d) h -> d hl h", d=64)
            S.dma("sp", stage[0:64, :, :], src[:, 0:16, :], writes=[Tstage])
            S.dma("sp", stage[64:128, :, :], src[:, 16:32, :], reads=[], writes=[Tstage])
            S.op("dve" if a == 0 else "pool", lambda e: e.tensor_copy(out=w1b[a][:], in_=stage[:]), reads=[Tstage], writes=[Tw1[a]])
            for hc in range(2):
                for ll in range(16):
                    S.op("pe", lambda e: e.matmul(pb[:, 0:1], lhsT=w1b[a][:, ll, hc * 128:(hc + 1) * 128], rhs=peb[:, a, ll:ll + 1], start=(ll == 0), stop=(ll == 15)),
                         reads=[Tw1[a], Tpe], writes=[Tpb])
                S.op("dve", lambda e: e.tensor_copy(out=bias[:, a, hc:hc + 1], in_=pb[:, 0:1]), reads=[Tpb], writes=[Tbias])
        for a, nm in enumerate(["cmp_k_w2", "cmp_v_w2"]):
            S.dma("sp", w2s[:, a, :, :], k.inp[nm][l].rearrange("(hc p) d -> p hc d", p=128), writes=[Tw2])
        S.op("dve", lambda e: e.tensor_copy(out=w2b[:], in_=w2s[:]), reads=[Tw2], writes=[Tw2])
        for hc in range(2):
            S.op("dve", lambda e: e.tensor_scalar(out=w2r[:, hc, 0:32], in0=w2s[:, 0, hc, 32:64], scalar1=-1.0, scalar2=None, op0=ALU.mult), reads=[Tw2], writes=[Tw2])
            S.op("dve", lambda e: e.tensor_copy(out=w2r[:, hc, 32:64], in_=w2s[:, 0, hc, 0:32]), reads=[Tw2], writes=[Tw2])

        X2 = [k.sb(es, f"p2_X2_{i}", [128, SEQ], BF16) for i in range(2)]
        TX2 = [S.T(f"p2_X2_{i}") for i in range(2)]
        for i in range(2):
            S.op("pool", lambda e: e.memset(X2[i][64:128, SEQ - 16:SEQ], 0.0), writes=[TX2[i]])
        hid = [k.sb(es, f"p2_hid{i}", [128, 2, 256], BF16) for i in range(2)]
        Thid = [S.T(f"p2_hid{i}") for i in range(2)]
        for i in range(2):
            S.op("pool", lambda e: e.memset(hid[i][:], 0.0), writes=[Thid[i]])
        ph = [k.ps(es, f"p2_ph{i}", [128, 512], F32) for i in range(2)]
        Tph = [S.T(f"p2_ph{i}") for i in range(2)]
        pk = k.ps(es, "p2_pk", [128, 512], F32)
        Tpk = S.T("p2_pk")
        pv = k.ps(es, "p2_pv", [128, 512], F32)
        Tpv = S.T("p2_pv")
        t1 = k.sb(es, "p2_t1", [64, 256], F32)
        t2 = k.sb(es, "p2_t2", [64, 256], F32)
        kco = k.sb(es, "p2_kco", [64, 256], BF16)
        Tko = S.T("p2_kco")
        S.op("dve", lambda e: e.memset(kco[:], 0.0), writes=[Tko])
        vco = [k.sb(es, f"p2_vco{i}", [128, 2, 64], BF16) for i in range(2)]
        Tvo = [S.T(f"p2_vco{i}") for i in range(2)]
        it = 0
        phi = 0
        for b in range(NB):
            for kv in range(2):
                for a in range(2):
                    u = it % 2
                    it += 1
                    src = k.scr["kcrT" if a == 0 else "vcrT"][b, kv * 64:(kv + 1) * 64, :]
                    S.dma("sp", X2[u][0:64, :], src, writes=[TX2[u]])
                    S.dma("sp", X2[u][64:128, 0:SEQ - 16], src[:, 16:SEQ], reads=[], writes=[TX2[u]])
                    xv = X2[u][:, :].rearrange("p (i s) -> p i s", s=16)
                    for hc in range(2):
                        pi = phi % 2
                        phi += 1
                        for ll in range(16):
                            S.op("pe", lambda e: e.matmul(ph[pi][:, 0:255], lhsT=w1b[a][:, ll, hc * 128:(hc + 1) * 128], rhs=xv[:, 0:255, ll], start=(ll == 0), stop=(ll == 15)),
                                 reads=[Tw1[a], TX2[u]], writes=[Tph[pi]])
                        S.op("act", lambda e: e.activation(out=hid[u][:, hc, 0:255], in_=ph[pi][:, 0:255], func=AF.Gelu_apprx_tanh, bias=bias[:, a, hc:hc + 1]),
                             reads=[Tph[pi], Tbias], writes=[Thid[u]])
                    if a == 0:
                        for r_, wsel in enumerate([w2b[:, 0, :, :], w2r[:, :, :]]):
                            for hc in range(2):
                                S.op("pe", lambda e: e.matmul(pk[0:64, r_ * 256:r_ * 256 + 256], lhsT=wsel[:, hc, :], rhs=hid[u][:, hc, :], start=(hc == 0), stop=(hc == 1)),
                                     reads=[Tw2, Thid[u]], writes=[Tpk])
                        S.op("dve", lambda e: e.tensor_tensor(out=t1[:], in0=pk[0:64, 0:256], in1=csC[:], op=ALU.mult), reads=[Tpk, TcsC], writes=[Tko])
                        S.op("dve", lambda e: e.tensor_tensor(out=t2[:], in0=pk[0:64, 256:512], in1=snC[:], op=ALU.mult), reads=[Tpk, TcsC], writes=[Tko])
                        S.op("dve", lambda e: e.tensor_tensor(out=kco[:, 0:255], in0=t1[:, 0:255], in1=t2[:, 0:255], op=ALU.add), reads=[Tko], writes=[Tko])
                        k.store(k.scr["kcT"][b, kv], kco[:], [Tko])
                    else:
                        for c in range(2):
                            for hc in range(2):
                                S.op("pe", lambda e: e.matmul(pv[:, c * 64:(c + 1) * 64], lhsT=hid[u][:, hc, c * 128:(c + 1) * 128], rhs=w2b[:, 1, hc, :], start=(hc == 0), stop=(hc == 1)),
                                     reads=[Tw2, Thid[u]], writes=[Tpv])
                        S.op("dve", lambda e: e.tensor_copy(out=vco[u][:].rearrange("p c d -> p (c d)"), in_=pv[:, 0:128]), reads=[Tpv], writes=[Tvo[u]])
                        k.store(k.scr["vc"][b, kv].rearrange("(c p) d -> p c d", p=128), vco[u][:], [Tvo[u]])
        S.barrier()


K.phase2 = phase2
PHASES.append("phase2")


def phase3(self, l):
    k, S = self, self.S
    with contextlib.ExitStack() as es:
        sb = lambda n, sh, dt=F32: k.sb(es, "p3_" + n, sh, dt)
        QS = [sb(f"QS{h}", [128, SEQ], BF16) for h in range(4)]
        TQd = [S.T(f"p3_Qd{h}") for h in range(4)]
        TQm = [[S.T(f"p3_Qm{h}_{g}") for g in range(NG)] for h in range(4)]
        KS = sb("KS", [128, SEQ], BF16)
        TKS, TE = S.T("p3_KS"), S.T("p3_E")
        KW = sb("KW", [64, SEQ], BF16)
        TKW = S.T("p3_KW")
        KC = sb("KC", [64, 256], BF16)
        TKC = S.T("p3_KC")
        VS = sb("VS", [128, NT, 80], BF16)
        VW = sb("VW", [128, NT, 80], BF16)
        VC = sb("VC", [128, 2, 144], BF16)
        TVS = [S.T(f"p3_VS{i}") for i in range(4)]
        TVW = [S.T(f"p3_VW{i}") for i in range(4)]
        TVC = S.T("p3_VC")
        G_ = sb("G", [128, NT, 32], F32)
        TG = S.T("p3_G")
        yacc = sb("yacc", [128, NT, 4, 64], F32)
        Ty = [[S.T(f"p3_y{g}_{h}") for h in range(4)] for g in range(NG)]
        cmaskS = sb("cmask", [128, 2, SEQ], BF16)
        wmaskS = sb("wmask", [128, 8, 512], BF16)
        dmaskS = sb("dmask", [128, 4, 512], BF16)
        addS = sb("add", [128, NT, 64], F32)
        Tc = S.T("p3_consts")
        S.dma("sp", cmaskS[:], k.inp["cmask"].rearrange("(c p) t -> p c t", p=128), writes=[Tc])
        Tc2 = S.T("p3_consts2")
        S.dma("sp", wmaskS[:], k.inp["wmask"].rearrange("r p u -> p r u"), writes=[Tc2])
        Tc3 = S.T("p3_consts3")
        S.dma("sp", dmaskS[:], k.inp["dmask"].rearrange("r p u -> p r u"), writes=[Tc3])
        Tc4 = S.T("p3_consts4")
        S.dma("sp", addS[:], k.inp["addtab"], writes=[Tc4])
        S.dma("sp", KS[64:128, :], k.inp["Erows"], writes=[TE])
        S.op("pool", lambda e: e.memset(VS[:, :, 64:65], 1.0), writes=TVS)
        S.op("pool", lambda e: e.memset(VW[:, :, 64:65], 1.0), writes=TVW)
        S.op("pool", lambda e: e.memset(VC[:, :, 64:65], 1.0), writes=[TVC])
        Tov = S.T("p3_ov")
        ovs = sb("ovs", [128, 2, 64], BF16)
        S.dma("sp", ovs[:], k.inp["overlap"].rearrange("(c p) j -> p c j", p=128), writes=[Tov])
        S.op("pool", lambda e: e.tensor_copy(out=VC[:, :, 65:129], in_=ovs[:]), reads=[Tov], writes=[TVC])
        import os
        if os.environ.get("P3STOP", "") == "c":
            S.barrier()
            return
        bank = [k.ps(es, f"p3_bank{i}", [128, 512], F32) for i in range(8)]
        Tb = [S.T(f"p3_bank{i}") for i in range(8)]
        NP = 6
        P = [sb(f"P{i}", [128, 512], BF16) for i in range(NP)]
        TP = [S.T(f"p3_P{i}") for i in range(NP)]
        EC = [sb(f"EC{i}", [128, 512], BF16) for i in range(8)]
        TEC = [S.T(f"p3_EC{i}") for i in range(8)]
        Mbp = [sb(f"Mbp{i}", [128, 128], BF16) for i in range(4)]
        TMb = [S.T(f"p3_Mbp{i}") for i in range(4)]
        for i in range(4):
            S.op("pool", lambda e: e.memset(Mbp[i][:], 0.0), writes=[TMb[i]])
        NS = 4
        rs = [sb(f"rs{i}", [128, 4], F32) for i in range(NS)]
        coef = [sb(f"coef{i}", [128, 4], F32) for i in range(NS)]
        impt = [sb(f"impt{i}", [128, 64], F32) for i in range(NS)]
        tmp = [sb(f"tmp{i}", [128, 64], F32) for i in range(NS)]
        m1 = [sb(f"m1_{i}", [128, 8], F32) for i in range(NS)]
        m2 = [sb(f"m2_{i}", [128, 8], F32) for i in range(NS)]
        Tsm = [S.T(f"p3_sm{i}") for i in range(NS)]
        ot = [sb(f"ot{i}", [65, 512], F32) for i in range(2)]
        Tot = [S.T(f"p3_ot{i}") for i in range(2)]
        cnt = {"p": 0, "s": 0, "sm": 0, "mb": 0, "ot": 0, "pa": 0, "ow": 0, "os": 0}

        def nxt(key, n):
            v = cnt[key] % n
            cnt[key] += 1
            return v

        for b in range(NB):
            for kv in range(2):
                for h in range(4):
                    r0 = (kv * 4 + h) * 64
                    S.dma("sp", QS[h][0:64, :], k.scr["qT"][b, r0:r0 + 64, :], writes=[TQd[h]])
                S.dma("sp", KS[0:64, :], k.scr["ksT"][b, kv * 64:(kv + 1) * 64, :], writes=[TKS])
                S.dma("sp", KW[:, :], k.scr["kwT"][b, kv * 64:(kv + 1) * 64, :], writes=[TKW])
                S.dma("sp", KC[:, :], k.scr["kcT"][b, kv], writes=[TKC])
                vsrc = k.scr["vsw"][b].rearrange("(n p) c -> p n c", p=128)
                for q4 in range(4):
                    nsl = slice(q4 * 8, q4 * 8 + 8)
                    S.dma("sp", VS[:, nsl, 0:64], vsrc[:, nsl, kv * 64:(kv + 1) * 64], writes=[TVS[q4]])
                    S.dma("sp", VW[:, nsl, 0:64], vsrc[:, nsl, 128 + kv * 64:128 + (kv + 1) * 64], writes=[TVW[q4]])
                S.dma("sp", VC[:, :, 0:64], k.scr["vc"][b, kv].rearrange("(c p) d -> p c d", p=128), writes=[TVC])
                if kv == 0:
                    S.dma("sp", G_[:], k.scr["gat"][b].rearrange("(n p) c -> p n c", p=128), writes=[TG])
                import os
                P3STOP = os.environ.get("P3STOP", "")
                for g in range(NG):
                    if P3STOP == "load":
                        break
                    gsl = slice(g * 512, (g + 1) * 512)
                    ncs = 2 if g >= 4 else 1
                    ecs = {}
                    for h in range(4):
                        for c in range(ncs):
                            bi = nxt("s", 2)
                            S.op("pe", lambda e: e.matmul(bank[bi][:], lhsT=KC[0:64, c * 128:(c + 1) * 128], rhs=QS[h][0:64, gsl], start=True, stop=False),
                                 reads=[TKC, TQd[h]], writes=[Tb[bi]])
                            S.op("pe", lambda e: e.matmul(bank[bi][:], lhsT=k.identb[:], rhs=cmaskS[:, c, gsl], start=False, stop=True),
                                 reads=[k.Tidb, Tc], writes=[Tb[bi]])
                            ei = h * 2 + c
                            S.op("act", lambda e: e.activation(out=EC[ei][:], in_=bank[bi][:], func=AF.Exp), reads=[Tb[bi]], writes=[TEC[ei]])
                            ecs[(h, c)] = ei
                    P3A = os.environ.get("P3A", "")
                    for jq in range(4):
                        if P3A == "s":
                            break
                        n = 4 * g + jq
                        pa = nxt("pa", 2)
                        bks = (2 + 2 * pa, 3 + 2 * pa)
                        for h in range(4):
                            bk = bks[h // 2]
                            o0 = (h % 2) * 256
                            for c in range(ncs):
                                ei = ecs[(h, c)]
                                S.op("pe", lambda e: e.matmul(bank[bk][:, o0:o0 + 129], lhsT=EC[ei][:, jq * 128:(jq + 1) * 128], rhs=VC[:, c, 0:129], start=(c == 0), stop=(c == ncs - 1)),
                                     reads=[TEC[ei], TVC], writes=[Tb[bk]])
                        if P3A == "pv":
                            continue
                        si = nxt("sm", NS)
                        T_s = Tsm[si]
                        for half in range(2):
                            bk = bks[half]
                            S.op("dve", lambda e: e.tensor_scalar(out=rs[si][:, 2 * half:2 * half + 2], in0=bank[bk][:].rearrange("p (a f) -> p a f", a=2)[:, :, 64], scalar1=1e-30, scalar2=None, op0=ALU.max),
                                 reads=[Tb[bk]], writes=[T_s])
                        S.op("dve", lambda e: e.reciprocal(out=rs[si][:], in_=rs[si][:]), reads=[T_s], writes=[T_s])
                        for half in range(0):
                            pass
                        for h in range(4):
                            bk = bks[h // 2]
                            o0 = (h % 2) * 256
                            in1 = addS[:, n, :] if h == 0 else impt[si][:]
                            S.op("dve", lambda e: e.scalar_tensor_tensor(out=impt[si][:], in0=bank[bk][:, o0 + 65:o0 + 129], scalar=rs[si][:, h:h + 1], in1=in1, op0=ALU.mult, op1=ALU.add),
                                 reads=[Tb[bk], T_s, Tc4], writes=[T_s])
                        S.op("dve", lambda e: e.tensor_tensor(out=coef[si][:], in0=rs[si][:], in1=G_[:, n, kv * 4:kv * 4 + 4], op=ALU.mult), reads=[T_s, TG], writes=[T_s])
                        if P3A == "d1":
                            continue
                        for h in range(4):
                            bk = bks[h // 2]
                            o0 = (h % 2) * 256
                            S.op("act", lambda e: e.activation(out=yacc[:, n, h, :], in_=bank[bk][:, o0:o0 + 64], func=AF.Identity, scale=coef[si][:, h:h + 1]),
                                 reads=[Tb[bk], T_s], writes=[Ty[g][h]])
                        if P3A == "y":
                            continue
                        S.op("dve", lambda e: e.max(out=m1[si][:], in_=impt[si][:]), reads=[T_s], writes=[T_s])
                        S.op("dve", lambda e: e.match_replace(out=tmp[si][:], in_to_replace=m1[si][:], in_values=impt[si][:], imm_value=-3e9), reads=[T_s], writes=[T_s])
                        S.op("dve", lambda e: e.max(out=m2[si][:], in_=tmp[si][:]), reads=[T_s], writes=[T_s])
                        if P3A == "tk":
                            continue
                        mi = nxt("mb", 4)
                        S.op("dve", lambda e: e.tensor_scalar(out=Mbp[mi][:, 64:128], in0=impt[si][:], scalar1=m2[si][:, 7:8], scalar2=NEG, op0=ALU.is_lt, op1=ALU.mult),
                             reads=[T_s], writes=[TMb[mi]])
                        S.op("pe", lambda e: e.matmul(bank[6][:, jq * 128:(jq + 1) * 128], lhsT=Mbp[mi][:], rhs=k.identb[:], start=True, stop=True),
                             reads=[TMb[mi], k.Tidb], writes=[Tb[6]])
                    S.op("dve", lambda e: e.tensor_copy(out=QS[0][64:128, gsl], in_=bank[6][64:128, :]), reads=[Tb[6]], writes=[TQm[0][g]])
                    for h in range(1, 4):
                        S.op("pool", lambda e: e.tensor_copy(out=QS[h][64:128, gsl], in_=QS[0][64:128, gsl]), reads=[TQm[0][g]], writes=[TQm[h][g]])
                    for h in range(4):
                        if P3STOP == "A":
                            break
                        for br in ((2,) if P3STOP == "W" else (2, 1)):
                            if br == 2:
                                kts = [kt for kt in range(4 * g - 4, 4 * g + 4) if kt >= 0]
                                ob = 3 + nxt("ow", 2)
                            else:
                                kts = list(range(0, 4 * g + 4))
                                ob = 5 + nxt("os", 2)
                            Vt, TV = (VW, TVW) if br == 2 else (VS, TVS)
                            pis = {}

                            def qk(idx, kt):
                                ksl = slice(kt * 128, (kt + 1) * 128)
                                bi = nxt("p", 3)
                                if br == 2:
                                    S.op("pe", lambda e: e.matmul(bank[bi][:], lhsT=KW[0:64, ksl], rhs=QS[h][0:64, gsl], start=True, stop=False),
                                         reads=[TKW, TQd[h]], writes=[Tb[bi]])
                                    S.op("pe", lambda e: e.matmul(bank[bi][:], lhsT=k.identb[:], rhs=wmaskS[:, kt - 4 * g + 4, :], start=False, stop=True),
                                         reads=[k.Tidb, Tc2], writes=[Tb[bi]])
                                else:
                                    diag = kt >= 4 * g
                                    S.op("pe", lambda e: e.matmul(bank[bi][:], lhsT=KS[:, ksl], rhs=QS[h][:, gsl], start=True, stop=not diag),
                                         reads=[TKS, TE, TQd[h], TQm[h][g]], writes=[Tb[bi]])
                                    if diag:
                                        S.op("pe", lambda e: e.matmul(bank[bi][:], lhsT=k.identb[:], rhs=dmaskS[:, kt - 4 * g, :], start=False, stop=True),
                                             reads=[k.Tidb, Tc3], writes=[Tb[bi]])
                                pi = nxt("s", NP)
                                S.op("act", lambda e: e.activation(out=P[pi][:], in_=bank[bi][:], func=AF.Exp), reads=[Tb[bi]], writes=[TP[pi]])
                                pis[idx] = pi

                            def pvf(idx, kt):
                                pi = pis.pop(idx)
                                S.op("pe", lambda e: e.matmul(bank[ob][0:65, :], lhsT=Vt[:, kt, 0:65], rhs=P[pi][:], start=(idx == 0), stop=(idx == len(kts) - 1)),
                                     reads=[TV[kt // 8], TP[pi]], writes=[Tb[ob]])
                            LA = 2
                            for i_ in range(len(kts) + LA):
                                if i_ < len(kts):
                                    qk(i_, kts[i_])
                                if i_ >= LA:
                                    pvf(i_ - LA, kts[i_ - LA])
                            oi = nxt("ot", 2)
                            S.op("dve", lambda e: e.tensor_copy(out=ot[oi][:], in_=bank[ob][0:65, :]), reads=[Tb[ob]], writes=[Tot[oi]])
                            for jq in range(4):
                                S.op("pe", lambda e: e.transpose(out=bank[7][:, jq * 128:jq * 128 + 65], in_=ot[oi][:, jq * 128:(jq + 1) * 128], identity=k.identf[0:65, 0:65]),
                                     reads=[Tot[oi], k.Tid], writes=[Tb[7]])
                            si = nxt("sm", NS)
                            T_s = Tsm[si]
                            b7 = bank[7][:].rearrange("p (a f) -> p a f", a=4)
                            S.op("dve", lambda e: e.reciprocal(out=rs[si][:], in_=b7[:, :, 64]), reads=[Tb[7]], writes=[T_s])
                            gc = br * 8 + kv * 4 + h
                            S.op("dve", lambda e: e.tensor_tensor(out=coef[si][:], in0=rs[si][:], in1=G_[:, 4 * g:4 * g + 4, gc], op=ALU.mult), reads=[T_s, TG], writes=[T_s])
                            for jq in range(4):
                                n = 4 * g + jq
                                S.op("dve", lambda e: e.scalar_tensor_tensor(out=yacc[:, n, h, :], in0=bank[7][:, jq * 128:jq * 128 + 64], scalar=coef[si][:, jq:jq + 1], in1=yacc[:, n, h, :], op0=ALU.mult, op1=ALU.add),
                                     reads=[Tb[7], T_s, Ty[g][h]], writes=[Ty[g][h]])
                if P3STOP == "load" and os.environ.get("P3NOST", ""):
                    continue
                ydst = k.scr["ycat"][b].rearrange("(n p) c -> p n c", p=128)
                for g in range(NG):
                    k.store(ydst[:, 4 * g:4 * g + 4, kv * 256:(kv + 1) * 256], yacc[:, 4 * g:4 * g + 4, :, :].rearrange("p n h d -> p n (h d)"), Ty[g])
        S.barrier()


K.phase3 = phase3
PHASES.append("phase3")


def phase5(self, l):
    k, S = self, self.S
    with contextlib.ExitStack() as es:
        sb = lambda n, sh, dt=F32: k.sb(es, "p5_" + n, sh, dt)
        wdw = sb("wdw", [128, 2, 31], F32)
        Twd = S.T("p5_wdw")
        S.dma("sp", wdw[:], k.inp["wdw_t"][l].rearrange("(ct p) kk -> p ct kk", p=128), writes=[Twd])
        Dg = sb("Dg", [128, 2, 31, 128], BF16)
        TDg = S.T("p5_Dg")
        for ct in range(2):
            for kk in range(31):
                S.op("pool" if (kk % 2) else "dve", lambda e: e.tensor_scalar(out=Dg[:, ct, kk, :], in0=k.identf[:], scalar1=wdw[:, ct, kk:kk + 1], scalar2=None, op0=ALU.mult),
                     reads=[Twd, k.Tid], writes=[TDg])
        rows = sb("rows", [1, 2, 256], F32)
        rowsb = sb("rowsb", [1, 2, 256], BF16)
        Trows = S.T("p5_rows")
        S.dma("sp", rows[:, 0, :], k.inp["conv_b_dw"][l:l + 1, :], writes=[Trows])
        S.dma("sp", rows[:, 1, :], k.inp["conv_b_pw"][l:l + 1, :], writes=[Trows])
        S.op("dve", lambda e: e.tensor_copy(out=rowsb[:], in_=rows[:]), reads=[Trows], writes=[Trows])
        lng = sb("lng", [128, 256], F32)
        lnb = sb("lnb", [128, 256], F32)
        Tln = S.T("p5_ln")
        S.dma("sp", lng[:], k.inp["conv_ln_g"][l].partition_broadcast(128), writes=[Tln])
        Tln2 = S.T("p5_ln2")
        S.dma("sp", lnb[:], k.inp["conv_ln_b"][l].partition_broadcast(128), writes=[Tln2])
        wps = sb("wps", [128, 2, 256], F32)
        wpb = sb("wpb", [128, 2, 256], BF16)
        Twp = S.T("p5_wp")
        S.dma("sp", wps[:], k.inp["conv_w_pw"][l].rearrange("(ct p) n -> p ct n", p=128), writes=[Twp])
        S.op("dve", lambda e: e.tensor_copy(out=wpb[:], in_=wps[:]), reads=[Twp], writes=[Twp])
        Ub = sb("Ub", [128, 2, 32 + SEQ], BF16)
        TUb = [S.T(f"p5_Ub{c}") for c in range(2)]
        S.op("pool", lambda e: e.memset(Ub[:, :, 0:32], 0.0), writes=TUb)
        pc = [k.ps(es, f"p5_pc{i}", [128, 512], F32) for i in range(2)]
        Tpc = [S.T(f"p5_pc{i}") for i in range(2)]
        pz = [k.ps(es, f"p5_pz{i}", [128, 256], BF16) for i in range(2)]
        Tpz = [S.T(f"p5_pz{i}") for i in range(2)]
        po = [k.ps(es, f"p5_po{i}", [128, 512], F32) for i in range(2)]
        Tpo = [S.T(f"p5_po{i}") for i in range(2)]
        NR = 3
        st = [sb(f"st{i}", [128, 6], F32) for i in range(NR)]
        mv = [sb(f"mv{i}", [128, 2], F32) for i in range(NR)]
        rstd = [sb(f"rstd{i}", [128, 1], F32) for i in range(NR)]
        xn = [sb(f"xn{i}", [128, 256], F32) for i in range(NR)]
        zb = [sb(f"zb{i}", [128, 256], BF16) for i in range(NR)]
        zT = [sb(f"zT{i}", [128, 2, 128], BF16) for i in range(NR)]
        Tr = [S.T(f"p5_r{i}") for i in range(NR)]
        TzT = [S.T(f"p5_zT{i}") for i in range(NR)]
        yo = [sb(f"yo{i}", [128, 4, 256], F32) for i in range(2)]
        Tyo = [S.T(f"p5_yo{i}") for i in range(2)]
        it = 0
        for b in range(NB):
            for ct in range(2):
                S.dma("sp", Ub[:, ct, 32:32 + SEQ], k.scr["ucT"][b, ct * 128:(ct + 1) * 128, :], writes=[TUb[ct]])
            for n in range(NT):
                u = it % 2
                r_ = it % NR
                it += 1
                t0 = n * 128 + 2
                for ct in range(2):
                    csl = slice(ct * 128, (ct + 1) * 128)
                    S.op("pe", lambda e: e.matmul(pc[u][:, csl], lhsT=k.ones_b[0:1, 0:128], rhs=rowsb[0:1, 0, csl], start=True, stop=False),
                         reads=[k.Tones, Trows], writes=[Tpc[u]])
                    for kk in range(31):
                        S.op("pe", lambda e: e.matmul(pc[u][:, csl], lhsT=Ub[:, ct, t0 + kk:t0 + kk + 128], rhs=Dg[:, ct, kk, :], start=False, stop=(kk == 30)),
                             reads=[TUb[ct], TDg], writes=[Tpc[u]])
                T_r = Tr[r_]
                S.op("dve", lambda e: e.bn_stats(out=st[r_][:], in_=pc[u][:, 0:256]), reads=[Tpc[u]], writes=[T_r])
                S.op("dve", lambda e: e.bn_aggr(out=mv[r_][:], in_=st[r_][:]), reads=[T_r], writes=[T_r])
                S.op("dve", lambda e: e.tensor_scalar(out=rstd[r_][:], in0=mv[r_][:, 1:2], scalar1=EPS, scalar2=None, op0=ALU.add), reads=[T_r], writes=[T_r])
                S.op("act", lambda e: e.activation(out=rstd[r_][:], in_=rstd[r_][:], func=AF.Sqrt), reads=[T_r], writes=[T_r])
                S.op("dve", lambda e: e.reciprocal(out=rstd[r_][:], in_=rstd[r_][:]), reads=[T_r], writes=[T_r])
                S.op("dve", lambda e: e.tensor_scalar(out=xn[r_][:], in0=pc[u][:, 0:256], scalar1=mv[r_][:, 0:1], scalar2=rstd[r_][:, 0:1], op0=ALU.subtract, op1=ALU.mult),
                     reads=[Tpc[u], T_r], writes=[T_r])
                S.op("pool", lambda e: e.tensor_tensor(out=xn[r_][:], in0=xn[r_][:], in1=lng[:], op=ALU.mult), reads=[T_r, Tln], writes=[T_r])
                S.op("pool", lambda e: e.tensor_tensor(out=xn[r_][:], in0=xn[r_][:], in1=lnb[:], op=ALU.add), reads=[T_r, Tln2], writes=[T_r])
                S.op("act", lambda e: e.activation(out=zb[r_][:], in_=xn[r_][:], func=AF.Silu), reads=[T_r], writes=[T_r])
                for ct in range(2):
                    S.op("pe", lambda e: e.transpose(out=pz[u][:, ct * 128:(ct + 1) * 128], in_=zb[r_][:, ct * 128:(ct + 1) * 128], identity=k.identb[:]),
                         reads=[T_r, k.Tidb], writes=[Tpz[u]])
                S.op("act", lambda e: e.activation(out=zT[r_][:].rearrange("p c t -> p (c t)"), in_=pz[u][:], func=AF.Copy), reads=[Tpz[u]], writes=[TzT[r_]])
                S.op("pe", lambda e: e.matmul(po[u][:, 0:256], lhsT=k.ones_b[0:1, 0:128], rhs=rowsb[0:1, 1, :], start=True, stop=False),
                     reads=[k.Tones, Trows], writes=[Tpo[u]])
                for ct in range(2):
                    S.op("pe", lambda e: e.matmul(po[u][:, 0:256], lhsT=zT[r_][:, ct, :], rhs=wpb[:, ct, :], start=False, stop=(ct == 1)),
                         reads=[TzT[r_], Twp], writes=[Tpo[u]])
                yi = (n // 4) % 2
                S.op("dve", lambda e: e.tensor_copy(out=yo[yi][:, n % 4, :], in_=po[u][:, 0:256]), reads=[Tpo[u]], writes=[Tyo[yi]])
                if n % 4 == 3:
                    ydst = k.scr["ycat"][b].rearrange("(n p) c -> p n c", p=128)
                    k.store(ydst[:, n - 3:n + 1, 768:1024], yo[yi][:], [Tyo[yi]])
        S.barrier()


K.phase5 = phase5
PHASES.append("phase5")


def phase6a(self, l):
    k, S = self, self.S
    with contextlib.ExitStack() as es:
        sb = lambda n, sh, dt=F32: k.sb(es, "p6a_" + n, sh, dt)
        Wo = sb("Wo", [128, 8, D], BF16)
        TWo = [S.T(f"p6a_Wo{i}") for i in range(8)]
        stage = [sb(f"stage{i}", [128, D], F32) for i in range(2)]
        Tst = [S.T(f"p6a_stage{i}") for i in range(2)]
        gcat = sb("gcat", [128, 8], F32)
        Tg = S.T("p6a_gcat")
        S.dma("sp", gcat[:], k.inp["gcat_t"][l], writes=[Tg])
        wsrc = k.inp["w_out"][l].rearrange("(kc p) n -> p kc n", p=128)
        for kc in range(8):
            S.dma("sp", stage[kc % 2][:], wsrc[:, kc, :], writes=[Tst[kc % 2]])
            S.op("pool" if kc % 2 else "dve", lambda e: e.tensor_scalar(out=Wo[:, kc, :], in0=stage[kc % 2][:], scalar1=gcat[:, kc:kc + 1], scalar2=None, op0=ALU.mult),
                 reads=[Tst[kc % 2], Tg], writes=[TWo[kc]])
        yg = [sb(f"yg{i}", [128, 4, D], F32) for i in range(2)]
        Tyg = [S.T(f"p6a_yg{i}") for i in range(2)]
        xg = [sb(f"xg{i}", [128, 4, D], F32) for i in range(2)]
        Txg = [[S.T(f"p6a_xg{i}_{j}") for j in range(4)] for i in range(2)]
        junk = sb("junk", [128, 512], BF16)
        Tjunk = S.T("p6a_junk")
        ss = sb("ss", [128, 4, 4], F32)
        Tss = S.T("p6a_ss")
        mb = sb("mb", [128, 4, D], BF16)
        Tmb = [S.T(f"p6a_mb{j}") for j in range(4)]
        mT = sb("mT", [128, 8, 512], BF16)
        TmT = [S.T(f"p6a_mT{c}") for c in range(8)]
        hb = sb("hb", [128, 4, D], BF16)
        Thb = [S.T(f"p6a_hb{j}") for j in range(4)]
        hT = [sb(f"hT{i}", [128, 8, 512], BF16) for i in range(2)]
        ThT = [S.T(f"p6a_hT{i}") for i in range(2)]
        pT = [k.ps(es, f"p6a_pT{i}", [128, 512], BF16) for i in range(2)]
        TpT = [S.T(f"p6a_pT{i}") for i in range(2)]
        pm = [k.ps(es, f"p6a_pm{i}", [128, 512], F32) for i in range(4)]
        Tpm = [S.T(f"p6a_pm{i}") for i in range(4)]
        xin = k.inp["x"] if l == 0 else k.scr["xres"]
        segs = [(0, 512), (512, 768), (768, 1024)]
        gi = 0
        pmi = 0
        for b in range(NB):
            for g in range(NG):
                u = gi % 2
                gi += 1
                tsl = slice(g * 512, (g + 1) * 512)
                S.dma("sp", yg[u][:], k.scr["ycat"][b, tsl, :].rearrange("(j p) d -> p j d", p=128), writes=[Tyg[u]])
                for j in range(4):
                    S.dma("sp", xg[u][:, j, :], xin[b, g * 512 + j * 128:g * 512 + (j + 1) * 128, :], writes=[Txg[u][j]])
                for j in range(4):
                    for si, (a0, a1) in enumerate(segs):
                        S.op("act", lambda e: e.activation(out=junk[:, 0:a1 - a0], in_=yg[u][:, j, a0:a1], func=AF.Square, accum_out=ss[:, j, si:si + 1]),
                             reads=[Tyg[u]], writes=[Tjunk, Tss])
                for si, (a0, a1) in enumerate(segs):
                    S.op("dve", lambda e: e.tensor_scalar(out=ss[:, :, si], in0=ss[:, :, si], scalar1=1.0 / (a1 - a0), scalar2=EPS, op0=ALU.mult, op1=ALU.add), reads=[Tss], writes=[Tss])
                S.op("act", lambda e: e.activation(out=ss[:, :, 0:3], in_=ss[:, :, 0:3], func=AF.Sqrt), reads=[Tss], writes=[Tss])
                S.op("dve", lambda e: e.reciprocal(out=ss[:, :, 0:3], in_=ss[:, :, 0:3]), reads=[Tss], writes=[Tss])
                for j in range(4):
                    for si, (a0, a1) in enumerate(segs):
                        S.op("pool" if (si == 0) else "dve", lambda e: e.tensor_scalar(out=mb[:, j, a0:a1], in0=yg[u][:, j, a0:a1], scalar1=ss[:, j, si:si + 1], scalar2=None, op0=ALU.mult),
                             reads=[Tyg[u], Tss], writes=[Tmb[j]])
                for kc in range(8):
                    pu = kc % 2
                    for j in range(4):
                        S.op("pe", lambda e: e.transpose(out=pT[pu][:, j * 128:(j + 1) * 128], in_=mb[:, j, kc * 128:(kc + 1) * 128], identity=k.identb[:]),
                             reads=[Tmb[j], k.Tidb], writes=[TpT[pu]])
                    S.op("act" if kc % 2 else "dve", (lambda e: e.activation(out=mT[:, kc, :], in_=pT[pu][:], func=AF.Copy)) if kc % 2 else (lambda e: e.tensor_copy(out=mT[:, kc, :], in_=pT[pu][:])),
                         reads=[TpT[pu]], writes=[TmT[kc]])
                for j in range(4):
                    for half in range(2):
                        i = pmi % 4
                        pmi += 1
                        hsl = slice(half * 512, (half + 1) * 512)
                        for kc in range(8):
                            S.op("pe", lambda e: e.matmul(pm[i][:], lhsT=mT[:, kc, j * 128:(j + 1) * 128], rhs=Wo[:, kc, hsl], start=(kc == 0), stop=(kc == 7)),
                                 reads=[TmT[kc], TWo[kc]], writes=[Tpm[i]])
                        S.op("dve", lambda e: e.tensor_tensor(out=xg[u][:, j, hsl], in0=pm[i][:], in1=xg[u][:, j, hsl], op=ALU.add), reads=[Tpm[i], Txg[u][j]], writes=[Txg[u][j]])
                    k.store(k.scr["xres"][b, g * 512 + j * 128:g * 512 + (j + 1) * 128, :], xg[u][:, j, :], [Txg[u][j]])
                    S.op("act", lambda e: e.activation(out=junk[:, 0:512], in_=xg[u][:, j, 0:512], func=AF.Square, accum_out=ss[:, j, 3:4]),
                         reads=[Txg[u][j]], writes=[Tjunk, Tss])
                    S.op("act", lambda e: e.activation(out=junk[:, 0:512], in_=xg[u][:, j, 512:1024], func=AF.Square, accum_out=ss[:, j, 0:1]),
                         reads=[Txg[u][j]], writes=[Tjunk, Tss])
                S.op("dve", lambda e: e.tensor_tensor(out=ss[:, :, 3], in0=ss[:, :, 3], in1=ss[:, :, 0], op=ALU.add), reads=[Tss], writes=[Tss])
                S.op("dve", lambda e: e.tensor_scalar(out=ss[:, :, 3], in0=ss[:, :, 3], scalar1=1.0 / D, scalar2=EPS, op0=ALU.mult, op1=ALU.add), reads=[Tss], writes=[Tss])
                S.op("act", lambda e: e.activation(out=ss[:, :, 3], in_=ss[:, :, 3], func=AF.Sqrt), reads=[Tss], writes=[Tss])
                S.op("dve", lambda e: e.reciprocal(out=ss[:, :, 3], in_=ss[:, :, 3]), reads=[Tss], writes=[Tss])
                for j in range(4):
                    S.op("pool" if j % 2 else "dve", lambda e: e.tensor_scalar(out=hb[:, j, :], in0=xg[u][:, j, :], scalar1=ss[:, j, 3:4], scalar2=None, op0=ALU.mult),
                         reads=[Txg[u][j], Tss], writes=[Thb[j]])
                for kc in range(8):
                    pu = kc % 2
                    for j in range(4):
                        S.op("pe", lambda e: e.transpose(out=pT[pu][:, j * 128:(j + 1) * 128], in_=hb[:, j, kc * 128:(kc + 1) * 128], identity=k.identb[:]),
                             reads=[Thb[j], k.Tidb], writes=[TpT[pu]])
                    S.op("act" if kc % 2 else "dve", (lambda e: e.activation(out=hT[u][:, kc, :], in_=pT[pu][:], func=AF.Copy)) if kc % 2 else (lambda e: e.tensor_copy(out=hT[u][:, kc, :], in_=pT[pu][:])),
                         reads=[TpT[pu]], writes=[ThT[u]])
                k.store(k.scr["h2T"][b, :, tsl].rearrange("(kc p) t -> p kc t", p=128), hT[u][:], [ThT[u]])
        S.barrier()


def phase6b(self, l):
    k, S = self, self.S
    with contextlib.ExitStack() as es:
        sb = lambda n, sh, dt=F32: k.sb(es, "p6b_" + n, sh, dt)
        Wg = sb("Wg", [128, 8, DFF], BF16)
        Wu = sb("Wu", [128, 8, DFF], BF16)
        Wd = sb("Wd", [128, NFF, D], BF16)
        TWg = [S.T(f"p6b_Wg{i}") for i in range(8)]
        TWu = [S.T(f"p6b_Wu{i}") for i in range(8)]
        TWd = [S.T(f"p6b_Wd{i}") for i in range(NFF)]
        with contextlib.ExitStack() as es2:
            stage = [k.sb(es2, f"p6b_stage{i}", [128, DFF], F32) for i in range(2)]
            Tst = [S.T(f"p6b_stage{i}") for i in range(2)]
            gn = k.sb(es2, "p6b_gn", [128, 8], F32)
            Tgn = S.T("p6b_gn")
            S.dma("sp", gn[:], k.inp["norm_ffn_t"][l], writes=[Tgn])
            si = 0
            for nm, Wt, TWt in [("w_gate", Wg, TWg), ("w_up", Wu, TWu)]:
                wsrc = k.inp[nm][l].rearrange("(kc p) n -> p kc n", p=128)
                for kc in range(8):
                    u = si % 2
                    si += 1
                    S.dma("sp", stage[u][:], wsrc[:, kc, :], writes=[Tst[u]])
                    S.op("pool" if u else "dve", lambda e: e.tensor_scalar(out=Wt[:, kc, :], in0=stage[u][:], scalar1=gn[:, kc:kc + 1], scalar2=None, op0=ALU.mult),
                         reads=[Tst[u], Tgn], writes=[TWt[kc]])
            wsrc = k.inp["w_down"][l].rearrange("(fc p) n -> p fc n", p=128)
            for fc in range(0, NFF, 2):
                u = si % 2
                si += 1
                S.dma("sp", stage[u][:, 0:2 * D].rearrange("p (a n) -> p a n", a=2), wsrc[:, fc:fc + 2, :], writes=[Tst[u]])
                S.op("pool" if u else "act", (lambda e: e.tensor_copy(out=Wd[:, fc:fc + 2, :].rearrange("p a n -> p (a n)"), in_=stage[u][:, 0:2 * D])) if u else
                     (lambda e: e.activation(out=Wd[:, fc:fc + 2, :].rearrange("p a n -> p (a n)"), in_=stage[u][:, 0:2 * D], func=AF.Copy)),
                     reads=[Tst[u]], writes=[TWd[fc], TWd[fc + 1]])
            S.barrier()
        hT = [sb(f"hT{i}", [128, 8, 512], BF16) for i in range(2)]
        ThT = [S.T(f"p6b_hT{i}") for i in range(2)]
        NX = 3
        xt = [sb(f"xt{i}", [128, D], F32) for i in range(NX)]
        Txt = [S.T(f"p6b_xt{i}") for i in range(NX)]
        actT = sb("actT", [128, NFF, 512], BF16)
        Tact = [S.T(f"p6b_act{i}") for i in range(NFF)]
        sg = [sb(f"sg{i}", [128, 512], BF16) for i in range(2)]
        Tsg = [S.T(f"p6b_sg{i}") for i in range(2)]
        pg = [k.ps(es, f"p6b_pg{i}", [128, 512], F32) for i in range(2)]
        pu_ = [k.ps(es, f"p6b_pu{i}", [128, 512], F32) for i in range(2)]
        pd = [k.ps(es, f"p6b_pd{i}", [128, 512], F32) for i in range(2)]
        Tpg = [S.T(f"p6b_pg{i}") for i in range(2)]
        Tpu = [S.T(f"p6b_pu{i}") for i in range(2)]
        Tpd = [S.T(f"p6b_pd{i}") for i in range(2)]
        gi = 0
        xi = 0
        pdi = 0
        for b in range(NB):
            for g in range(NG):
                u = gi % 2
                gi += 1
                tsl = slice(g * 512, (g + 1) * 512)
                S.dma("sp", hT[u][:], k.scr["h2T"][b, :, tsl].rearrange("(kc p) t -> p kc t", p=128), writes=[ThT[u]])
                for fc in range(NFF):
                    v = fc % 2
                    fsl = slice(fc * 128, (fc + 1) * 128)
                    for kc in range(8):
                        S.op("pe", lambda e: e.matmul(pg[v][:], lhsT=Wg[:, kc, fsl], rhs=hT[u][:, kc, :], start=(kc == 0), stop=(kc == 7)),
                             reads=[TWg[kc], ThT[u]], writes=[Tpg[v]])
                    for kc in range(8):
                        S.op("pe", lambda e: e.matmul(pu_[v][:], lhsT=Wu[:, kc, fsl], rhs=hT[u][:, kc, :], start=(kc == 0), stop=(kc == 7)),
                             reads=[TWu[kc], ThT[u]], writes=[Tpu[v]])
                    S.op("act", lambda e: e.activation(out=sg[v][:], in_=pg[v][:], func=AF.Silu), reads=[Tpg[v]], writes=[Tsg[v]])
                    S.op("dve", lambda e: e.tensor_tensor(out=actT[:, fc, :], in0=pu_[v][:], in1=sg[v][:], op=ALU.mult), reads=[Tpu[v], Tsg[v]], writes=[Tact[fc]])
                for j in range(4):
                    xx = xi % NX
                    xi += 1
                    r0 = g * 512 + j * 128
                    S.dma("sp", xt[xx][:], k.scr["xres"][b, r0:r0 + 128, :], writes=[Txt[xx]])
                    for half in range(2):
                        pi = pdi % 2
                        pdi += 1
                        hsl = slice(half * 512, (half + 1) * 512)
                        for fc in range(NFF):
                            S.op("pe", lambda e: e.matmul(pd[pi][:], lhsT=actT[:, fc, j * 128:(j + 1) * 128], rhs=Wd[:, fc, hsl], start=(fc == 0), stop=(fc == NFF - 1)),
                                 reads=[Tact[fc], TWd[fc]], writes=[Tpd[pi]])
                        S.op("dve", lambda e: e.tensor_tensor(out=xt[xx][:, hsl], in0=pd[pi][:], in1=xt[xx][:, hsl], op=ALU.add), reads=[Tpd[pi], Txt[xx]], writes=[Txt[xx]])
                    k.store(k.scr["xres"][b, r0:r0 + 128, :], xt[xx][:], [Txt[xx]])
        S.barrier()


K.phase6a = phase6a
K.phase6b = phase6b


def phase4(self, l):
    k, S = self, self.S
    PI = float(np.pi)
    C1 = float(np.float32(2 * np.pi))
    C2 = float(2 * np.pi - float(np.float32(2 * np.pi)))
    with contextlib.ExitStack() as es:
        sb = lambda n, sh, dt=F32: k.sb(es, "p4_" + n, sh, dt)
        Tp = S.T("p4_par")
        are, aim, ldt = sb("are", [128, 8]), sb("aim", [128, 8]), sb("ldt", [128, 8])
        S.dma("sp", are[:], k.inp["ssm_are_t"][l], writes=[Tp])
        Tp2 = S.T("p4_par2")
        S.dma("sp", aim[:], k.inp["ssm_aim_t"][l], writes=[Tp2])
        Tp3 = S.T("p4_par3")
        S.dma("sp", ldt[:], k.inp["ssm_ldt_t"][l], writes=[Tp3])
        dtt, z, th, mag, q = sb("dtt", [128, 8]), sb("z", [128, 8]), sb("th", [128, 8]), sb("mag", [128, 8]), sb("q", [128, 8])

        def dv(fn, reads=(), writes=(Tp,)):
            S.op("dve", fn, reads=list(reads) + [Tp], writes=list(writes))

        S.op("act", lambda e: e.activation(out=dtt[:], in_=ldt[:], func=AF.Exp), reads=[Tp3], writes=[Tp])
        dv(lambda e: e.tensor_tensor(out=z[:], in0=are[:], in1=dtt[:], op=ALU.mult))
        dv(lambda e: e.tensor_tensor(out=th[:], in0=aim[:], in1=dtt[:], op=ALU.mult), reads=[Tp2])
        dv(lambda e: e.tensor_scalar(out=q[:], in0=z[:], scalar1=1.0 / 6.0, scalar2=1.0, op0=ALU.mult, op1=ALU.add))
        for kk in (5.0, 4.0, 3.0, 2.0, 1.0):
            dv(lambda e: e.tensor_tensor(out=q[:], in0=q[:], in1=z[:], op=ALU.mult))
            dv(lambda e: e.tensor_scalar(out=q[:], in0=q[:], scalar1=1.0 / kk, scalar2=1.0, op0=ALU.mult, op1=ALU.add))
        dv(lambda e: e.tensor_copy(out=mag[:], in_=q[:]))

        iot = sb("iota", [128, 512])
        Tio = S.T("p4_iota")
        S.dma("sp", iot[:], k.inp["iota512"].partition_broadcast(128), writes=[Tio])
        Ct = sb("Ct", [128, 8, 512])
        St = sb("St", [128, 8, 512])
        TCt = [S.T(f"p4_Ct{m}") for m in range(8)]
        ang = [sb(f"ang{i}", [128, 512]) for i in range(2)]
        kf = [sb(f"kf{i}", [128, 512]) for i in range(2)]
        ki = [sb(f"ki{i}", [128, 512], mybir.dt.int32) for i in range(2)]
        Tang = [S.T(f"p4_ang{i}") for i in range(2)]
        ai = 0
        for m in range(8):
            for which in range(2):
                a_ = ai % 2
                ai += 1
                eng = "dve" if which == 0 else "pool"
                Ta = Tang[a_]
                A, KF, KI = ang[a_], kf[a_], ki[a_]
                off = 0.0 if which == 0 else PI / 2
                S.op("dve", lambda e: e.tensor_scalar(out=A[:], in0=iot[:], scalar1=th[:, m:m + 1], scalar2=off, op0=ALU.mult, op1=ALU.add), reads=[Tio, Tp], writes=[Ta])
                S.op("dve", lambda e: e.tensor_scalar(out=KI[:], in0=A[:], scalar1=1.0 / (2 * PI), scalar2=None, op0=ALU.mult), reads=[Ta], writes=[Ta])
                S.op("dve", lambda e: e.tensor_copy(out=KF[:], in_=KI[:]), reads=[Ta], writes=[Ta])
                S.op("dve", lambda e: e.scalar_tensor_tensor(out=A[:], in0=KF[:], scalar=-C1, in1=A[:], op0=ALU.mult, op1=ALU.add), reads=[Ta], writes=[Ta])
                S.op("dve", lambda e: e.scalar_tensor_tensor(out=A[:], in0=KF[:], scalar=-C2, in1=A[:], op0=ALU.mult, op1=ALU.add), reads=[Ta], writes=[Ta])
                S.op("dve", lambda e: e.tensor_scalar(out=KF[:], in0=A[:], scalar1=PI, scalar2=-2 * PI, op0=ALU.is_gt, op1=ALU.mult), reads=[Ta], writes=[Ta])
                S.op("dve", lambda e: e.tensor_tensor(out=A[:], in0=A[:], in1=KF[:], op=ALU.add), reads=[Ta], writes=[Ta])
                S.op("dve", lambda e: e.tensor_scalar(out=KF[:], in0=A[:], scalar1=-PI, scalar2=2 * PI, op0=ALU.is_lt, op1=ALU.mult), reads=[Ta], writes=[Ta])
                S.op("dve", lambda e: e.tensor_tensor(out=A[:], in0=A[:], in1=KF[:], op=ALU.add), reads=[Ta], writes=[Ta])
                S.op("dve", lambda e: e.tensor_scalar(out=A[:], in0=A[:], scalar1=PI, scalar2=-PI, op0=ALU.min, op1=ALU.max), reads=[Ta], writes=[Ta])
                dst = St if which == 0 else Ct
                S.op("act", lambda e: e.activation(out=dst[:, m, :], in_=A[:], func=AF.Sin), reads=[Ta], writes=[TCt[m]])
        c1, s1, ns1 = sb("c1", [128, 8]), sb("s1", [128, 8]), sb("ns1", [128, 8])
        TC1 = S.T("p4_c1")
        S.op("dve", lambda e: e.tensor_copy(out=c1[:], in_=Ct[:, :, 1]), reads=TCt, writes=[TC1])
        S.op("dve", lambda e: e.tensor_copy(out=s1[:], in_=St[:, :, 1]), reads=TCt, writes=[TC1])
        S.op("dve", lambda e: e.tensor_scalar(out=ns1[:], in0=s1[:], scalar1=-1.0, scalar2=None, op0=ALU.mult), reads=[TC1], writes=[TC1])
        lre, lim, den, nr, fre, fim, nfim, t8 = [sb(n, [128, 8]) for n in ("lre", "lim", "den", "nr", "fre", "fim", "nfim", "t8")]

        def d2(fn):
            S.op("dve", fn, reads=[Tp, Tp2, TC1], writes=[TC1])

        d2(lambda e: e.tensor_tensor(out=lre[:], in0=mag[:], in1=c1[:], op=ALU.mult))
        d2(lambda e: e.tensor_tensor(out=lim[:], in0=mag[:], in1=s1[:], op=ALU.mult))
        d2(lambda e: e.tensor_tensor(out=den[:], in0=are[:], in1=are[:], op=ALU.mult))
        d2(lambda e: e.tensor_tensor(out=t8[:], in0=aim[:], in1=aim[:], op=ALU.mult))
        d2(lambda e: e.tensor_tensor(out=den[:], in0=den[:], in1=t8[:], op=ALU.add))
        d2(lambda e: e.reciprocal(out=den[:], in_=den[:]))
        d2(lambda e: e.tensor_scalar(out=nr[:], in0=lre[:], scalar1=-1.0, scalar2=None, op0=ALU.add))
        d2(lambda e: e.tensor_tensor(out=fre[:], in0=nr[:], in1=are[:], op=ALU.mult))
        d2(lambda e: e.tensor_tensor(out=t8[:], in0=lim[:], in1=aim[:], op=ALU.mult))
        d2(lambda e: e.tensor_tensor(out=fre[:], in0=fre[:], in1=t8[:], op=ALU.add))
        d2(lambda e: e.tensor_tensor(out=fre[:], in0=fre[:], in1=den[:], op=ALU.mult))
        d2(lambda e: e.tensor_tensor(out=fim[:], in0=lim[:], in1=are[:], op=ALU.mult))
        d2(lambda e: e.tensor_tensor(out=t8[:], in0=nr[:], in1=aim[:], op=ALU.mult))
        d2(lambda e: e.tensor_tensor(out=fim[:], in0=fim[:], in1=t8[:], op=ALU.subtract))
        d2(lambda e: e.tensor_tensor(out=fim[:], in0=fim[:], in1=den[:], op=ALU.mult))
        d2(lambda e: e.tensor_scalar(out=nfim[:], in0=fim[:], scalar1=-1.0, scalar2=None, op0=ALU.mult))
        Mre = sb("Mre", [128, 8, 128], BF16)
        Mim = sb("Mim", [128, 8, 128], BF16)
        Cre = sb("Cre", [128, 8, 128], BF16)
        Cim = sb("Cim", [128, 8, 128], BF16)
        TM = S.T("p4_M")
        TCp = S.T("p4_Cp")
        pset = k.ps(es, "p4_pset", [128, 512], BF16)
        Tpset = S.T("p4_pset")
        with contextlib.ExitStack() as es2:
            zbr = k.sb(es2, "p4_zbr", [128, 8, 128], F32)
            zbi = k.sb(es2, "p4_zbi", [128, 8, 128], F32)
            Tz1, Tz2 = S.T("p4_zbr"), S.T("p4_zbi")
            tt = k.sb(es2, "p4_tt", [128, 128], F32)
            bbr = k.sb(es2, "p4_bbr", [128, 128], BF16)
            bbi = k.sb(es2, "p4_bbi", [128, 128], BF16)
            Tbb = S.T("p4_bb")
            S.dma("sp", zbr[:], k.inp["Zb_re"][l], writes=[Tz1])
            S.dma("sp", zbi[:], k.inp["Zb_im"][l], writes=[Tz2])
            for m in range(8):
                S.op("dve", lambda e: e.tensor_scalar(out=tt[:], in0=zbr[:, m, :], scalar1=fre[:, m:m + 1], scalar2=None, op0=ALU.mult), reads=[Tz1, TC1], writes=[Tbb])
                S.op("dve", lambda e: e.scalar_tensor_tensor(out=bbr[:], in0=zbi[:, m, :], scalar=nfim[:, m:m + 1], in1=tt[:], op0=ALU.mult, op1=ALU.add), reads=[Tz2, TC1, Tbb], writes=[Tbb])
                S.op("dve", lambda e: e.tensor_scalar(out=tt[:], in0=zbi[:, m, :], scalar1=fre[:, m:m + 1], scalar2=None, op0=ALU.mult), reads=[Tz2, TC1, Tbb], writes=[Tbb])
                S.op("dve", lambda e: e.scalar_tensor_tensor(out=bbi[:], in0=zbr[:, m, :], scalar=fim[:, m:m + 1], in1=tt[:], op0=ALU.mult, op1=ALU.add), reads=[Tz1, TC1, Tbb], writes=[Tbb])
                S.op("pe", lambda e: e.transpose(out=pset[:, 0:128], in_=bbr[:], identity=k.identb[:]), reads=[Tbb, k.Tidb], writes=[Tpset])
                S.op("pe", lambda e: e.transpose(out=pset[:, 128:256], in_=bbi[:], identity=k.identb[:]), reads=[Tbb, k.Tidb], writes=[Tpset])
                S.op("act", lambda e: e.activation(out=Mre[:, m, :], in_=pset[:, 0:128], func=AF.Copy), reads=[Tpset], writes=[TM])
                S.op("act", lambda e: e.activation(out=Mim[:, m, :], in_=pset[:, 128:256], func=AF.Copy), reads=[Tpset], writes=[TM])
            S.dma("sp", zbr[:], k.inp["Cp_re"][l], writes=[Tz1])
            S.dma("sp", zbi[:], k.inp["Cp_im"][l], writes=[Tz2])
            S.op("dve", lambda e: e.tensor_copy(out=Cre[:], in_=zbr[:]), reads=[Tz1], writes=[TCp])
            S.op("dve", lambda e: e.tensor_scalar(out=Cim[:], in0=zbi[:], scalar1=-1.0, scalar2=None, op0=ALU.mult), reads=[Tz2], writes=[TCp])
            S.barrier()
        dvec = sb("dvec", [128, 2])
        Td = S.T("p4_d")
        S.dma("sp", dvec[:], k.inp["ssm_d_t"][l], writes=[Td])
        wgs = sb("wgs", [128, 2, 512])
        wgb = sb("wgb", [128, 2, 512], BF16)
        Twg = S.T("p4_wg")
        S.dma("sp", wgs[:], k.inp["ssm_w_glu"][l].rearrange("(mt p) n -> p mt n", p=128), writes=[Twg])
        S.op("dve", lambda e: e.tensor_copy(out=wgb[:], in_=wgs[:]), reads=[Twg], writes=[Twg])
        U = sb("U", [128, 2, SEQ], BF16)
        TU = [S.T(f"p4_U{i}") for i in range(2)]
        NW = 2
        pw = [[k.ps(es, f"p4_pw{i}_{c}", [128, 512], F32) for c in range(2)] for i in range(NW)]
        Tpw = [[S.T(f"p4_pw{i}_{c}") for c in range(2)] for i in range(NW)]
        py = [k.ps(es, f"p4_py{i}", [128, 512], F32) for i in range(2)]
        Tpy = [S.T(f"p4_py{i}") for i in range(2)]
        pgl = k.ps(es, "p4_pgl", [128, 512], F32)
        Tpgl = S.T("p4_pgl")
        ta = [sb(f"ta{i}", [128, 512]) for i in range(NW)]
        tb_ = [sb(f"tb{i}", [128, 512]) for i in range(NW)]
        vre = [sb(f"vre{i}", [128, 512]) for i in range(NW)]
        vim = [sb(f"vim{i}", [128, 512]) for i in range(NW)]
        sre = [sb(f"sre{i}", [128, 512]) for i in range(NW)]
        sim_ = [sb(f"sim{i}", [128, 512]) for i in range(NW)]
        xre = [sb(f"xre{i}", [128, 512], BF16) for i in range(NW)]
        xim = [sb(f"xim{i}", [128, 512], BF16) for i in range(NW)]
        Twk = [S.T(f"p4_wk{i}") for i in range(NW)]
        Tv = [S.T(f"p4_v{i}") for i in range(NW)]
        Ts = [S.T(f"p4_s{i}") for i in range(NW)]
        Tx = [S.T(f"p4_x{i}") for i in range(NW)]
        init = sb("init", [128, 8, 2])
        Tinit = [S.T(f"p4_init{m}") for m in range(8)]
        xl = sb("xl", [128, 8, 2])
        i4 = sb("i4", [128, 8, 2])
        yf = [sb(f"yf{i}", [128, 512]) for i in range(2)]
        gT = [sb(f"gT{i}", [128, 512], BF16) for i in range(2)]
        TgT = [S.T(f"p4_gT{i}") for i in range(2)]
        Tyf = [S.T(f"p4_yf{i}") for i in range(2)]
        sgl = [sb(f"sgl{i}", [128, 256]) for i in range(2)]
        Tsgl = [S.T(f"p4_sgl{i}") for i in range(2)]
        yo = [sb(f"yo{i}", [128, 4, 256]) for i in range(2)]
        Tyo = [S.T(f"p4_yo{i}") for i in range(2)]
        wi = 0
        gi = 0
        for b in range(NB):
            for mt in range(2):
                S.dma("sp", U[:, mt, :], k.scr["usT"][b, mt * 128:(mt + 1) * 128, :], writes=[TU[mt]])
            for m in range(8):
                S.op("pool", lambda e: e.memset(init[:, m, :], 0.0), writes=[Tinit[m]])
            for g in range(NG):
                tsl = slice(g * 512, (g + 1) * 512)
                for mt in range(2):
                    yb = (g * 2 + mt) % 2
                    for m in range(4 * mt, 4 * mt + 4):
                        w = wi % NW
                        wi += 1
                        Cm, Sm = Ct[:, m, :], St[:, m, :]
                        S.op("pe", lambda e: e.matmul(pw[w][0][:], lhsT=Mre[:, m, :], rhs=U[:, mt, tsl], start=True, stop=True), reads=[TM, TU[mt]], writes=[Tpw[w][0]])
                        S.op("pe", lambda e: e.matmul(pw[w][1][:], lhsT=Mim[:, m, :], rhs=U[:, mt, tsl], start=True, stop=True), reads=[TM, TU[mt]], writes=[Tpw[w][1]])
                        S.op("dve", lambda e: e.tensor_tensor(out=ta[w][:], in0=pw[w][0][:], in1=Cm, op=ALU.mult), reads=[Tpw[w][0], TCt[m]], writes=[Twk[w]])
                        S.op("dve", lambda e: e.tensor_tensor(out=tb_[w][:], in0=pw[w][1][:], in1=Sm, op=ALU.mult), reads=[Tpw[w][1], TCt[m]], writes=[Twk[w]])
                        S.op("pool", lambda e: e.tensor_tensor(out=vre[w][:], in0=ta[w][:], in1=tb_[w][:], op=ALU.add), reads=[Twk[w]], writes=[Tv[w]])
                        S.op("dve", lambda e: e.tensor_tensor(out=ta[w][:], in0=pw[w][1][:], in1=Cm, op=ALU.mult), reads=[Tpw[w][1], TCt[m], Tv[w]], writes=[Twk[w]])
                        S.op("dve", lambda e: e.tensor_tensor(out=tb_[w][:], in0=pw[w][0][:], in1=Sm, op=ALU.mult), reads=[Tpw[w][0], TCt[m], Tv[w]], writes=[Twk[w]])
                        S.op("pool", lambda e: e.tensor_tensor(out=vim[w][:], in0=ta[w][:], in1=tb_[w][:], op=ALU.subtract), reads=[Twk[w]], writes=[Tv[w]])
                        S.op("dve", lambda e: e.tensor_tensor_scan(out=sre[w][:], data0=mag[:, m:m + 1].broadcast_to([128, 512]), data1=vre[w][:], initial=init[:, m, 0:1], op0=ALU.mult, op1=ALU.add),
                             reads=[Tv[w], Tp, Tinit[m]], writes=[Ts[w]])
                        S.op("dve", lambda e: e.tensor_tensor_scan(out=sim_[w][:], data0=mag[:, m:m + 1].broadcast_to([128, 512]), data1=vim[w][:], initial=init[:, m, 1:2], op0=ALU.mult, op1=ALU.add),
                             reads=[Tv[w], Tp, Tinit[m]], writes=[Ts[w]])
                        S.op("pool", lambda e: e.tensor_tensor(out=i4[:, m, 0:1], in0=sre[w][:, 511:512], in1=Ct[:, m, 511:512], op=ALU.mult), reads=[Ts[w], TCt[m]], writes=[Tinit[m]])
                        S.op("pool", lambda e: e.tensor_tensor(out=i4[:, m, 1:2], in0=sim_[w][:, 511:512], in1=St[:, m, 511:512], op=ALU.mult), reads=[Ts[w], TCt[m]], writes=[Tinit[m]])
                        S.op("pool", lambda e: e.tensor_tensor(out=xl[:, m, 0:1], in0=i4[:, m, 0:1], in1=i4[:, m, 1:2], op=ALU.subtract), reads=[Tinit[m]], writes=[Tinit[m]])
                        S.op("pool", lambda e: e.tensor_tensor(out=i4[:, m, 0:1], in0=sim_[w][:, 511:512], in1=Ct[:, m, 511:512], op=ALU.mult), reads=[Ts[w], TCt[m]], writes=[Tinit[m]])
                        S.op("pool", lambda e: e.tensor_tensor(out=i4[:, m, 1:2], in0=sre[w][:, 511:512], in1=St[:, m, 511:512], op=ALU.mult), reads=[Ts[w], TCt[m]], writes=[Tinit[m]])
                        S.op("pool", lambda e: e.tensor_tensor(out=xl[:, m, 1:2], in0=i4[:, m, 0:1], in1=i4[:, m, 1:2], op=ALU.add), reads=[Tinit[m]], writes=[Tinit[m]])
                        S.op("pool", lambda e: e.tensor_tensor(out=i4[:, m, 0:1], in0=xl[:, m, 0:1], in1=c1[:, m:m + 1], op=ALU.mult), reads=[Tinit[m], TC1], writes=[Tinit[m]])
                        S.op("pool", lambda e: e.tensor_tensor(out=i4[:, m, 1:2], in0=xl[:, m, 1:2], in1=s1[:, m:m + 1], op=ALU.mult), reads=[Tinit[m], TC1], writes=[Tinit[m]])
                        S.op("pool", lambda e: e.tensor_tensor(out=init[:, m, 0:1], in0=i4[:, m, 0:1], in1=i4[:, m, 1:2], op=ALU.subtract), reads=[Tinit[m]], writes=[Tinit[m]])
                        S.op("pool", lambda e: e.tensor_tensor(out=i4[:, m, 0:1], in0=xl[:, m, 1:2], in1=c1[:, m:m + 1], op=ALU.mult), reads=[Tinit[m], TC1], writes=[Tinit[m]])
                        S.op("pool", lambda e: e.tensor_tensor(out=i4[:, m, 1:2], in0=xl[:, m, 0:1], in1=s1[:, m:m + 1], op=ALU.mult), reads=[Tinit[m], TC1], writes=[Tinit[m]])
                        S.op("pool", lambda e: e.tensor_tensor(out=init[:, m, 1:2], in0=i4[:, m, 0:1], in1=i4[:, m, 1:2], op=ALU.add), reads=[Tinit[m]], writes=[Tinit[m]])
                        S.op("pool", lambda e: e.tensor_tensor(out=ta[w][:], in0=sre[w][:], in1=Cm, op=ALU.mult), reads=[Ts[w], TCt[m]], writes=[Twk[w]])
                        S.op("pool", lambda e: e.tensor_tensor(out=tb_[w][:], in0=sim_[w][:], in1=Sm, op=ALU.mult), reads=[Ts[w], TCt[m]], writes=[Twk[w]])
                        S.op("pool", lambda e: e.tensor_tensor(out=xre[w][:], in0=ta[w][:], in1=tb_[w][:], op=ALU.subtract), reads=[Twk[w]], writes=[Tx[w]])
                        S.op("dve", lambda e: e.tensor_tensor(out=vre[w][:], in0=sim_[w][:], in1=Cm, op=ALU.mult), reads=[Ts[w], TCt[m]], writes=[Tv[w]])
                        S.op("dve", lambda e: e.tensor_tensor(out=vim[w][:], in0=sre[w][:], in1=Sm, op=ALU.mult), reads=[Ts[w], TCt[m]], writes=[Tv[w]])
                        S.op("pool", lambda e: e.tensor_tensor(out=xim[w][:], in0=vre[w][:], in1=vim[w][:], op=ALU.add), reads=[Tv[w]], writes=[Tx[w]])
                        first = (m == 4 * mt)
                        last = (m == 4 * mt + 3)
                        S.op("pe", lambda e: e.matmul(py[yb][:], lhsT=Cre[:, m, :], rhs=xre[w][:], start=first, stop=False), reads=[TCp, Tx[w]], writes=[Tpy[yb]])
                        S.op("pe", lambda e: e.matmul(py[yb][:], lhsT=Cim[:, m, :], rhs=xim[w][:], start=False, stop=last), reads=[TCp, Tx[w]], writes=[Tpy[yb]])
                    S.op("dve", lambda e: e.scalar_tensor_tensor(out=yf[mt][:], in0=U[:, mt, tsl], scalar=dvec[:, mt:mt + 1], in1=py[yb][:], op0=ALU.mult, op1=ALU.add),
                         reads=[TU[mt], Td, Tpy[yb]], writes=[Tyf[mt]])
                    S.op("act", lambda e: e.activation(out=gT[mt][:], in_=yf[mt][:], func=AF.Gelu_apprx_tanh), reads=[Tyf[mt]], writes=[TgT[mt]])
                yi = gi % 2
                gi += 1
                for j in range(4):
                    for mt in range(2):
                        S.op("pe", lambda e: e.matmul(pgl[:], lhsT=gT[mt][:, j * 128:(j + 1) * 128], rhs=wgb[:, mt, :], start=(mt == 0), stop=(mt == 1)),
                             reads=[TgT[mt], Twg], writes=[Tpgl])
                    sj = j % 2
                    S.op("act", lambda e: e.activation(out=sgl[sj][:], in_=pgl[:, 256:512], func=AF.Sigmoid), reads=[Tpgl], writes=[Tsgl[sj]])
                    S.op("dve", lambda e: e.tensor_tensor(out=yo[yi][:, j, :], in0=pgl[:, 0:256], in1=sgl[sj][:], op=ALU.mult), reads=[Tpgl, Tsgl[sj]], writes=[Tyo[yi]])
                ydst = k.scr["ycat"][b].rearrange("(n p) c -> p n c", p=128)
                k.store(ydst[:, 4 * g:4 * g + 4, 512:768], yo[yi][:], [Tyo[yi]])
        S.barrier()


K.phase4 = phase4
PHASES.extend(["phase4", "phase6a", "phase6b"])
```

```python
import contextlib
import numpy as np
import ml_dtypes
import concourse.bass as bass
import concourse.mybir as mybir
from concourse.bass_utils import run_bass_kernel_spmd

F32 = mybir.dt.float32
BF16 = mybir.dt.bfloat16
AF = mybir.ActivationFunctionType
ALU = mybir.AluOpType
AX = mybir.AxisListType

SEM_L = 20000
DMA_L = 16 * 1200
NB = 2
SEQ = 4096
D = 1024
NT = SEQ // 128
NG = SEQ // 512
DFF = 2816
NFF = DFF // 128
EPS = 1e-6
NEG = -30000.0
L = 2


class T:
    __slots__ = ("name", "w", "r", "sem", "semv", "semcls")

    def __init__(self, name=""):
        self.name = name
        self.w = None
        self.r = {}
        self.sem = None
        self.semv = 0
        self.semcls = None


class Sched:
    def __init__(self, nc):
        self.nc = nc
        self.h = {"pe": nc.tensor, "act": nc.scalar, "dve": nc.vector,
                  "pool": nc.gpsimd, "sp": nc.sync}
        self.cnt = {k: 0 for k in self.h}
        self.sems = {k: [] for k in self.h}
        self.waited = {}
        self.same = {"dve", "act", "pool"}
        self.nsem = 0
        self.ninst = 0
        self.all_T = []

    def T(self, name=""):
        t = T(name)
        self.all_T.append(t)
        return t

    def new_sem(self, name):
        self.nsem += 1
        return self.nc.alloc_semaphore(name + f"_{self.nsem}")

    def dma_sem(self, d, cls):
        if not hasattr(self, "free_sems"):
            self.free_sems = {"sw": [], "hw": []}
        free = self.free_sems[cls]
        if free:
            d.sem, d.semv = free.pop()
        else:
            d.sem, d.semv = self.new_sem("d" + cls), 0
        d.semcls = cls

    def _eng_sem(self, e):
        idx = self.cnt[e] // SEM_L
        while len(self.sems[e]) <= idx:
            self.sems[e].append(self.new_sem(f"s_{e}"))
        return self.sems[e][idx]

    def _wait(self, e, tok):
        if tok is None:
            return
        sem, val, src = tok
        if src == e and e not in self.same:
            return
        key = (e, id(sem))
        if self.waited.get(key, 0) >= val:
            return
        self.waited[key] = val
        self.h[e].wait_ge(sem, val)

    def _deps(self, e, reads, writes):
        for t in reads:
            self._wait(e, t.w)
        for t in writes:
            self._wait(e, t.w)
            for tok in t.r.values():
                self._wait(e, tok)

    def op(self, e, fn, reads=(), writes=()):
        self._deps(e, reads, writes)
        sem = self._eng_sem(e)
        val = self.cnt[e] % SEM_L + 1
        fn(self.h[e]).then_inc(sem, 1)
        self.cnt[e] += 1
        self.ninst += 1
        tok = (sem, val, e)
        for t in reads:
            t.r[id(sem)] = tok
        for t in writes:
            t.w = tok
            t.r = {}
        return tok

    def dma(self, q, out, in_, reads=(), writes=(), **kw):
        self._deps(q, reads, writes)
        d = writes[0]
        cls = "sw" if q == "pool" else "hw"
        if d.sem is None or d.semv >= DMA_L or d.semcls != cls:
            self.dma_sem(d, cls)
        d.semv += 16
        self.h[q].dma_start(out=out, in_=in_, **kw).then_inc(d.sem, 16)
        self.ninst += 1
        tok = (d.sem, d.semv, "dma")
        for t in reads:
            t.r[id(d.sem)] = tok
        d.w = tok
        d.r = {}
        return tok

    def barrier(self):
        e0 = "dve"
        for t in self.all_T:
            self._wait(e0, t.w)
            for tok in t.r.values():
                self._wait(e0, tok)
            t.r = {}
        for e in self.h:
            if self.cnt[e] > 0 and e != e0:
                idx = (self.cnt[e] - 1) // SEM_L
                self._wait(e0, (self.sems[e][idx], (self.cnt[e] - 1) % SEM_L + 1, e))
        if not hasattr(self, "Tbar"):
            self.Tbar = T("bar")
        tok = self.op(e0, lambda h: h.memset(self.bar_tile[:], 0.0), writes=[self.Tbar])
        for e in self.h:
            if e != e0:
                self._wait(e, tok)
        if not hasattr(self, "free_sems"):
            self.free_sems = {"sw": [], "hw": []}
        for t in self.all_T:
            t.w = None
            t.r = {}
            if t.sem is not None:
                if t.semv < DMA_L - 4096:
                    self.free_sems[t.semcls].append((t.sem, t.semv))
                t.sem = None
                t.semv = 0


class K:
    def __init__(self, debug=False, phases=None, nlayers=L):
        self.debug = debug
        self.phases = phases
        self.nlayers = nlayers
        nc = bass.Bass("TRN2", target_bir_lowering=False)
        self.nc = nc
        self.S = Sched(nc)
        self.inp = {}
        self.scr = {}
        self.S.bar_tile = nc.alloc_sbuf_tensor("bar_tile", [128, 8], F32)
        self.st_i = 0
        self.ld_i = 0

    def din(self, name, shape, dt=F32):
        self.inp[name] = self.nc.dram_tensor(name, list(shape), dt, kind="ExternalInput").ap()
        return self.inp[name]

    def dscr(self, name, shape, dt=F32, out=False):
        kind = "ExternalOutput" if (self.debug or out) else "Internal"
        self.scr[name] = self.nc.dram_tensor(name, list(shape), dt, kind=kind).ap()
        return self.scr[name]

    def sb(self, es, name, shape, dt=F32):
        self.uid = getattr(self, "uid", 0) + 1
        return es.enter_context(self.nc.sbuf_tensor(f"sb{self.uid}_{name}", list(shape), dt))

    def ps(self, es, name, shape, dt=F32):
        self.uid = getattr(self, "uid", 0) + 1
        return es.enter_context(self.nc.psum_tensor(f"ps{self.uid}_{name}", list(shape), dt))

    def store(self, out, in_, reads, q=None):
        import os
        q = q or os.environ.get("STQ", "pool")
        if not hasattr(self, "_st"):
            self._st = [self.S.T(f"st{i}") for i in range(8)]
        i = self.st_i % 8
        self.st_i += 1
        self.S.dma(q, out, in_, reads=reads, writes=[self._st[i]])

    def load(self, out, in_, Tt, q="sp"):
        self.S.dma(q, out, in_, writes=[Tt])


def _rope_tables():
    half = 32
    freqs = (np.float32(10000.0) ** (-np.arange(half, dtype=np.float32) / np.float32(half))).astype(np.float32)
    pos = np.arange(SEQ, dtype=np.float32)
    ang = (pos[:, None] * freqs[None, :]).astype(np.float32)
    cos = np.cos(ang).astype(np.float32).T
    sin = np.sin(ang).astype(np.float32).T
    cosT = np.tile(cos, (4, 1))
    sinT = np.tile(sin, (4, 1))
    posc = (np.arange(256, dtype=np.float32) * 16 + 31).astype(np.float32)
    angc = (posc[:, None] * freqs[None, :]).astype(np.float32)
    cosC = np.tile(np.cos(angc).astype(np.float32).T, (2, 1))
    sinC = np.tile(np.sin(angc).astype(np.float32).T, (2, 1))
    return cosT, sinT, cosC, sinC


def _consts():
    c = {}
    cosT, sinT, cosC, sinC = _rope_tables()
    c["cosT"], c["sinT"], c["cosC"], c["sinC"] = cosT, sinT, cosC, sinC
    c["identf"] = np.eye(128, dtype=np.float32)
    c["identb"] = np.eye(128, dtype=np.float32).astype(ml_dtypes.bfloat16)
    key = np.arange(SEQ)
    c["Erows"] = (key[None, :] // 64 == np.arange(64)[:, None]).astype(np.float32).astype(ml_dtypes.bfloat16)
    i = np.arange(256)
    c["cmask"] = np.where((i[:, None] * 16 + 31) <= key[None, :], 0.0, NEG).astype(np.float32).astype(ml_dtypes.bfloat16)
    v = np.arange(128)[:, None]
    u = np.arange(512)[None, :]
    wm = np.zeros((8, 128, 512), np.float32)
    for ri, r in enumerate(range(-4, 4)):
        kk = 128 * r + v
        wm[ri] = np.where((kk <= u) & (kk > u - 512), 0.0, NEG)
    c["wmask"] = wm.astype(ml_dtypes.bfloat16)
    cm = np.zeros((4, 128, 512), np.float32)
    for r in range(4):
        cm[r] = np.where(128 * r + v <= u, 0.0, NEG)
    c["dmask"] = cm.astype(ml_dtypes.bfloat16)
    t = np.arange(SEQ)[:, None]
    j = np.arange(64)[None, :]
    cur = t // 64
    forced = (j == 0) | (j == cur) | (j == cur - 1)
    causal = j * 64 <= t
    add = np.where(forced, 1e9, np.where(causal, 0.0, -1e9)).astype(np.float32)
    c["addtab"] = np.ascontiguousarray(add.reshape(NT, 128, 64).transpose(1, 0, 2))
    ii = np.arange(256)[:, None]
    ov = ((ii * 16 < (j + 1) * 64) & (ii * 16 + 32 > j * 64) & (ii < 255)).astype(np.float32)
    c["overlap"] = ov.astype(ml_dtypes.bfloat16)
    return c


def declare_io(self):
    k = self
    k.din("x", [NB, SEQ, D])
    k.din("w_in_p", [L, D, 2072])
    k.din("norm_mix_t", [L, 128, 8])
    k.din("norm_final", [D])
    for n, sh, dt in [("cosT", [128, SEQ], F32), ("sinT", [128, SEQ], F32), ("cosC", [64, 256], F32),
                      ("sinC", [64, 256], F32), ("identf", [128, 128], F32), ("identb", [128, 128], BF16),
                      ("Erows", [64, SEQ], BF16), ("cmask", [256, SEQ], BF16), ("wmask", [8, 128, 512], BF16),
                      ("dmask", [4, 128, 512], BF16), ("addtab", [128, NT, 64], F32), ("overlap", [256, 64], BF16)]:
        k.din(n, sh, dt)
    k.dscr("qT", [NB, 512, SEQ], BF16)
    k.dscr("ksT", [NB, 128, SEQ], BF16)
    k.dscr("kwT", [NB, 128, SEQ], BF16)
    k.dscr("kcrT", [NB, 128, SEQ], BF16)
    k.dscr("vcrT", [NB, 128, SEQ], BF16)
    k.dscr("ucT", [NB, 256, SEQ], BF16)
    k.dscr("usT", [NB, 256, SEQ], BF16)
    k.dscr("vsw", [NB, SEQ, 256], BF16)
    k.dscr("gat", [NB, SEQ, 32], F32)
    k.dscr("ycat", [NB, SEQ, D], F32)
    k.dscr("xres", [NB, SEQ, D], F32)
    k.dscr("h2T", [NB, D, SEQ], BF16)
    k.dscr("kcT", [NB, 2, 64, 256], BF16)
    k.dscr("vc", [NB, 2, 256, 64], BF16)
    for n in ["cmp_k_w1", "cmp_v_w1"]:
        k.din(n, [L, 2048, 256])
    for n in ["cmp_k_w2", "cmp_v_w2"]:
        k.din(n, [L, 256, 64])
    k.din("pe_t", [L, 2, 128, 16])
    k.din("wdw_t", [L, 256, 31])
    for n in ["ssm_are_t", "ssm_aim_t", "ssm_ldt_t"]:
        k.din(n, [L, 128, 8])
    for n in ["Zb_re", "Zb_im", "Cp_re", "Cp_im"]:
        k.din(n, [L, 128, 8, 128])
    k.din("ssm_d_t", [L, 128, 2])
    k.din("ssm_w_glu", [L, 256, 512])
    k.din("iota512", [512])
    k.din("gcat_t", [L, 128, 8])
    k.din("norm_ffn_t", [L, 128, 8])
    k.din("w_out", [L, D, D])
    k.din("w_gate", [L, D, DFF])
    k.din("w_up", [L, D, DFF])
    k.din("w_down", [L, DFF, D])
    for n in ["conv_b_dw", "conv_b_pw", "conv_ln_g", "conv_ln_b"]:
        k.din(n, [L, 256])
    k.din("conv_w_pw", [L, 256, 256])
    k.dscr("out", [NB, SEQ, D], F32, out=True)


def load_consts(self, es):
    k, S = self, self.S
    k.identf = k.sb(es, "identf", [128, 128], F32)
    k.identb = k.sb(es, "identb", [128, 128], BF16)
    k.Tid = S.T("ident")
    S.dma("sp", k.identf[:], k.inp["identf"], writes=[k.Tid])
    k.Tidb = S.T("identb")
    S.dma("sp", k.identb[:], k.inp["identb"], writes=[k.Tidb])
    k.ones_b = k.sb(es, "ones_b", [128, 128], BF16)
    k.Tones = S.T("ones")
    S.op("dve", lambda e: e.memset(k.ones_b[:], 1.0), writes=[k.Tones])


def phase1(self, l):
    k, S, nc = self, self.S, self.nc
    with contextlib.ExitStack() as es:
        NCOL = 2840
        W = k.sb(es, "p1_W", [128, 8, NCOL], BF16)
        TW = [S.T(f"p1_W{i}") for i in range(8)]
        stage = [k.sb(es, f"p1_stage{i}", [128, 2072], F32) for i in range(2)]
        Tstage = [S.T(f"p1_stage{i}") for i in range(2)]
        gain = k.sb(es, "p1_gain", [128, 8], F32)
        gq = k.sb(es, "p1_gq", [128, 8], F32)
        ngq = k.sb(es, "p1_ngq", [128, 8], F32)
        ngain = k.sb(es, "p1_ngain", [128, 8], F32)
        Tgain = S.T("p1_gain")
        S.dma("sp", gain[:], k.inp["norm_mix_t"][l], writes=[Tgain])
        S.op("dve", lambda e: e.tensor_scalar(out=ngain[:], in0=gain[:], scalar1=-1.0, scalar2=None, op0=ALU.mult), reads=[Tgain], writes=[Tgain])
        S.op("dve", lambda e: e.tensor_scalar(out=gq[:], in0=gain[:], scalar1=0.125, scalar2=None, op0=ALU.mult), reads=[Tgain], writes=[Tgain])
        S.op("dve", lambda e: e.tensor_scalar(out=ngq[:], in0=gain[:], scalar1=-0.125, scalar2=None, op0=ALU.mult), reads=[Tgain], writes=[Tgain])
        wsrc = k.inp["w_in_p"][l].rearrange("(kc p) n -> p kc n", p=128)
        engs = ["dve", "pool"]
        ei = 0

        def ts(out, in_, sc):
            nonlocal ei
            e_ = engs[kc % 2]
            S.op(e_, lambda e: e.tensor_scalar(out=out, in0=in_, scalar1=sc, scalar2=None, op0=ALU.mult),
                 reads=[Tstage[kc % 2], Tgain], writes=[TW[kc]])

        for kc in range(8):
            st = stage[kc % 2]
            S.dma("sp", st[:], wsrc[:, kc, :], writes=[Tstage[kc % 2]])
            g1, ng1, q1, nq1 = gain[:, kc:kc + 1], ngain[:, kc:kc + 1], gq[:, kc:kc + 1], ngq[:, kc:kc + 1]
            ts(W[:, kc, 0:512], st[:, 0:512], q1)
            sv = st[:, 0:512].rearrange("p (h t f) -> p h t f", h=8, t=2)
            dv = W[:, kc, 512:1024].rearrange("p (h t f) -> p h t f", h=8, t=2)
            ts(dv[:, :, 0, :], sv[:, :, 1, :], nq1)
            ts(dv[:, :, 1, :], sv[:, :, 0, :], q1)
            for (s0, d0) in [(512, 1024), (640, 1280)]:
                ts(W[:, kc, d0:d0 + 128], st[:, s0:s0 + 128], g1)
                sv = st[:, s0:s0 + 128].rearrange("p (h t f) -> p h t f", h=2, t=2)
                dv = W[:, kc, d0 + 128:d0 + 256].rearrange("p (h t f) -> p h t f", h=2, t=2)
                ts(dv[:, :, 0, :], sv[:, :, 1, :], ng1)
                ts(dv[:, :, 1, :], sv[:, :, 0, :], g1)
            ts(W[:, kc, 1536:2840], st[:, 768:2072], g1)

        import os
        STOP = os.environ.get("P1STOP", "")
        if STOP == "w":
            S.barrier()
            return
        xg = [k.sb(es, f"p1_xg{i}", [128, 4, D], F32) for i in range(2)]
        Txg = [S.T(f"p1_xg{i}") for i in range(2)]
        junk = k.sb(es, "p1_junk", [128, D], BF16)
        Tjunk = S.T("p1_junk")
        ss = k.sb(es, "p1_ss", [128, 4], F32)
        rstd = k.sb(es, "p1_rstd", [128, 4], F32)
        Tss = S.T("p1_ss")
        hb = k.sb(es, "p1_hb", [128, 4, D], BF16)
        Thb = [S.T(f"p1_hb{i}") for i in range(4)]
        hT = [k.sb(es, f"p1_hT{i}", [128, 8, 512], BF16) for i in range(2)]
        ThT = [[S.T(f"p1_hT{i}_{c}") for c in range(8)] for i in range(2)]
        cs = [k.sb(es, f"p1_cos{i}", [128, 512], F32) for i in range(2)]
        sn = [k.sb(es, f"p1_sin{i}", [128, 512], F32) for i in range(2)]
        Tcs = [S.T(f"p1_cs{i}") for i in range(2)]
        pT = [k.ps(es, f"p1_pT{i}", [128, 512], BF16) for i in range(2)]
        TpT = [S.T(f"p1_pT{i}") for i in range(2)]
        pm = [k.ps(es, f"p1_pm{i}", [128, 512], F32) for i in range(4)]
        Tpm = [S.T(f"p1_pm{i}") for i in range(4)]
        pk = [k.ps(es, f"p1_pk{i}", [128, 512], F32) for i in range(2)]
        Tpk = [S.T(f"p1_pk{i}") for i in range(2)]
        NE = 6
        t1 = [k.sb(es, f"p1_t1_{i}", [128, 512], F32) for i in range(NE)]
        t2 = [k.sb(es, f"p1_t2_{i}", [128, 512], F32) for i in range(NE)]
        ob = [k.sb(es, f"p1_ob{i}", [128, 512], BF16) for i in range(NE)]
        Tt1 = [S.T(f"p1_t1_{i}") for i in range(NE)]
        Tt2 = [S.T(f"p1_t2_{i}") for i in range(NE)]
        Tob = [S.T(f"p1_ob{i}") for i in range(NE)]
        Ttg_pre = [S.T(f"p1_tg{i}") for i in range(2)]
        tk = [k.sb(es, f"p1_tk{i}", [128, 256], BF16) for i in range(2)]
        tg = [k.sb(es, f"p1_tg{i}", [128, 32], F32) for i in range(2)]
        for i in range(2):
            S.op("dve", lambda e: e.memset(tg[i][:], 0.0), writes=[Ttg_pre[i]])
        Ttk = [S.T(f"p1_tk{i}") for i in range(2)]
        Ttg = Ttg_pre
        xin = k.inp["x"] if l == 0 else k.scr["xres"]
        gi = 0
        ecnt = 0
        pmi = 0
        for b in range(NB):
            for g in range(NG):
                u = gi % 2
                gi += 1
                tsl = slice(g * 512, (g + 1) * 512)
                S.dma("sp", xg[u][:], xin[b, tsl, :].rearrange("(j p) d -> p j d", p=128), writes=[Txg[u]])
                S.dma("sp", cs[u][:], k.inp["cosT"][:, tsl], writes=[Tcs[u]])
                S.dma("sp", sn[u][:], k.inp["sinT"][:, tsl], writes=[Tcs[u]])
                for j in range(4):
                    S.op("act", lambda e: e.activation(out=junk[:], in_=xg[u][:, j, :], func=AF.Square, accum_out=ss[:, j:j + 1]),
                         reads=[Txg[u]], writes=[Tjunk, Tss])
                S.op("dve", lambda e: e.tensor_scalar(out=rstd[:], in0=ss[:], scalar1=1.0 / D, scalar2=EPS, op0=ALU.mult, op1=ALU.add), reads=[Tss], writes=[Tss])
                S.op("act", lambda e: e.activation(out=rstd[:], in_=rstd[:], func=AF.Sqrt), reads=[Tss], writes=[Tss])
                S.op("dve", lambda e: e.reciprocal(out=rstd[:], in_=rstd[:]), reads=[Tss], writes=[Tss])
                for j in range(4):
                    S.op("pool" if j % 2 else "dve", lambda e: e.tensor_scalar(out=hb[:, j, :], in0=xg[u][:, j, :], scalar1=rstd[:, j:j + 1], scalar2=None, op0=ALU.mult),
                         reads=[Txg[u], Tss], writes=[Thb[j]])
                if STOP == "n":
                    continue
                for kc in range(8):
                    pu = kc % 2
                    for j in range(4):
                        S.op("pe", lambda e: e.transpose(out=pT[pu][:, j * 128:(j + 1) * 128], in_=hb[:, j, kc * 128:(kc + 1) * 128], identity=k.identb[:]),
                             reads=[Thb[j], k.Tidb], writes=[TpT[pu]])
                    if kc % 2:
                        S.op("act", lambda e: e.activation(out=hT[u][:, kc, :], in_=pT[pu][:], func=AF.Copy), reads=[TpT[pu]], writes=[ThT[u][kc]])
                    else:
                        S.op("dve", lambda e: e.tensor_copy(out=hT[u][:, kc, :], in_=pT[pu][:]), reads=[TpT[pu]], writes=[ThT[u][kc]])

                if STOP == "t":
                    continue

                def mm(c):
                    nonlocal pmi
                    i = pmi % 4
                    pmi += 1
                    for kc in range(8):
                        S.op("pe", lambda e: e.matmul(pm[i][:], lhsT=W[:, kc, c * 128:(c + 1) * 128], rhs=hT[u][:, kc, :], start=(kc == 0), stop=(kc == 7)),
                             reads=[TW[kc], ThT[u][kc]], writes=[Tpm[i]])
                    return i

                def roped(cx, cr, dst):
                    nonlocal ecnt
                    ix = mm(cx)
                    ir = mm(cr)
                    n = ecnt % NE
                    ecnt += 1
                    S.op("dve", lambda e: e.tensor_tensor(out=t1[n][:], in0=pm[ix][:], in1=cs[u][:], op=ALU.mult), reads=[Tpm[ix], Tcs[u]], writes=[Tt1[n]])
                    S.op("dve", lambda e: e.tensor_tensor(out=t2[n][:], in0=pm[ir][:], in1=sn[u][:], op=ALU.mult), reads=[Tpm[ir], Tcs[u]], writes=[Tt2[n]])
                    S.op("pool", lambda e: e.tensor_tensor(out=ob[n][:], in0=t1[n][:], in1=t2[n][:], op=ALU.add), reads=[Tt1[n], Tt2[n]], writes=[Tob[n]])
                    k.store(dst, ob[n][:], [Tob[n]])

                def plain(c, dst):
                    nonlocal ecnt
                    i = mm(c)
                    n = ecnt % NE
                    ecnt += 1
                    S.op("act", lambda e: e.activation(out=ob[n][:], in_=pm[i][:], func=AF.Copy), reads=[Tpm[i]], writes=[Tob[n]])
                    k.store(dst, ob[n][:], [Tob[n]])

                for c in range(4):
                    roped(c, c + 4, k.scr["qT"][b, c * 128:(c + 1) * 128, tsl])
                roped(8, 9, k.scr["ksT"][b, :, tsl])
                roped(10, 11, k.scr["kwT"][b, :, tsl])
                plain(12, k.scr["kcrT"][b, :, tsl])
                plain(13, k.scr["vcrT"][b, :, tsl])
                for c in range(2):
                    ia = mm(14 + c)
                    ig = mm(16 + c)
                    n = ecnt % NE
                    ecnt += 1
                    S.op("act", lambda e: e.activation(out=t1[n][:], in_=pm[ig][:], func=AF.Sigmoid), reads=[Tpm[ig]], writes=[Tt1[n]])
                    S.op("dve", lambda e: e.tensor_tensor(out=ob[n][:], in0=pm[ia][:], in1=t1[n][:], op=ALU.mult), reads=[Tpm[ia], Tt1[n]], writes=[Tob[n]])
                    k.store(k.scr["ucT"][b, c * 128:(c + 1) * 128, tsl], ob[n][:], [Tob[n]])
                for c in range(2):
                    plain(18 + c, k.scr["usT"][b, c * 128:(c + 1) * 128, tsl])
                if STOP == "f":
                    continue
                for j in range(4):
                    pu = j % 2
                    for kc in range(8):
                        S.op("pe", lambda e: e.matmul(pk[pu][:, 0:280], lhsT=hT[u][:, kc, j * 128:(j + 1) * 128], rhs=W[:, kc, 2560:2840], start=(kc == 0), stop=(kc == 7)),
                             reads=[TW[kc], ThT[u][kc]], writes=[Tpk[pu]])
                    S.op("dve", lambda e: e.tensor_copy(out=tk[pu][:], in_=pk[pu][:, 0:256]), reads=[Tpk[pu]], writes=[Ttk[pu]])
                    r0 = g * 512 + j * 128
                    if STOP != "g":
                        S.op("act", lambda e: e.activation(out=tg[pu][:, 0:24], in_=pk[pu][:, 256:280], func=AF.Sigmoid), reads=[Tpk[pu]], writes=[Ttg[pu]])
                        if STOP != "h":
                            k.store(k.scr["gat"][b, r0:r0 + 128, :], tg[pu][:], [Ttg[pu]])
                    k.store(k.scr["vsw"][b, r0:r0 + 128, :], tk[pu][:], [Ttk[pu]])
        S.barrier()


K.declare_io = declare_io
K.load_consts = load_consts
K.phase1 = phase1


def build(debug=False, phases=None, nlayers=L):
    k = K(debug=debug, phases=phases, nlayers=nlayers)
    k.declare_io()
    S = k.S
    with contextlib.ExitStack() as es:
        k.load_consts(es)
        for l in range(nlayers):
            for ph in PHASES:
                if phases is not None and (l, ph) not in phases and ph not in phases:
                    continue
                getattr(k, ph)(l)
        if phases is None or "final" in phases:
            k.final()
        S.barrier()
    return k


PHASES = ["phase1"]


def host_inputs(inputs):
    f = lambda a: np.ascontiguousarray(np.asarray(a, dtype=np.float32))
    w_in = f(inputs["w_in"])
    cols = np.concatenate([np.arange(0, 512), np.arange(768, 896), np.arange(1024, 1152), np.arange(512, 640),
                           np.arange(640, 768), np.arange(1560, 2072), np.arange(1304, 1560), np.arange(896, 1024),
                           np.arange(1152, 1280), np.arange(1280, 1304)])
    shared = {}
    shared["w_in_p"] = np.ascontiguousarray(w_in[:, :, cols])
    shared["norm_mix_t"] = np.ascontiguousarray(f(inputs["norm_mix"]).reshape(L, 8, 128).transpose(0, 2, 1))
    shared["norm_final"] = f(inputs["norm_final"])
    tl = lambda a: np.ascontiguousarray(a.reshape(L, 8, 128).transpose(0, 2, 1))
    shared["ssm_are_t"] = tl(f(inputs["ssm_a_re"]).reshape(L, 16 * 64))
    shared["ssm_aim_t"] = tl(f(inputs["ssm_a_im"]).reshape(L, 16 * 64))
    shared["ssm_ldt_t"] = tl(np.repeat(f(inputs["ssm_log_dt"]), 64, axis=1))
    def padB(bm):
        o = np.zeros((L, 128, 8, 128), np.float32)
        for g_ in range(16):
            m_, h_ = g_ // 2, g_ % 2
            col = (g_ % 8) * 16
            o[:, h_ * 64:(h_ + 1) * 64, m_, col:col + 16] = bm[:, g_]
        return o
    shared["Zb_re"] = padB(f(inputs["ssm_b_re"]))
    shared["Zb_im"] = padB(f(inputs["ssm_b_im"]))
    shared["Cp_re"] = padB(f(inputs["ssm_c_re"]).transpose(0, 1, 3, 2))
    shared["Cp_im"] = padB(f(inputs["ssm_c_im"]).transpose(0, 1, 3, 2))
    shared["ssm_d_t"] = np.ascontiguousarray(f(inputs["ssm_d"]).reshape(L, 2, 128).transpose(0, 2, 1))
    shared["ssm_w_glu"] = f(inputs["ssm_w_glu"])
    shared["iota512"] = np.arange(512, dtype=np.float32)
    gcat = np.concatenate([f(inputs["norm_out_attn"]), f(inputs["norm_out_ssm"]), f(inputs["norm_out_conv"])], axis=1)
    shared["gcat_t"] = np.ascontiguousarray(gcat.reshape(L, 8, 128).transpose(0, 2, 1))
    shared["norm_ffn_t"] = np.ascontiguousarray(f(inputs["norm_ffn"]).reshape(L, 8, 128).transpose(0, 2, 1))
    for n in ["w_out", "w_gate", "w_up", "w_down"]:
        shared[n] = f(inputs[n])
    shared["wdw_t"] = np.ascontiguousarray(f(inputs["conv_w_dw"]).transpose(0, 2, 1))
    for n in ["conv_b_dw", "conv_b_pw", "conv_ln_g", "conv_ln_b", "conv_w_pw"]:
        shared[n] = f(inputs[n])
    for n in ["cmp_k_w1", "cmp_v_w1", "cmp_k_w2", "cmp_v_w2"]:
        shared[n] = f(inputs[n])
    pe = np.stack([f(inputs["cmp_pe_k"]), f(inputs["cmp_pe_v"])], 1)
    shared["pe_t"] = np.ascontiguousarray(pe.reshape(L, 2, 2, 16, 64).transpose(0, 1, 2, 4, 3).reshape(L, 2, 128, 16))
    shared.update(_consts())
    return shared


def kernel(**inputs):
    k = build()
    shared = host_inputs(inputs)
    x = np.ascontiguousarray(np.asarray(inputs["x"], dtype=np.float32))
    names = set(k.inp.keys())
    in_maps = []
    for c in range(8):
        m = {n: shared[n] for n in names if n != "x"}
        m["x"] = np.ascontiguousarray(x[c * NB:(c + 1) * NB])
        in_maps.append(m)
    res = run_bass_kernel_spmd(k.nc, in_maps, core_ids=list(range(8)))
    return np.concatenate([np.asarray(r["out"]) for r in res.results], axis=0).astype(np.float32)


def final(self):
    k, S = self, self.S
    src = k.scr["xres"] if self.nlayers > 0 else k.inp["x"]
    with contextlib.ExitStack() as es:
        gf = k.sb(es, "fin_g", [128, D], F32)
        Tgf = S.T("fin_g")
        S.dma("sp", gf[:], k.inp["norm_final"].partition_broadcast(128), writes=[Tgf])
        xg = [k.sb(es, f"fin_x{i}", [128, 4, D], F32) for i in range(2)]
        Txg = [S.T(f"fin_x{i}") for i in range(2)]
        og = [k.sb(es, f"fin_o{i}", [128, 4, D], F32) for i in range(2)]
        Tog = [S.T(f"fin_o{i}") for i in range(2)]
        junk = k.sb(es, "fin_junk", [128, D], BF16)
        Tjunk = S.T("fin_junk")
        ss = [k.sb(es, f"fin_ss{i}", [128, 4], F32) for i in range(2)]
        Tss = [S.T(f"fin_ss{i}") for i in range(2)]
        gi = 0
        for b in range(NB):
            for g in range(NG):
                u = gi % 2
                gi += 1
                tsl = slice(g * 512, (g + 1) * 512)
                S.dma("sp", xg[u][:], src[b, tsl, :].rearrange("(j p) d -> p j d", p=128), writes=[Txg[u]])
                for j in range(4):
                    S.op("act", lambda e: e.activation(out=junk[:], in_=xg[u][:, j, :], func=AF.Square, accum_out=ss[u][:, j:j + 1]),
                         reads=[Txg[u]], writes=[Tjunk, Tss[u]])
                S.op("dve", lambda e: e.tensor_scalar(out=ss[u][:], in0=ss[u][:], scalar1=1.0 / D, scalar2=EPS, op0=ALU.mult, op1=ALU.add), reads=[Tss[u]], writes=[Tss[u]])
                S.op("act", lambda e: e.activation(out=ss[u][:], in_=ss[u][:], func=AF.Sqrt), reads=[Tss[u]], writes=[Tss[u]])
                S.op("dve", lambda e: e.reciprocal(out=ss[u][:], in_=ss[u][:]), reads=[Tss[u]], writes=[Tss[u]])
                for j in range(4):
                    S.op("dve", lambda e: e.scalar_tensor_tensor(out=og[u][:, j, :], in0=xg[u][:, j, :], scalar=ss[u][:, j:j + 1], in1=gf[:], op0=ALU.mult, op1=ALU.mult),
                         reads=[Txg[u], Tss[u], Tgf], writes=[Tog[u]])
                k.store(k.scr["out"][b, tsl, :].rearrange("(j p) d -> p j d", p=128), og[u][:], [Tog[u]])
        S.barrier()


K.final = final


def phase2(self, l):
    k, S = self, self.S
    with contextlib.ExitStack() as es:
        stage = k.sb(es, "p2_stage", [128, 16, 256], F32)
        Tstage = S.T("p2_stage")
        w1b = [k.sb(es, f"p2_w1b{i}", [128, 16, 256], BF16) for i in range(2)]
        Tw1 = [S.T(f"p2_w1b{i}") for i in range(2)]
        pes = k.sb(es, "p2_pes", [128, 2, 16], F32)
        peb = k.sb(es, "p2_peb", [128, 2, 16], BF16)
        Tpe = S.T("p2_pe")
        w2s = k.sb(es, "p2_w2s", [128, 2, 2, 64], F32)
        w2b = k.sb(es, "p2_w2b", [128, 2, 2, 64], BF16)
        w2r = k.sb(es, "p2_w2r", [128, 2, 64], BF16)
        Tw2 = S.T("p2_w2")
        bias = k.sb(es, "p2_bias", [128, 2, 2], F32)
        Tbias = S.T("p2_bias")
        csC = k.sb(es, "p2_cosC", [64, 256], F32)
        snC = k.sb(es, "p2_sinC", [64, 256], F32)
        TcsC = S.T("p2_csC")
        S.dma("sp", csC[:], k.inp["cosC"], writes=[TcsC])
        S.dma("sp", snC[:], k.inp["sinC"], writes=[TcsC])
        pb = k.ps(es, "p2_pb", [128, 512], F32)
        Tpb = S.T("p2_pb")
        S.dma("sp", pes[:], k.inp["pe_t"][l].rearrange("a p l -> p a l"), writes=[Tpe])
        S.op("dve", lambda e: e.tensor_copy(out=peb[:], in_=pes[:]), reads=[Tpe], writes=[Tpe])
        for a, nm in enumerate(["cmp_k_w1", "cmp_v_w1"]):
            src = k.inp[nm][l].rearrange("(hl d) h -> d hl h", d=64)
            S.dma("sp", stage[0:64, :, :], src[:, 0:16, :], writes=[Tstage])
            S.dma("sp", stage[64:128, :, :], src[:, 16:32, :], reads=[], writes=[Tstage])
            S.op("dve" if a == 0 else "pool", lambda e: e.tensor_copy(out=w1b[a][:], in_=stage[:]), reads=[Tstage], writes=[Tw1[a]])
            for hc in range(2):
                for ll in range(16):
                    S.op("pe", lambda e: e.matmul(pb[:, 0:1], lhsT=w1b[a][:, ll, hc * 128:(hc + 1) * 128], rhs=peb[:, a, ll:ll + 1], start=(ll == 0), stop=(ll == 15)),
                         reads=[Tw1[a], Tpe], writes=[Tpb])
                S.op("dve", lambda e: e.tensor_copy(out=bias[:, a, hc:hc + 1], in_=pb[:, 0:1]), reads=[Tpb], writes=[Tbias])
        for a, nm in enumerate(["cmp_k_w2", "cmp_v_w2"]):
            S.dma("sp", w2s[:, a, :, :], k.inp[nm][l].rearrange("(hc p) d -> p hc d", p=128), writes=[Tw2])
        S.op("dve", lambda e: e.tensor_copy(out=w2b[:], in_=w2s[:]), reads=[Tw2], writes=[Tw2])
        for hc in range(2):
            S.op("dve", lambda e: e.tensor_scalar(out=w2r[:, hc, 0:32], in0=w2s[:, 0, hc, 32:64], scalar1=-1.0, scalar2=None, op0=ALU.mult), reads=[Tw2], writes=[Tw2])
            S.op("dve", lambda e: e.tensor_copy(out=w2r[:, hc, 32:64], in_=w2s[:, 0, hc, 0:32]), reads=[Tw2], writes=[Tw2])

        X2 = [k.sb(es, f"p2_X2_{i}", [128, SEQ], BF16) for i in range(2)]
        TX2 = [S.T(f"p2_X2_{i}") for i in range(2)]
        for i in range(2):
            S.op("pool", lambda e: e.memset(X2[i][64:128, SEQ - 16:SEQ], 0.0), writes=[TX2[i]])
        hid = [k.sb(es, f"p2_hid{i}", [128, 2, 256], BF16) for i in range(2)]
        Thid = [S.T(f"p2_hid{i}") for i in range(2)]
        for i in range(2):
            S.op("pool", lambda e: e.memset(hid[i][:], 0.0), writes=[Thid[i]])
        ph = [k.ps(es, f"p2_ph{i}", [128, 512], F32) for i in range(2)]
        Tph = [S.T(f"p2_ph{i}") for i in range(2)]
        pk = k.ps(es, "p2_pk", [128, 512], F32)
        Tpk = S.T("p2_pk")
        pv = k.ps(es, "p2_pv", [128, 512], F32)
        Tpv = S.T("p2_pv")
        t1 = k.sb(es, "p2_t1", [64, 256], F32)
        t2 = k.sb(es, "p2_t2", [64, 256], F32)
        kco = k.sb(es, "p2_kco", [64, 256], BF16)
        Tko = S.T("p2_kco")
        S.op("dve", lambda e: e.memset(kco[:], 0.0), writes=[Tko])
        vco = [k.sb(es, f"p2_vco{i}", [128, 2, 64], BF16) for i in range(2)]
        Tvo = [S.T(f"p2_vco{i}") for i in range(2)]
        it = 0
        phi = 0
        for b in range(NB):
            for kv in range(2):
                for a in range(2):
                    u = it % 2
                    it += 1
                    src = k.scr["kcrT" if a == 0 else "vcrT"][b, kv * 64:(kv + 1) * 64, :]
                    S.dma("sp", X2[u][0:64, :], src, writes=[TX2[u]])
                    S.dma("sp", X2[u][64:128, 0:SEQ - 16], src[:, 16:SEQ], reads=[], writes=[TX2[u]])
                    xv = X2[u][:, :].rearrange("p (i s) -> p i s", s=16)
                    for hc in range(2):
                        pi = phi % 2
                        phi += 1
                        for ll in range(16):
                            S.op("pe", lambda e: e.matmul(ph[pi][:, 0:255], lhsT=w1b[a][:, ll, hc * 128:(hc + 1) * 128], rhs=xv[:, 0:255, ll], start=(ll == 0), stop=(ll == 15)),
                                 reads=[Tw1[a], TX2[u]], writes=[Tph[pi]])
                        S.op("act", lambda e: e.activation(out=hid[u][:, hc, 0:255], in_=ph[pi][:, 0:255], func=AF.Gelu_apprx_tanh, bias=bias[:, a, hc:hc + 1]),
                             reads=[Tph[pi], Tbias], writes=[Thid[u]])
                    if a == 0:
                        for r_, wsel in enumerate([w2b[:, 0, :, :], w2r[:, :, :]]):
                            for hc in range(2):
                                S.op("pe", lambda e: e.matmul(pk[0:64, r_ * 256:r_ * 256 + 256], lhsT=wsel[:, hc, :], rhs=hid[u][:, hc, :], start=(hc == 0), stop=(hc == 1)),
                                     reads=[Tw2, Thid[u]], writes=[Tpk])
                        S.op("dve", lambda e: e.tensor_tensor(out=t1[:], in0=pk[0:64, 0:256], in1=csC[:], op=ALU.mult), reads=[Tpk, TcsC], writes=[Tko])
                        S.op("dve", lambda e: e.tensor_tensor(out=t2[:], in0=pk[0:64, 256:512], in1=snC[:], op=ALU.mult), reads=[Tpk, TcsC], writes=[Tko])
                        S.op("dve", lambda e: e.tensor_tensor(out=kco[:, 0:255], in0=t1[:, 0:255], in1=t2[:, 0:255], op=ALU.add), reads=[Tko], writes=[Tko])
                        k.store(k.scr["kcT"][b, kv], kco[:], [Tko])
                    else:
                        for c in range(2):
                            for hc in range(2):
                                S.op("pe", lambda e: e.matmul(pv[:, c * 64:(c + 1) * 64], lhsT=hid[u][:, hc, c * 128:(c + 1) * 128], rhs=w2b[:, 1, hc, :], start=(hc == 0), stop=(hc == 1)),
                                     reads=[Tw2, Thid[u]], writes=[Tpv])
                        S.op("dve", lambda e: e.tensor_copy(out=vco[u][:].rearrange("p c d -> p (c d)"), in_=pv[:, 0:128]), reads=[Tpv], writes=[Tvo[u]])
                        k.store(k.scr["vc"][b, kv].rearrange("(c p) d -> p c d", p=128), vco[u][:], [Tvo[u]])
        S.barrier()


K.phase2 = phase2
PHASES.append("phase2")


def phase3(self, l):
    k, S = self, self.S
    with contextlib.ExitStack() as es:
        sb = lambda n, sh, dt=F32: k.sb(es, "p3_" + n, sh, dt)
        QS = [sb(f"QS{h}", [128, SEQ], BF16) for h in range(4)]
        TQd = [S.T(f"p3_Qd{h}") for h in range(4)]
        TQm = [[S.T(f"p3_Qm{h}_{g}") for g in range(NG)] for h in range(4)]
        KS = sb("KS", [128, SEQ], BF16)
        TKS, TE = S.T("p3_KS"), S.T("p3_E")
        KW = sb("KW", [64, SEQ], BF16)
        TKW = S.T("p3_KW")
        KC = sb("KC", [64, 256], BF16)
        TKC = S.T("p3_KC")
        VS = sb("VS", [128, NT, 80], BF16)
        VW = sb("VW", [128, NT, 80], BF16)
        VC = sb("VC", [128, 2, 144], BF16)
        TVS = [S.T(f"p3_VS{i}") for i in range(4)]
        TVW = [S.T(f"p3_VW{i}") for i in range(4)]
        TVC = S.T("p3_VC")
        G_ = sb("G", [128, NT, 32], F32)
        TG = S.T("p3_G")
        yacc = sb("yacc", [128, NT, 4, 64], F32)
        Ty = [[S.T(f"p3_y{g}_{h}") for h in range(4)] for g in range(NG)]
        cmaskS = sb("cmask", [128, 2, SEQ], BF16)
        wmaskS = sb("wmask", [128, 8, 512], BF16)
        dmaskS = sb("dmask", [128, 4, 512], BF16)
        addS = sb("add", [128, NT, 64], F32)
        Tc = S.T("p3_consts")
        S.dma("sp", cmaskS[:], k.inp["cmask"].rearrange("(c p) t -> p c t", p=128), writes=[Tc])
        Tc2 = S.T("p3_consts2")
        S.dma("sp", wmaskS[:], k.inp["wmask"].rearrange("r p u -> p r u"), writes=[Tc2])
        Tc3 = S.T("p3_consts3")
        S.dma("sp", dmaskS[:], k.inp["dmask"].rearrange("r p u -> p r u"), writes=[Tc3])
        Tc4 = S.T("p3_consts4")
        S.dma("sp", addS[:], k.inp["addtab"], writes=[Tc4])
        S.dma("sp", KS[64:128, :], k.inp["Erows"], writes=[TE])
        S.op("pool", lambda e: e.memset(VS[:, :, 64:65], 1.0), writes=TVS)
        S.op("pool", lambda e: e.memset(VW[:, :, 64:65], 1.0), writes=TVW)
        S.op("pool", lambda e: e.memset(VC[:, :, 64:65], 1.0), writes=[TVC])
        Tov = S.T("p3_ov")
        ovs = sb("ovs", [128, 2, 64], BF16)
        S.dma("sp", ovs[:], k.inp["overlap"].rearrange("(c p) j -> p c j", p=128), writes=[Tov])
        S.op("pool", lambda e: e.tensor_copy(out=VC[:, :, 65:129], in_=ovs[:]), reads=[Tov], writes=[TVC])
        import os
        if os.environ.get("P3STOP", "") == "c":
            S.barrier()
            return
        bank = [k.ps(es, f"p3_bank{i}", [128, 512], F32) for i in range(8)]
        Tb = [S.T(f"p3_bank{i}") for i in range(8)]
        NP = 6
        P = [sb(f"P{i}", [128, 512], BF16) for i in range(NP)]
        TP = [S.T(f"p3_P{i}") for i in range(NP)]
        EC = [sb(f"EC{i}", [128, 512], BF16) for i in range(8)]
        TEC = [S.T(f"p3_EC{i}") for i in range(8)]
        Mbp = [sb(f"Mbp{i}", [128, 128], BF16) for i in range(4)]
        TMb = [S.T(f"p3_Mbp{i}") for i in range(4)]
        for i in range(4):
            S.op("pool", lambda e: e.memset(Mbp[i][:], 0.0), writes=[TMb[i]])
        NS = 4
        rs = [sb(f"rs{i}", [128, 4], F32) for i in range(NS)]
        coef = [sb(f"coef{i}", [128, 4], F32) for i in range(NS)]
        impt = [sb(f"impt{i}", [128, 64], F32) for i in range(NS)]
        tmp = [sb(f"tmp{i}", [128, 64], F32) for i in range(NS)]
        m1 = [sb(f"m1_{i}", [128, 8], F32) for i in range(NS)]
        m2 = [sb(f"m2_{i}", [128, 8], F32) for i in range(NS)]
        Tsm = [S.T(f"p3_sm{i}") for i in range(NS)]
        ot = [sb(f"ot{i}", [65, 512], F32) for i in range(2)]
        Tot = [S.T(f"p3_ot{i}") for i in range(2)]
        cnt = {"p": 0, "s": 0, "sm": 0, "mb": 0, "ot": 0, "pa": 0, "ow": 0, "os": 0}

        def nxt(key, n):
            v = cnt[key] % n
            cnt[key] += 1
            return v

        for b in range(NB):
            for kv in range(2):
                for h in range(4):
                    r0 = (kv * 4 + h) * 64
                    S.dma("sp", QS[h][0:64, :], k.scr["qT"][b, r0:r0 + 64, :], writes=[TQd[h]])
                S.dma("sp", KS[0:64, :], k.scr["ksT"][b, kv * 64:(kv + 1) * 64, :], writes=[TKS])
                S.dma("sp", KW[:, :], k.scr["kwT"][b, kv * 64:(kv + 1) * 64, :], writes=[TKW])
                S.dma("sp", KC[:, :], k.scr["kcT"][b, kv], writes=[TKC])
                vsrc = k.scr["vsw"][b].rearrange("(n p) c -> p n c", p=128)
                for q4 in range(4):
                    nsl = slice(q4 * 8, q4 * 8 + 8)
                    S.dma("sp", VS[:, nsl, 0:64], vsrc[:, nsl, kv * 64:(kv + 1) * 64], writes=[TVS[q4]])
                    S.dma("sp", VW[:, nsl, 0:64], vsrc[:, nsl, 128 + kv * 64:128 + (kv + 1) * 64], writes=[TVW[q4]])
                S.dma("sp", VC[:, :, 0:64], k.scr["vc"][b, kv].rearrange("(c p) d -> p c d", p=128), writes=[TVC])
                if kv == 0:
                    S.dma("sp", G_[:], k.scr["gat"][b].rearrange("(n p) c -> p n c", p=128), writes=[TG])
                import os
                P3STOP = os.environ.get("P3STOP", "")
                for g in range(NG):
                    if P3STOP == "load":
                        break
                    gsl = slice(g * 512, (g + 1) * 512)
                    ncs = 2 if g >= 4 else 1
                    ecs = {}
                    for h in range(4):
                        for c in range(ncs):
                            bi = nxt("s", 2)
                            S.op("pe", lambda e: e.matmul(bank[bi][:], lhsT=KC[0:64, c * 128:(c + 1) * 128], rhs=QS[h][0:64, gsl], start=True, stop=False),
                                 reads=[TKC, TQd[h]], writes=[Tb[bi]])
                            S.op("pe", lambda e: e.matmul(bank[bi][:], lhsT=k.identb[:], rhs=cmaskS[:, c, gsl], start=False, stop=True),
                                 reads=[k.Tidb, Tc], writes=[Tb[bi]])
                            ei = h * 2 + c
                            S.op("act", lambda e: e.activation(out=EC[ei][:], in_=bank[bi][:], func=AF.Exp), reads=[Tb[bi]], writes=[TEC[ei]])
                            ecs[(h, c)] = ei
                    P3A = os.environ.get("P3A", "")
                    for jq in range(4):
                        if P3A == "s":
                            break
                        n = 4 * g + jq
                        pa = nxt("pa", 2)
                        bks = (2 + 2 * pa, 3 + 2 * pa)
                        for h in range(4):
                            bk = bks[h // 2]
                            o0 = (h % 2) * 256
                            for c in range(ncs):
                                ei = ecs[(h, c)]
                                S.op("pe", lambda e: e.matmul(bank[bk][:, o0:o0 + 129], lhsT=EC[ei][:, jq * 128:(jq + 1) * 128], rhs=VC[:, c, 0:129], start=(c == 0), stop=(c == ncs - 1)),
                                     reads=[TEC[ei], TVC], writes=[Tb[bk]])
                        if P3A == "pv":
                            continue
                        si = nxt("sm", NS)
                        T_s = Tsm[si]
                        for half in range(2):
                            bk = bks[half]
                            S.op("dve", lambda e: e.tensor_scalar(out=rs[si][:, 2 * half:2 * half + 2], in0=bank[bk][:].rearrange("p (a f) -> p a f", a=2)[:, :, 64], scalar1=1e-30, scalar2=None, op0=ALU.max),
                                 reads=[Tb[bk]], writes=[T_s])
                        S.op("dve", lambda e: e.reciprocal(out=rs[si][:], in_=rs[si][:]), reads=[T_s], writes=[T_s])
                        for half in range(0):
                            pass
                        for h in range(4):
                            bk = bks[h // 2]
                            o0 = (h % 2) * 256
                            in1 = addS[:, n, :] if h == 0 else impt[si][:]
                            S.op("dve", lambda e: e.scalar_tensor_tensor(out=impt[si][:], in0=bank[bk][:, o0 + 65:o0 + 129], scalar=rs[si][:, h:h + 1], in1=in1, op0=ALU.mult, op1=ALU.add),
                                 reads=[Tb[bk], T_s, Tc4], writes=[T_s])
                        S.op("dve", lambda e: e.tensor_tensor(out=coef[si][:], in0=rs[si][:], in1=G_[:, n, kv * 4:kv * 4 + 4], op=ALU.mult), reads=[T_s, TG], writes=[T_s])
                        if P3A == "d1":
                            continue
                        for h in range(4):
                            bk = bks[h // 2]
                            o0 = (h % 2) * 256
                            S.op("act", lambda e: e.activation(out=yacc[:, n, h, :], in_=bank[bk][:, o0:o0 + 64], func=AF.Identity, scale=coef[si][:, h:h + 1]),
                                 reads=[Tb[bk], T_s], writes=[Ty[g][h]])
                        if P3A == "y":
                            continue
                        S.op("dve", lambda e: e.max(out=m1[si][:], in_=impt[si][:]), reads=[T_s], writes=[T_s])
                        S.op("dve", lambda e: e.match_replace(out=tmp[si][:], in_to_replace=m1[si][:], in_values=impt[si][:], imm_value=-3e9), reads=[T_s], writes=[T_s])
                        S.op("dve", lambda e: e.max(out=m2[si][:], in_=tmp[si][:]), reads=[T_s], writes=[T_s])
                        if P3A == "tk":
                            continue
                        mi = nxt("mb", 4)
                        S.op("dve", lambda e: e.tensor_scalar(out=Mbp[mi][:, 64:128], in0=impt[si][:], scalar1=m2[si][:, 7:8], scalar2=NEG, op0=ALU.is_lt, op1=ALU.mult),
                             reads=[T_s], writes=[TMb[mi]])
                        S.op("pe", lambda e: e.matmul(bank[6][:, jq * 128:(jq + 1) * 128], lhsT=Mbp[mi][:], rhs=k.identb[:], start=True, stop=True),
                             reads=[TMb[mi], k.Tidb], writes=[Tb[6]])
                    S.op("dve", lambda e: e.tensor_copy(out=QS[0][64:128, gsl], in_=bank[6][64:128, :]), reads=[Tb[6]], writes=[TQm[0][g]])
                    for h in range(1, 4):
                        S.op("pool", lambda e: e.tensor_copy(out=QS[h][64:128, gsl], in_=QS[0][64:128, gsl]), reads=[TQm[0][g]], writes=[TQm[h][g]])
                    for h in range(4):
                        if P3STOP == "A":
                            break
                        for br in ((2,) if P3STOP == "W" else (2, 1)):
                            if br == 2:
                                kts = [kt for kt in range(4 * g - 4, 4 * g + 4) if kt >= 0]
                                ob = 3 + nxt("ow", 2)
                            else:
                                kts = list(range(0, 4 * g + 4))
                                ob = 5 + nxt("os", 2)
                            Vt, TV = (VW, TVW) if br == 2 else (VS, TVS)
                            pis = {}

                            def qk(idx, kt):
                                ksl = slice(kt * 128, (kt + 1) * 128)
                                bi = nxt("p", 3)
                                if br == 2:
                                    S.op("pe", lambda e: e.matmul(bank[bi][:], lhsT=KW[0:64, ksl], rhs=QS[h][0:64, gsl], start=True, stop=False),
                                         reads=[TKW, TQd[h]], writes=[Tb[bi]])
                                    S.op("pe", lambda e: e.matmul(bank[bi][:], lhsT=k.identb[:], rhs=wmaskS[:, kt - 4 * g + 4, :], start=False, stop=True),
                                         reads=[k.Tidb, Tc2], writes=[Tb[bi]])
                                else:
                                    diag = kt >= 4 * g
                                    S.op("pe", lambda e: e.matmul(bank[bi][:], lhsT=KS[:, ksl], rhs=QS[h][:, gsl], start=True, stop=not diag),
                                         reads=[TKS, TE, TQd[h], TQm[h][g]], writes=[Tb[bi]])
                                    if diag:
                                        S.op("pe", lambda e: e.matmul(bank[bi][:], lhsT=k.identb[:], rhs=dmaskS[:, kt - 4 * g, :], start=False, stop=True),
                                             reads=[k.Tidb, Tc3], writes=[Tb[bi]])
                                pi = nxt("s", NP)
                                S.op("act", lambda e: e.activation(out=P[pi][:], in_=bank[bi][:], func=AF.Exp), reads=[Tb[bi]], writes=[TP[pi]])
                                pis[idx] = pi

                            def pvf(idx, kt):
                                pi = pis.pop(idx)
                                S.op("pe", lambda e: e.matmul(bank[ob][0:65, :], lhsT=Vt[:, kt, 0:65], rhs=P[pi][:], start=(idx == 0), stop=(idx == len(kts) - 1)),
                                     reads=[TV[kt // 8], TP[pi]], writes=[Tb[ob]])
                            LA = 2
                            for i_ in range(len(kts) + LA):
                                if i_ < len(kts):
                                    qk(i_, kts[i_])
                                if i_ >= LA:
                                    pvf(i_ - LA, kts[i_ - LA])
                            oi = nxt("ot", 2)
                            S.op("dve", lambda e: e.tensor_copy(out=ot[oi][:], in_=bank[ob][0:65, :]), reads=[Tb[ob]], writes=[Tot[oi]])
                            for jq in range(4):
                                S.op("pe", lambda e: e.transpose(out=bank[7][:, jq * 128:jq * 128 + 65], in_=ot[oi][:, jq * 128:(jq + 1) * 128], identity=k.identf[0:65, 0:65]),
                                     reads=[Tot[oi], k.Tid], writes=[Tb[7]])
                            si = nxt("sm", NS)
                            T_s = Tsm[si]
                            b7 = bank[7][:].rearrange("p (a f) -> p a f", a=4)
                            S.op("dve", lambda e: e.reciprocal(out=rs[si][:], in_=b7[:, :, 64]), reads=[Tb[7]], writes=[T_s])
                            gc = br * 8 + kv * 4 + h
                            S.op("dve", lambda e: e.tensor_tensor(out=coef[si][:], in0=rs[si][:], in1=G_[:, 4 * g:4 * g + 4, gc], op=ALU.mult), reads=[T_s, TG], writes=[T_s])
                            for jq in range(4):
                                n = 4 * g + jq
                                S.op("dve", lambda e: e.scalar_tensor_tensor(out=yacc[:, n, h, :], in0=bank[7][:, jq * 128:jq * 128 + 64], scalar=coef[si][:, jq:jq + 1], in1=yacc[:, n, h, :], op0=ALU.mult, op1=ALU.add),
                                     reads=[Tb[7], T_s, Ty[g][h]], writes=[Ty[g][h]])
                if P3STOP == "load" and os.environ.get("P3NOST", ""):
                    continue
                ydst = k.scr["ycat"][b].rearrange("(n p) c -> p n c", p=128)
                for g in range(NG):
                    k.store(ydst[:, 4 * g:4 * g + 4, kv * 256:(kv + 1) * 256], yacc[:, 4 * g:4 * g + 4, :, :].rearrange("p n h d -> p n (h d)"), Ty[g])
        S.barrier()


K.phase3 = phase3
PHASES.append("phase3")


def phase5(self, l):
    k, S = self, self.S
    with contextlib.ExitStack() as es:
        sb = lambda n, sh, dt=F32: k.sb(es, "p5_" + n, sh, dt)
        wdw = sb("wdw", [128, 2, 31], F32)
        Twd = S.T("p5_wdw")
        S.dma("sp", wdw[:], k.inp["wdw_t"][l].rearrange("(ct p) kk -> p ct kk", p=128), writes=[Twd])
        Dg = sb("Dg", [128, 2, 31, 128], BF16)
        TDg = S.T("p5_Dg")
        for ct in range(2):
            for kk in range(31):
                S.op("pool" if (kk % 2) else "dve", lambda e: e.tensor_scalar(out=Dg[:, ct, kk, :], in0=k.identf[:], scalar1=wdw[:, ct, kk:kk + 1], scalar2=None, op0=ALU.mult),
                     reads=[Twd, k.Tid], writes=[TDg])
        rows = sb("rows", [1, 2, 256], F32)
        rowsb = sb("rowsb", [1, 2, 256], BF16)
        Trows = S.T("p5_rows")
        S.dma("sp", rows[:, 0, :], k.inp["conv_b_dw"][l:l + 1, :], writes=[Trows])
        S.dma("sp", rows[:, 1, :], k.inp["conv_b_pw"][l:l + 1, :], writes=[Trows])
        S.op("dve", lambda e: e.tensor_copy(out=rowsb[:], in_=rows[:]), reads=[Trows], writes=[Trows])
        lng = sb("lng", [128, 256], F32)
        lnb = sb("lnb", [128, 256], F32)
        Tln = S.T("p5_ln")
        S.dma("sp", lng[:], k.inp["conv_ln_g"][l].partition_broadcast(128), writes=[Tln])
        Tln2 = S.T("p5_ln2")
        S.dma("sp", lnb[:], k.inp["conv_ln_b"][l].partition_broadcast(128), writes=[Tln2])
        wps = sb("wps", [128, 2, 256], F32)
        wpb = sb("wpb", [128, 2, 256], BF16)
        Twp = S.T("p5_wp")
        S.dma("sp", wps[:], k.inp["conv_w_pw"][l].rearrange("(ct p) n -> p ct n", p=128), writes=[Twp])
        S.op("dve", lambda e: e.tensor_copy(out=wpb[:], in_=wps[:]), reads=[Twp], writes=[Twp])
        Ub = sb("Ub", [128, 2, 32 + SEQ], BF16)
        TUb = [S.T(f"p5_Ub{c}") for c in range(2)]
        S.op("pool", lambda e: e.memset(Ub[:, :, 0:32], 0.0), writes=TUb)
        pc = [k.ps(es, f"p5_pc{i}", [128, 512], F32) for i in range(2)]
        Tpc = [S.T(f"p5_pc{i}") for i in range(2)]
        pz = [k.ps(es, f"p5_pz{i}", [128, 256], BF16) for i in range(2)]
        Tpz = [S.T(f"p5_pz{i}") for i in range(2)]
        po = [k.ps(es, f"p5_po{i}", [128, 512], F32) for i in range(2)]
        Tpo = [S.T(f"p5_po{i}") for i in range(2)]
        NR = 3
        st = [sb(f"st{i}", [128, 6], F32) for i in range(NR)]
        mv = [sb(f"mv{i}", [128, 2], F32) for i in range(NR)]
        rstd = [sb(f"rstd{i}", [128, 1], F32) for i in range(NR)]
        xn = [sb(f"xn{i}", [128, 256], F32) for i in range(NR)]
        zb = [sb(f"zb{i}", [128, 256], BF16) for i in range(NR)]
        zT = [sb(f"zT{i}", [128, 2, 128], BF16) for i in range(NR)]
        Tr = [S.T(f"p5_r{i}") for i in range(NR)]
        TzT = [S.T(f"p5_zT{i}") for i in range(NR)]
        yo = [sb(f"yo{i}", [128, 4, 256], F32) for i in range(2)]
        Tyo = [S.T(f"p5_yo{i}") for i in range(2)]
        it = 0
        for b in range(NB):
            for ct in range(2):
                S.dma("sp", Ub[:, ct, 32:32 + SEQ], k.scr["ucT"][b, ct * 128:(ct + 1) * 128, :], writes=[TUb[ct]])
            for n in range(NT):
                u = it % 2
                r_ = it % NR
                it += 1
                t0 = n * 128 + 2
                for ct in range(2):
                    csl = slice(ct * 128, (ct + 1) * 128)
                    S.op("pe", lambda e: e.matmul(pc[u][:, csl], lhsT=k.ones_b[0:1, 0:128], rhs=rowsb[0:1, 0, csl], start=True, stop=False),
                         reads=[k.Tones, Trows], writes=[Tpc[u]])
                    for kk in range(31):
                        S.op("pe", lambda e: e.matmul(pc[u][:, csl], lhsT=Ub[:, ct, t0 + kk:t0 + kk + 128], rhs=Dg[:, ct, kk, :], start=False, stop=(kk == 30)),
                             reads=[TUb[ct], TDg], writes=[Tpc[u]])
                T_r = Tr[r_]
                S.op("dve", lambda e: e.bn_stats(out=st[r_][:], in_=pc[u][:, 0:256]), reads=[Tpc[u]], writes=[T_r])
                S.op("dve", lambda e: e.bn_aggr(out=mv[r_][:], in_=st[r_][:]), reads=[T_r], writes=[T_r])
                S.op("dve", lambda e: e.tensor_scalar(out=rstd[r_][:], in0=mv[r_][:, 1:2], scalar1=EPS, scalar2=None, op0=ALU.add), reads=[T_r], writes=[T_r])
                S.op("act", lambda e: e.activation(out=rstd[r_][:], in_=rstd[r_][:], func=AF.Sqrt), reads=[T_r], writes=[T_r])
                S.op("dve", lambda e: e.reciprocal(out=rstd[r_][:], in_=rstd[r_][:]), reads=[T_r], writes=[T_r])
                S.op("dve", lambda e: e.tensor_scalar(out=xn[r_][:], in0=pc[u][:, 0:256], scalar1=mv[r_][:, 0:1], scalar2=rstd[r_][:, 0:1], op0=ALU.subtract, op1=ALU.mult),
                     reads=[Tpc[u], T_r], writes=[T_r])
                S.op("pool", lambda e: e.tensor_tensor(out=xn[r_][:], in0=xn[r_][:], in1=lng[:], op=ALU.mult), reads=[T_r, Tln], writes=[T_r])
                S.op("pool", lambda e: e.tensor_tensor(out=xn[r_][:], in0=xn[r_][:], in1=lnb[:], op=ALU.add), reads=[T_r, Tln2], writes=[T_r])
                S.op("act", lambda e: e.activation(out=zb[r_][:], in_=xn[r_][:], func=AF.Silu), reads=[T_r], writes=[T_r])
                for ct in range(2):
                    S.op("pe", lambda e: e.transpose(out=pz[u][:, ct * 128:(ct + 1) * 128], in_=zb[r_][:, ct * 128:(ct + 1) * 128], identity=k.identb[:]),
                         reads=[T_r, k.Tidb], writes=[Tpz[u]])
                S.op("act", lambda e: e.activation(out=zT[r_][:].rearrange("p c t -> p (c t)"), in_=pz[u][:], func=AF.Copy), reads=[Tpz[u]], writes=[TzT[r_]])
                S.op("pe", lambda e: e.matmul(po[u][:, 0:256], lhsT=k.ones_b[0:1, 0:128], rhs=rowsb[0:1, 1, :], start=True, stop=False),
                     reads=[k.Tones, Trows], writes=[Tpo[u]])
                for ct in range(2):
                    S.op("pe", lambda e: e.matmul(po[u][:, 0:256], lhsT=zT[r_][:, ct, :], rhs=wpb[:, ct, :], start=False, stop=(ct == 1)),
                         reads=[TzT[r_], Twp], writes=[Tpo[u]])
                yi = (n // 4) % 2
                S.op("dve", lambda e: e.tensor_copy(out=yo[yi][:, n % 4, :], in_=po[u][:, 0:256]), reads=[Tpo[u]], writes=[Tyo[yi]])
                if n % 4 == 3:
                    ydst = k.scr["ycat"][b].rearrange("(n p) c -> p n c", p=128)
                    k.store(ydst[:, n - 3:n + 1, 768:1024], yo[yi][:], [Tyo[yi]])
        S.barrier()


K.phase5 = phase5
PHASES.append("phase5")


def phase6a(self, l):
    k, S = self, self.S
    with contextlib.ExitStack() as es:
        sb = lambda n, sh, dt=F32: k.sb(es, "p6a_" + n, sh, dt)
        Wo = sb("Wo", [128, 8, D], BF16)
        TWo = [S.T(f"p6a_Wo{i}") for i in range(8)]
        stage = [sb(f"stage{i}", [128, D], F32) for i in range(2)]
        Tst = [S.T(f"p6a_stage{i}") for i in range(2)]
        gcat = sb("gcat", [128, 8], F32)
        Tg = S.T("p6a_gcat")
        S.dma("sp", gcat[:], k.inp["gcat_t"][l], writes=[Tg])
        wsrc = k.inp["w_out"][l].rearrange("(kc p) n -> p kc n", p=128)
        for kc in range(8):
            S.dma("sp", stage[kc % 2][:], wsrc[:, kc, :], writes=[Tst[kc % 2]])
            S.op("pool" if kc % 2 else "dve", lambda e: e.tensor_scalar(out=Wo[:, kc, :], in0=stage[kc % 2][:], scalar1=gcat[:, kc:kc + 1], scalar2=None, op0=ALU.mult),
                 reads=[Tst[kc % 2], Tg], writes=[TWo[kc]])
        yg = [sb(f"yg{i}", [128, 4, D], F32) for i in range(2)]
        Tyg = [S.T(f"p6a_yg{i}") for i in range(2)]
        xg = [sb(f"xg{i}", [128, 4, D], F32) for i in range(2)]
        Txg = [[S.T(f"p6a_xg{i}_{j}") for j in range(4)] for i in range(2)]
        junk = sb("junk", [128, 512], BF16)
        Tjunk = S.T("p6a_junk")
        ss = sb("ss", [128, 4, 4], F32)
        Tss = S.T("p6a_ss")
        mb = sb("mb", [128, 4, D], BF16)
        Tmb = [S.T(f"p6a_mb{j}") for j in range(4)]
        mT = sb("mT", [128, 8, 512], BF16)
        TmT = [S.T(f"p6a_mT{c}") for c in range(8)]
        hb = sb("hb", [128, 4, D], BF16)
        Thb = [S.T(f"p6a_hb{j}") for j in range(4)]
        hT = [sb(f"hT{i}", [128, 8, 512], BF16) for i in range(2)]
        ThT = [S.T(f"p6a_hT{i}") for i in range(2)]
        pT = [k.ps(es, f"p6a_pT{i}", [128, 512], BF16) for i in range(2)]
        TpT = [S.T(f"p6a_pT{i}") for i in range(2)]
        pm = [k.ps(es, f"p6a_pm{i}", [128, 512], F32) for i in range(4)]
        Tpm = [S.T(f"p6a_pm{i}") for i in range(4)]
        xin = k.inp["x"] if l == 0 else k.scr["xres"]
        segs = [(0, 512), (512, 768), (768, 1024)]
        gi = 0
        pmi = 0
        for b in range(NB):
            for g in range(NG):
                u = gi % 2
                gi += 1
                tsl = slice(g * 512, (g + 1) * 512)
                S.dma("sp", yg[u][:], k.scr["ycat"][b, tsl, :].rearrange("(j p) d -> p j d", p=128), writes=[Tyg[u]])
                for j in range(4):
                    S.dma("sp", xg[u][:, j, :], xin[b, g * 512 + j * 128:g * 512 + (j + 1) * 128, :], writes=[Txg[u][j]])
                for j in range(4):
                    for si, (a0, a1) in enumerate(segs):
                        S.op("act", lambda e: e.activation(out=junk[:, 0:a1 - a0], in_=yg[u][:, j, a0:a1], func=AF.Square, accum_out=ss[:, j, si:si + 1]),
                             reads=[Tyg[u]], writes=[Tjunk, Tss])
                for si, (a0, a1) in enumerate(segs):
                    S.op("dve", lambda e: e.tensor_scalar(out=ss[:, :, si], in0=ss[:, :, si], scalar1=1.0 / (a1 - a0), scalar2=EPS, op0=ALU.mult, op1=ALU.add), reads=[Tss], writes=[Tss])
                S.op("act", lambda e: e.activation(out=ss[:, :, 0:3], in_=ss[:, :, 0:3], func=AF.Sqrt), reads=[Tss], writes=[Tss])
                S.op("dve", lambda e: e.reciprocal(out=ss[:, :, 0:3], in_=ss[:, :, 0:3]), reads=[Tss], writes=[Tss])
                for j in range(4):
                    for si, (a0, a1) in enumerate(segs):
                        S.op("pool" if (si == 0) else "dve", lambda e: e.tensor_scalar(out=mb[:, j, a0:a1], in0=yg[u][:, j, a0:a1], scalar1=ss[:, j, si:si + 1], scalar2=None, op0=ALU.mult),
                             reads=[Tyg[u], Tss], writes=[Tmb[j]])
                for kc in range(8):
                    pu = kc % 2
                    for j in range(4):
                        S.op("pe", lambda e: e.transpose(out=pT[pu][:, j * 128:(j + 1) * 128], in_=mb[:, j, kc * 128:(kc + 1) * 128], identity=k.identb[:]),
                             reads=[Tmb[j], k.Tidb], writes=[TpT[pu]])
                    S.op("act" if kc % 2 else "dve", (lambda e: e.activation(out=mT[:, kc, :], in_=pT[pu][:], func=AF.Copy)) if kc % 2 else (lambda e: e.tensor_copy(out=mT[:, kc, :], in_=pT[pu][:])),
                         reads=[TpT[pu]], writes=[TmT[kc]])
                for j in range(4):
                    for half in range(2):
                        i = pmi % 4
                        pmi += 1
                        hsl = slice(half * 512, (half + 1) * 512)
                        for kc in range(8):
                            S.op("pe", lambda e: e.matmul(pm[i][:], lhsT=mT[:, kc, j * 128:(j + 1) * 128], rhs=Wo[:, kc, hsl], start=(kc == 0), stop=(kc == 7)),
                                 reads=[TmT[kc], TWo[kc]], writes=[Tpm[i]])
                        S.op("dve", lambda e: e.tensor_tensor(out=xg[u][:, j, hsl], in0=pm[i][:], in1=xg[u][:, j, hsl], op=ALU.add), reads=[Tpm[i], Txg[u][j]], writes=[Txg[u][j]])
                    k.store(k.scr["xres"][b, g * 512 + j * 128:g * 512 + (j + 1) * 128, :], xg[u][:, j, :], [Txg[u][j]])
                    S.op("act", lambda e: e.activation(out=junk[:, 0:512], in_=xg[u][:, j, 0:512], func=AF.Square, accum_out=ss[:, j, 3:4]),
                         reads=[Txg[u][j]], writes=[Tjunk, Tss])
                    S.op("act", lambda e: e.activation(out=junk[:, 0:512], in_=xg[u][:, j, 512:1024], func=AF.Square, accum_out=ss[:, j, 0:1]),
                         reads=[Txg[u][j]], writes=[Tjunk, Tss])
                S.op("dve", lambda e: e.tensor_tensor(out=ss[:, :, 3], in0=ss[:, :, 3], in1=ss[:, :, 0], op=ALU.add), reads=[Tss], writes=[Tss])
                S.op("dve", lambda e: e.tensor_scalar(out=ss[:, :, 3], in0=ss[:, :, 3], scalar1=1.0 / D, scalar2=EPS, op0=ALU.mult, op1=ALU.add), reads=[Tss], writes=[Tss])
                S.op("act", lambda e: e.activation(out=ss[:, :, 3], in_=ss[:, :, 3], func=AF.Sqrt), reads=[Tss], writes=[Tss])
                S.op("dve", lambda e: e.reciprocal(out=ss[:, :, 3], in_=ss[:, :, 3]), reads=[Tss], writes=[Tss])
                for j in range(4):
                    S.op("pool" if j % 2 else "dve", lambda e: e.tensor_scalar(out=hb[:, j, :], in0=xg[u][:, j, :], scalar1=ss[:, j, 3:4], scalar2=None, op0=ALU.mult),
                         reads=[Txg[u][j], Tss], writes=[Thb[j]])
                for kc in range(8):
                    pu = kc % 2
                    for j in range(4):
                        S.op("pe", lambda e: e.transpose(out=pT[pu][:, j * 128:(j + 1) * 128], in_=hb[:, j, kc * 128:(kc + 1) * 128], identity=k.identb[:]),
                             reads=[Thb[j], k.Tidb], writes=[TpT[pu]])
                    S.op("act" if kc % 2 else "dve", (lambda e: e.activation(out=hT[u][:, kc, :], in_=pT[pu][:], func=AF.Copy)) if kc % 2 else (lambda e: e.tensor_copy(out=hT[u][:, kc, :], in_=pT[pu][:])),
                         reads=[TpT[pu]], writes=[ThT[u]])
                k.store(k.scr["h2T"][b, :, tsl].rearrange("(kc p) t -> p kc t", p=128), hT[u][:], [ThT[u]])
        S.barrier()


def phase6b(self, l):
    k, S = self, self.S
    with contextlib.ExitStack() as es:
        sb = lambda n, sh, dt=F32: k.sb(es, "p6b_" + n, sh, dt)
        Wg = sb("Wg", [128, 8, DFF], BF16)
        Wu = sb("Wu", [128, 8, DFF], BF16)
        Wd = sb("Wd", [128, NFF, D], BF16)
        TWg = [S.T(f"p6b_Wg{i}") for i in range(8)]
        TWu = [S.T(f"p6b_Wu{i}") for i in range(8)]
        TWd = [S.T(f"p6b_Wd{i}") for i in range(NFF)]
        with contextlib.ExitStack() as es2:
            stage = [k.sb(es2, f"p6b_stage{i}", [128, DFF], F32) for i in range(2)]
            Tst = [S.T(f"p6b_stage{i}") for i in range(2)]
            gn = k.sb(es2, "p6b_gn", [128, 8], F32)
            Tgn = S.T("p6b_gn")
            S.dma("sp", gn[:], k.inp["norm_ffn_t"][l], writes=[Tgn])
            si = 0
            for nm, Wt, TWt in [("w_gate", Wg, TWg), ("w_up", Wu, TWu)]:
                wsrc = k.inp[nm][l].rearrange("(kc p) n -> p kc n", p=128)
                for kc in range(8):
                    u = si % 2
                    si += 1
                    S.dma("sp", stage[u][:], wsrc[:, kc, :], writes=[Tst[u]])
                    S.op("pool" if u else "dve", lambda e: e.tensor_scalar(out=Wt[:, kc, :], in0=stage[u][:], scalar1=gn[:, kc:kc + 1], scalar2=None, op0=ALU.mult),
                         reads=[Tst[u], Tgn], writes=[TWt[kc]])
            wsrc = k.inp["w_down"][l].rearrange("(fc p) n -> p fc n", p=128)
            for fc in range(0, NFF, 2):
                u = si % 2
                si += 1
                S.dma("sp", stage[u][:, 0:2 * D].rearrange("p (a n) -> p a n", a=2), wsrc[:, fc:fc + 2, :], writes=[Tst[u]])
                S.op("pool" if u else "act", (lambda e: e.tensor_copy(out=Wd[:, fc:fc + 2, :].rearrange("p a n -> p (a n)"), in_=stage[u][:, 0:2 * D])) if u else
                     (lambda e: e.activation(out=Wd[:, fc:fc + 2, :].rearrange("p a n -> p (a n)"), in_=stage[u][:, 0:2 * D], func=AF.Copy)),
                     reads=[Tst[u]], writes=[TWd[fc], TWd[fc + 1]])
            S.barrier()
        hT = [sb(f"hT{i}", [128, 8, 512], BF16) for i in range(2)]
        ThT = [S.T(f"p6b_hT{i}") for i in range(2)]
        NX = 3
        xt = [sb(f"xt{i}", [128, D], F32) for i in range(NX)]
        Txt = [S.T(f"p6b_xt{i}") for i in range(NX)]
        actT = sb("actT", [128, NFF, 512], BF16)
        Tact = [S.T(f"p6b_act{i}") for i in range(NFF)]
        sg = [sb(f"sg{i}", [128, 512], BF16) for i in range(2)]
        Tsg = [S.T(f"p6b_sg{i}") for i in range(2)]
        pg = [k.ps(es, f"p6b_pg{i}", [128, 512], F32) for i in range(2)]
        pu_ = [k.ps(es, f"p6b_pu{i}", [128, 512], F32) for i in range(2)]
        pd = [k.ps(es, f"p6b_pd{i}", [128, 512], F32) for i in range(2)]
        Tpg = [S.T(f"p6b_pg{i}") for i in range(2)]
        Tpu = [S.T(f"p6b_pu{i}") for i in range(2)]
        Tpd = [S.T(f"p6b_pd{i}") for i in range(2)]
        gi = 0
        xi = 0
        pdi = 0
        for b in range(NB):
            for g in range(NG):
                u = gi % 2
                gi += 1
                tsl = slice(g * 512, (g + 1) * 512)
                S.dma("sp", hT[u][:], k.scr["h2T"][b, :, tsl].rearrange("(kc p) t -> p kc t", p=128), writes=[ThT[u]])
                for fc in range(NFF):
                    v = fc % 2
                    fsl = slice(fc * 128, (fc + 1) * 128)
                    for kc in range(8):
                        S.op("pe", lambda e: e.matmul(pg[v][:], lhsT=Wg[:, kc, fsl], rhs=hT[u][:, kc, :], start=(kc == 0), stop=(kc == 7)),
                             reads=[TWg[kc], ThT[u]], writes=[Tpg[v]])
                    for kc in range(8):
                        S.op("pe", lambda e: e.matmul(pu_[v][:], lhsT=Wu[:, kc, fsl], rhs=hT[u][:, kc, :], start=(kc == 0), stop=(kc == 7)),
                             reads=[TWu[kc], ThT[u]], writes=[Tpu[v]])
                    S.op("act", lambda e: e.activation(out=sg[v][:], in_=pg[v][:], func=AF.Silu), reads=[Tpg[v]], writes=[Tsg[v]])
                    S.op("dve", lambda e: e.tensor_tensor(out=actT[:, fc, :], in0=pu_[v][:], in1=sg[v][:], op=ALU.mult), reads=[Tpu[v], Tsg[v]], writes=[Tact[fc]])
                for j in range(4):
                    xx = xi % NX
                    xi += 1
                    r0 = g * 512 + j * 128
                    S.dma("sp", xt[xx][:], k.scr["xres"][b, r0:r0 + 128, :], writes=[Txt[xx]])
                    for half in range(2):
                        pi = pdi % 2
                        pdi += 1
                        hsl = slice(half * 512, (half + 1) * 512)
                        for fc in range(NFF):
                            S.op("pe", lambda e: e.matmul(pd[pi][:], lhsT=actT[:, fc, j * 128:(j + 1) * 128], rhs=Wd[:, fc, hsl], start=(fc == 0), stop=(fc == NFF - 1)),
                                 reads=[Tact[fc], TWd[fc]], writes=[Tpd[pi]])
                        S.op("dve", lambda e: e.tensor_tensor(out=xt[xx][:, hsl], in0=pd[pi][:], in1=xt[xx][:, hsl], op=ALU.add), reads=[Tpd[pi], Txt[xx]], writes=[Txt[xx]])
                    k.store(k.scr["xres"][b, r0:r0 + 128, :], xt[xx][:], [Txt[xx]])
        S.barrier()


K.phase6a = phase6a
K.phase6b = phase6b


def phase4(self, l):
    k, S = self, self.S
    PI = float(np.pi)
    C1 = float(np.float32(2 * np.pi))
    C2 = float(2 * np.pi - float(np.float32(2 * np.pi)))
    with contextlib.ExitStack() as es:
        sb = lambda n, sh, dt=F32: k.sb(es, "p4_" + n, sh, dt)
        Tp = S.T("p4_par")
        are, aim, ldt = sb("are", [128, 8]), sb("aim", [128, 8]), sb("ldt", [128, 8])
        S.dma("sp", are[:], k.inp["ssm_are_t"][l], writes=[Tp])
        Tp2 = S.T("p4_par2")
        S.dma("sp", aim[:], k.inp["ssm_aim_t"][l], writes=[Tp2])
        Tp3 = S.T("p4_par3")
        S.dma("sp", ldt[:], k.inp["ssm_ldt_t"][l], writes=[Tp3])
        dtt, z, th, mag, q = sb("dtt", [128, 8]), sb("z", [128, 8]), sb("th", [128, 8]), sb("mag", [128, 8]), sb("q", [128, 8])

        def dv(fn, reads=(), writes=(Tp,)):
            S.op("dve", fn, reads=list(reads) + [Tp], writes=list(writes))

        S.op("act", lambda e: e.activation(out=dtt[:], in_=ldt[:], func=AF.Exp), reads=[Tp3], writes=[Tp])
        dv(lambda e: e.tensor_tensor(out=z[:], in0=are[:], in1=dtt[:], op=ALU.mult))
        dv(lambda e: e.tensor_tensor(out=th[:], in0=aim[:], in1=dtt[:], op=ALU.mult), reads=[Tp2])
        dv(lambda e: e.tensor_scalar(out=q[:], in0=z[:], scalar1=1.0 / 6.0, scalar2=1.0, op0=ALU.mult, op1=ALU.add))
        for kk in (5.0, 4.0, 3.0, 2.0, 1.0):
            dv(lambda e: e.tensor_tensor(out=q[:], in0=q[:], in1=z[:], op=ALU.mult))
            dv(lambda e: e.tensor_scalar(out=q[:], in0=q[:], scalar1=1.0 / kk, scalar2=1.0, op0=ALU.mult, op1=ALU.add))
        dv(lambda e: e.tensor_copy(out=mag[:], in_=q[:]))

        iot = sb("iota", [128, 512])
        Tio = S.T("p4_iota")
        S.dma("sp", iot[:], k.inp["iota512"].partition_broadcast(128), writes=[Tio])
        Ct = sb("Ct", [128, 8, 512])
        St = sb("St", [128, 8, 512])
        TCt = [S.T(f"p4_Ct{m}") for m in range(8)]
        ang = [sb(f"ang{i}", [128, 512]) for i in range(2)]
        kf = [sb(f"kf{i}", [128, 512]) for i in range(2)]
        ki = [sb(f"ki{i}", [128, 512], mybir.dt.int32) for i in range(2)]
        Tang = [S.T(f"p4_ang{i}") for i in range(2)]
        ai = 0
        for m in range(8):
            for which in range(2):
                a_ = ai % 2
                ai += 1
                eng = "dve" if which == 0 else "pool"
                Ta = Tang[a_]
                A, KF, KI = ang[a_], kf[a_], ki[a_]
                off = 0.0 if which == 0 else PI / 2
                S.op("dve", lambda e: e.tensor_scalar(out=A[:], in0=iot[:], scalar1=th[:, m:m + 1], scalar2=off, op0=ALU.mult, op1=ALU.add), reads=[Tio, Tp], writes=[Ta])
                S.op("dve", lambda e: e.tensor_scalar(out=KI[:], in0=A[:], scalar1=1.0 / (2 * PI), scalar2=None, op0=ALU.mult), reads=[Ta], writes=[Ta])
                S.op("dve", lambda e: e.tensor_copy(out=KF[:], in_=KI[:]), reads=[Ta], writes=[Ta])
                S.op("dve", lambda e: e.scalar_tensor_tensor(out=A[:], in0=KF[:], scalar=-C1, in1=A[:], op0=ALU.mult, op1=ALU.add), reads=[Ta], writes=[Ta])
                S.op("dve", lambda e: e.scalar_tensor_tensor(out=A[:], in0=KF[:], scalar=-C2, in1=A[:], op0=ALU.mult, op1=ALU.add), reads=[Ta], writes=[Ta])
                S.op("dve", lambda e: e.tensor_scalar(out=KF[:], in0=A[:], scalar1=PI, scalar2=-2 * PI, op0=ALU.is_gt, op1=ALU.mult), reads=[Ta], writes=[Ta])
                S.op("dve", lambda e: e.tensor_tensor(out=A[:], in0=A[:], in1=KF[:], op=ALU.add), reads=[Ta], writes=[Ta])
                S.op("dve", lambda e: e.tensor_scalar(out=KF[:], in0=A[:], scalar1=-PI, scalar2=2 * PI, op0=ALU.is_lt, op1=ALU.mult), reads=[Ta], writes=[Ta])
                S.op("dve", lambda e: e.tensor_tensor(out=A[:], in0=A[:], in1=KF[:], op=ALU.add), reads=[Ta], writes=[Ta])
                S.op("dve", lambda e: e.tensor_scalar(out=A[:], in0=A[:], scalar1=PI, scalar2=-PI, op0=ALU.min, op1=ALU.max), reads=[Ta], writes=[Ta])
                dst = St if which == 0 else Ct
                S.op("act", lambda e: e.activation(out=dst[:, m, :], in_=A[:], func=AF.Sin), reads=[Ta], writes=[TCt[m]])
        c1, s1, ns1 = sb("c1", [128, 8]), sb("s1", [128, 8]), sb("ns1", [128, 8])
        TC1 = S.T("p4_c1")
        S.op("dve", lambda e: e.tensor_copy(out=c1[:], in_=Ct[:, :, 1]), reads=TCt, writes=[TC1])
        S.op("dve", lambda e: e.tensor_copy(out=s1[:], in_=St[:, :, 1]), reads=TCt, writes=[TC1])
        S.op("dve", lambda e: e.tensor_scalar(out=ns1[:], in0=s1[:], scalar1=-1.0, scalar2=None, op0=ALU.mult), reads=[TC1], writes=[TC1])
        lre, lim, den, nr, fre, fim, nfim, t8 = [sb(n, [128, 8]) for n in ("lre", "lim", "den", "nr", "fre", "fim", "nfim", "t8")]
        c512, s512, ns512, t9 = [sb(n, [128, 8]) for n in ("c512", "s512", "ns512", "t9")]
        S.op("dve", lambda e: e.tensor_tensor(out=c512[:], in0=Ct[:, :, 511], in1=c1[:], op=ALU.mult), reads=TCt + [TC1], writes=[TC1])
        S.op("dve", lambda e: e.tensor_tensor(out=t9[:], in0=St[:, :, 511], in1=s1[:], op=ALU.mult), reads=TCt + [TC1], writes=[TC1])
        S.op("dve", lambda e: e.tensor_tensor(out=c512[:], in0=c512[:], in1=t9[:], op=ALU.subtract), reads=[TC1], writes=[TC1])
        S.op("dve", lambda e: e.tensor_tensor(out=s512[:], in0=St[:, :, 511], in1=c1[:], op=ALU.mult), reads=TCt + [TC1], writes=[TC1])
        S.op("dve", lambda e: e.tensor_tensor(out=t9[:], in0=Ct[:, :, 511], in1=s1[:], op=ALU.mult), reads=TCt + [TC1], writes=[TC1])
        S.op("dve", lambda e: e.tensor_tensor(out=s512[:], in0=s512[:], in1=t9[:], op=ALU.add), reads=[TC1], writes=[TC1])
        S.op("dve", lambda e: e.tensor_scalar(out=ns512[:], in0=s512[:], scalar1=-1.0, scalar2=None, op0=ALU.mult), reads=[TC1], writes=[TC1])

        def d2(fn):
            S.op("dve", fn, reads=[Tp, Tp2, TC1], writes=[TC1])

        d2(lambda e: e.tensor_tensor(out=lre[:], in0=mag[:], in1=c1[:], op=ALU.mult))
        d2(lambda e: e.tensor_tensor(out=lim[:], in0=mag[:], in1=s1[:], op=ALU.mult))
        d2(lambda e: e.tensor_tensor(out=den[:], in0=are[:], in1=are[:], op=ALU.mult))
        d2(lambda e: e.tensor_tensor(out=t8[:], in0=aim[:], in1=aim[:], op=ALU.mult))
        d2(lambda e: e.tensor_tensor(out=den[:], in0=den[:], in1=t8[:], op=ALU.add))
        d2(lambda e: e.reciprocal(out=den[:], in_=den[:]))
        d2(lambda e: e.tensor_scalar(out=nr[:], in0=lre[:], scalar1=-1.0, scalar2=None, op0=ALU.add))
        d2(lambda e: e.tensor_tensor(out=fre[:], in0=nr[:], in1=are[:], op=ALU.mult))
        d2(lambda e: e.tensor_tensor(out=t8[:], in0=lim[:], in1=aim[:], op=ALU.mult))
        d2(lambda e: e.tensor_tensor(out=fre[:], in0=fre[:], in1=t8[:], op=ALU.add))
        d2(lambda e: e.tensor_tensor(out=fre[:], in0=fre[:], in1=den[:], op=ALU.mult))
        d2(lambda e: e.tensor_tensor(out=fim[:], in0=lim[:], in1=are[:], op=ALU.mult))
        d2(lambda e: e.tensor_tensor(out=t8[:], in0=nr[:], in1=aim[:], op=ALU.mult))
        d2(lambda e: e.tensor_tensor(out=fim[:], in0=fim[:], in1=t8[:], op=ALU.subtract))
        d2(lambda e: e.tensor_tensor(out=fim[:], in0=fim[:], in1=den[:], op=ALU.mult))
        d2(lambda e: e.tensor_scalar(out=nfim[:], in0=fim[:], scalar1=-1.0, scalar2=None, op0=ALU.mult))
        Mre = sb("Mre", [128, 8, 128], BF16)
        Mim = sb("Mim", [128, 8, 128], BF16)
        Cre = sb("Cre", [128, 8, 128], BF16)
        Cim = sb("Cim", [128, 8, 128], BF16)
        TM = S.T("p4_M")
        TCp = S.T("p4_Cp")
        pset = k.ps(es, "p4_pset", [128, 512], BF16)
        Tpset = S.T("p4_pset")
        with contextlib.ExitStack() as es2:
            zbr = k.sb(es2, "p4_zbr", [128, 8, 128], F32)
            zbi = k.sb(es2, "p4_zbi", [128, 8, 128], F32)
            Tz1, Tz2 = S.T("p4_zbr"), S.T("p4_zbi")
            tt = k.sb(es2, "p4_tt", [128, 128], F32)
            bbr = k.sb(es2, "p4_bbr", [128, 128], BF16)
            bbi = k.sb(es2, "p4_bbi", [128, 128], BF16)
            Tbb = S.T("p4_bb")
            S.dma("sp", zbr[:], k.inp["Zb_re"][l], writes=[Tz1])
            S.dma("sp", zbi[:], k.inp["Zb_im"][l], writes=[Tz2])
            for m in range(8):
                S.op("dve", lambda e: e.tensor_scalar(out=tt[:], in0=zbr[:, m, :], scalar1=fre[:, m:m + 1], scalar2=None, op0=ALU.mult), reads=[Tz1, TC1], writes=[Tbb])
                S.op("dve", lambda e: e.scalar_tensor_tensor(out=bbr[:], in0=zbi[:, m, :], scalar=nfim[:, m:m + 1], in1=tt[:], op0=ALU.mult, op1=ALU.add), reads=[Tz2, TC1, Tbb], writes=[Tbb])
                S.op("dve", lambda e: e.tensor_scalar(out=tt[:], in0=zbi[:, m, :], scalar1=fre[:, m:m + 1], scalar2=None, op0=ALU.mult), reads=[Tz2, TC1, Tbb], writes=[Tbb])
                S.op("dve", lambda e: e.scalar_tensor_tensor(out=bbi[:], in0=zbr[:, m, :], scalar=fim[:, m:m + 1], in1=tt[:], op0=ALU.mult, op1=ALU.add), reads=[Tz1, TC1, Tbb], writes=[Tbb])
                S.op("pe", lambda e: e.transpose(out=pset[:, 0:128], in_=bbr[:], identity=k.identb[:]), reads=[Tbb, k.Tidb], writes=[Tpset])
                S.op("pe", lambda e: e.transpose(out=pset[:, 128:256], in_=bbi[:], identity=k.identb[:]), reads=[Tbb, k.Tidb], writes=[Tpset])
                S.op("act", lambda e: e.activation(out=Mre[:, m, :], in_=pset[:, 0:128], func=AF.Copy), reads=[Tpset], writes=[TM])
                S.op("act", lambda e: e.activation(out=Mim[:, m, :], in_=pset[:, 128:256], func=AF.Copy), reads=[Tpset], writes=[TM])
            S.dma("sp", zbr[:], k.inp["Cp_re"][l], writes=[Tz1])
            S.dma("sp", zbi[:], k.inp["Cp_im"][l], writes=[Tz2])
            S.op("dve", lambda e: e.tensor_copy(out=Cre[:], in_=zbr[:]), reads=[Tz1], writes=[TCp])
            S.op("dve", lambda e: e.tensor_scalar(out=Cim[:], in0=zbi[:], scalar1=-1.0, scalar2=None, op0=ALU.mult), reads=[Tz2], writes=[TCp])
            S.barrier()
        dvec = sb("dvec", [128, 2])
        Td = S.T("p4_d")
        S.dma("sp", dvec[:], k.inp["ssm_d_t"][l], writes=[Td])
        wgs = sb("wgs", [128, 2, 512])
        wgb = sb("wgb", [128, 2, 512], BF16)
        Twg = S.T("p4_wg")
        S.dma("sp", wgs[:], k.inp["ssm_w_glu"][l].rearrange("(mt p) n -> p mt n", p=128), writes=[Twg])
        S.op("dve", lambda e: e.tensor_copy(out=wgb[:], in_=wgs[:]), reads=[Twg], writes=[Twg])
        U = sb("U", [128, 2, SEQ], BF16)
        TU = [S.T(f"p4_U{i}") for i in range(2)]
        NW = 3
        NPW = 2
        pw = [[k.ps(es, f"p4_pw{i}_{c}", [128, 512], F32) for c in range(2)] for i in range(NPW)]
        Tpw = [[S.T(f"p4_pw{i}_{c}") for c in range(2)] for i in range(NPW)]
        py = [k.ps(es, f"p4_py{i}", [128, 512], F32) for i in range(2)]
        Tpy = [S.T(f"p4_py{i}") for i in range(2)]
        pgl = k.ps(es, "p4_pgl", [128, 512], F32)
        Tpgl = S.T("p4_pgl")
        ta = [sb(f"ta{i}", [128, 512]) for i in range(NW)]
        tb_ = [sb(f"tb{i}", [128, 512]) for i in range(NW)]
        tc_ = [sb(f"tc{i}", [128, 512]) for i in range(NW)]
        td_ = [sb(f"td{i}", [128, 512]) for i in range(NW)]
        te_ = [sb(f"te{i}", [128, 512]) for i in range(NW)]
        tf_ = [sb(f"tf{i}", [128, 512]) for i in range(NW)]
        tg_ = [sb(f"tg{i}", [128, 512]) for i in range(NW)]
        th_ = [sb(f"th{i}", [128, 512]) for i in range(NW)]
        Twa = [S.T(f"p4_wa{i}") for i in range(NW)]
        Twc = [S.T(f"p4_wc{i}") for i in range(NW)]
        Twe = [S.T(f"p4_we{i}") for i in range(NW)]
        Twgg = [S.T(f"p4_wgg{i}") for i in range(NW)]
        Tvr = [S.T(f"p4_vr{i}") for i in range(NW)]
        Tvi = [S.T(f"p4_vi{i}") for i in range(NW)]
        Tsr = [S.T(f"p4_sr{i}") for i in range(NW)]
        Tsi = [S.T(f"p4_si{i}") for i in range(NW)]
        vre = [sb(f"vre{i}", [128, 512]) for i in range(NW)]
        vim = [sb(f"vim{i}", [128, 512]) for i in range(NW)]
        sre = [sb(f"sre{i}", [128, 512]) for i in range(NW)]
        sim_ = [sb(f"sim{i}", [128, 512]) for i in range(NW)]
        xre = [sb(f"xre{i}", [128, 512], BF16) for i in range(NW)]
        xim = [sb(f"xim{i}", [128, 512], BF16) for i in range(NW)]
        Twk = [S.T(f"p4_wk{i}") for i in range(NW)]
        Tv = [S.T(f"p4_v{i}") for i in range(NW)]
        Ts = [S.T(f"p4_s{i}") for i in range(NW)]
        Tx = [S.T(f"p4_x{i}") for i in range(NW)]
        init = sb("init", [128, 8, 2])
        Tinit = [S.T(f"p4_init{m}") for m in range(8)]
        xl = sb("xl", [128, 8, 2])
        i4 = sb("i4", [128, 8, 2])
        yf = [sb(f"yf{i}", [128, 512]) for i in range(2)]
        gT = [sb(f"gT{i}", [128, 512], BF16) for i in range(2)]
        TgT = [S.T(f"p4_gT{i}") for i in range(2)]
        Tyf = [S.T(f"p4_yf{i}") for i in range(2)]
        sgl = [sb(f"sgl{i}", [128, 256]) for i in range(2)]
        Tsgl = [S.T(f"p4_sgl{i}") for i in range(2)]
        yo = [sb(f"yo{i}", [128, 4, 256]) for i in range(2)]
        Tyo = [S.T(f"p4_yo{i}") for i in range(2)]
        wi = 0
        gi = 0
        for b in range(NB):
            for mt in range(2):
                S.dma("sp", U[:, mt, :], k.scr["usT"][b, mt * 128:(mt + 1) * 128, :], writes=[TU[mt]])
            for m in range(8):
                S.op("pool", lambda e: e.memset(init[:, m, :], 0.0), writes=[Tinit[m]])
            units = [(g, mt, m) for g in range(NG) for mt in range(2) for m in range(4 * mt, 4 * mt + 4)]
            ustate = {}

            def stageA(un):
                nonlocal wi
                g, mt, m = un
                tsl = slice(g * 512, (g + 1) * 512)
                w = wi % NW
                pwi = wi % NPW
                wi += 1
                ustate[un] = w
                Cm, Sm = Ct[:, m, :], St[:, m, :]
                S.op("pe", lambda e: e.matmul(pw[pwi][0][:], lhsT=Mre[:, m, :], rhs=U[:, mt, tsl], start=True, stop=True), reads=[TM, TU[mt]], writes=[Tpw[pwi][0]])
                S.op("pe", lambda e: e.matmul(pw[pwi][1][:], lhsT=Mim[:, m, :], rhs=U[:, mt, tsl], start=True, stop=True), reads=[TM, TU[mt]], writes=[Tpw[pwi][1]])
                S.op("dve", lambda e: e.tensor_tensor(out=ta[w][:], in0=pw[pwi][0][:], in1=Cm, op=ALU.mult), reads=[Tpw[pwi][0], TCt[m]], writes=[Twa[w]])
                S.op("dve", lambda e: e.tensor_tensor(out=tb_[w][:], in0=pw[pwi][1][:], in1=Sm, op=ALU.mult), reads=[Tpw[pwi][1], TCt[m]], writes=[Twa[w]])
                S.op("dve", lambda e: e.tensor_tensor(out=tc_[w][:], in0=pw[pwi][1][:], in1=Cm, op=ALU.mult), reads=[Tpw[pwi][1], TCt[m]], writes=[Twc[w]])
                S.op("dve", lambda e: e.tensor_tensor(out=td_[w][:], in0=pw[pwi][0][:], in1=Sm, op=ALU.mult), reads=[Tpw[pwi][0], TCt[m]], writes=[Twc[w]])
                S.op("pool", lambda e: e.tensor_tensor(out=vre[w][:], in0=ta[w][:], in1=tb_[w][:], op=ALU.add), reads=[Twa[w]], writes=[Tvr[w]])
                S.op("pool", lambda e: e.tensor_tensor(out=vim[w][:], in0=tc_[w][:], in1=td_[w][:], op=ALU.subtract), reads=[Twc[w]], writes=[Tvi[w]])

            def stageB(un):
                nonlocal gi
                g, mt, m = un
                tsl = slice(g * 512, (g + 1) * 512)
                w = ustate.pop(un)
                yb = (g * 2 + mt) % 2
                Cm, Sm = Ct[:, m, :], St[:, m, :]
                S.op("dve", lambda e: e.tensor_tensor_scan(out=sre[w][:], data0=mag[:, m:m + 1].broadcast_to([128, 512]), data1=vre[w][:], initial=init[:, m, 0:1], op0=ALU.mult, op1=ALU.add),
                     reads=[Tvr[w], Tp, Tinit[m]], writes=[Tsr[w]])
                S.op("dve", lambda e: e.tensor_tensor_scan(out=sim_[w][:], data0=mag[:, m:m + 1].broadcast_to([128, 512]), data1=vim[w][:], initial=init[:, m, 1:2], op0=ALU.mult, op1=ALU.add),
                     reads=[Tvi[w], Tp, Tinit[m]], writes=[Tsi[w]])
                S.op("dve", lambda e: e.tensor_scalar(out=i4[:, m, 0:1], in0=sim_[w][:, 511:512], scalar1=ns512[:, m:m + 1], scalar2=None, op0=ALU.mult), reads=[Tsi[w], TC1], writes=[Tinit[m]])
                S.op("dve", lambda e: e.tensor_scalar(out=i4[:, m, 1:2], in0=sre[w][:, 511:512], scalar1=s512[:, m:m + 1], scalar2=None, op0=ALU.mult), reads=[Tsr[w], TC1], writes=[Tinit[m]])
                S.op("dve", lambda e: e.scalar_tensor_tensor(out=init[:, m, 0:1], in0=sre[w][:, 511:512], scalar=c512[:, m:m + 1], in1=i4[:, m, 0:1], op0=ALU.mult, op1=ALU.add), reads=[Tsr[w], TC1, Tinit[m]], writes=[Tinit[m]])
                S.op("dve", lambda e: e.scalar_tensor_tensor(out=init[:, m, 1:2], in0=sim_[w][:, 511:512], scalar=c512[:, m:m + 1], in1=i4[:, m, 1:2], op0=ALU.mult, op1=ALU.add), reads=[Tsi[w], TC1, Tinit[m]], writes=[Tinit[m]])
                S.op("pool", lambda e: e.tensor_tensor(out=te_[w][:], in0=sre[w][:], in1=Cm, op=ALU.mult), reads=[Tsr[w], TCt[m]], writes=[Twe[w]])
                S.op("pool", lambda e: e.tensor_tensor(out=tf_[w][:], in0=sim_[w][:], in1=Sm, op=ALU.mult), reads=[Tsi[w], TCt[m]], writes=[Twe[w]])
                S.op("pool", lambda e: e.tensor_tensor(out=xre[w][:], in0=te_[w][:], in1=tf_[w][:], op=ALU.subtract), reads=[Twe[w]], writes=[Tx[w]])
                S.op("dve", lambda e: e.tensor_tensor(out=tg_[w][:], in0=sim_[w][:], in1=Cm, op=ALU.mult), reads=[Tsi[w], TCt[m]], writes=[Twgg[w]])
                S.op("pool", lambda e: e.tensor_tensor(out=th_[w][:], in0=sre[w][:], in1=Sm, op=ALU.mult), reads=[Tsr[w], TCt[m]], writes=[Twgg[w]])
                S.op("pool", lambda e: e.tensor_tensor(out=xim[w][:], in0=tg_[w][:], in1=th_[w][:], op=ALU.add), reads=[Twgg[w]], writes=[Tx[w]])
                first = (m == 4 * mt)
                last = (m == 4 * mt + 3)
                S.op("pe", lambda e: e.matmul(py[yb][:], lhsT=Cre[:, m, :], rhs=xre[w][:], start=first, stop=False), reads=[TCp, Tx[w]], writes=[Tpy[yb]])
                S.op("pe", lambda e: e.matmul(py[yb][:], lhsT=Cim[:, m, :], rhs=xim[w][:], start=False, stop=last), reads=[TCp, Tx[w]], writes=[Tpy[yb]])
                if last:
                    S.op("dve", lambda e: e.scalar_tensor_tensor(out=yf[mt][:], in0=U[:, mt, tsl], scalar=dvec[:, mt:mt + 1], in1=py[yb][:], op0=ALU.mult, op1=ALU.add),
                         reads=[TU[mt], Td, Tpy[yb]], writes=[Tyf[mt]])
                    S.op("act", lambda e: e.activation(out=gT[mt][:], in_=yf[mt][:], func=AF.Gelu_apprx_tanh), reads=[Tyf[mt]], writes=[TgT[mt]])
                if last and mt == 1:
                    yi = gi % 2
                    gi += 1
                    for j in range(4):
                        for mt2 in range(2):
                            S.op("pe", lambda e: e.matmul(pgl[:], lhsT=gT[mt2][:, j * 128:(j + 1) * 128], rhs=wgb[:, mt2, :], start=(mt2 == 0), stop=(mt2 == 1)),
                                 reads=[TgT[mt2], Twg], writes=[Tpgl])
                        sj = j % 2
                        S.op("act", lambda e: e.activation(out=sgl[sj][:], in_=pgl[:, 256:512], func=AF.Sigmoid), reads=[Tpgl], writes=[Tsgl[sj]])
                        S.op("dve", lambda e: e.tensor_tensor(out=yo[yi][:, j, :], in0=pgl[:, 0:256], in1=sgl[sj][:], op=ALU.mult), reads=[Tpgl, Tsgl[sj]], writes=[Tyo[yi]])
                    ydst = k.scr["ycat"][b].rearrange("(n p) c -> p n c", p=128)
                    k.store(ydst[:, 4 * g:4 * g + 4, 512:768], yo[yi][:], [Tyo[yi]])

            for un in units:
                stageA(un)
                stageB(un)
        S.barrier()


K.phase4 = phase4
PHASES.extend(["phase4", "phase6a", "phase6b"])
```

```python
import contextlib
import numpy as np
import ml_dtypes
import concourse.bass as bass
import concourse.mybir as mybir
from concourse.bass_utils import run_bass_kernel_spmd

F32 = mybir.dt.float32
BF16 = mybir.dt.bfloat16
AF = mybir.ActivationFunctionType
ALU = mybir.AluOpType
AX = mybir.AxisListType

SEM_L = 20000
DMA_L = 16 * 1200
NB = 2
SEQ = 4096
D = 1024
NT = SEQ // 128
NG = SEQ // 512
DFF = 2816
NFF = DFF // 128
EPS = 1e-6
NEG = -30000.0
L = 2


class T:
    __slots__ = ("name", "w", "r", "sem", "semv", "semcls")

    def __init__(self, name=""):
        self.name = name
        self.w = None
        self.r = {}
        self.sem = None
        self.semv = 0
        self.semcls = None


class Sched:
    def __init__(self, nc):
        self.nc = nc
        self.h = {"pe": nc.tensor, "act": nc.scalar, "dve": nc.vector,
                  "pool": nc.gpsimd, "sp": nc.sync}
        self.cnt = {k: 0 for k in self.h}
        self.sems = {k: [] for k in self.h}
        self.waited = {}
        self.same = {"dve", "act", "pool"}
        self.nsem = 0
        self.ninst = 0
        self.all_T = []

    def T(self, name=""):
        t = T(name)
        self.all_T.append(t)
        return t

    def new_sem(self, name):
        self.nsem += 1
        return self.nc.alloc_semaphore(name + f"_{self.nsem}")

    def dma_sem(self, d, cls):
        if not hasattr(self, "free_sems"):
            self.free_sems = {"sw": [], "hw": []}
        free = self.free_sems[cls]
        if free:
            d.sem, d.semv = free.pop()
        else:
            d.sem, d.semv = self.new_sem("d" + cls), 0
        d.semcls = cls

    def _eng_sem(self, e):
        idx = self.cnt[e] // SEM_L
        while len(self.sems[e]) <= idx:
            self.sems[e].append(self.new_sem(f"s_{e}"))
        return self.sems[e][idx]

    def _wait(self, e, tok):
        if tok is None:
            return
        sem, val, src = tok
        if src == e and e not in self.same:
            return
        key = (e, id(sem))
        if self.waited.get(key, 0) >= val:
            return
        self.waited[key] = val
        self.h[e].wait_ge(sem, val)

    def _deps(self, e, reads, writes):
        for t in reads:
            self._wait(e, t.w)
        for t in writes:
            self._wait(e, t.w)
            for tok in t.r.values():
                self._wait(e, tok)

    def op(self, e, fn, reads=(), writes=()):
        self._deps(e, reads, writes)
        sem = self._eng_sem(e)
        val = self.cnt[e] % SEM_L + 1
        fn(self.h[e]).then_inc(sem, 1)
        self.cnt[e] += 1
        self.ninst += 1
        tok = (sem, val, e)
        for t in reads:
            t.r[id(sem)] = tok
        for t in writes:
            t.w = tok
            t.r = {}
        return tok

    def dma(self, q, out, in_, reads=(), writes=(), **kw):
        self._deps(q, reads, writes)
        d = writes[0]
        cls = "sw" if q == "pool" else "hw"
        if d.sem is None or d.semv >= DMA_L or d.semcls != cls:
            self.dma_sem(d, cls)
        d.semv += 16
        self.h[q].dma_start(out=out, in_=in_, **kw).then_inc(d.sem, 16)
        self.ninst += 1
        tok = (d.sem, d.semv, "dma")
        for t in reads:
            t.r[id(d.sem)] = tok
        d.w = tok
        d.r = {}
        return tok

    def barrier(self):
        e0 = "dve"
        for t in self.all_T:
            self._wait(e0, t.w)
            for tok in t.r.values():
                self._wait(e0, tok)
            t.r = {}
        for e in self.h:
            if self.cnt[e] > 0 and e != e0:
                idx = (self.cnt[e] - 1) // SEM_L
                self._wait(e0, (self.sems[e][idx], (self.cnt[e] - 1) % SEM_L + 1, e))
        if not hasattr(self, "Tbar"):
            self.Tbar = T("bar")
        tok = self.op(e0, lambda h: h.memset(self.bar_tile[:], 0.0), writes=[self.Tbar])
        for e in self.h:
            if e != e0:
                self._wait(e, tok)
        if not hasattr(self, "free_sems"):
            self.free_sems = {"sw": [], "hw": []}
        for t in self.all_T:
            t.w = None
            t.r = {}
            if t.sem is not None:
                if t.semv < DMA_L - 4096:
                    self.free_sems[t.semcls].append((t.sem, t.semv))
                t.sem = None
                t.semv = 0


class K:
    def __init__(self, debug=False, phases=None, nlayers=L):
        self.debug = debug
        self.phases = phases
        self.nlayers = nlayers
        nc = bass.Bass("TRN2", target_bir_lowering=False)
        self.nc = nc
        self.S = Sched(nc)
        self.inp = {}
        self.scr = {}
        self.S.bar_tile = nc.alloc_sbuf_tensor("bar_tile", [128, 8], F32)
        self.st_i = 0
        self.ld_i = 0

    def din(self, name, shape, dt=F32):
        self.inp[name] = self.nc.dram_tensor(name, list(shape), dt, kind="ExternalInput").ap()
        return self.inp[name]

    def dscr(self, name, shape, dt=F32, out=False):
        kind = "ExternalOutput" if (self.debug or out) else "Internal"
        self.scr[name] = self.nc.dram_tensor(name, list(shape), dt, kind=kind).ap()
        return self.scr[name]

    def sb(self, es, name, shape, dt=F32):
        self.uid = getattr(self, "uid", 0) + 1
        return es.enter_context(self.nc.sbuf_tensor(f"sb{self.uid}_{name}", list(shape), dt))

    def ps(self, es, name, shape, dt=F32):
        self.uid = getattr(self, "uid", 0) + 1
        return es.enter_context(self.nc.psum_tensor(f"ps{self.uid}_{name}", list(shape), dt))

    def store(self, out, in_, reads, q=None):
        import os
        q = q or os.environ.get("STQ", "pool")
        if not hasattr(self, "_st"):
            self._st = [self.S.T(f"st{i}") for i in range(8)]
        i = self.st_i % 8
        self.st_i += 1
        self.S.dma(q, out, in_, reads=reads, writes=[self._st[i]])

    def load(self, out, in_, Tt, q="sp"):
        self.S.dma(q, out, in_, writes=[Tt])


def _rope_tables():
    half = 32
    freqs = (np.float32(10000.0) ** (-np.arange(half, dtype=np.float32) / np.float32(half))).astype(np.float32)
    pos = np.arange(SEQ, dtype=np.float32)
    ang = (pos[:, None] * freqs[None, :]).astype(np.float32)
    cos = np.cos(ang).astype(np.float32).T
    sin = np.sin(ang).astype(np.float32).T
    cosT = np.tile(cos, (4, 1))
    sinT = np.tile(sin, (4, 1))
    posc = (np.arange(256, dtype=np.float32) * 16 + 31).astype(np.float32)
    angc = (posc[:, None] * freqs[None, :]).astype(np.float32)
    cosC = np.tile(np.cos(angc).astype(np.float32).T, (2, 1))
    sinC = np.tile(np.sin(angc).astype(np.float32).T, (2, 1))
    return cosT, sinT, cosC, sinC


def _consts():
    c = {}
    cosT, sinT, cosC, sinC = _rope_tables()
    c["cosT"], c["sinT"], c["cosC"], c["sinC"] = cosT, sinT, cosC, sinC
    c["identf"] = np.eye(128, dtype=np.float32)
    c["identb"] = np.eye(128, dtype=np.float32).astype(ml_dtypes.bfloat16)
    key = np.arange(SEQ)
    c["Erows"] = (key[None, :] // 64 == np.arange(64)[:, None]).astype(np.float32).astype(ml_dtypes.bfloat16)
    i = np.arange(256)
    c["cmask"] = np.where((i[:, None] * 16 + 31) <= key[None, :], 0.0, NEG).astype(np.float32).astype(ml_dtypes.bfloat16)
    v = np.arange(128)[:, None]
    u = np.arange(512)[None, :]
    wm = np.zeros((8, 128, 512), np.float32)
    for ri, r in enumerate(range(-4, 4)):
        kk = 128 * r + v
        wm[ri] = np.where((kk <= u) & (kk > u - 512), 0.0, NEG)
    c["wmask"] = wm.astype(ml_dtypes.bfloat16)
    cm = np.zeros((4, 128, 512), np.float32)
    for r in range(4):
        cm[r] = np.where(128 * r + v <= u, 0.0, NEG)
    c["dmask"] = cm.astype(ml_dtypes.bfloat16)
    t = np.arange(SEQ)[:, None]
    j = np.arange(64)[None, :]
    cur = t // 64
    forced = (j == 0) | (j == cur) | (j == cur - 1)
    causal = j * 64 <= t
    add = np.where(forced, 1e9, np.where(causal, 0.0, -1e9)).astype(np.float32)
    c["addtab"] = np.ascontiguousarray(add.reshape(NT, 128, 64).transpose(1, 0, 2))
    ii = np.arange(256)[:, None]
    ov = ((ii * 16 < (j + 1) * 64) & (ii * 16 + 32 > j * 64) & (ii < 255)).astype(np.float32)
    c["overlap"] = ov.astype(ml_dtypes.bfloat16)
    return c


def declare_io(self):
    k = self
    k.din("x", [NB, SEQ, D])
    k.din("w_in_p", [L, D, 2072])
    k.din("norm_mix_t", [L, 128, 8])
    k.din("norm_final", [D])
    for n, sh, dt in [("cosT", [128, SEQ], F32), ("sinT", [128, SEQ], F32), ("cosC", [64, 256], F32),
                      ("sinC", [64, 256], F32), ("identf", [128, 128], F32), ("identb", [128, 128], BF16),
                      ("Erows", [64, SEQ], BF16), ("cmask", [256, SEQ], BF16), ("wmask", [8, 128, 512], BF16),
                      ("dmask", [4, 128, 512], BF16), ("addtab", [128, NT, 64], F32), ("overlap", [256, 64], BF16)]:
        k.din(n, sh, dt)
    k.dscr("qT", [NB, 512, SEQ], BF16)
    k.dscr("ksT", [NB, 128, SEQ], BF16)
    k.dscr("kwT", [NB, 128, SEQ], BF16)
    k.dscr("kcrT", [NB, 128, SEQ], BF16)
    k.dscr("vcrT", [NB, 128, SEQ], BF16)
    k.dscr("ucT", [NB, 256, SEQ], BF16)
    k.dscr("usT", [NB, 256, SEQ], BF16)
    k.dscr("vsw", [NB, SEQ, 256], BF16)
    k.dscr("gat", [NB, SEQ, 32], F32)
    k.dscr("ycat", [NB, SEQ, D], F32)
    k.dscr("xres", [NB, SEQ, D], F32)
    k.dscr("h2T", [NB, D, SEQ], BF16)
    k.dscr("kcT", [NB, 2, 64, 256], BF16)
    k.dscr("vc", [NB, 2, 256, 64], BF16)
    for n in ["cmp_k_w1", "cmp_v_w1"]:
        k.din(n, [L, 2048, 256])
    for n in ["cmp_k_w2", "cmp_v_w2"]:
        k.din(n, [L, 256, 64])
    k.din("pe_t", [L, 2, 128, 16])
    k.din("wdw_t", [L, 256, 31])
    for n in ["ssm_are_t", "ssm_aim_t", "ssm_ldt_t"]:
        k.din(n, [L, 128, 8])
    for n in ["Zb_re", "Zb_im", "Cp_re", "Cp_im"]:
        k.din(n, [L, 128, 8, 128])
    k.din("ssm_d_t", [L, 128, 2])
    k.din("ssm_w_glu", [L, 256, 512])
    k.din("iota512", [512])
    k.din("gcat_t", [L, 128, 8])
    k.din("norm_ffn_t", [L, 128, 8])
    k.din("w_out", [L, D, D])
    k.din("w_gate", [L, D, DFF])
    k.din("w_up", [L, D, DFF])
    k.din("w_down", [L, DFF, D])
    for n in ["conv_b_dw", "conv_b_pw", "conv_ln_g", "conv_ln_b"]:
        k.din(n, [L, 256])
    k.din("conv_w_pw", [L, 256, 256])
    k.dscr("out", [NB, SEQ, D], F32, out=True)


def load_consts(self, es):
    k, S = self, self.S
    k.identf = k.sb(es, "identf", [128, 128], F32)
    k.identb = k.sb(es, "identb", [128, 128], BF16)
    k.Tid = S.T("ident")
    S.dma("sp", k.identf[:], k.inp["identf"], writes=[k.Tid])
    k.Tidb = S.T("identb")
    S.dma("sp", k.identb[:], k.inp["identb"], writes=[k.Tidb])
    k.ones_b = k.sb(es, "ones_b", [128, 128], BF16)
    k.Tones = S.T("ones")
    S.op("dve", lambda e: e.memset(k.ones_b[:], 1.0), writes=[k.Tones])


def phase1(self, l):
    k, S, nc = self, self.S, self.nc
    with contextlib.ExitStack() as es:
        NCOL = 2840
        W = k.sb(es, "p1_W", [128, 8, NCOL], BF16)
        TW = [S.T(f"p1_W{i}") for i in range(8)]
        stage = [k.sb(es, f"p1_stage{i}", [128, 2072], F32) for i in range(2)]
        Tstage = [S.T(f"p1_stage{i}") for i in range(2)]
        gain = k.sb(es, "p1_gain", [128, 8], F32)
        gq = k.sb(es, "p1_gq", [128, 8], F32)
        ngq = k.sb(es, "p1_ngq", [128, 8], F32)
        ngain = k.sb(es, "p1_ngain", [128, 8], F32)
        Tgain = S.T("p1_gain")
        S.dma("sp", gain[:], k.inp["norm_mix_t"][l], writes=[Tgain])
        S.op("dve", lambda e: e.tensor_scalar(out=ngain[:], in0=gain[:], scalar1=-1.0, scalar2=None, op0=ALU.mult), reads=[Tgain], writes=[Tgain])
        S.op("dve", lambda e: e.tensor_scalar(out=gq[:], in0=gain[:], scalar1=0.125, scalar2=None, op0=ALU.mult), reads=[Tgain], writes=[Tgain])
        S.op("dve", lambda e: e.tensor_scalar(out=ngq[:], in0=gain[:], scalar1=-0.125, scalar2=None, op0=ALU.mult), reads=[Tgain], writes=[Tgain])
        wsrc = k.inp["w_in_p"][l].rearrange("(kc p) n -> p kc n", p=128)
        engs = ["dve", "pool"]
        ei = 0

        def ts(out, in_, sc):
            nonlocal ei
            e_ = engs[kc % 2]
            S.op(e_, lambda e: e.tensor_scalar(out=out, in0=in_, scalar1=sc, scalar2=None, op0=ALU.mult),
                 reads=[Tstage[kc % 2], Tgain], writes=[TW[kc]])

        for kc in range(8):
            st = stage[kc % 2]
            S.dma("sp", st[:], wsrc[:, kc, :], writes=[Tstage[kc % 2]])
            g1, ng1, q1, nq1 = gain[:, kc:kc + 1], ngain[:, kc:kc + 1], gq[:, kc:kc + 1], ngq[:, kc:kc + 1]
            ts(W[:, kc, 0:512], st[:, 0:512], q1)
            sv = st[:, 0:512].rearrange("p (h t f) -> p h t f", h=8, t=2)
            dv = W[:, kc, 512:1024].rearrange("p (h t f) -> p h t f", h=8, t=2)
            ts(dv[:, :, 0, :], sv[:, :, 1, :], nq1)
            ts(dv[:, :, 1, :], sv[:, :, 0, :], q1)
            for (s0, d0) in [(512, 1024), (640, 1280)]:
                ts(W[:, kc, d0:d0 + 128], st[:, s0:s0 + 128], g1)
                sv = st[:, s0:s0 + 128].rearrange("p (h t f) -> p h t f", h=2, t=2)
                dv = W[:, kc, d0 + 128:d0 + 256].rearrange("p (h t f) -> p h t f", h=2, t=2)
                ts(dv[:, :, 0, :], sv[:, :, 1, :], ng1)
                ts(dv[:, :, 1, :], sv[:, :, 0, :], g1)
            ts(W[:, kc, 1536:2840], st[:, 768:2072], g1)

        import os
        STOP = os.environ.get("P1STOP", "")
        if STOP == "w":
            S.barrier()
            return
        xg = [k.sb(es, f"p1_xg{i}", [128, 4, D], F32) for i in range(2)]
        Txg = [S.T(f"p1_xg{i}") for i in range(2)]
        junk = k.sb(es, "p1_junk", [128, D], BF16)
        Tjunk = S.T("p1_junk")
        ss = k.sb(es, "p1_ss", [128, 4], F32)
        rstd = k.sb(es, "p1_rstd", [128, 4], F32)
        Tss = S.T("p1_ss")
        hb = k.sb(es, "p1_hb", [128, 4, D], BF16)
        Thb = [S.T(f"p1_hb{i}") for i in range(4)]
        hT = [k.sb(es, f"p1_hT{i}", [128, 8, 512], BF16) for i in range(2)]
        ThT = [[S.T(f"p1_hT{i}_{c}") for c in range(8)] for i in range(2)]
        cs = [k.sb(es, f"p1_cos{i}", [128, 512], F32) for i in range(2)]
        sn = [k.sb(es, f"p1_sin{i}", [128, 512], F32) for i in range(2)]
        Tcs = [S.T(f"p1_cs{i}") for i in range(2)]
        pT = [k.ps(es, f"p1_pT{i}", [128, 512], BF16) for i in range(2)]
        TpT = [S.T(f"p1_pT{i}") for i in range(2)]
        pm = [k.ps(es, f"p1_pm{i}", [128, 512], F32) for i in range(4)]
        Tpm = [S.T(f"p1_pm{i}") for i in range(4)]
        pk = [k.ps(es, f"p1_pk{i}", [128, 512], F32) for i in range(2)]
        Tpk = [S.T(f"p1_pk{i}") for i in range(2)]
        NE = 6
        t1 = [k.sb(es, f"p1_t1_{i}", [128, 512], F32) for i in range(NE)]
        t2 = [k.sb(es, f"p1_t2_{i}", [128, 512], F32) for i in range(NE)]
        ob = [k.sb(es, f"p1_ob{i}", [128, 512], BF16) for i in range(NE)]
        Tt1 = [S.T(f"p1_t1_{i}") for i in range(NE)]
        Tt2 = [S.T(f"p1_t2_{i}") for i in range(NE)]
        Tob = [S.T(f"p1_ob{i}") for i in range(NE)]
        Ttg_pre = [S.T(f"p1_tg{i}") for i in range(2)]
        tk = [k.sb(es, f"p1_tk{i}", [128, 256], BF16) for i in range(2)]
        tg = [k.sb(es, f"p1_tg{i}", [128, 32], F32) for i in range(2)]
        for i in range(2):
            S.op("dve", lambda e: e.memset(tg[i][:], 0.0), writes=[Ttg_pre[i]])
        Ttk = [S.T(f"p1_tk{i}") for i in range(2)]
        Ttg = Ttg_pre
        xin = k.inp["x"] if l == 0 else k.scr["xres"]
        gi = 0
        ecnt = 0
        pmi = 0
        for b in range(NB):
            for g in range(NG):
                u = gi % 2
                gi += 1
                tsl = slice(g * 512, (g + 1) * 512)
                S.dma("sp", xg[u][:], xin[b, tsl, :].rearrange("(j p) d -> p j d", p=128), writes=[Txg[u]])
                S.dma("sp", cs[u][:], k.inp["cosT"][:, tsl], writes=[Tcs[u]])
                S.dma("sp", sn[u][:], k.inp["sinT"][:, tsl], writes=[Tcs[u]])
                for j in range(4):
                    S.op("act", lambda e: e.activation(out=junk[:], in_=xg[u][:, j, :], func=AF.Square, accum_out=ss[:, j:j + 1]),
                         reads=[Txg[u]], writes=[Tjunk, Tss])
                S.op("dve", lambda e: e.tensor_scalar(out=rstd[:], in0=ss[:], scalar1=1.0 / D, scalar2=EPS, op0=ALU.mult, op1=ALU.add), reads=[Tss], writes=[Tss])
                S.op("act", lambda e: e.activation(out=rstd[:], in_=rstd[:], func=AF.Sqrt), reads=[Tss], writes=[Tss])
                S.op("dve", lambda e: e.reciprocal(out=rstd[:], in_=rstd[:]), reads=[Tss], writes=[Tss])
                for j in range(4):
                    S.op("pool" if j % 2 else "dve", lambda e: e.tensor_scalar(out=hb[:, j, :], in0=xg[u][:, j, :], scalar1=rstd[:, j:j + 1], scalar2=None, op0=ALU.mult),
                         reads=[Txg[u], Tss], writes=[Thb[j]])
                if STOP == "n":
                    continue
                for kc in range(8):
                    pu = kc % 2
                    for j in range(4):
                        S.op("pe", lambda e: e.transpose(out=pT[pu][:, j * 128:(j + 1) * 128], in_=hb[:, j, kc * 128:(kc + 1) * 128], identity=k.identb[:]),
                             reads=[Thb[j], k.Tidb], writes=[TpT[pu]])
                    if kc % 2:
                        S.op("act", lambda e: e.activation(out=hT[u][:, kc, :], in_=pT[pu][:], func=AF.Copy), reads=[TpT[pu]], writes=[ThT[u][kc]])
                    else:
                        S.op("dve", lambda e: e.tensor_copy(out=hT[u][:, kc, :], in_=pT[pu][:]), reads=[TpT[pu]], writes=[ThT[u][kc]])

                if STOP == "t":
                    continue

                def mm(c):
                    nonlocal pmi
                    i = pmi % 4
                    pmi += 1
                    for kc in range(8):
                        S.op("pe", lambda e: e.matmul(pm[i][:], lhsT=W[:, kc, c * 128:(c + 1) * 128], rhs=hT[u][:, kc, :], start=(kc == 0), stop=(kc == 7)),
                             reads=[TW[kc], ThT[u][kc]], writes=[Tpm[i]])
                    return i

                def roped(cx, cr, dst):
                    nonlocal ecnt
                    ix = mm(cx)
                    ir = mm(cr)
                    n = ecnt % NE
                    ecnt += 1
                    S.op("dve", lambda e: e.tensor_tensor(out=t1[n][:], in0=pm[ix][:], in1=cs[u][:], op=ALU.mult), reads=[Tpm[ix], Tcs[u]], writes=[Tt1[n]])
                    S.op("dve", lambda e: e.tensor_tensor(out=t2[n][:], in0=pm[ir][:], in1=sn[u][:], op=ALU.mult), reads=[Tpm[ir], Tcs[u]], writes=[Tt2[n]])
                    S.op("pool", lambda e: e.tensor_tensor(out=ob[n][:], in0=t1[n][:], in1=t2[n][:], op=ALU.add), reads=[Tt1[n], Tt2[n]], writes=[Tob[n]])
                    k.store(dst, ob[n][:], [Tob[n]])

                def plain(c, dst):
                    nonlocal ecnt
                    i = mm(c)
                    n = ecnt % NE
                    ecnt += 1
                    S.op("act", lambda e: e.activation(out=ob[n][:], in_=pm[i][:], func=AF.Copy), reads=[Tpm[i]], writes=[Tob[n]])
                    k.store(dst, ob[n][:], [Tob[n]])

                for c in range(4):
                    roped(c, c + 4, k.scr["qT"][b, c * 128:(c + 1) * 128, tsl])
                roped(8, 9, k.scr["ksT"][b, :, tsl])
                roped(10, 11, k.scr["kwT"][b, :, tsl])
                plain(12, k.scr["kcrT"][b, :, tsl])
                plain(13, k.scr["vcrT"][b, :, tsl])
                for c in range(2):
                    ia = mm(14 + c)
                    ig = mm(16 + c)
                    n = ecnt % NE
                    ecnt += 1
                    S.op("act", lambda e: e.activation(out=t1[n][:], in_=pm[ig][:], func=AF.Sigmoid), reads=[Tpm[ig]], writes=[Tt1[n]])
                    S.op("dve", lambda e: e.tensor_tensor(out=ob[n][:], in0=pm[ia][:], in1=t1[n][:], op=ALU.mult), reads=[Tpm[ia], Tt1[n]], writes=[Tob[n]])
                    k.store(k.scr["ucT"][b, c * 128:(c + 1) * 128, tsl], ob[n][:], [Tob[n]])
                for c in range(2):
                    plain(18 + c, k.scr["usT"][b, c * 128:(c + 1) * 128, tsl])
                if STOP == "f":
                    continue
                for j in range(4):
                    pu = j % 2
                    for kc in range(8):
                        S.op("pe", lambda e: e.matmul(pk[pu][:, 0:280], lhsT=hT[u][:, kc, j * 128:(j + 1) * 128], rhs=W[:, kc, 2560:2840], start=(kc == 0), stop=(kc == 7)),
                             reads=[TW[kc], ThT[u][kc]], writes=[Tpk[pu]])
                    S.op("dve", lambda e: e.tensor_copy(out=tk[pu][:], in_=pk[pu][:, 0:256]), reads=[Tpk[pu]], writes=[Ttk[pu]])
                    r0 = g * 512 + j * 128
                    if STOP != "g":
                        S.op("act", lambda e: e.activation(out=tg[pu][:, 0:24], in_=pk[pu][:, 256:280], func=AF.Sigmoid), reads=[Tpk[pu]], writes=[Ttg[pu]])
                        if STOP != "h":
                            k.store(k.scr["gat"][b, r0:r0 + 128, :], tg[pu][:], [Ttg[pu]])
                    k.store(k.scr["vsw"][b, r0:r0 + 128, :], tk[pu][:], [Ttk[pu]])
        S.barrier()


K.declare_io = declare_io
K.load_consts = load_consts
K.phase1 = phase1


def build(debug=False, phases=None, nlayers=L):
    k = K(debug=debug, phases=phases, nlayers=nlayers)
    k.declare_io()
    S = k.S
    with contextlib.ExitStack() as es:
        k.load_consts(es)
        for l in range(nlayers):
            for ph in PHASES:
                if phases is not None and (l, ph) not in phases and ph not in phases:
                    continue
                getattr(k, ph)(l)
        if phases is None or "final" in phases:
            k.final()
        S.barrier()
    return k


PHASES = ["phase1"]


def host_inputs(inputs):
    f = lambda a: np.ascontiguousarray(np.asarray(a, dtype=np.float32))
    w_in = f(inputs["w_in"])
    cols = np.concatenate([np.arange(0, 512), np.arange(768, 896), np.arange(1024, 1152), np.arange(512, 640),
                           np.arange(640, 768), np.arange(1560, 2072), np.arange(1304, 1560), np.arange(896, 1024),
                           np.arange(1152, 1280), np.arange(1280, 1304)])
    shared = {}
    shared["w_in_p"] = np.ascontiguousarray(w_in[:, :, cols])
    shared["norm_mix_t"] = np.ascontiguousarray(f(inputs["norm_mix"]).reshape(L, 8, 128).transpose(0, 2, 1))
    shared["norm_final"] = f(inputs["norm_final"])
    tl = lambda a: np.ascontiguousarray(a.reshape(L, 8, 128).transpose(0, 2, 1))
    shared["ssm_are_t"] = tl(f(inputs["ssm_a_re"]).reshape(L, 16 * 64))
    shared["ssm_aim_t"] = tl(f(inputs["ssm_a_im"]).reshape(L, 16 * 64))
    shared["ssm_ldt_t"] = tl(np.repeat(f(inputs["ssm_log_dt"]), 64, axis=1))
    def padB(bm):
        o = np.zeros((L, 128, 8, 128), np.float32)
        for g_ in range(16):
            m_, h_ = g_ // 2, g_ % 2
            col = (g_ % 8) * 16
            o[:, h_ * 64:(h_ + 1) * 64, m_, col:col + 16] = bm[:, g_]
        return o
    shared["Zb_re"] = padB(f(inputs["ssm_b_re"]))
    shared["Zb_im"] = padB(f(inputs["ssm_b_im"]))
    shared["Cp_re"] = padB(f(inputs["ssm_c_re"]).transpose(0, 1, 3, 2))
    shared["Cp_im"] = padB(f(inputs["ssm_c_im"]).transpose(0, 1, 3, 2))
    shared["ssm_d_t"] = np.ascontiguousarray(f(inputs["ssm_d"]).reshape(L, 2, 128).transpose(0, 2, 1))
    shared["ssm_w_glu"] = f(inputs["ssm_w_glu"])
    shared["iota512"] = np.arange(512, dtype=np.float32)
    gcat = np.concatenate([f(inputs["norm_out_attn"]), f(inputs["norm_out_ssm"]), f(inputs["norm_out_conv"])], axis=1)
    shared["gcat_t"] = np.ascontiguousarray(gcat.reshape(L, 8, 128).transpose(0, 2, 1))
    shared["norm_ffn_t"] = np.ascontiguousarray(f(inputs["norm_ffn"]).reshape(L, 8, 128).transpose(0, 2, 1))
    for n in ["w_out", "w_gate", "w_up", "w_down"]:
        shared[n] = f(inputs[n])
    shared["wdw_t"] = np.ascontiguousarray(f(inputs["conv_w_dw"]).transpose(0, 2, 1))
    for n in ["conv_b_dw", "conv_b_pw", "conv_ln_g", "conv_ln_b", "conv_w_pw"]:
        shared[n] = f(inputs[n])
    for n in ["cmp_k_w1", "cmp_v_w1", "cmp_k_w2", "cmp_v_w2"]:
        shared[n] = f(inputs[n])
    pe = np.stack([f(inputs["cmp_pe_k"]), f(inputs["cmp_pe_v"])], 1)
    shared["pe_t"] = np.ascontiguousarray(pe.reshape(L, 2, 2, 16, 64).transpose(0, 1, 2, 4, 3).reshape(L, 2, 128, 16))
    shared.update(_consts())
    return shared


def kernel(**inputs):
    k = build()
    shared = host_inputs(inputs)
    x = np.ascontiguousarray(np.asarray(inputs["x"], dtype=np.float32))
    names = set(k.inp.keys())
    in_maps = []
    for c in range(8):
        m = {n: shared[n] for n in names if n != "x"}
        m["x"] = np.ascontiguousarray(x[c * NB:(c + 1) * NB])
        in_maps.append(m)
    res = run_bass_kernel_spmd(k.nc, in_maps, core_ids=list(range(8)))
    return np.concatenate([np.asarray(r["out"]) for r in res.results], axis=0).astype(np.float32)


def final(self):
    k, S = self, self.S
    src = k.scr["xres"] if self.nlayers > 0 else k.inp["x"]
    with contextlib.ExitStack() as es:
        gf = k.sb(es, "fin_g", [128, D], F32)
        Tgf = S.T("fin_g")
        S.dma("sp", gf[:], k.inp["norm_final"].partition_broadcast(128), writes=[Tgf])
        xg = [k.sb(es, f"fin_x{i}", [128, 4, D], F32) for i in range(2)]
        Txg = [S.T(f"fin_x{i}") for i in range(2)]
        og = [k.sb(es, f"fin_o{i}", [128, 4, D], F32) for i in range(2)]
        Tog = [S.T(f"fin_o{i}") for i in range(2)]
        junk = k.sb(es, "fin_junk", [128, D], BF16)
        Tjunk = S.T("fin_junk")
        ss = [k.sb(es, f"fin_ss{i}", [128, 4], F32) for i in range(2)]
        Tss = [S.T(f"fin_ss{i}") for i in range(2)]
        gi = 0
        for b in range(NB):
            for g in range(NG):
                u = gi % 2
                gi += 1
                tsl = slice(g * 512, (g + 1) * 512)
                S.dma("sp", xg[u][:], src[b, tsl, :].rearrange("(j p) d -> p j d", p=128), writes=[Txg[u]])
                for j in range(4):
                    S.op("act", lambda e: e.activation(out=junk[:], in_=xg[u][:, j, :], func=AF.Square, accum_out=ss[u][:, j:j + 1]),
                         reads=[Txg[u]], writes=[Tjunk, Tss[u]])
                S.op("dve", lambda e: e.tensor_scalar(out=ss[u][:], in0=ss[u][:], scalar1=1.0 / D, scalar2=EPS, op0=ALU.mult, op1=ALU.add), reads=[Tss[u]], writes=[Tss[u]])
                S.op("act", lambda e: e.activation(out=ss[u][:], in_=ss[u][:], func=AF.Sqrt), reads=[Tss[u]], writes=[Tss[u]])
                S.op("dve", lambda e: e.reciprocal(out=ss[u][:], in_=ss[u][:]), reads=[Tss[u]], writes=[Tss[u]])
                for j in range(4):
                    S.op("dve", lambda e: e.scalar_tensor_tensor(out=og[u][:, j, :], in0=xg[u][:, j, :], scalar=ss[u][:, j:j + 1], in1=gf[:], op0=ALU.mult, op1=ALU.mult),
                         reads=[Txg[u], Tss[u], Tgf], writes=[Tog[u]])
                k.store(k.scr["out"][b, tsl, :].rearrange("(j p) d -> p j d", p=128), og[u][:], [Tog[u]])
        S.barrier()


K.final = final


def phase2(self, l):
    k, S = self, self.S
    with contextlib.ExitStack() as es:
        stage = k.sb(es, "p2_stage", [128, 16, 256], F32)
        Tstage = S.T("p2_stage")
        w1b = [k.sb(es, f"p2_w1b{i}", [128, 16, 256], BF16) for i in range(2)]
        Tw1 = [S.T(f"p2_w1b{i}") for i in range(2)]
        pes = k.sb(es, "p2_pes", [128, 2, 16], F32)
        peb = k.sb(es, "p2_peb", [128, 2, 16], BF16)
        Tpe = S.T("p2_pe")
        w2s = k.sb(es, "p2_w2s", [128, 2, 2, 64], F32)
        w2b = k.sb(es, "p2_w2b", [128, 2, 2, 64], BF16)
        w2r = k.sb(es, "p2_w2r", [128, 2, 64], BF16)
        Tw2 = S.T("p2_w2")
        bias = k.sb(es, "p2_bias", [128, 2, 2], F32)
        Tbias = S.T("p2_bias")
        csC = k.sb(es, "p2_cosC", [64, 256], F32)
        snC = k.sb(es, "p2_sinC", [64, 256], F32)
        TcsC = S.T("p2_csC")
        S.dma("sp", csC[:], k.inp["cosC"], writes=[TcsC])
        S.dma("sp", snC[:], k.inp["sinC"], writes=[TcsC])
        pb = k.ps(es, "p2_pb", [128, 512], F32)
        Tpb = S.T("p2_pb")
        S.dma("sp", pes[:], k.inp["pe_t"][l].rearrange("a p l -> p a l"), writes=[Tpe])
        S.op("dve", lambda e: e.tensor_copy(out=peb[:], in_=pes[:]), reads=[Tpe], writes=[Tpe])
        for a, nm in enumerate(["cmp_k_w1", "cmp_v_w1"]):
            src = k.inp[nm][l].rearrange("(hl d) h -> d hl h", d=64)
            S.dma("sp", stage[0:64, :, :], src[:, 0:16, :], writes=[Tstage])
            S.dma("sp", stage[64:128, :, :], src[:, 16:32, :], reads=[], writes=[Tstage])
            S.op("dve" if a == 0 else "pool", lambda e: e.tensor_copy(out=w1b[a][:], in_=stage[:]), reads=[Tstage], writes=[Tw1[a]])
            for hc in range(2):
                for ll in range(16):
                    S.op("pe", lambda e: e.matmul(pb[:, 0:1], lhsT=w1b[a][:, ll, hc * 128:(hc + 1) * 128], rhs=peb[:, a, ll:ll + 1], start=(ll == 0), stop=(ll == 15)),
                         reads=[Tw1[a], Tpe], writes=[Tpb])
                S.op("dve", lambda e: e.tensor_copy(out=bias[:, a, hc:hc + 1], in_=pb[:, 0:1]), reads=[Tpb], writes=[Tbias])
        for a, nm in enumerate(["cmp_k_w2", "cmp_v_w2"]):
            S.dma("sp", w2s[:, a, :, :], k.inp[nm][l].rearrange("(hc p) d -> p hc d", p=128), writes=[Tw2])
        S.op("dve", lambda e: e.tensor_copy(out=w2b[:], in_=w2s[:]), reads=[Tw2], writes=[Tw2])
        for hc in range(2):
            S.op("dve", lambda e: e.tensor_scalar(out=w2r[:, hc, 0:32], in0=w2s[:, 0, hc, 32:64], scalar1=-1.0, scalar2=None, op0=ALU.mult), reads=[Tw2], writes=[Tw2])
            S.op("dve", lambda e: e.tensor_copy(out=w2r[:, hc, 32:64], in_=w2s[:, 0, hc, 0:32]), reads=[Tw2], writes=[Tw2])

        X2 = [k.sb(es, f"p2_X2_{i}", [128, SEQ], BF16) for i in range(2)]
        TX2 = [S.T(f"p2_X2_{i}") for i in range(2)]
        for i in range(2):
            S.op("pool", lambda e: e.memset(X2[i][64:128, SEQ - 16:SEQ], 0.0), writes=[TX2[i]])
        hid = [k.sb(es, f"p2_hid{i}", [128, 2, 256], BF16) for i in range(2)]
        Thid = [S.T(f"p2_hid{i}") for i in range(2)]
        for i in range(2):
            S.op("pool", lambda e: e.memset(hid[i][:], 0.0), writes=[Thid[i]])
        ph = [k.ps(es, f"p2_ph{i}", [128, 512], F32) for i in range(2)]
        Tph = [S.T(f"p2_ph{i}") for i in range(2)]
        pk = k.ps(es, "p2_pk", [128, 512], F32)
        Tpk = S.T("p2_pk")
        pv = k.ps(es, "p2_pv", [128, 512], F32)
        Tpv = S.T("p2_pv")
        t1 = k.sb(es, "p2_t1", [64, 256], F32)
        t2 = k.sb(es, "p2_t2", [64, 256], F32)
        kco = k.sb(es, "p2_kco", [64, 256], BF16)
        Tko = S.T("p2_kco")
        S.op("dve", lambda e: e.memset(kco[:], 0.0), writes=[Tko])
        vco = [k.sb(es, f"p2_vco{i}", [128, 2, 64], BF16) for i in range(2)]
        Tvo = [S.T(f"p2_vco{i}") for i in range(2)]
        it = 0
        phi = 0
        for b in range(NB):
            for kv in range(2):
                for a in range(2):
                    u = it % 2
                    it += 1
                    src = k.scr["kcrT" if a == 0 else "vcrT"][b, kv * 64:(kv + 1) * 64, :]
                    S.dma("sp", X2[u][0:64, :], src, writes=[TX2[u]])
                    S.dma("sp", X2[u][64:128, 0:SEQ - 16], src[:, 16:SEQ], reads=[], writes=[TX2[u]])
                    xv = X2[u][:, :].rearrange("p (i s) -> p i s", s=16)
                    for hc in range(2):
                        pi = phi % 2
                        phi += 1
                        for ll in range(16):
                            S.op("pe", lambda e: e.matmul(ph[pi][:, 0:255], lhsT=w1b[a][:, ll, hc * 128:(hc + 1) * 128], rhs=xv[:, 0:255, ll], start=(ll == 0), stop=(ll == 15)),
                                 reads=[Tw1[a], TX2[u]], writes=[Tph[pi]])
                        S.op("act", lambda e: e.activation(out=hid[u][:, hc, 0:255], in_=ph[pi][:, 0:255], func=AF.Gelu_apprx_tanh, bias=bias[:, a, hc:hc + 1]),
                             reads=[Tph[pi], Tbias], writes=[Thid[u]])
                    if a == 0:
                        for r_, wsel in enumerate([w2b[:, 0, :, :], w2r[:, :, :]]):
                            for hc in range(2):
                                S.op("pe", lambda e: e.matmul(pk[0:64, r_ * 256:r_ * 256 + 256], lhsT=wsel[:, hc, :], rhs=hid[u][:, hc, :], start=(hc == 0), stop=(hc == 1)),
                                     reads=[Tw2, Thid[u]], writes=[Tpk])
                        S.op("dve", lambda e: e.tensor_tensor(out=t1[:], in0=pk[0:64, 0:256], in1=csC[:], op=ALU.mult), reads=[Tpk, TcsC], writes=[Tko])
                        S.op("dve", lambda e: e.tensor_tensor(out=t2[:], in0=pk[0:64, 256:512], in1=snC[:], op=ALU.mult), reads=[Tpk, TcsC], writes=[Tko])
                        S.op("dve", lambda e: e.tensor_tensor(out=kco[:, 0:255], in0=t1[:, 0:255], in1=t2[:, 0:255], op=ALU.add), reads=[Tko], writes=[Tko])
                        k.store(k.scr["kcT"][b, kv], kco[:], [Tko])
                    else:
                        for c in range(2):
                            for hc in range(2):
                                S.op("pe", lambda e: e.matmul(pv[:, c * 64:(c + 1) * 64], lhsT=hid[u][:, hc, c * 128:(c + 1) * 128], rhs=w2b[:, 1, hc, :], start=(hc == 0), stop=(hc == 1)),
                                     reads=[Tw2, Thid[u]], writes=[Tpv])
                        S.op("dve", lambda e: e.tensor_copy(out=vco[u][:].rearrange("p c d -> p (c d)"), in_=pv[:, 0:128]), reads=[Tpv], writes=[Tvo[u]])
                        k.store(k.scr["vc"][b, kv].rearrange("(c p) d -> p c d", p=128), vco[u][:], [Tvo[u]])
        S.barrier()


K.phase2 = phase2
PHASES.append("phase2")


def phase3(self, l):
    k, S = self, self.S
    with contextlib.ExitStack() as es:
        sb = lambda n, sh, dt=F32: k.sb(es, "p3_" + n, sh, dt)
        QS = [sb(f"QS{h}", [128, SEQ], BF16) for h in range(4)]
        TQd = [S.T(f"p3_Qd{h}") for h in range(4)]
        TQm = [[S.T(f"p3_Qm{h}_{g}") for g in range(NG)] for h in range(4)]
        KS = sb("KS", [128, SEQ], BF16)
        TKS, TE = S.T("p3_KS"), S.T("p3_E")
        KW = sb("KW", [64, SEQ], BF16)
        TKW = S.T("p3_KW")
        KC = sb("KC", [64, 256], BF16)
        TKC = S.T("p3_KC")
        VS = sb("VS", [128, NT, 80], BF16)
        VW = sb("VW", [128, NT, 80], BF16)
        VC = sb("VC", [128, 2, 144], BF16)
        TVS = [S.T(f"p3_VS{i}") for i in range(4)]
        TVW = [S.T(f"p3_VW{i}") for i in range(4)]
        TVC = S.T("p3_VC")
        G_ = sb("G", [128, NT, 32], F32)
        TG = S.T("p3_G")
        yacc = sb("yacc", [128, NT, 4, 64], F32)
        Ty = [[S.T(f"p3_y{g}_{h}") for h in range(4)] for g in range(NG)]
        cmaskS = sb("cmask", [128, 2, SEQ], BF16)
        wmaskS = sb("wmask", [128, 8, 512], BF16)
        dmaskS = sb("dmask", [128, 4, 512], BF16)
        addS = sb("add", [128, NT, 64], F32)
        Tc = S.T("p3_consts")
        S.dma("sp", cmaskS[:], k.inp["cmask"].rearrange("(c p) t -> p c t", p=128), writes=[Tc])
        Tc2 = S.T("p3_consts2")
        S.dma("sp", wmaskS[:], k.inp["wmask"].rearrange("r p u -> p r u"), writes=[Tc2])
        Tc3 = S.T("p3_consts3")
        S.dma("sp", dmaskS[:], k.inp["dmask"].rearrange("r p u -> p r u"), writes=[Tc3])
        Tc4 = S.T("p3_consts4")
        S.dma("sp", addS[:], k.inp["addtab"], writes=[Tc4])
        S.dma("sp", KS[64:128, :], k.inp["Erows"], writes=[TE])
        S.op("pool", lambda e: e.memset(VS[:, :, 64:65], 1.0), writes=TVS)
        S.op("pool", lambda e: e.memset(VW[:, :, 64:65], 1.0), writes=TVW)
        S.op("pool", lambda e: e.memset(VC[:, :, 64:65], 1.0), writes=[TVC])
        Tov = S.T("p3_ov")
        ovs = sb("ovs", [128, 2, 64], BF16)
        S.dma("sp", ovs[:], k.inp["overlap"].rearrange("(c p) j -> p c j", p=128), writes=[Tov])
        S.op("pool", lambda e: e.tensor_copy(out=VC[:, :, 65:129], in_=ovs[:]), reads=[Tov], writes=[TVC])
        import os
        if os.environ.get("P3STOP", "") == "c":
            S.barrier()
            return
        bank = [k.ps(es, f"p3_bank{i}", [128, 512], F32) for i in range(8)]
        Tb = [S.T(f"p3_bank{i}") for i in range(8)]
        NP = 6
        P = [sb(f"P{i}", [128, 512], BF16) for i in range(NP)]
        TP = [S.T(f"p3_P{i}") for i in range(NP)]
        EC = [sb(f"EC{i}", [128, 512], BF16) for i in range(8)]
        TEC = [S.T(f"p3_EC{i}") for i in range(8)]
        Mbp = [sb(f"Mbp{i}", [128, 128], BF16) for i in range(4)]
        TMb = [S.T(f"p3_Mbp{i}") for i in range(4)]
        for i in range(4):
            S.op("pool", lambda e: e.memset(Mbp[i][:], 0.0), writes=[TMb[i]])
        NS = 4
        rs = [sb(f"rs{i}", [128, 4], F32) for i in range(NS)]
        coef = [sb(f"coef{i}", [128, 4], F32) for i in range(NS)]
        impt = [sb(f"impt{i}", [128, 64], F32) for i in range(NS)]
        tmp = [sb(f"tmp{i}", [128, 64], F32) for i in range(NS)]
        m1 = [sb(f"m1_{i}", [128, 8], F32) for i in range(NS)]
        m2 = [sb(f"m2_{i}", [128, 8], F32) for i in range(NS)]
        Tsm = [S.T(f"p3_sm{i}") for i in range(NS)]
        ot = [sb(f"ot{i}", [65, 512], F32) for i in range(2)]
        Tot = [S.T(f"p3_ot{i}") for i in range(2)]
        cnt = {"p": 0, "s": 0, "sm": 0, "mb": 0, "ot": 0, "pa": 0, "ow": 0, "os": 0}

        def nxt(key, n):
            v = cnt[key] % n
            cnt[key] += 1
            return v

        for b in range(NB):
            for kv in range(2):
                for h in range(4):
                    r0 = (kv * 4 + h) * 64
                    S.dma("sp", QS[h][0:64, :], k.scr["qT"][b, r0:r0 + 64, :], writes=[TQd[h]])
                S.dma("sp", KS[0:64, :], k.scr["ksT"][b, kv * 64:(kv + 1) * 64, :], writes=[TKS])
                S.dma("sp", KW[:, :], k.scr["kwT"][b, kv * 64:(kv + 1) * 64, :], writes=[TKW])
                S.dma("sp", KC[:, :], k.scr["kcT"][b, kv], writes=[TKC])
                vsrc = k.scr["vsw"][b].rearrange("(n p) c -> p n c", p=128)
                for q4 in range(4):
                    nsl = slice(q4 * 8, q4 * 8 + 8)
                    S.dma("sp", VS[:, nsl, 0:64], vsrc[:, nsl, kv * 64:(kv + 1) * 64], writes=[TVS[q4]])
                    S.dma("sp", VW[:, nsl, 0:64], vsrc[:, nsl, 128 + kv * 64:128 + (kv + 1) * 64], writes=[TVW[q4]])
                S.dma("sp", VC[:, :, 0:64], k.scr["vc"][b, kv].rearrange("(c p) d -> p c d", p=128), writes=[TVC])
                if kv == 0:
                    S.dma("sp", G_[:], k.scr["gat"][b].rearrange("(n p) c -> p n c", p=128), writes=[TG])
                import os
                P3STOP = os.environ.get("P3STOP", "")
                for g in range(NG):
                    if P3STOP == "load":
                        break
                    gsl = slice(g * 512, (g + 1) * 512)
                    ncs = 2 if g >= 4 else 1
                    ecs = {}
                    for h in range(4):
                        for c in range(ncs):
                            bi = nxt("s", 2)
                            S.op("pe", lambda e: e.matmul(bank[bi][:], lhsT=KC[0:64, c * 128:(c + 1) * 128], rhs=QS[h][0:64, gsl], start=True, stop=False),
                                 reads=[TKC, TQd[h]], writes=[Tb[bi]])
                            S.op("pe", lambda e: e.matmul(bank[bi][:], lhsT=k.identb[:], rhs=cmaskS[:, c, gsl], start=False, stop=True),
                                 reads=[k.Tidb, Tc], writes=[Tb[bi]])
                            ei = h * 2 + c
                            S.op("act", lambda e: e.activation(out=EC[ei][:], in_=bank[bi][:], func=AF.Exp), reads=[Tb[bi]], writes=[TEC[ei]])
                            ecs[(h, c)] = ei
                    P3A = os.environ.get("P3A", "")
                    for jq in range(4):
                        if P3A == "s":
                            break
                        n = 4 * g + jq
                        pa = nxt("pa", 2)
                        bks = (2 + 2 * pa, 3 + 2 * pa)
                        for h in range(4):
                            bk = bks[h // 2]
                            o0 = (h % 2) * 256
                            for c in range(ncs):
                                ei = ecs[(h, c)]
                                S.op("pe", lambda e: e.matmul(bank[bk][:, o0:o0 + 129], lhsT=EC[ei][:, jq * 128:(jq + 1) * 128], rhs=VC[:, c, 0:129], start=(c == 0), stop=(c == ncs - 1)),
                                     reads=[TEC[ei], TVC], writes=[Tb[bk]])
                        if P3A == "pv":
                            continue
                        si = nxt("sm", NS)
                        T_s = Tsm[si]
                        for half in range(2):
                            bk = bks[half]
                            S.op("dve", lambda e: e.tensor_scalar(out=rs[si][:, 2 * half:2 * half + 2], in0=bank[bk][:].rearrange("p (a f) -> p a f", a=2)[:, :, 64], scalar1=1e-30, scalar2=None, op0=ALU.max),
                                 reads=[Tb[bk]], writes=[T_s])
                        S.op("dve", lambda e: e.reciprocal(out=rs[si][:], in_=rs[si][:]), reads=[T_s], writes=[T_s])
                        for half in range(0):
                            pass
                        for h in range(4):
                            bk = bks[h // 2]
                            o0 = (h % 2) * 256
                            in1 = addS[:, n, :] if h == 0 else impt[si][:]
                            S.op("dve", lambda e: e.scalar_tensor_tensor(out=impt[si][:], in0=bank[bk][:, o0 + 65:o0 + 129], scalar=rs[si][:, h:h + 1], in1=in1, op0=ALU.mult, op1=ALU.add),
                                 reads=[Tb[bk], T_s, Tc4], writes=[T_s])
                        S.op("dve", lambda e: e.tensor_tensor(out=coef[si][:], in0=rs[si][:], in1=G_[:, n, kv * 4:kv * 4 + 4], op=ALU.mult), reads=[T_s, TG], writes=[T_s])
                        if P3A == "d1":
                            continue
                        for h in range(4):
                            bk = bks[h // 2]
                            o0 = (h % 2) * 256
                            S.op("act", lambda e: e.activation(out=yacc[:, n, h, :], in_=bank[bk][:, o0:o0 + 64], func=AF.Identity, scale=coef[si][:, h:h + 1]),
                                 reads=[Tb[bk], T_s], writes=[Ty[g][h]])
                        if P3A == "y":
                            continue
                        S.op("dve", lambda e: e.max(out=m1[si][:], in_=impt[si][:]), reads=[T_s], writes=[T_s])
                        S.op("dve", lambda e: e.match_replace(out=tmp[si][:], in_to_replace=m1[si][:], in_values=impt[si][:], imm_value=-3e9), reads=[T_s], writes=[T_s])
                        S.op("dve", lambda e: e.max(out=m2[si][:], in_=tmp[si][:]), reads=[T_s], writes=[T_s])
                        if P3A == "tk":
                            continue
                        mi = nxt("mb", 4)
                        S.op("dve", lambda e: e.tensor_scalar(out=Mbp[mi][:, 64:128], in0=impt[si][:], scalar1=m2[si][:, 7:8], scalar2=NEG, op0=ALU.is_lt, op1=ALU.mult),
                             reads=[T_s], writes=[TMb[mi]])
                        S.op("pe", lambda e: e.matmul(bank[6][:, jq * 128:(jq + 1) * 128], lhsT=Mbp[mi][:], rhs=k.identb[:], start=True, stop=True),
                             reads=[TMb[mi], k.Tidb], writes=[Tb[6]])
                    S.op("dve", lambda e: e.tensor_copy(out=QS[0][64:128, gsl], in_=bank[6][64:128, :]), reads=[Tb[6]], writes=[TQm[0][g]])
                    for h in range(1, 4):
                        S.op("pool", lambda e: e.tensor_copy(out=QS[h][64:128, gsl], in_=QS[0][64:128, gsl]), reads=[TQm[0][g]], writes=[TQm[h][g]])
                    for h in range(4):
                        if P3STOP == "A":
                            break
                        for br in ((2,) if P3STOP == "W" else (2, 1)):
                            if br == 2:
                                kts = [kt for kt in range(4 * g - 4, 4 * g + 4) if kt >= 0]
                                ob = 3 + nxt("ow", 2)
                            else:
                                kts = list(range(0, 4 * g + 4))
                                ob = 5 + nxt("os", 2)
                            Vt, TV = (VW, TVW) if br == 2 else (VS, TVS)
                            pis = {}

                            def qk(idx, kt):
                                ksl = slice(kt * 128, (kt + 1) * 128)
                                bi = nxt("p", 3)
                                if br == 2:
                                    S.op("pe", lambda e: e.matmul(bank[bi][:], lhsT=KW[0:64, ksl], rhs=QS[h][0:64, gsl], start=True, stop=False),
                                         reads=[TKW, TQd[h]], writes=[Tb[bi]])
                                    S.op("pe", lambda e: e.matmul(bank[bi][:], lhsT=k.identb[:], rhs=wmaskS[:, kt - 4 * g + 4, :], start=False, stop=True),
                                         reads=[k.Tidb, Tc2], writes=[Tb[bi]])
                                else:
                                    diag = kt >= 4 * g
                                    S.op("pe", lambda e: e.matmul(bank[bi][:], lhsT=KS[:, ksl], rhs=QS[h][:, gsl], start=True, stop=not diag),
                                         reads=[TKS, TE, TQd[h], TQm[h][g]], writes=[Tb[bi]])
                                    if diag:
                                        S.op("pe", lambda e: e.matmul(bank[bi][:], lhsT=k.identb[:], rhs=dmaskS[:, kt - 4 * g, :], start=False, stop=True),
                                             reads=[k.Tidb, Tc3], writes=[Tb[bi]])
                                pi = nxt("s", NP)
                                S.op("act", lambda e: e.activation(out=P[pi][:], in_=bank[bi][:], func=AF.Exp), reads=[Tb[bi]], writes=[TP[pi]])
                                pis[idx] = pi

                            def pvf(idx, kt):
                                pi = pis.pop(idx)
                                S.op("pe", lambda e: e.matmul(bank[ob][0:65, :], lhsT=Vt[:, kt, 0:65], rhs=P[pi][:], start=(idx == 0), stop=(idx == len(kts) - 1)),
                                     reads=[TV[kt // 8], TP[pi]], writes=[Tb[ob]])
                            LA = 2
                            for i_ in range(len(kts) + LA):
                                if i_ < len(kts):
                                    qk(i_, kts[i_])
                                if i_ >= LA:
                                    pvf(i_ - LA, kts[i_ - LA])
                            oi = nxt("ot", 2)
                            S.op("dve", lambda e: e.tensor_copy(out=ot[oi][:], in_=bank[ob][0:65, :]), reads=[Tb[ob]], writes=[Tot[oi]])
                            for jq in range(4):
                                S.op("pe", lambda e: e.transpose(out=bank[7][:, jq * 128:jq * 128 + 65], in_=ot[oi][:, jq * 128:(jq + 1) * 128], identity=k.identf[0:65, 0:65]),
                                     reads=[Tot[oi], k.Tid], writes=[Tb[7]])
                            si = nxt("sm", NS)
                            T_s = Tsm[si]
                            b7 = bank[7][:].rearrange("p (a f) -> p a f", a=4)
                            S.op("dve", lambda e: e.reciprocal(out=rs[si][:], in_=b7[:, :, 64]), reads=[Tb[7]], writes=[T_s])
                            gc = br * 8 + kv * 4 + h
                            S.op("dve", lambda e: e.tensor_tensor(out=coef[si][:], in0=rs[si][:], in1=G_[:, 4 * g:4 * g + 4, gc], op=ALU.mult), reads=[T_s, TG], writes=[T_s])
                            for jq in range(4):
                                n = 4 * g + jq
                                S.op("dve", lambda e: e.scalar_tensor_tensor(out=yacc[:, n, h, :], in0=bank[7][:, jq * 128:jq * 128 + 64], scalar=coef[si][:, jq:jq + 1], in1=yacc[:, n, h, :], op0=ALU.mult, op1=ALU.add),
                                     reads=[Tb[7], T_s, Ty[g][h]], writes=[Ty[g][h]])
                if P3STOP == "load" and os.environ.get("P3NOST", ""):
                    continue
                ydst = k.scr["ycat"][b].rearrange("(n p) c -> p n c", p=128)
                for g in range(NG):
                    k.store(ydst[:, 4 * g:4 * g + 4, kv * 256:(kv + 1) * 256], yacc[:, 4 * g:4 * g + 4, :, :].rearrange("p n h d -> p n (h d)"), Ty[g])
        S.barrier()


K.phase3 = phase3
PHASES.append("phase3")


def phase5(self, l):
    k, S = self, self.S
    with contextlib.ExitStack() as es:
        sb = lambda n, sh, dt=F32: k.sb(es, "p5_" + n, sh, dt)
        wdw = sb("wdw", [128, 2, 31], F32)
        Twd = S.T("p5_wdw")
        S.dma("sp", wdw[:], k.inp["wdw_t"][l].rearrange("(ct p) kk -> p ct kk", p=128), writes=[Twd])
        Dg = sb("Dg", [128, 2, 31, 128], BF16)
        TDg = S.T("p5_Dg")
        for ct in range(2):
            for kk in range(31):
                S.op("pool" if (kk % 2) else "dve", lambda e: e.tensor_scalar(out=Dg[:, ct, kk, :], in0=k.identf[:], scalar1=wdw[:, ct, kk:kk + 1], scalar2=None, op0=ALU.mult),
                     reads=[Twd, k.Tid], writes=[TDg])
        rows = sb("rows", [1, 2, 256], F32)
        rowsb = sb("rowsb", [1, 2, 256], BF16)
        Trows = S.T("p5_rows")
        S.dma("sp", rows[:, 0, :], k.inp["conv_b_dw"][l:l + 1, :], writes=[Trows])
        S.dma("sp", rows[:, 1, :], k.inp["conv_b_pw"][l:l + 1, :], writes=[Trows])
        S.op("dve", lambda e: e.tensor_copy(out=rowsb[:], in_=rows[:]), reads=[Trows], writes=[Trows])
        lng = sb("lng", [128, 256], F32)
        lnb = sb("lnb", [128, 256], F32)
        Tln = S.T("p5_ln")
        S.dma("sp", lng[:], k.inp["conv_ln_g"][l].partition_broadcast(128), writes=[Tln])
        Tln2 = S.T("p5_ln2")
        S.dma("sp", lnb[:], k.inp["conv_ln_b"][l].partition_broadcast(128), writes=[Tln2])
        wps = sb("wps", [128, 2, 256], F32)
        wpb = sb("wpb", [128, 2, 256], BF16)
        Twp = S.T("p5_wp")
        S.dma("sp", wps[:], k.inp["conv_w_pw"][l].rearrange("(ct p) n -> p ct n", p=128), writes=[Twp])
        S.op("dve", lambda e: e.tensor_copy(out=wpb[:], in_=wps[:]), reads=[Twp], writes=[Twp])
        Ub = sb("Ub", [128, 2, 32 + SEQ], BF16)
        TUb = [S.T(f"p5_Ub{c}") for c in range(2)]
        S.op("pool", lambda e: e.memset(Ub[:, :, 0:32], 0.0), writes=TUb)
        pc = [k.ps(es, f"p5_pc{i}", [128, 512], F32) for i in range(2)]
        Tpc = [S.T(f"p5_pc{i}") for i in range(2)]
        pz = [k.ps(es, f"p5_pz{i}", [128, 256], BF16) for i in range(2)]
        Tpz = [S.T(f"p5_pz{i}") for i in range(2)]
        po = [k.ps(es, f"p5_po{i}", [128, 512], F32) for i in range(2)]
        Tpo = [S.T(f"p5_po{i}") for i in range(2)]
        NR = 3
        st = [sb(f"st{i}", [128, 6], F32) for i in range(NR)]
        mv = [sb(f"mv{i}", [128, 2], F32) for i in range(NR)]
        rstd = [sb(f"rstd{i}", [128, 1], F32) for i in range(NR)]
        xn = [sb(f"xn{i}", [128, 256], F32) for i in range(NR)]
        zb = [sb(f"zb{i}", [128, 256], BF16) for i in range(NR)]
        zT = [sb(f"zT{i}", [128, 2, 128], BF16) for i in range(NR)]
        Tr = [S.T(f"p5_r{i}") for i in range(NR)]
        TzT = [S.T(f"p5_zT{i}") for i in range(NR)]
        yo = [sb(f"yo{i}", [128, 4, 256], F32) for i in range(2)]
        Tyo = [S.T(f"p5_yo{i}") for i in range(2)]
        it = 0
        for b in range(NB):
            for ct in range(2):
                S.dma("sp", Ub[:, ct, 32:32 + SEQ], k.scr["ucT"][b, ct * 128:(ct + 1) * 128, :], writes=[TUb[ct]])
            def conv_mm(n):
                u = n % 2
                t0 = n * 128 + 2
                for ct in range(2):
                    csl = slice(ct * 128, (ct + 1) * 128)
                    S.op("pe", lambda e: e.matmul(pc[u][:, csl], lhsT=k.ones_b[0:1, 0:128], rhs=rowsb[0:1, 0, csl], start=True, stop=False),
                         reads=[k.Tones, Trows], writes=[Tpc[u]])
                    for kk in range(31):
                        S.op("pe", lambda e: e.matmul(pc[u][:, csl], lhsT=Ub[:, ct, t0 + kk:t0 + kk + 128], rhs=Dg[:, ct, kk, :], start=False, stop=(kk == 30)),
                             reads=[TUb[ct], TDg], writes=[Tpc[u]])

            def conv_tail(n):
                u = n % 2
                r_ = n % NR
                T_r = Tr[r_]
                S.op("dve", lambda e: e.bn_stats(out=st[r_][:], in_=pc[u][:, 0:256]), reads=[Tpc[u]], writes=[T_r])
                S.op("dve", lambda e: e.bn_aggr(out=mv[r_][:], in_=st[r_][:]), reads=[T_r], writes=[T_r])
                S.op("dve", lambda e: e.tensor_scalar(out=rstd[r_][:], in0=mv[r_][:, 1:2], scalar1=EPS, scalar2=None, op0=ALU.add), reads=[T_r], writes=[T_r])
                S.op("act", lambda e: e.activation(out=rstd[r_][:], in_=rstd[r_][:], func=AF.Sqrt), reads=[T_r], writes=[T_r])
                S.op("dve", lambda e: e.reciprocal(out=rstd[r_][:], in_=rstd[r_][:]), reads=[T_r], writes=[T_r])
                S.op("dve", lambda e: e.tensor_scalar(out=xn[r_][:], in0=pc[u][:, 0:256], scalar1=mv[r_][:, 0:1], scalar2=rstd[r_][:, 0:1], op0=ALU.subtract, op1=ALU.mult),
                     reads=[Tpc[u], T_r], writes=[T_r])
                S.op("pool", lambda e: e.tensor_tensor(out=xn[r_][:], in0=xn[r_][:], in1=lng[:], op=ALU.mult), reads=[T_r, Tln], writes=[T_r])
                S.op("pool", lambda e: e.tensor_tensor(out=xn[r_][:], in0=xn[r_][:], in1=lnb[:], op=ALU.add), reads=[T_r, Tln2], writes=[T_r])
                S.op("act", lambda e: e.activation(out=zb[r_][:], in_=xn[r_][:], func=AF.Silu), reads=[T_r], writes=[T_r])
                for ct in range(2):
                    S.op("pe", lambda e: e.transpose(out=pz[u][:, ct * 128:(ct + 1) * 128], in_=zb[r_][:, ct * 128:(ct + 1) * 128], identity=k.identb[:]),
                         reads=[T_r, k.Tidb], writes=[Tpz[u]])
                S.op("act", lambda e: e.activation(out=zT[r_][:].rearrange("p c t -> p (c t)"), in_=pz[u][:], func=AF.Copy), reads=[Tpz[u]], writes=[TzT[r_]])
                S.op("pe", lambda e: e.matmul(po[u][:, 0:256], lhsT=k.ones_b[0:1, 0:128], rhs=rowsb[0:1, 1, :], start=True, stop=False),
                     reads=[k.Tones, Trows], writes=[Tpo[u]])
                for ct in range(2):
                    S.op("pe", lambda e: e.matmul(po[u][:, 0:256], lhsT=zT[r_][:, ct, :], rhs=wpb[:, ct, :], start=False, stop=(ct == 1)),
                         reads=[TzT[r_], Twp], writes=[Tpo[u]])
                yi = (n // 4) % 2
                S.op("dve", lambda e: e.tensor_copy(out=yo[yi][:, n % 4, :], in_=po[u][:, 0:256]), reads=[Tpo[u]], writes=[Tyo[yi]])
                if n % 4 == 3:
                    ydst = k.scr["ycat"][b].rearrange("(n p) c -> p n c", p=128)
                    k.store(ydst[:, n - 3:n + 1, 768:1024], yo[yi][:], [Tyo[yi]])

            for n in range(NT):
                conv_mm(n)
                if n > 0:
                    conv_tail(n - 1)
            conv_tail(NT - 1)
        S.barrier()


K.phase5 = phase5
PHASES.append("phase5")


def phase6a(self, l):
    k, S = self, self.S
    with contextlib.ExitStack() as es:
        sb = lambda n, sh, dt=F32: k.sb(es, "p6a_" + n, sh, dt)
        Wo = sb("Wo", [128, 8, D], BF16)
        TWo = [S.T(f"p6a_Wo{i}") for i in range(8)]
        stage = [sb(f"stage{i}", [128, D], F32) for i in range(2)]
        Tst = [S.T(f"p6a_stage{i}") for i in range(2)]
        gcat = sb("gcat", [128, 8], F32)
        Tg = S.T("p6a_gcat")
        S.dma("sp", gcat[:], k.inp["gcat_t"][l], writes=[Tg])
        wsrc = k.inp["w_out"][l].rearrange("(kc p) n -> p kc n", p=128)
        for kc in range(8):
            S.dma("sp", stage[kc % 2][:], wsrc[:, kc, :], writes=[Tst[kc % 2]])
            S.op("pool" if kc % 2 else "dve", lambda e: e.tensor_scalar(out=Wo[:, kc, :], in0=stage[kc % 2][:], scalar1=gcat[:, kc:kc + 1], scalar2=None, op0=ALU.mult),
                 reads=[Tst[kc % 2], Tg], writes=[TWo[kc]])
        yg = [sb(f"yg{i}", [128, 4, D], F32) for i in range(2)]
        Tyg = [S.T(f"p6a_yg{i}") for i in range(2)]
        xg = [sb(f"xg{i}", [128, 4, D], F32) for i in range(2)]
        Txg = [[S.T(f"p6a_xg{i}_{j}") for j in range(4)] for i in range(2)]
        junk = sb("junk", [128, 512], BF16)
        Tjunk = S.T("p6a_junk")
        ss = sb("ss", [128, 4, 4], F32)
        Tss = S.T("p6a_ss")
        mb = sb("mb", [128, 4, D], BF16)
        Tmb = [S.T(f"p6a_mb{j}") for j in range(4)]
        mT = sb("mT", [128, 8, 512], BF16)
        TmT = [S.T(f"p6a_mT{c}") for c in range(8)]
        hb = sb("hb", [128, 4, D], BF16)
        Thb = [S.T(f"p6a_hb{j}") for j in range(4)]
        hT = [sb(f"hT{i}", [128, 8, 512], BF16) for i in range(2)]
        ThT = [S.T(f"p6a_hT{i}") for i in range(2)]
        pT = [k.ps(es, f"p6a_pT{i}", [128, 512], BF16) for i in range(2)]
        TpT = [S.T(f"p6a_pT{i}") for i in range(2)]
        pm = [k.ps(es, f"p6a_pm{i}", [128, 512], F32) for i in range(4)]
        Tpm = [S.T(f"p6a_pm{i}") for i in range(4)]
        xin = k.inp["x"] if l == 0 else k.scr["xres"]
        segs = [(0, 512), (512, 768), (768, 1024)]
        gi = 0
        pmi = 0
        for b in range(NB):
            for g in range(NG):
                u = gi % 2
                gi += 1
                tsl = slice(g * 512, (g + 1) * 512)
                S.dma("sp", yg[u][:], k.scr["ycat"][b, tsl, :].rearrange("(j p) d -> p j d", p=128), writes=[Tyg[u]])
                for j in range(4):
                    S.dma("sp", xg[u][:, j, :], xin[b, g * 512 + j * 128:g * 512 + (j + 1) * 128, :], writes=[Txg[u][j]])
                for j in range(4):
                    for si, (a0, a1) in enumerate(segs):
                        S.op("act", lambda e: e.activation(out=junk[:, 0:a1 - a0], in_=yg[u][:, j, a0:a1], func=AF.Square, accum_out=ss[:, j, si:si + 1]),
                             reads=[Tyg[u]], writes=[Tjunk, Tss])
                for si, (a0, a1) in enumerate(segs):
                    S.op("dve", lambda e: e.tensor_scalar(out=ss[:, :, si], in0=ss[:, :, si], scalar1=1.0 / (a1 - a0), scalar2=EPS, op0=ALU.mult, op1=ALU.add), reads=[Tss], writes=[Tss])
                S.op("act", lambda e: e.activation(out=ss[:, :, 0:3], in_=ss[:, :, 0:3], func=AF.Sqrt), reads=[Tss], writes=[Tss])
                S.op("dve", lambda e: e.reciprocal(out=ss[:, :, 0:3], in_=ss[:, :, 0:3]), reads=[Tss], writes=[Tss])
                for j in range(4):
                    for si, (a0, a1) in enumerate(segs):
                        S.op("pool" if (si == 0) else "dve", lambda e: e.tensor_scalar(out=mb[:, j, a0:a1], in0=yg[u][:, j, a0:a1], scalar1=ss[:, j, si:si + 1], scalar2=None, op0=ALU.mult),
                             reads=[Tyg[u], Tss], writes=[Tmb[j]])
                for kc in range(8):
                    pu = kc % 2
                    for j in range(4):
                        S.op("pe", lambda e: e.transpose(out=pT[pu][:, j * 128:(j + 1) * 128], in_=mb[:, j, kc * 128:(kc + 1) * 128], identity=k.identb[:]),
                             reads=[Tmb[j], k.Tidb], writes=[TpT[pu]])
                    S.op("act" if kc % 2 else "dve", (lambda e: e.activation(out=mT[:, kc, :], in_=pT[pu][:], func=AF.Copy)) if kc % 2 else (lambda e: e.tensor_copy(out=mT[:, kc, :], in_=pT[pu][:])),
                         reads=[TpT[pu]], writes=[TmT[kc]])
                for j in range(4):
                    for half in range(2):
                        i = pmi % 4
                        pmi += 1
                        hsl = slice(half * 512, (half + 1) * 512)
                        for kc in range(8):
                            S.op("pe", lambda e: e.matmul(pm[i][:], lhsT=mT[:, kc, j * 128:(j + 1) * 128], rhs=Wo[:, kc, hsl], start=(kc == 0), stop=(kc == 7)),
                                 reads=[TmT[kc], TWo[kc]], writes=[Tpm[i]])
                        S.op("dve", lambda e: e.tensor_tensor(out=xg[u][:, j, hsl], in0=pm[i][:], in1=xg[u][:, j, hsl], op=ALU.add), reads=[Tpm[i], Txg[u][j]], writes=[Txg[u][j]])
                    k.store(k.scr["xres"][b, g * 512 + j * 128:g * 512 + (j + 1) * 128, :], xg[u][:, j, :], [Txg[u][j]])
                    S.op("act", lambda e: e.activation(out=junk[:, 0:512], in_=xg[u][:, j, 0:512], func=AF.Square, accum_out=ss[:, j, 3:4]),
                         reads=[Txg[u][j]], writes=[Tjunk, Tss])
                    S.op("act", lambda e: e.activation(out=junk[:, 0:512], in_=xg[u][:, j, 512:1024], func=AF.Square, accum_out=ss[:, j, 0:1]),
                         reads=[Txg[u][j]], writes=[Tjunk, Tss])
                S.op("dve", lambda e: e.tensor_tensor(out=ss[:, :, 3], in0=ss[:, :, 3], in1=ss[:, :, 0], op=ALU.add), reads=[Tss], writes=[Tss])
                S.op("dve", lambda e: e.tensor_scalar(out=ss[:, :, 3], in0=ss[:, :, 3], scalar1=1.0 / D, scalar2=EPS, op0=ALU.mult, op1=ALU.add), reads=[Tss], writes=[Tss])
                S.op("act", lambda e: e.activation(out=ss[:, :, 3], in_=ss[:, :, 3], func=AF.Sqrt), reads=[Tss], writes=[Tss])
                S.op("dve", lambda e: e.reciprocal(out=ss[:, :, 3], in_=ss[:, :, 3]), reads=[Tss], writes=[Tss])
                for j in range(4):
                    S.op("pool" if j % 2 else "dve", lambda e: e.tensor_scalar(out=hb[:, j, :], in0=xg[u][:, j, :], scalar1=ss[:, j, 3:4], scalar2=None, op0=ALU.mult),
                         reads=[Txg[u][j], Tss], writes=[Thb[j]])
                for kc in range(8):
                    pu = kc % 2
                    for j in range(4):
                        S.op("pe", lambda e: e.transpose(out=pT[pu][:, j * 128:(j + 1) * 128], in_=hb[:, j, kc * 128:(kc + 1) * 128], identity=k.identb[:]),
                             reads=[Thb[j], k.Tidb], writes=[TpT[pu]])
                    S.op("act" if kc % 2 else "dve", (lambda e: e.activation(out=hT[u][:, kc, :], in_=pT[pu][:], func=AF.Copy)) if kc % 2 else (lambda e: e.tensor_copy(out=hT[u][:, kc, :], in_=pT[pu][:])),
                         reads=[TpT[pu]], writes=[ThT[u]])
                k.store(k.scr["h2T"][b, :, tsl].rearrange("(kc p) t -> p kc t", p=128), hT[u][:], [ThT[u]])
        S.barrier()


def phase6b(self, l):
    k, S = self, self.S
    with contextlib.ExitStack() as es:
        sb = lambda n, sh, dt=F32: k.sb(es, "p6b_" + n, sh, dt)
        Wg = sb("Wg", [128, 8, DFF], BF16)
        Wu = sb("Wu", [128, 8, DFF], BF16)
        Wd = sb("Wd", [128, NFF, D], BF16)
        TWg = [S.T(f"p6b_Wg{i}") for i in range(8)]
        TWu = [S.T(f"p6b_Wu{i}") for i in range(8)]
        TWd = [S.T(f"p6b_Wd{i}") for i in range(NFF)]
        with contextlib.ExitStack() as es2:
            stage = [k.sb(es2, f"p6b_stage{i}", [128, DFF], F32) for i in range(2)]
            Tst = [S.T(f"p6b_stage{i}") for i in range(2)]
            gn = k.sb(es2, "p6b_gn", [128, 8], F32)
            Tgn = S.T("p6b_gn")
            S.dma("sp", gn[:], k.inp["norm_ffn_t"][l], writes=[Tgn])
            si = 0
            for nm, Wt, TWt in [("w_gate", Wg, TWg), ("w_up", Wu, TWu)]:
                wsrc = k.inp[nm][l].rearrange("(kc p) n -> p kc n", p=128)
                for kc in range(8):
                    u = si % 2
                    si += 1
                    S.dma("sp", stage[u][:], wsrc[:, kc, :], writes=[Tst[u]])
                    S.op("pool" if u else "dve", lambda e: e.tensor_scalar(out=Wt[:, kc, :], in0=stage[u][:], scalar1=gn[:, kc:kc + 1], scalar2=None, op0=ALU.mult),
                         reads=[Tst[u], Tgn], writes=[TWt[kc]])
            wsrc = k.inp["w_down"][l].rearrange("(fc p) n -> p fc n", p=128)
            for fc in range(0, NFF, 2):
                u = si % 2
                si += 1
                S.dma("sp", stage[u][:, 0:2 * D].rearrange("p (a n) -> p a n", a=2), wsrc[:, fc:fc + 2, :], writes=[Tst[u]])
                S.op("pool" if u else "act", (lambda e: e.tensor_copy(out=Wd[:, fc:fc + 2, :].rearrange("p a n -> p (a n)"), in_=stage[u][:, 0:2 * D])) if u else
                     (lambda e: e.activation(out=Wd[:, fc:fc + 2, :].rearrange("p a n -> p (a n)"), in_=stage[u][:, 0:2 * D], func=AF.Copy)),
                     reads=[Tst[u]], writes=[TWd[fc], TWd[fc + 1]])
            S.barrier()
        hT = [sb(f"hT{i}", [128, 8, 512], BF16) for i in range(2)]
        ThT = [S.T(f"p6b_hT{i}") for i in range(2)]
        NX = 3
        xt = [sb(f"xt{i}", [128, D], F32) for i in range(NX)]
        Txt = [S.T(f"p6b_xt{i}") for i in range(NX)]
        actT = sb("actT", [128, NFF, 512], BF16)
        Tact = [S.T(f"p6b_act{i}") for i in range(NFF)]
        sg = [sb(f"sg{i}", [128, 512], BF16) for i in range(2)]
        Tsg = [S.T(f"p6b_sg{i}") for i in range(2)]
        pg = [k.ps(es, f"p6b_pg{i}", [128, 512], F32) for i in range(2)]
        pu_ = [k.ps(es, f"p6b_pu{i}", [128, 512], F32) for i in range(2)]
        pd = [k.ps(es, f"p6b_pd{i}", [128, 512], F32) for i in range(2)]
        Tpg = [S.T(f"p6b_pg{i}") for i in range(2)]
        Tpu = [S.T(f"p6b_pu{i}") for i in range(2)]
        Tpd = [S.T(f"p6b_pd{i}") for i in range(2)]
        gi = 0
        xi = 0
        pdi = 0
        for b in range(NB):
            for g in range(NG):
                u = gi % 2
                gi += 1
                tsl = slice(g * 512, (g + 1) * 512)
                S.dma("sp", hT[u][:], k.scr["h2T"][b, :, tsl].rearrange("(kc p) t -> p kc t", p=128), writes=[ThT[u]])
                for fc in range(NFF):
                    v = fc % 2
                    fsl = slice(fc * 128, (fc + 1) * 128)
                    for kc in range(8):
                        S.op("pe", lambda e: e.matmul(pg[v][:], lhsT=Wg[:, kc, fsl], rhs=hT[u][:, kc, :], start=(kc == 0), stop=(kc == 7)),
                             reads=[TWg[kc], ThT[u]], writes=[Tpg[v]])
                    for kc in range(8):
                        S.op("pe", lambda e: e.matmul(pu_[v][:], lhsT=Wu[:, kc, fsl], rhs=hT[u][:, kc, :], start=(kc == 0), stop=(kc == 7)),
                             reads=[TWu[kc], ThT[u]], writes=[Tpu[v]])
                    S.op("act", lambda e: e.activation(out=sg[v][:], in_=pg[v][:], func=AF.Silu), reads=[Tpg[v]], writes=[Tsg[v]])
                    S.op("dve", lambda e: e.tensor_tensor(out=actT[:, fc, :], in0=pu_[v][:], in1=sg[v][:], op=ALU.mult), reads=[Tpu[v], Tsg[v]], writes=[Tact[fc]])
                for j in range(4):
                    xx = xi % NX
                    xi += 1
                    r0 = g * 512 + j * 128
                    S.dma("sp", xt[xx][:], k.scr["xres"][b, r0:r0 + 128, :], writes=[Txt[xx]])
                    for half in range(2):
                        pi = pdi % 2
                        pdi += 1
                        hsl = slice(half * 512, (half + 1) * 512)
                        for fc in range(NFF):
                            S.op("pe", lambda e: e.matmul(pd[pi][:], lhsT=actT[:, fc, j * 128:(j + 1) * 128], rhs=Wd[:, fc, hsl], start=(fc == 0), stop=(fc == NFF - 1)),
                                 reads=[Tact[fc], TWd[fc]], writes=[Tpd[pi]])
                        S.op("dve", lambda e: e.tensor_tensor(out=xt[xx][:, hsl], in0=pd[pi][:], in1=xt[xx][:, hsl], op=ALU.add), reads=[Tpd[pi], Txt[xx]], writes=[Txt[xx]])
                    k.store(k.scr["xres"][b, r0:r0 + 128, :], xt[xx][:], [Txt[xx]])
        S.barrier()


K.phase6a = phase6a
K.phase6b = phase6b


def phase4(self, l):
    k, S = self, self.S
    PI = float(np.pi)
    C1 = float(np.float32(2 * np.pi))
    C2 = float(2 * np.pi - float(np.float32(2 * np.pi)))
    with contextlib.ExitStack() as es:
        sb = lambda n, sh, dt=F32: k.sb(es, "p4_" + n, sh, dt)
        Tp = S.T("p4_par")
        are, aim, ldt = sb("are", [128, 8]), sb("aim", [128, 8]), sb("ldt", [128, 8])
        S.dma("sp", are[:], k.inp["ssm_are_t"][l], writes=[Tp])
        Tp2 = S.T("p4_par2")
        S.dma("sp", aim[:], k.inp["ssm_aim_t"][l], writes=[Tp2])
        Tp3 = S.T("p4_par3")
        S.dma("sp", ldt[:], k.inp["ssm_ldt_t"][l], writes=[Tp3])
        dtt, z, th, mag, q = sb("dtt", [128, 8]), sb("z", [128, 8]), sb("th", [128, 8]), sb("mag", [128, 8]), sb("q", [128, 8])

        def dv(fn, reads=(), writes=(Tp,)):
            S.op("dve", fn, reads=list(reads) + [Tp], writes=list(writes))

        S.op("act", lambda e: e.activation(out=dtt[:], in_=ldt[:], func=AF.Exp), reads=[Tp3], writes=[Tp])
        dv(lambda e: e.tensor_tensor(out=z[:], in0=are[:], in1=dtt[:], op=ALU.mult))
        dv(lambda e: e.tensor_tensor(out=th[:], in0=aim[:], in1=dtt[:], op=ALU.mult), reads=[Tp2])
        dv(lambda e: e.tensor_scalar(out=q[:], in0=z[:], scalar1=1.0 / 6.0, scalar2=1.0, op0=ALU.mult, op1=ALU.add))
        for kk in (5.0, 4.0, 3.0, 2.0, 1.0):
            dv(lambda e: e.tensor_tensor(out=q[:], in0=q[:], in1=z[:], op=ALU.mult))
            dv(lambda e: e.tensor_scalar(out=q[:], in0=q[:], scalar1=1.0 / kk, scalar2=1.0, op0=ALU.mult, op1=ALU.add))
        dv(lambda e: e.tensor_copy(out=mag[:], in_=q[:]))

        iot = sb("iota", [128, 512])
        Tio = S.T("p4_iota")
        S.dma("sp", iot[:], k.inp["iota512"].partition_broadcast(128), writes=[Tio])
        Ct = sb("Ct", [128, 8, 512])
        St = sb("St", [128, 8, 512])
        TCt = [S.T(f"p4_Ct{m}") for m in range(8)]
        ang = [sb(f"ang{i}", [128, 512]) for i in range(2)]
        kf = [sb(f"kf{i}", [128, 512]) for i in range(2)]
        ki = [sb(f"ki{i}", [128, 512], mybir.dt.int32) for i in range(2)]
        Tang = [S.T(f"p4_ang{i}") for i in range(2)]
        ai = 0
        for m in range(8):
            for which in range(2):
                a_ = ai % 2
                ai += 1
                eng = "dve" if which == 0 else "pool"
                Ta = Tang[a_]
                A, KF, KI = ang[a_], kf[a_], ki[a_]
                off = 0.0 if which == 0 else PI / 2
                S.op("dve", lambda e: e.tensor_scalar(out=A[:], in0=iot[:], scalar1=th[:, m:m + 1], scalar2=off, op0=ALU.mult, op1=ALU.add), reads=[Tio, Tp], writes=[Ta])
                S.op("dve", lambda e: e.tensor_scalar(out=KI[:], in0=A[:], scalar1=1.0 / (2 * PI), scalar2=None, op0=ALU.mult), reads=[Ta], writes=[Ta])
                S.op("dve", lambda e: e.tensor_copy(out=KF[:], in_=KI[:]), reads=[Ta], writes=[Ta])
                S.op("dve", lambda e: e.scalar_tensor_tensor(out=A[:], in0=KF[:], scalar=-C1, in1=A[:], op0=ALU.mult, op1=ALU.add), reads=[Ta], writes=[Ta])
                S.op("dve", lambda e: e.scalar_tensor_tensor(out=A[:], in0=KF[:], scalar=-C2, in1=A[:], op0=ALU.mult, op1=ALU.add), reads=[Ta], writes=[Ta])
                S.op("dve", lambda e: e.tensor_scalar(out=KF[:], in0=A[:], scalar1=PI, scalar2=-2 * PI, op0=ALU.is_gt, op1=ALU.mult), reads=[Ta], writes=[Ta])
                S.op("dve", lambda e: e.tensor_tensor(out=A[:], in0=A[:], in1=KF[:], op=ALU.add), reads=[Ta], writes=[Ta])
                S.op("dve", lambda e: e.tensor_scalar(out=KF[:], in0=A[:], scalar1=-PI, scalar2=2 * PI, op0=ALU.is_lt, op1=ALU.mult), reads=[Ta], writes=[Ta])
                S.op("dve", lambda e: e.tensor_tensor(out=A[:], in0=A[:], in1=KF[:], op=ALU.add), reads=[Ta], writes=[Ta])
                S.op("dve", lambda e: e.tensor_scalar(out=A[:], in0=A[:], scalar1=PI, scalar2=-PI, op0=ALU.min, op1=ALU.max), reads=[Ta], writes=[Ta])
                dst = St if which == 0 else Ct
                S.op("act", lambda e: e.activation(out=dst[:, m, :], in_=A[:], func=AF.Sin), reads=[Ta], writes=[TCt[m]])
        c1, s1, ns1 = sb("c1", [128, 8]), sb("s1", [128, 8]), sb("ns1", [128, 8])
        TC1 = S.T("p4_c1")
        S.op("dve", lambda e: e.tensor_copy(out=c1[:], in_=Ct[:, :, 1]), reads=TCt, writes=[TC1])
        S.op("dve", lambda e: e.tensor_copy(out=s1[:], in_=St[:, :, 1]), reads=TCt, writes=[TC1])
        S.op("dve", lambda e: e.tensor_scalar(out=ns1[:], in0=s1[:], scalar1=-1.0, scalar2=None, op0=ALU.mult), reads=[TC1], writes=[TC1])
        lre, lim, den, nr, fre, fim, nfim, t8 = [sb(n, [128, 8]) for n in ("lre", "lim", "den", "nr", "fre", "fim", "nfim", "t8")]
        c512, s512, ns512, t9 = [sb(n, [128, 8]) for n in ("c512", "s512", "ns512", "t9")]
        S.op("dve", lambda e: e.tensor_tensor(out=c512[:], in0=Ct[:, :, 511], in1=c1[:], op=ALU.mult), reads=TCt + [TC1], writes=[TC1])
        S.op("dve", lambda e: e.tensor_tensor(out=t9[:], in0=St[:, :, 511], in1=s1[:], op=ALU.mult), reads=TCt + [TC1], writes=[TC1])
        S.op("dve", lambda e: e.tensor_tensor(out=c512[:], in0=c512[:], in1=t9[:], op=ALU.subtract), reads=[TC1], writes=[TC1])
        S.op("dve", lambda e: e.tensor_tensor(out=s512[:], in0=St[:, :, 511], in1=c1[:], op=ALU.mult), reads=TCt + [TC1], writes=[TC1])
        S.op("dve", lambda e: e.tensor_tensor(out=t9[:], in0=Ct[:, :, 511], in1=s1[:], op=ALU.mult), reads=TCt + [TC1], writes=[TC1])
        S.op("dve", lambda e: e.tensor_tensor(out=s512[:], in0=s512[:], in1=t9[:], op=ALU.add), reads=[TC1], writes=[TC1])
        S.op("dve", lambda e: e.tensor_scalar(out=ns512[:], in0=s512[:], scalar1=-1.0, scalar2=None, op0=ALU.mult), reads=[TC1], writes=[TC1])

        def d2(fn):
            S.op("dve", fn, reads=[Tp, Tp2, TC1], writes=[TC1])

        d2(lambda e: e.tensor_tensor(out=lre[:], in0=mag[:], in1=c1[:], op=ALU.mult))
        d2(lambda e: e.tensor_tensor(out=lim[:], in0=mag[:], in1=s1[:], op=ALU.mult))
        d2(lambda e: e.tensor_tensor(out=den[:], in0=are[:], in1=are[:], op=ALU.mult))
        d2(lambda e: e.tensor_tensor(out=t8[:], in0=aim[:], in1=aim[:], op=ALU.mult))
        d2(lambda e: e.tensor_tensor(out=den[:], in0=den[:], in1=t8[:], op=ALU.add))
        d2(lambda e: e.reciprocal(out=den[:], in_=den[:]))
        d2(lambda e: e.tensor_scalar(out=nr[:], in0=lre[:], scalar1=-1.0, scalar2=None, op0=ALU.add))
        d2(lambda e: e.tensor_tensor(out=fre[:], in0=nr[:], in1=are[:], op=ALU.mult))
        d2(lambda e: e.tensor_tensor(out=t8[:], in0=lim[:], in1=aim[:], op=ALU.mult))
        d2(lambda e: e.tensor_tensor(out=fre[:], in0=fre[:], in1=t8[:], op=ALU.add))
        d2(lambda e: e.tensor_tensor(out=fre[:], in0=fre[:], in1=den[:], op=ALU.mult))
        d2(lambda e: e.tensor_tensor(out=fim[:], in0=lim[:], in1=are[:], op=ALU.mult))
        d2(lambda e: e.tensor_tensor(out=t8[:], in0=nr[:], in1=aim[:], op=ALU.mult))
        d2(lambda e: e.tensor_tensor(out=fim[:], in0=fim[:], in1=t8[:], op=ALU.subtract))
        d2(lambda e: e.tensor_tensor(out=fim[:], in0=fim[:], in1=den[:], op=ALU.mult))
        d2(lambda e: e.tensor_scalar(out=nfim[:], in0=fim[:], scalar1=-1.0, scalar2=None, op0=ALU.mult))
        Mre = sb("Mre", [128, 8, 128], BF16)
        Mim = sb("Mim", [128, 8, 128], BF16)
        Cre = sb("Cre", [128, 8, 128], BF16)
        Cim = sb("Cim", [128, 8, 128], BF16)
        TM = S.T("p4_M")
        TCp = S.T("p4_Cp")
        pset = k.ps(es, "p4_pset", [128, 512], BF16)
        Tpset = S.T("p4_pset")
        with contextlib.ExitStack() as es2:
            zbr = k.sb(es2, "p4_zbr", [128, 8, 128], F32)
            zbi = k.sb(es2, "p4_zbi", [128, 8, 128], F32)
            Tz1, Tz2 = S.T("p4_zbr"), S.T("p4_zbi")
            tt = k.sb(es2, "p4_tt", [128, 128], F32)
            bbr = k.sb(es2, "p4_bbr", [128, 128], BF16)
            bbi = k.sb(es2, "p4_bbi", [128, 128], BF16)
            Tbb = S.T("p4_bb")
            S.dma("sp", zbr[:], k.inp["Zb_re"][l], writes=[Tz1])
            S.dma("sp", zbi[:], k.inp["Zb_im"][l], writes=[Tz2])
            for m in range(8):
                S.op("dve", lambda e: e.tensor_scalar(out=tt[:], in0=zbr[:, m, :], scalar1=fre[:, m:m + 1], scalar2=None, op0=ALU.mult), reads=[Tz1, TC1], writes=[Tbb])
                S.op("dve", lambda e: e.scalar_tensor_tensor(out=bbr[:], in0=zbi[:, m, :], scalar=nfim[:, m:m + 1], in1=tt[:], op0=ALU.mult, op1=ALU.add), reads=[Tz2, TC1, Tbb], writes=[Tbb])
                S.op("dve", lambda e: e.tensor_scalar(out=tt[:], in0=zbi[:, m, :], scalar1=fre[:, m:m + 1], scalar2=None, op0=ALU.mult), reads=[Tz2, TC1, Tbb], writes=[Tbb])
                S.op("dve", lambda e: e.scalar_tensor_tensor(out=bbi[:], in0=zbr[:, m, :], scalar=fim[:, m:m + 1], in1=tt[:], op0=ALU.mult, op1=ALU.add), reads=[Tz1, TC1, Tbb], writes=[Tbb])
                S.op("pe", lambda e: e.transpose(out=pset[:, 0:128], in_=bbr[:], identity=k.identb[:]), reads=[Tbb, k.Tidb], writes=[Tpset])
                S.op("pe", lambda e: e.transpose(out=pset[:, 128:256], in_=bbi[:], identity=k.identb[:]), reads=[Tbb, k.Tidb], writes=[Tpset])
                S.op("act", lambda e: e.activation(out=Mre[:, m, :], in_=pset[:, 0:128], func=AF.Copy), reads=[Tpset], writes=[TM])
                S.op("act", lambda e: e.activation(out=Mim[:, m, :], in_=pset[:, 128:256], func=AF.Copy), reads=[Tpset], writes=[TM])
            S.dma("sp", zbr[:], k.inp["Cp_re"][l], writes=[Tz1])
            S.dma("sp", zbi[:], k.inp["Cp_im"][l], writes=[Tz2])
            S.op("dve", lambda e: e.tensor_copy(out=Cre[:], in_=zbr[:]), reads=[Tz1], writes=[TCp])
            S.op("dve", lambda e: e.tensor_scalar(out=Cim[:], in0=zbi[:], scalar1=-1.0, scalar2=None, op0=ALU.mult), reads=[Tz2], writes=[TCp])
            S.barrier()
        dvec = sb("dvec", [128, 2])
        Td = S.T("p4_d")
        S.dma("sp", dvec[:], k.inp["ssm_d_t"][l], writes=[Td])
        wgs = sb("wgs", [128, 2, 512])
        wgb = sb("wgb", [128, 2, 512], BF16)
        Twg = S.T("p4_wg")
        S.dma("sp", wgs[:], k.inp["ssm_w_glu"][l].rearrange("(mt p) n -> p mt n", p=128), writes=[Twg])
        S.op("dve", lambda e: e.tensor_copy(out=wgb[:], in_=wgs[:]), reads=[Twg], writes=[Twg])
        U = sb("U", [128, 2, SEQ], BF16)
        TU = [S.T(f"p4_U{i}") for i in range(2)]
        NW = 3
        NPW = 2
        pw = [[k.ps(es, f"p4_pw{i}_{c}", [128, 512], F32) for c in range(2)] for i in range(NPW)]
        Tpw = [[S.T(f"p4_pw{i}_{c}") for c in range(2)] for i in range(NPW)]
        py = [k.ps(es, f"p4_py{i}", [128, 512], F32) for i in range(2)]
        Tpy = [S.T(f"p4_py{i}") for i in range(2)]
        pgl = k.ps(es, "p4_pgl", [128, 512], F32)
        Tpgl = S.T("p4_pgl")
        ta = [sb(f"ta{i}", [128, 512]) for i in range(NW)]
        tb_ = [sb(f"tb{i}", [128, 512]) for i in range(NW)]
        tc_ = [sb(f"tc{i}", [128, 512]) for i in range(NW)]
        td_ = [sb(f"td{i}", [128, 512]) for i in range(NW)]
        te_ = [sb(f"te{i}", [128, 512]) for i in range(NW)]
        tf_ = [sb(f"tf{i}", [128, 512]) for i in range(NW)]
        tg_ = [sb(f"tg{i}", [128, 512]) for i in range(NW)]
        th_ = [sb(f"th{i}", [128, 512]) for i in range(NW)]
        Twa = [S.T(f"p4_wa{i}") for i in range(NW)]
        Twc = [S.T(f"p4_wc{i}") for i in range(NW)]
        Twe = [S.T(f"p4_we{i}") for i in range(NW)]
        Twgg = [S.T(f"p4_wgg{i}") for i in range(NW)]
        Tvr = [S.T(f"p4_vr{i}") for i in range(NW)]
        Tvi = [S.T(f"p4_vi{i}") for i in range(NW)]
        Tsr = [S.T(f"p4_sr{i}") for i in range(NW)]
        Tsi = [S.T(f"p4_si{i}") for i in range(NW)]
        vre = [sb(f"vre{i}", [128, 512]) for i in range(NW)]
        vim = [sb(f"vim{i}", [128, 512]) for i in range(NW)]
        sre = [sb(f"sre{i}", [128, 512]) for i in range(NW)]
        sim_ = [sb(f"sim{i}", [128, 512]) for i in range(NW)]
        xre = [sb(f"xre{i}", [128, 512], BF16) for i in range(NW)]
        xim = [sb(f"xim{i}", [128, 512], BF16) for i in range(NW)]
        Twk = [S.T(f"p4_wk{i}") for i in range(NW)]
        Tv = [S.T(f"p4_v{i}") for i in range(NW)]
        Ts = [S.T(f"p4_s{i}") for i in range(NW)]
        Tx = [S.T(f"p4_x{i}") for i in range(NW)]
        init = sb("init", [128, 8, 2])
        Tinit = [S.T(f"p4_init{m}") for m in range(8)]
        xl = sb("xl", [128, 8, 2])
        i4 = sb("i4", [128, 8, 2])
        yf = [sb(f"yf{i}", [128, 512]) for i in range(2)]
        gT = [sb(f"gT{i}", [128, 512], BF16) for i in range(2)]
        TgT = [S.T(f"p4_gT{i}") for i in range(2)]
        Tyf = [S.T(f"p4_yf{i}") for i in range(2)]
        sgl = [sb(f"sgl{i}", [128, 256]) for i in range(2)]
        Tsgl = [S.T(f"p4_sgl{i}") for i in range(2)]
        yo = [sb(f"yo{i}", [128, 4, 256]) for i in range(2)]
        Tyo = [S.T(f"p4_yo{i}") for i in range(2)]
        wi = 0
        gi = 0
        for b in range(NB):
            for mt in range(2):
                S.dma("sp", U[:, mt, :], k.scr["usT"][b, mt * 128:(mt + 1) * 128, :], writes=[TU[mt]])
            for m in range(8):
                S.op("pool", lambda e: e.memset(init[:, m, :], 0.0), writes=[Tinit[m]])
            units = [(g, mt, m) for g in range(NG) for mt in range(2) for m in range(4 * mt, 4 * mt + 4)]
            ustate = {}

            def stageA(un):
                nonlocal wi
                g, mt, m = un
                tsl = slice(g * 512, (g + 1) * 512)
                w = wi % NW
                pwi = wi % NPW
                wi += 1
                ustate[un] = w
                Cm, Sm = Ct[:, m, :], St[:, m, :]
                S.op("pe", lambda e: e.matmul(pw[pwi][0][:], lhsT=Mre[:, m, :], rhs=U[:, mt, tsl], start=True, stop=True), reads=[TM, TU[mt]], writes=[Tpw[pwi][0]])
                S.op("pe", lambda e: e.matmul(pw[pwi][1][:], lhsT=Mim[:, m, :], rhs=U[:, mt, tsl], start=True, stop=True), reads=[TM, TU[mt]], writes=[Tpw[pwi][1]])
                S.op("dve", lambda e: e.tensor_tensor(out=ta[w][:], in0=pw[pwi][0][:], in1=Cm, op=ALU.mult), reads=[Tpw[pwi][0], TCt[m]], writes=[Twa[w]])
                S.op("dve", lambda e: e.tensor_tensor(out=tb_[w][:], in0=pw[pwi][1][:], in1=Sm, op=ALU.mult), reads=[Tpw[pwi][1], TCt[m]], writes=[Twa[w]])
                S.op("dve", lambda e: e.tensor_tensor(out=tc_[w][:], in0=pw[pwi][1][:], in1=Cm, op=ALU.mult), reads=[Tpw[pwi][1], TCt[m]], writes=[Twc[w]])
                S.op("dve", lambda e: e.tensor_tensor(out=td_[w][:], in0=pw[pwi][0][:], in1=Sm, op=ALU.mult), reads=[Tpw[pwi][0], TCt[m]], writes=[Twc[w]])
                S.op("pool", lambda e: e.tensor_tensor(out=vre[w][:], in0=ta[w][:], in1=tb_[w][:], op=ALU.add), reads=[Twa[w]], writes=[Tvr[w]])
                S.op("pool", lambda e: e.tensor_tensor(out=vim[w][:], in0=tc_[w][:], in1=td_[w][:], op=ALU.subtract), reads=[Twc[w]], writes=[Tvi[w]])

            def stageB(un):
                nonlocal gi
                g, mt, m = un
                tsl = slice(g * 512, (g + 1) * 512)
                w = ustate.pop(un)
                yb = (g * 2 + mt) % 2
                Cm, Sm = Ct[:, m, :], St[:, m, :]
                S.op("dve", lambda e: e.tensor_tensor_scan(out=sre[w][:], data0=mag[:, m:m + 1].broadcast_to([128, 512]), data1=vre[w][:], initial=init[:, m, 0:1], op0=ALU.mult, op1=ALU.add),
                     reads=[Tvr[w], Tp, Tinit[m]], writes=[Tsr[w]])
                S.op("dve", lambda e: e.tensor_tensor_scan(out=sim_[w][:], data0=mag[:, m:m + 1].broadcast_to([128, 512]), data1=vim[w][:], initial=init[:, m, 1:2], op0=ALU.mult, op1=ALU.add),
                     reads=[Tvi[w], Tp, Tinit[m]], writes=[Tsi[w]])
                S.op("dve", lambda e: e.tensor_scalar(out=i4[:, m, 0:1], in0=sim_[w][:, 511:512], scalar1=ns512[:, m:m + 1], scalar2=None, op0=ALU.mult), reads=[Tsi[w], TC1], writes=[Tinit[m]])
                S.op("dve", lambda e: e.tensor_scalar(out=i4[:, m, 1:2], in0=sre[w][:, 511:512], scalar1=s512[:, m:m + 1], scalar2=None, op0=ALU.mult), reads=[Tsr[w], TC1], writes=[Tinit[m]])
                S.op("dve", lambda e: e.scalar_tensor_tensor(out=init[:, m, 0:1], in0=sre[w][:, 511:512], scalar=c512[:, m:m + 1], in1=i4[:, m, 0:1], op0=ALU.mult, op1=ALU.add), reads=[Tsr[w], TC1, Tinit[m]], writes=[Tinit[m]])
                S.op("dve", lambda e: e.scalar_tensor_tensor(out=init[:, m, 1:2], in0=sim_[w][:, 511:512], scalar=c512[:, m:m + 1], in1=i4[:, m, 1:2], op0=ALU.mult, op1=ALU.add), reads=[Tsi[w], TC1, Tinit[m]], writes=[Tinit[m]])
                S.op("pool", lambda e: e.tensor_tensor(out=te_[w][:], in0=sre[w][:], in1=Cm, op=ALU.mult), reads=[Tsr[w], TCt[m]], writes=[Twe[w]])
                S.op("pool", lambda e: e.tensor_tensor(out=tf_[w][:], in0=sim_[w][:], in1=Sm, op=ALU.mult), reads=[Tsi[w], TCt[m]], writes=[Twe[w]])
                S.op("pool", lambda e: e.tensor_tensor(out=xre[w][:], in0=te_[w][:], in1=tf_[w][:], op=ALU.subtract), reads=[Twe[w]], writes=[Tx[w]])
                S.op("dve", lambda e: e.tensor_tensor(out=tg_[w][:], in0=sim_[w][:], in1=Cm, op=ALU.mult), reads=[Tsi[w], TCt[m]], writes=[Twgg[w]])
                S.op("pool", lambda e: e.tensor_tensor(out=th_[w][:], in0=sre[w][:], in1=Sm, op=ALU.mult), reads=[Tsr[w], TCt[m]], writes=[Twgg[w]])
                S.op("pool", lambda e: e.tensor_tensor(out=xim[w][:], in0=tg_[w][:], in1=th_[w][:], op=ALU.add), reads=[Twgg[w]], writes=[Tx[w]])
                first = (m == 4 * mt)
                last = (m == 4 * mt + 3)
                S.op("pe", lambda e: e.matmul(py[yb][:], lhsT=Cre[:, m, :], rhs=xre[w][:], start=first, stop=False), reads=[TCp, Tx[w]], writes=[Tpy[yb]])
                S.op("pe", lambda e: e.matmul(py[yb][:], lhsT=Cim[:, m, :], rhs=xim[w][:], start=False, stop=last), reads=[TCp, Tx[w]], writes=[Tpy[yb]])
                if last:
                    S.op("dve", lambda e: e.scalar_tensor_tensor(out=yf[mt][:], in0=U[:, mt, tsl], scalar=dvec[:, mt:mt + 1], in1=py[yb][:], op0=ALU.mult, op1=ALU.add),
                         reads=[TU[mt], Td, Tpy[yb]], writes=[Tyf[mt]])
                    S.op("act", lambda e: e.activation(out=gT[mt][:], in_=yf[mt][:], func=AF.Gelu_apprx_tanh), reads=[Tyf[mt]], writes=[TgT[mt]])
                if last and mt == 1:
                    yi = gi % 2
                    gi += 1
                    for j in range(4):
                        for mt2 in range(2):
                            S.op("pe", lambda e: e.matmul(pgl[:], lhsT=gT[mt2][:, j * 128:(j + 1) * 128], rhs=wgb[:, mt2, :], start=(mt2 == 0), stop=(mt2 == 1)),
                                 reads=[TgT[mt2], Twg], writes=[Tpgl])
                        sj = j % 2
                        S.op("act", lambda e: e.activation(out=sgl[sj][:], in_=pgl[:, 256:512], func=AF.Sigmoid), reads=[Tpgl], writes=[Tsgl[sj]])
                        S.op("dve", lambda e: e.tensor_tensor(out=yo[yi][:, j, :], in0=pgl[:, 0:256], in1=sgl[sj][:], op=ALU.mult), reads=[Tpgl, Tsgl[sj]], writes=[Tyo[yi]])
                    ydst = k.scr["ycat"][b].rearrange("(n p) c -> p n c", p=128)
                    k.store(ydst[:, 4 * g:4 * g + 4, 512:768], yo[yi][:], [Tyo[yi]])

            for un in units:
                stageA(un)
                stageB(un)
        S.barrier()


K.phase4 = phase4
PHASES.extend(["phase4", "phase6a", "phase6b"])
```
